# Optimizing a Trainium2 kernel written in Bass

```python
import math
import jax, jax.numpy as jnp
from jax import lax
import numpy as np

D_MODEL = 2048
BATCH = 4
SEQ = 4096
DEPTH = 2

ROPE_THETA = 10000.0
LN_EPS = 1e-5
RET_HEADS = 4
RET_HEAD_DIM = 256
RET_WIDTH = RET_HEADS * RET_HEAD_DIM
RET_CHUNK = 128
CONV_CHANNELS = D_MODEL - RET_WIDTH
CONV_TAPS = 31
EVEN_IN = 4 * RET_WIDTH + 2 * CONV_CHANNELS
DSA_HEADS = 16
DSA_HEAD_DIM = D_MODEL // DSA_HEADS
DSA_KV_HEADS = 4
IDX_HEADS = 16
IDX_DIM = 64
TOPK_MAX = 256
QUERY_BLOCK = 128
ODD_IN = DSA_HEADS * DSA_HEAD_DIM + 2 * DSA_KV_HEADS * DSA_HEAD_DIM + IDX_HEADS * IDX_DIM + IDX_DIM + IDX_HEADS
N_EXPERTS = 16
N_GROUPS = 4
EXPERTS_PER_GROUP = N_EXPERTS // N_GROUPS
TOP_K = 2
EXPERT_FF = 512
N_EVEN = (DEPTH + 1) // 2
N_ODD = DEPTH // 2
DEEPNORM_ALPHA = (2 * DEPTH) ** 0.25
DEEPNORM_BETA = (8 * DEPTH) ** -0.25

kernel_name = 'hybrid_retention_conformer_dsa_moe'

f32 = jnp.float32


def layer_norm(x, g, b):
    xf = x.astype(f32)
    mu = jnp.mean(xf, axis=-1, keepdims=True)
    var = jnp.mean(jnp.square(xf - mu), axis=-1, keepdims=True)
    y = (xf - mu) * lax.rsqrt(var + LN_EPS)
    return (y * g.astype(f32) + b.astype(f32)).astype(x.dtype)


def rope(x, pos):
    d = x.shape[-1]
    half = d // 2
    inv = ROPE_THETA ** (-jnp.arange(half, dtype=f32) / half)
    ang = pos.astype(f32)[:, None] * inv[None, :]
    cos = jnp.cos(ang)[None, :, None, :]
    sin = jnp.sin(ang)[None, :, None, :]
    xf = x.astype(f32)
    x1, x2 = xf[..., :half], xf[..., half:]
    return jnp.concatenate([x1 * cos - x2 * sin, x2 * cos + x1 * sin], axis=-1).astype(x.dtype)


def retention_chunkwise(q, k, v):
    B, S, H, Dk = q.shape
    Dv = v.shape[-1]
    C = RET_CHUNK
    nc = S // C
    log_g = jnp.log(1.0 - 2.0 ** (-5.0 - jnp.arange(H, dtype=f32)))
    j = jnp.arange(C, dtype=f32)
    diff = j[:, None] - j[None, :]
    decay_in = jnp.where(diff[None] >= 0, jnp.exp(jnp.maximum(diff, 0.0)[None] * log_g[:, None, None]), 0.0)
    xi = jnp.exp((j[None, :] + 1.0) * log_g[:, None])
    zeta = jnp.exp((C - 1.0 - j[None, :]) * log_g[:, None])
    chunk_decay = jnp.exp(C * log_g)

    def to_chunks(t):
        return t.astype(f32).reshape(B, nc, C, H, t.shape[-1]).transpose(1, 0, 3, 2, 4)

    def step(state, inp):
        qc, kc, vc = inp
        attn = jnp.einsum('bhqd,bhsd->bhqs', qc, kc) * decay_in[None]
        inner = jnp.einsum('bhqs,bhse->bhqe', attn, vc)
        cross = jnp.einsum('bhqd,bhde->bhqe', qc, state) * xi[None, :, :, None]
        new_state = state * chunk_decay[None, :, None, None] + jnp.einsum('bhsd,bhse->bhde', kc * zeta[None, :, :, None], vc)
        return new_state, inner + cross

    state0 = jnp.zeros((B, H, Dk, Dv), f32)
    _, ys = lax.scan(step, state0, (to_chunks(q), to_chunks(k), to_chunks(v)))
    return ys.transpose(1, 0, 3, 2, 4).reshape(B, S, H, Dv)


def conformer_conv(a, b, conv_w, conv_b, ln_g, ln_b):
    u = a * jax.nn.sigmoid(b)
    y = lax.conv_general_dilated(u, conv_w[:, None, :].astype(u.dtype), window_strides=(1,),
                                 padding=[(CONV_TAPS - 1, 0)], dimension_numbers=('NWC', 'WIO', 'NWC'),
                                 feature_group_count=u.shape[-1])
    y = layer_norm(y + conv_b, ln_g, ln_b)
    return jax.nn.silu(y)


def retention_conv_mixer(x, w_in, ret_gn_g, conv_w, conv_b, conv_ln_g, conv_ln_b, w_out):
    B, S, _ = x.shape
    pos = jnp.arange(S)
    R = RET_WIDTH
    proj = x @ w_in
    q, k, v, g, ga, gb = jnp.split(proj, [R, 2 * R, 3 * R, 4 * R, 4 * R + CONV_CHANNELS], axis=-1)
    q = rope(q.reshape(B, S, RET_HEADS, RET_HEAD_DIM), pos)
    k = rope(k.reshape(B, S, RET_HEADS, RET_HEAD_DIM), pos) * (RET_HEAD_DIM ** -0.5)
    v = v.reshape(B, S, RET_HEADS, RET_HEAD_DIM)
    y = retention_chunkwise(q, k, v)
    mu = jnp.mean(y, axis=-1, keepdims=True)
    var = jnp.mean(jnp.square(y - mu), axis=-1, keepdims=True)
    yn = (y - mu) * lax.rsqrt(var + LN_EPS) * ret_gn_g.astype(f32).reshape(RET_HEADS, RET_HEAD_DIM)
    ret = (jax.nn.silu(g.astype(f32)) * yn.reshape(B, S, R)).astype(x.dtype)
    conv = conformer_conv(ga, gb, conv_w, conv_b, conv_ln_g, conv_ln_b)
    return jnp.concatenate([ret, conv], axis=-1) @ w_out


def dsa_mixer(x, w_in, w_out):
    B, S, _ = x.shape
    pos = jnp.arange(S)
    hq = DSA_HEADS * DSA_HEAD_DIM
    hkv = DSA_KV_HEADS * DSA_HEAD_DIM
    hi = IDX_HEADS * IDX_DIM
    proj = x @ w_in
    q, k, v, qi, ki, wi = jnp.split(proj, [hq, hq + hkv, hq + 2 * hkv, hq + 2 * hkv + hi, hq + 2 * hkv + hi + IDX_DIM], axis=-1)
    q = rope(q.reshape(B, S, DSA_HEADS, DSA_HEAD_DIM), pos)
    k = rope(k.reshape(B, S, DSA_KV_HEADS, DSA_HEAD_DIM), pos)
    v = v.reshape(B, S, DSA_KV_HEADS, DSA_HEAD_DIM)
    qi = rope(qi.reshape(B, S, IDX_HEADS, IDX_DIM), pos).astype(f32)
    ki = rope(ki.reshape(B, S, 1, IDX_DIM), pos)[:, :, 0].astype(f32)
    wi = wi.astype(f32) * (IDX_HEADS ** -0.5)
    n_sel = min(TOPK_MAX, S // 4)
    nb = S // QUERY_BLOCK
    group = DSA_HEADS // DSA_KV_HEADS
    key_pos = jnp.arange(S)

    def blockify(t):
        return t.reshape((B, nb, QUERY_BLOCK) + t.shape[2:]).swapaxes(0, 1)

    def attend_block(args):
        qb, qib, wib, blk = args
        q_pos = blk * QUERY_BLOCK + jnp.arange(QUERY_BLOCK)
        rel = jax.nn.relu(jnp.einsum('bqhd,bsd->bqhs', qib, ki) * (IDX_DIM ** -0.5))
        score = jnp.einsum('bqhs,bqh->bqs', rel, wib)
        causal = key_pos[None, :] <= q_pos[:, None]
        score = jnp.where(causal[None], score, -jnp.inf)
        _, idx = lax.top_k(score, n_sel)
        valid = idx <= q_pos[None, :, None]
        k_sel = jax.vmap(lambda kk, ii: kk[ii])(k, idx)
        v_sel = jax.vmap(lambda vv, ii: vv[ii])(v, idx)
        qg = qb.reshape(B, QUERY_BLOCK, DSA_KV_HEADS, group, DSA_HEAD_DIM).astype(f32)
        logits = jnp.einsum('bqkgd,bqnkd->bqkgn', qg, k_sel.astype(f32)) * (DSA_HEAD_DIM ** -0.5)
        logits = jnp.where(valid[:, :, None, None, :], logits, -jnp.inf)
        p = jax.nn.softmax(logits, axis=-1)
        o = jnp.einsum('bqkgn,bqnkd->bqkgd', p, v_sel.astype(f32))
        return o.reshape(B, QUERY_BLOCK, hq).astype(x.dtype)

    out = lax.map(attend_block, (blockify(q), blockify(qi), blockify(wi), jnp.arange(nb)))
    out = out.swapaxes(0, 1).reshape(B, S, hq)
    return out @ w_out


def grouped_moe(x, router_w, router_b, w_gu, w_down):
    B, S, D = x.shape
    xt = x.reshape(B * S, D)
    affinity = jax.nn.sigmoid(xt.astype(f32) @ router_w.astype(f32))
    sel = affinity + router_b.astype(f32)
    grp = sel.reshape(-1, N_GROUPS, EXPERTS_PER_GROUP)
    group_score = jnp.sum(lax.top_k(grp, 2)[0], axis=-1)
    best = jnp.argmax(group_score, axis=-1)
    in_group = (jnp.arange(N_EXPERTS) // EXPERTS_PER_GROUP)[None, :] == best[:, None]
    _, eidx = lax.top_k(jnp.where(in_group, sel, -jnp.inf), TOP_K)
    w = jnp.take_along_axis(affinity, eidx, axis=-1)
    w = w / jnp.sum(w, axis=-1, keepdims=True)
    gate = jnp.sum(jax.nn.one_hot(eidx, N_EXPERTS, dtype=f32) * w[..., None], axis=1)
    y = jnp.zeros((B * S, D), f32)
    for e in range(N_EXPERTS):
        a, b = jnp.split(xt @ w_gu[e], 2, axis=-1)
        y = y + gate[:, e:e + 1] * ((jax.nn.silu(a) * b) @ w_down[e]).astype(f32)
    return y.reshape(B, S, D).astype(x.dtype)


def setup_inputs(seed: int = 0) -> dict:
    key = jax.random.key(seed)
    ks = jax.random.split(key, 18)

    def nrm(k, shape, scale):
        return jax.random.normal(k, shape, f32) * scale

    return {
        'x': nrm(ks[0], (BATCH, SEQ, D_MODEL), 1.0),
        'even_w_in': nrm(ks[1], (N_EVEN, D_MODEL, EVEN_IN), D_MODEL ** -0.5),
        'even_ret_gn_g': 1.0 + nrm(ks[2], (N_EVEN, RET_WIDTH), 0.02),
        'even_conv_w': nrm(ks[3], (N_EVEN, CONV_TAPS, CONV_CHANNELS), CONV_TAPS ** -0.5),
        'even_conv_b': nrm(ks[4], (N_EVEN, CONV_CHANNELS), 0.02),
        'even_conv_ln_g': 1.0 + nrm(ks[5], (N_EVEN, CONV_CHANNELS), 0.02),
        'even_conv_ln_b': nrm(ks[6], (N_EVEN, CONV_CHANNELS), 0.02),
        'even_w_out': nrm(ks[7], (N_EVEN, D_MODEL, D_MODEL), D_MODEL ** -0.5 * DEEPNORM_BETA),
        'odd_w_in': nrm(ks[8], (N_ODD, D_MODEL, ODD_IN), D_MODEL ** -0.5),
        'odd_w_out': nrm(ks[9], (N_ODD, DSA_HEADS * DSA_HEAD_DIM, D_MODEL), (DSA_HEADS * DSA_HEAD_DIM) ** -0.5 * DEEPNORM_BETA),
        'mix_ln_g': 1.0 + nrm(ks[10], (DEPTH, D_MODEL), 0.02),
        'mix_ln_b': nrm(ks[11], (DEPTH, D_MODEL), 0.02),
        'moe_w_gu': nrm(ks[12], (DEPTH, N_EXPERTS, D_MODEL, 2 * EXPERT_FF), D_MODEL ** -0.5),
        'moe_w_down': nrm(ks[13], (DEPTH, N_EXPERTS, EXPERT_FF, D_MODEL), EXPERT_FF ** -0.5 * DEEPNORM_BETA),
        'ffn_ln_g': 1.0 + nrm(ks[14], (DEPTH, D_MODEL), 0.02),
        'ffn_ln_b': nrm(ks[15], (DEPTH, D_MODEL), 0.02),
        'router_w': nrm(ks[16], (D_MODEL, N_EXPERTS), D_MODEL ** -0.5),
        'router_b': nrm(ks[17], (N_EXPERTS,), 0.01),
    }


def reference(x, even_w_in, even_ret_gn_g, even_conv_w, even_conv_b, even_conv_ln_g, even_conv_ln_b, even_w_out,
              odd_w_in, odd_w_out, mix_ln_g, mix_ln_b, moe_w_gu, moe_w_down, ffn_ln_g, ffn_ln_b, router_w, router_b):
    h = x
    for layer in range(DEPTH):
        i = layer // 2
        if layer % 2 == 0:
            m = retention_conv_mixer(h, even_w_in[i], even_ret_gn_g[i], even_conv_w[i], even_conv_b[i],
                                     even_conv_ln_g[i], even_conv_ln_b[i], even_w_out[i])
        else:
            m = dsa_mixer(h, odd_w_in[i], odd_w_out[i])
        h = layer_norm(DEEPNORM_ALPHA * h + m, mix_ln_g[layer], mix_ln_b[layer])
        f = grouped_moe(h, router_w, router_b, moe_w_gu[layer], moe_w_down[layer])
        h = layer_norm(DEEPNORM_ALPHA * h + f, ffn_ln_g[layer], ffn_ln_b[layer])
    return h
```

```python
import math
import numpy as np
from contextlib import ExitStack
import concourse.bass as bass
import concourse.mybir as mybir
from concourse.bass_utils import run_bass_kernel_spmd

F32 = mybir.dt.float32
BF16 = mybir.dt.bfloat16
AF = mybir.ActivationFunctionType
ALU = mybir.AluOpType
AX = mybir.AxisListType

ENGS = ("pe", "act", "dve", "pool", "sp")
D = 2048
T = 2048
NT = 16
ALPHA = 4.0 ** 0.25
EPS = 1e-5
NEG = -1.0e30


class Op:
    __slots__ = ("eng", "fn", "deps", "dma_slot", "dma_val", "signal", "sigval", "idx", "stage")


class Prog:
    def __init__(self, nc):
        self.nc = nc
        self.ops = []
        self.last_w = {}
        self.readers = {}
        self.slot_cnt = {}
        self.slot_last = {}
        self.last_on = {}
        self.stage = "init"
        self.profile = False

    def _rec(self, eng, fn, reads, writes, dma_slot=None, extra_deps=()):
        op = Op()
        op.eng = eng; op.fn = fn; op.dma_slot = dma_slot; op.signal = False; op.sigval = 0
        op.idx = len(self.ops)
        op.stage = self.stage
        deps = set(extra_deps)
        for k in reads:
            w = self.last_w.get(k)
            if w is not None:
                deps.add(w)
        for k in writes:
            w = self.last_w.get(k)
            if w is not None:
                deps.add(w)
            for r in self.readers.get(k, ()):
                deps.add(r)
        op.deps = deps
        if dma_slot is not None:
            c = self.slot_cnt.get(dma_slot, 0) + 1
            self.slot_cnt[dma_slot] = c
            op.dma_val = 16 * c
            self.slot_last[dma_slot] = op.idx
        else:
            op.dma_val = 0
            self.last_on[eng] = op.idx
        for k in reads:
            self.readers.setdefault(k, []).append(op.idx)
        for k in writes:
            self.last_w[k] = op.idx
            self.readers[k] = []
        self.ops.append(op)
        return op

    def op(self, eng, fn, reads=(), writes=()):
        return self._rec(eng, fn, reads, writes)

    def dma(self, eng, fn, slot, reads=(), writes=()):
        return self._rec(eng, fn, reads, writes, dma_slot=slot)

    def barrier(self):
        deps = set(self.last_on.values()) | set(self.slot_last.values())
        for e in ENGS:
            self._rec(e, None, (), (), extra_deps=deps)
        self.last_w = {}
        self.readers = {}

    def emit(self):
        nc = self.nc
        self.barrier()
        ops = self.ops
        for op in ops:
            for d in op.deps:
                p = ops[d]
                if p.dma_slot is None and p.fn is not None and (p.eng != op.eng or p.eng != "pe"):
                    p.signal = True
        cnt = {e: 0 for e in ENGS}
        for op in ops:
            if op.signal:
                cnt[op.eng] += 1
                op.sigval = cnt[op.eng]
        slots = list(self.slot_cnt.keys())
        EPOCH = 30000
        with ExitStack() as es:
            eng_sems = {}
            for e in ENGS:
                n_ep = cnt[e] // EPOCH + 1
                eng_sems[e] = [es.enter_context(nc.semaphore(f"s_{e}_{i}")) for i in range(n_ep)]
            slot_sem = {s: es.enter_context(nc.semaphore(f"d_{i}")) for i, s in enumerate(slots)}
            block = es.enter_context(nc.Block())

            def run_engine(ename, eng):
                waited = {}
                cur = None
                for op in ops:
                    if op.eng != ename:
                        continue
                    if self.profile and op.stage != cur:
                        if cur is not None:
                            nc.pop_named_scope(cur)
                        cur = op.stage
                        nc.push_named_scope(cur)
                    for d in sorted(op.deps):
                        p = ops[d]
                        if p.dma_slot is not None:
                            sem = slot_sem[p.dma_slot]; val = p.dma_val
                        else:
                            if p.eng == ename and ename == "pe":
                                continue
                            if p.fn is None:
                                continue
                            ep = (p.sigval - 1) // EPOCH
                            sem = eng_sems[p.eng][ep]; val = p.sigval - ep * EPOCH
                        if waited.get(sem.num, 0) >= val:
                            continue
                        waited[sem.num] = val
                        eng.wait_ge(sem, val)
                    if op.fn is None:
                        continue
                    ins = op.fn(eng)
                    if op.dma_slot is not None:
                        ins.then_inc(slot_sem[op.dma_slot], 16)
                    elif op.signal:
                        ep = (op.sigval - 1) // EPOCH
                        ins.then_inc(eng_sems[ename][ep], 1)
                if self.profile and cur is not None:
                    nc.pop_named_scope(cur)

            block.sync(lambda e: run_engine("sp", e))
            block.tensor(lambda e: run_engine("pe", e))
            block.scalar(lambda e: run_engine("act", e))
            block.vector(lambda e: run_engine("dve", e))
            block.gpsimd(lambda e: run_engine("pool", e))


class Ctx:
    pass


def _mk_ctx(nc, es):
    C = Ctx()
    C.nc = nc
    C.P = Prog(nc)
    C.es = es
    C.ps_all = es.enter_context(nc.psum_tensor("ps_all", [128, 4096], F32))
    C.pb = [C.ps_all[:, i * 512:(i + 1) * 512] for i in range(8)]
    C.uid = 0
    C.pfx = ""
    return C


def _sb(C, st, name, shape, dt=F32):
    C.uid += 1
    return st.enter_context(C.nc.sbuf_tensor(f"{name}_{C.uid}", shape, dt))


def _load_consts(C, ident_d):
    nc, P = C.nc, C.P
    C.ident = C.es.enter_context(nc.sbuf_tensor("ident_sb", [128, 128], F32))
    C.ones = C.es.enter_context(nc.sbuf_tensor("ones_sb", [128, 128], F32))
    P.dma("sp", lambda e: e.dma_start(out=C.ident[:], in_=ident_d[:, :]), "ident", writes=["ident"])
    P.op("dve", lambda e: e.memset(C.ones[:], 1.0), writes=["ones"])


def build_xT(C, st, src, n_tok, tag, f32_copy=None):
    nc, P = C.nc, C.P
    nt = n_tok // 128
    xT = _sb(C, st, "xT" + tag, [128, 16, n_tok], BF16)
    stg = [_sb(C, st, "xs" + tag, [128, D]) for _ in range(2)]
    for t in range(nt):
        s = stg[t % 2]
        sk = ("xs", tag, t % 2)
        P.dma("sp", (lambda s=s, t=t: lambda e: e.dma_start(out=s[:], in_=src[t * 128:(t + 1) * 128, :]))(), sk, writes=[sk])
        for q in range(4):
            bank = C.pb[q % 2]
            bk = f"pb{q % 2}"
            for i in range(4):
                c = q * 4 + i
                P.op("pe", (lambda s=s, c=c, bank=bank, i=i: lambda e: e.transpose(bank[:, i * 128:(i + 1) * 128], s[:, c * 128:(c + 1) * 128], C.ident[:]))(),
                     reads=[sk, "ident"], writes=[bk])
            eng = "act" if q % 2 == 0 else "dve"
            dst = xT[:, q * 4:(q + 1) * 4, t * 128:(t + 1) * 128]
            srcp = bank[:].rearrange("p (i k) -> p i k", i=4)
            if eng == "act":
                P.op("act", (lambda dst=dst, srcp=srcp: lambda e: e.copy(out=dst, in_=srcp))(), reads=[bk], writes=[("xT", tag)])
            else:
                P.op("dve", (lambda dst=dst, srcp=srcp: lambda e: e.tensor_copy(out=dst, in_=srcp))(), reads=[bk], writes=[("xT", tag)])
            if f32_copy is not None:
                dst2 = f32_copy[:, q * 4:(q + 1) * 4, t * 128:(t + 1) * 128]
                P.op("dve", (lambda dst2=dst2, srcp=srcp: lambda e: e.tensor_copy(out=dst2, in_=srcp))(), reads=[bk], writes=[("xT32", tag)])
    return xT


def linear(C, src, n_tok, w, col_ranges, dst, tag):
    nc, P = C.nc, C.P
    P.stage = C.pfx + "lin_" + tag
    nt = n_tok // 128
    with ExitStack() as st:
        xT = build_xT(C, st, src, n_tok, tag)
        wb = [_sb(C, st, "wb" + tag, [128, 16, 512], BF16) for _ in range(2)]
        ost = [_sb(C, st, "os" + tag, [128, 512]) for _ in range(4)]
        groups = []
        o = 0
        for (c0, c1) in col_ranges:
            c = c0
            while c < c1:
                n = min(512, c1 - c)
                groups.append((c, n, o))
                c += n; o += n
        wv = w.rearrange("(c p) n -> p c n", p=128)
        k = 0
        for gi, (c0, n, o0) in enumerate(groups):
            wt = wb[gi % 2]
            wk = ("wb", tag, gi % 2)
            P.dma("pool", (lambda wt=wt, c0=c0, n=n: lambda e: e.dma_start(out=wt[:, :, 0:n], in_=wv[:, :, c0:c0 + n]))(), wk, writes=[wk])
            for t in range(nt):
                bank = C.pb[2 + k % 4]; bk = f"pb{2 + k % 4}"
                for c in range(16):
                    P.op("pe", (lambda bank=bank, c=c, t=t, wt=wt, n=n: lambda e: e.matmul(bank[:, 0:n], lhsT=xT[:, c, t * 128:(t + 1) * 128], rhs=wt[:, c, 0:n], start=(c == 0), stop=(c == 15)))(),
                         reads=[("xT", tag), wk], writes=[bk])
                os_ = ost[k % 4]; ok = ("os", tag, k % 4)
                if k % 2 == 0:
                    P.op("act", (lambda os_=os_, bank=bank, n=n: lambda e: e.copy(out=os_[:, 0:n], in_=bank[:, 0:n]))(), reads=[bk], writes=[ok])
                else:
                    P.op("dve", (lambda os_=os_, bank=bank, n=n: lambda e: e.tensor_copy(out=os_[:, 0:n], in_=bank[:, 0:n]))(), reads=[bk], writes=[ok])
                P.dma("sp", (lambda os_=os_, t=t, o0=o0, n=n: lambda e: e.dma_start(out=dst[t * 128:(t + 1) * 128, o0:o0 + n], in_=os_[:, 0:n]))(), ok, reads=[ok], writes=[("dst", tag, t, gi)])
                k += 1
    P.barrier()


def resid_ln(C, x, m, gB_d, bB_d, out, n_tok, tag):
    nc, P = C.nc, C.P
    P.stage = C.pfx + "ln_" + tag
    nt = n_tok // 128
    with ExitStack() as st:
        gB = _sb(C, st, "gB", [128, D]); bB = _sb(C, st, "bB", [128, D])
        P.dma("sp", _dma(gB[:], gB_d[:, :]), ("gB", tag), writes=[("gB", tag)])
        P.dma("sp", _dma(bB[:], bB_d[:, :]), ("bB", tag), writes=[("bB", tag)])
        xs = [_sb(C, st, "rx", [128, D]) for _ in range(2)]
        ms = [_sb(C, st, "rm", [128, D]) for _ in range(2)]
        rs = [_sb(C, st, "rr", [128, D]) for _ in range(2)]
        mv = [_sb(C, st, "rmv", [128, 2]) for _ in range(2)]
        rstd = [_sb(C, st, "rrs", [128, 1]) for _ in range(2)]
        nb = [_sb(C, st, "rnb", [128, 1]) for _ in range(2)]

        def front(t):
            i = t % 2
            xk, mk, rk, vk = ("rx", tag, i), ("rm", tag, i), ("rr", tag, i), ("rmv", i)
            P.dma("sp", _dma(xs[i][:], x[t * 128:(t + 1) * 128, :]), xk, writes=[xk])
            P.dma("sp", _dma(ms[i][:], m[t * 128:(t + 1) * 128, :]), mk, writes=[mk])
            P.op("dve", _stt(rs[i][:], xs[i][:], ALPHA, ms[i][:], ALU.mult, ALU.add), reads=[xk, mk], writes=[rk])
            P.op("act", _act(xs[i][:], rs[i][:], AF.Identity, accum=mv[i][:, 0:1]), reads=[rk], writes=[vk, xk])
            P.op("act", _act(xs[i][:], rs[i][:], AF.Square, accum=mv[i][:, 1:2]), reads=[rk], writes=[vk, xk])

        def back(t):
            i = t % 2
            xk, mk, rk, vk, sk, nk = ("rx", tag, i), ("rm", tag, i), ("rr", tag, i), ("rmv", i), ("rrs", i), ("rnb", i)
            P.op("dve", _ts(mv[i][:], mv[i][:], 1.0 / D), reads=[vk], writes=[vk])
            P.op("dve", _tt(nb[i][:], mv[i][:, 0:1], mv[i][:, 0:1], ALU.mult), reads=[vk], writes=[nk])
            P.op("dve", _tt(rstd[i][:], mv[i][:, 1:2], nb[i][:], ALU.subtract), reads=[vk, nk], writes=[sk])
            P.op("dve", _ts(rstd[i][:], rstd[i][:], EPS, None, ALU.add), reads=[sk], writes=[sk])
            P.op("act", _sqrt(rstd[i][:], rstd[i][:]), reads=[sk], writes=[sk])
            P.op("dve", _rcp(rstd[i][:], rstd[i][:]), reads=[sk], writes=[sk])
            P.op("dve", _stt(nb[i][:], mv[i][:, 0:1], -1.0, rstd[i][:], ALU.mult, ALU.mult), reads=[vk, sk], writes=[nk])
            P.op("act", _act(rs[i][:], rs[i][:], AF.Identity, bias=nb[i][:, 0:1], scale=rstd[i][:, 0:1]), reads=[rk, sk, nk], writes=[rk])
            P.op("dve", _tt(ms[i][:], rs[i][:], gB[:], ALU.mult), reads=[rk, ("gB", tag)], writes=[mk])
            P.op("pool", _tt(ms[i][:], ms[i][:], bB[:], ALU.add), reads=[mk, ("bB", tag)], writes=[mk])
            P.dma("sp", _dma(out[t * 128:(t + 1) * 128, :], ms[i][:]), mk, reads=[mk], writes=[("rout", tag, t)])

        for t in range(nt + 1):
            if t < nt:
                front(t)
            if t >= 1:
                back(t - 1)
    P.barrier()


class LNEpi:
    def __init__(self, C, st, x, gB_d, bB_d, out, tag):
        self.C, self.x, self.out, self.tag = C, x, out, tag
        P = C.P
        self.gB = _sb(C, st, "egB", [128, D]); self.bB = _sb(C, st, "ebB", [128, D])
        P.dma("sp", _dma(self.gB[:], gB_d[:, :]), ("egB", tag), writes=[("egB", tag)])
        P.dma("sp", _dma(self.bB[:], bB_d[:, :]), ("ebB", tag), writes=[("ebB", tag)])
        self.xs = [_sb(C, st, "ex", [128, D]) for _ in range(2)]
        self.rs = [_sb(C, st, "er", [128, D]) for _ in range(2)]
        self.mv = [_sb(C, st, "emv", [128, 2]) for _ in range(2)]
        self.rstd = [_sb(C, st, "ers", [128, 1]) for _ in range(2)]
        self.nb = [_sb(C, st, "enb", [128, 1]) for _ in range(2)]

    def front(self, t, row0, msrc, mkeys):
        P, tag, i = self.C.P, self.tag, t % 2
        xk, rk, vk = ("ex", tag, i), ("er", tag, i), ("emv", tag, i)
        P.dma("sp", _dma(self.xs[i][:], self.x[row0:row0 + 128, :]), xk, writes=[xk])
        P.op("dve", _stt(self.rs[i][:], self.xs[i][:], ALPHA, msrc, ALU.mult, ALU.add), reads=[xk] + list(mkeys), writes=[rk] + list(mkeys))
        P.op("act", _act(self.xs[i][:], self.rs[i][:], AF.Identity, accum=self.mv[i][:, 0:1]), reads=[rk], writes=[vk, xk])
        P.op("act", _act(self.xs[i][:], self.rs[i][:], AF.Square, accum=self.mv[i][:, 1:2]), reads=[rk], writes=[vk, xk])

    def back(self, t, row0):
        P, tag, i = self.C.P, self.tag, t % 2
        mv, rstd, nb, rs, xs = self.mv[i], self.rstd[i], self.nb[i], self.rs[i], self.xs[i]
        xk, rk, vk, sk, nk = ("ex", tag, i), ("er", tag, i), ("emv", tag, i), ("ers", tag, i), ("enb", tag, i)
        P.op("dve", _ts(mv[:], mv[:], 1.0 / D), reads=[vk], writes=[vk])
        P.op("dve", _tt(nb[:], mv[:, 0:1], mv[:, 0:1], ALU.mult), reads=[vk], writes=[nk])
        P.op("dve", _tt(rstd[:], mv[:, 1:2], nb[:], ALU.subtract), reads=[vk, nk], writes=[sk])
        P.op("dve", _ts(rstd[:], rstd[:], EPS, None, ALU.add), reads=[sk], writes=[sk])
        P.op("act", _sqrt(rstd[:], rstd[:]), reads=[sk], writes=[sk])
        P.op("dve", _rcp(rstd[:], rstd[:]), reads=[sk], writes=[sk])
        P.op("dve", _stt(nb[:], mv[:, 0:1], -1.0, rstd[:], ALU.mult, ALU.mult), reads=[vk, sk], writes=[nk])
        P.op("act", _act(rs[:], rs[:], AF.Identity, bias=nb[:, 0:1], scale=rstd[:, 0:1]), reads=[rk, sk, nk], writes=[rk])
        P.op("dve", _tt(xs[:], rs[:], self.gB[:], ALU.mult), reads=[rk, ("egB", tag)], writes=[xk])
        P.op("pool", _tt(xs[:], xs[:], self.bB[:], ALU.add), reads=[xk, ("ebB", tag)], writes=[xk])
        P.dma("sp", _dma(self.out[row0:row0 + 128, :], xs[:]), xk, reads=[xk], writes=[("eout", tag, t)])


def linear_ln(C, src, w, x, gB_d, bB_d, out, tag):
    nc, P = C.nc, C.P
    P.stage = C.pfx + "linln_" + tag
    nt = T // 128
    with ExitStack() as st:
        xT = _sb(C, st, "lxT", [128, 16, T], BF16)
        wt = _sb(C, st, "lw", [128, 16, D], BF16)
        wv = w.rearrange("(c p) n -> p c n", p=128)
        for g in range(4):
            P.dma("pool", _dma(wt[:, :, g * 512:(g + 1) * 512], wv[:, :, g * 512:(g + 1) * 512]), ("lw", g), writes=[("lw", g)])
        with ExitStack() as s1:
            stg = [_sb(C, s1, "lxs", [128, D]) for _ in range(2)]
            for t in range(nt):
                sk = ("lxs", t % 2)
                P.dma("sp", _dma(stg[t % 2][:], src[t * 128:(t + 1) * 128, :]), sk, writes=[sk])
                _transpose_blocks(C, stg[t % 2], 16, xT[:, :, t * 128:(t + 1) * 128], [sk], "lxT", 0)
        P.barrier()
        ep = LNEpi(C, st, x, gB_d, bB_d, out, tag)
        for t in range(nt):
            base = (t % 2) * 4
            keys = [f"pb{base + g}" for g in range(4)]
            for g in range(4):
                for c in range(16):
                    P.op("pe", _mm(C.pb[base + g][:, :], xT[:, c, t * 128:(t + 1) * 128], wt[:, c, g * 512:(g + 1) * 512], c == 0, c == 15), reads=["lxT", ("lw", g)], writes=[keys[g]])
            ep.front(t, t * 128, C.ps_all[:, base * 512:base * 512 + 2048], keys)
            if t >= 1:
                ep.back(t - 1, (t - 1) * 128)
        ep.back(nt - 1, (nt - 1) * 128)
    P.barrier()


MOE_STOP = None
MOE_DBG = None


def moe(C, h, w_gu, w_down, rw_d, rbB_d, f_out, tag, ln=None):
    nc, P = C.nc, C.P
    HT = 1024
    for half_ in range(2 if MOE_STOP is None else 1):
        _moe_half(C, h, w_gu, w_down, rw_d, rbB_d, f_out, tag, half_, ln)


def _moe_half(C, h, w_gu, w_down, rw_d, rbB_d, f_out, tag, half, ln=None):
    nc, P = C.nc, C.P
    P.stage = C.pfx + "moe"
    HT = 1024
    if True:
        with ExitStack() as st:
            hsrc = h[half * HT:(half + 1) * HT, :]
            gateT = _sb(C, st, "gateT", [16, HT], BF16)
            sel16 = _sb(C, st, "sel16", [16, 16, 128], BF16)
            xT = _sb(C, st, "mxT", [128, 16, HT], BF16)
            yacc = _sb(C, st, "yacc", [128, 16, HT])
            dbg_sb = _sb(C, st, "dbg_sb", [128, 512])
            P.op("dve", lambda e: e.memset(dbg_sb[:], 0.0), writes=["dbg"])
            st1 = ExitStack()
            xT32 = _sb(C, st1, "xT32", [128, 16, 128])
            rw = _sb(C, st1, "rw", [128, 16, 16])
            rbB = _sb(C, st1, "rbB", [128, 16])
            gate = _sb(C, st1, "gate", [128, 8, 16])
            P.dma("sp", lambda e: e.dma_start(out=rw[:], in_=rw_d.rearrange("(c p) n -> p c n", p=128)), ("rw", tag, half), writes=["rw"])
            P.dma("sp", lambda e: e.dma_start(out=rbB[:], in_=rbB_d[:, :]), ("rbB", tag, half), writes=["rbB"])
            for ex_ in range(16):
                P.op("dve", (lambda ex_=ex_: lambda e: e.tensor_copy(out=sel16[:, ex_, :], in_=C.ident[0:16, ex_:ex_ + 1].to_broadcast([16, 128])))(), reads=["ident"], writes=["sel16"])
            stg = [_sb(C, st1, "mxs", [128, D]) for _ in range(2)]
            sm = {n: _sb(C, st1, "g" + n, [128, 16]) for n in ["aff", "sel", "msk", "t1", "t2", "w"]}
            sm4 = {n: _sb(C, st1, "g4" + n, [128, 4]) for n in ["m1", "m2", "gs", "gm"]}
            sm1 = {n: _sb(C, st1, "g1" + n, [128, 1]) for n in ["a", "b"]}
            for t in range(HT // 128):
                s = stg[t % 2]; sk = ("mxs", t % 2)
                P.dma("sp", (lambda s=s, t=t: lambda e: e.dma_start(out=s[:], in_=hsrc[t * 128:(t + 1) * 128, :]))(), sk, writes=[sk])
                for q in range(4):
                    bank = C.pb[q % 2]; bk = f"pb{q % 2}"
                    for i in range(4):
                        c = q * 4 + i
                        P.op("pe", (lambda s=s, c=c, bank=bank, i=i: lambda e: e.transpose(bank[:, i * 128:(i + 1) * 128], s[:, c * 128:(c + 1) * 128], C.ident[:]))(), reads=[sk, "ident"], writes=[bk])
                    srcp = bank[:].rearrange("p (i k) -> p i k", i=4)
                    P.op("act", (lambda q=q, t=t, srcp=srcp: lambda e: e.copy(out=xT[:, q * 4:(q + 1) * 4, t * 128:(t + 1) * 128], in_=srcp))(), reads=[bk], writes=["mxT", bk])
                    P.op("dve", (lambda q=q, srcp=srcp: lambda e: e.tensor_copy(out=xT32[:, q * 4:(q + 1) * 4, :], in_=srcp))(), reads=[bk], writes=["xT32", bk])
                lb = C.pb[2]
                for c in range(16):
                    P.op("pe", (lambda c=c: lambda e: e.matmul(lb[:, 0:16], lhsT=xT32[:, c, :], rhs=rw[:, c, :], start=(c == 0), stop=(c == 15)))(), reads=["xT32", "rw"], writes=["pb2"])
                aff, sel, msk, t1, t2, wv = sm["aff"], sm["sel"], sm["msk"], sm["t1"], sm["t2"], sm["w"]
                m1, m2, gs, gm = sm4["m1"], sm4["m2"], sm4["gs"], sm4["gm"]
                a1, b1 = sm1["a"], sm1["b"]
                G = ["gsm"]
                P.op("act", lambda e: e.activation(out=aff[:], in_=lb[:, 0:16], func=AF.Sigmoid), reads=["pb2"], writes=G)
                P.op("dve", lambda e: e.tensor_tensor(out=sel[:], in0=aff[:], in1=rbB[:], op=ALU.add), reads=G + ["rbB"], writes=G)
                sel3 = sel[:].rearrange("p (g k) -> p g k", g=4)
                t13 = t1[:].rearrange("p (g k) -> p g k", g=4)
                P.op("dve", lambda e: e.tensor_reduce(out=m1[:], in_=sel3, axis=AX.X, op=ALU.max), reads=G, writes=G)
                P.op("dve", lambda e: e.tensor_tensor(out=t13, in0=sel3, in1=m1[:, :, None].to_broadcast([128, 4, 4]), op=ALU.is_equal), reads=G, writes=G)
                P.op("dve", lambda e: e.scalar_tensor_tensor(out=t1[:], in0=t1[:], scalar=NEG, in1=sel[:], op0=ALU.mult, op1=ALU.add), reads=G, writes=G)
                P.op("dve", lambda e: e.tensor_reduce(out=m2[:], in_=t13, axis=AX.X, op=ALU.max), reads=G, writes=G)
                P.op("dve", lambda e: e.tensor_tensor(out=gs[:], in0=m1[:], in1=m2[:], op=ALU.add), reads=G, writes=G)
                P.op("dve", lambda e: e.tensor_reduce(out=a1[:], in_=gs[:], axis=AX.X, op=ALU.max), reads=G, writes=G)
                P.op("dve", lambda e: e.tensor_scalar(out=gm[:], in0=gs[:], scalar1=a1[:, 0:1], scalar2=None, op0=ALU.is_equal), reads=G, writes=G)
                msk3 = msk[:].rearrange("p (g k) -> p g k", g=4)
                P.op("dve", lambda e: e.tensor_copy(out=msk3, in_=gm[:, :, None].to_broadcast([128, 4, 4])), reads=G, writes=G)
                P.op("dve", lambda e: e.tensor_tensor(out=t1[:], in0=sel[:], in1=msk[:], op=ALU.mult), reads=G, writes=G)
                P.op("dve", lambda e: e.tensor_scalar(out=t2[:], in0=msk[:], scalar1=-1.0, scalar2=-NEG, op0=ALU.add, op1=ALU.mult), reads=G, writes=G)
                P.op("dve", lambda e: e.tensor_tensor(out=t1[:], in0=t1[:], in1=t2[:], op=ALU.add), reads=G, writes=G)
                P.op("dve", lambda e: e.tensor_reduce(out=a1[:], in_=t1[:], axis=AX.X, op=ALU.max), reads=G, writes=G)
                P.op("dve", lambda e: e.tensor_scalar(out=wv[:], in0=t1[:], scalar1=a1[:, 0:1], scalar2=None, op0=ALU.is_equal), reads=G, writes=G)
                P.op("dve", lambda e: e.scalar_tensor_tensor(out=t1[:], in0=wv[:], scalar=NEG, in1=t1[:], op0=ALU.mult, op1=ALU.add), reads=G, writes=G)
                P.op("dve", lambda e: e.tensor_reduce(out=b1[:], in_=t1[:], axis=AX.X, op=ALU.max), reads=G, writes=G)
                P.op("dve", lambda e: e.tensor_scalar(out=t2[:], in0=t1[:], scalar1=b1[:, 0:1], scalar2=None, op0=ALU.is_equal), reads=G, writes=G)
                P.op("dve", lambda e: e.tensor_tensor(out=wv[:], in0=wv[:], in1=t2[:], op=ALU.add), reads=G, writes=G)
                P.op("dve", lambda e: e.tensor_tensor(out=wv[:], in0=wv[:], in1=aff[:], op=ALU.mult), reads=G, writes=G)
                P.op("dve", lambda e: e.tensor_reduce(out=a1[:], in_=wv[:], axis=AX.X, op=ALU.add), reads=G, writes=G)
                P.op("dve", lambda e: e.reciprocal(out=a1[:], in_=a1[:]), reads=G, writes=G)
                P.op("dve", (lambda t=t: lambda e: e.tensor_scalar(out=gate[:, t, :], in0=wv[:], scalar1=a1[:, 0:1], scalar2=None, op0=ALU.mult))(), reads=G, writes=["gate"])
                if t == 0 and MOE_DBG is not None:
                    P.op("dve", lambda e: e.tensor_copy(out=dbg_sb[:, 0:16], in_=gate[:, 0, :]), reads=["gate"], writes=["dbg"])
                    P.op("dve", lambda e: e.tensor_copy(out=dbg_sb[:, 400:416], in_=aff[:]), reads=G, writes=["dbg"])
                    P.op("dve", lambda e: e.tensor_copy(out=dbg_sb[:, 416:432], in_=wv[:]), reads=G, writes=["dbg"])
                P.op("pe", (lambda t=t: lambda e: e.transpose(C.pb[3][0:16, 0:128], gate[:, t, :], C.ident[:]))(), reads=["gate", "ident"], writes=["pb3"])
                P.op("act", (lambda t=t: lambda e: e.copy(out=gateT[:, t * 128:(t + 1) * 128], in_=C.pb[3][0:16, 0:128]))(), reads=["pb3"], writes=["gateT"])
            P.barrier()
            st1.close()
            if MOE_STOP == "build":
                return
            st2 = ExitStack()
            wgu = [_sb(C, st2, "wgu", [128, 16, 1024], BF16) for _ in range(2)]
            wdn = _sb(C, st2, "wdn", [128, 4, D], BF16)
            gB = _sb(C, st2, "mgB", [128, HT], BF16)
            hm = [_sb(C, st2, "hm", [128, 4, 512], BF16) for _ in range(2)]
            sa = [_sb(C, st2, "sa", [128, 512], BF16) for _ in range(2)]
            sg = [_sb(C, st2, "sg", [128, 512], BF16) for _ in range(2)]
            gv = w_gu.rearrange("x (c p) n -> x p c n", p=128)
            dv = w_down.rearrange("x (c p) n -> x p c n", p=128)
            P.dma("pool", lambda e: e.dma_start(out=wgu[0][:], in_=gv[0]), ("wgu", 0), writes=[("wgu", 0)])
            kk = 0
            for ex in range(16 if MOE_STOP is None else 1):
                wg = wgu[ex % 2]; wgk = ("wgu", ex % 2)
                if ex + 1 < 16:
                    P.dma("pool", (lambda ex=ex: lambda e: e.dma_start(out=wgu[(ex + 1) % 2][:], in_=gv[ex + 1]))(), ("wgu", (ex + 1) % 2), writes=[("wgu", (ex + 1) % 2)])
                P.dma("pool", (lambda ex=ex: lambda e: e.dma_start(out=wdn[:], in_=dv[ex]))(), "wdn", writes=["wdn"])
                for tb in range(HT // 512):
                    P.op("pe", (lambda ex=ex, tb=tb: lambda e: e.matmul(C.pb[2][:, :], lhsT=sel16[:, ex, :], rhs=gateT[:, tb * 512:(tb + 1) * 512], start=True, stop=True))(), reads=["sel16", "gateT"], writes=["pb2"])
                    P.op("act", (lambda tb=tb: lambda e: e.copy(out=gB[:, tb * 512:(tb + 1) * 512], in_=C.pb[2][:, :]))(), reads=["pb2"], writes=[("mgB", tb)])
                for tb in range(HT // 512):
                    hmt = hm[tb % 2]; hk = ("hm", tb % 2)
                    for ffc in range(4):
                        pa = C.pb[4 + (kk % 2) * 2]; pak = f"pb{4 + (kk % 2) * 2}"
                        pbb = C.pb[5 + (kk % 2) * 2]; pbk = f"pb{5 + (kk % 2) * 2}"
                        for c in range(16):
                            P.op("pe", (lambda pa=pa, wg=wg, c=c, ffc=ffc, tb=tb: lambda e: e.matmul(pa[:, :], lhsT=wg[:, c, ffc * 128:(ffc + 1) * 128], rhs=xT[:, c, tb * 512:(tb + 1) * 512], start=(c == 0), stop=(c == 15)))(), reads=[wgk, "mxT"], writes=[pak])
                        for c in range(16):
                            P.op("pe", (lambda pbb=pbb, wg=wg, c=c, ffc=ffc, tb=tb: lambda e: e.matmul(pbb[:, :], lhsT=wg[:, c, 512 + ffc * 128:512 + (ffc + 1) * 128], rhs=xT[:, c, tb * 512:(tb + 1) * 512], start=(c == 0), stop=(c == 15)))(), reads=[wgk, "mxT"], writes=[pbk])
                        sat = sa[kk % 2]; sgt = sg[kk % 2]; sak = ("sa", kk % 2); sgk = ("sg", kk % 2)
                        P.op("act", (lambda sat=sat, pa=pa: lambda e: e.activation(out=sat[:], in_=pa[:, :], func=AF.Silu))(), reads=[pak], writes=[sak])
                        P.op("dve", (lambda sgt=sgt, sat=sat, tb=tb: lambda e: e.tensor_tensor(out=sgt[:], in0=sat[:], in1=gB[:, tb * 512:(tb + 1) * 512], op=ALU.mult))(), reads=[sak, ("mgB", tb)], writes=[sgk])
                        P.op("dve", (lambda hmt=hmt, ffc=ffc, sgt=sgt, pbb=pbb: lambda e: e.tensor_tensor(out=hmt[:, ffc, :], in0=sgt[:], in1=pbb[:, :], op=ALU.mult))(), reads=[sgk, pbk], writes=[hk])
                        kk += 1
                for tb in range(HT // 512):
                    hmt = hm[tb % 2]; hk = ("hm", tb % 2)
                    for dc in range(16):
                        po = C.pb[dc % 2]; pok = f"pb{dc % 2}"
                        for ffc in range(4):
                            P.op("pe", (lambda po=po, ffc=ffc, dc=dc, hmt=hmt: lambda e: e.matmul(po[:, :], lhsT=wdn[:, ffc, dc * 128:(dc + 1) * 128], rhs=hmt[:, ffc, :], start=(ffc == 0), stop=(ffc == 3)))(), reads=["wdn", hk], writes=[pok])
                        ydst = yacc[:, dc, tb * 512:(tb + 1) * 512]
                        if ex == 0:
                            P.op("dve", (lambda ydst=ydst, po=po: lambda e: e.tensor_copy(out=ydst, in_=po[:, :]))(), reads=[pok], writes=[("yacc", dc, tb)])
                        else:
                            P.op("dve", (lambda ydst=ydst, po=po: lambda e: e.tensor_tensor(out=ydst, in0=ydst, in1=po[:, :], op=ALU.add))(), reads=[pok, ("yacc", dc, tb)], writes=[("yacc", dc, tb)])
            if MOE_DBG is not None:
                P.op("dve", lambda e: e.tensor_copy(out=dbg_sb[:, 16:144], in_=gB[:, 0:128]), reads=[("mgB", 0)], writes=["dbg"])
                P.op("dve", lambda e: e.tensor_copy(out=dbg_sb[:, 144:272], in_=hm[0][:, 0, 0:128]), reads=[("hm", 0)], writes=["dbg"])
                P.op("dve", lambda e: e.tensor_copy(out=dbg_sb[:, 272:400], in_=yacc[:, 0, 0:128]), reads=[("yacc", 0, 0)], writes=["dbg"])
                P.op("dve", lambda e: e.tensor_copy(out=dbg_sb[0:16, 432:512], in_=gateT[:, 0:80]), reads=["gateT"], writes=["dbg"])
                P.dma("sp", lambda e: e.dma_start(out=MOE_DBG[:, :], in_=dbg_sb[:]), "dbg", reads=["dbg"], writes=["dbgout"])
            P.barrier()
            st2.close()
            if ln is None:
                fo = [_sb(C, st, "fo", [128, D]) for _ in range(2)]
                for t in range(HT // 128):
                    fot = fo[t % 2]; fk = ("fo", t % 2)
                    for q in range(4):
                        bank = C.pb[q % 2]; bk = f"pb{q % 2}"
                        for i in range(4):
                            dc = q * 4 + i
                            P.op("pe", _tr(bank[:, i * 128:(i + 1) * 128], yacc[:, dc, t * 128:(t + 1) * 128], C.ident[:]), reads=[("yacc", dc, t // 4), "ident"], writes=[bk])
                        P.op("act", _acp(fot[:, q * 512:(q + 1) * 512], bank[:, :]), reads=[bk], writes=[fk])
                    P.dma("sp", _dma(f_out[half * HT + t * 128: half * HT + (t + 1) * 128, :], fot[:]), fk, reads=[fk], writes=[("fout", half, t)])
            else:
                gB_d, bB_d, out_d = ln
                ep = LNEpi(C, st, h, gB_d, bB_d, out_d, "moe")
                ntl = HT // 128
                for t in range(ntl):
                    base = (t % 2) * 4
                    keys = [f"pb{base + q}" for q in range(4)]
                    for q in range(4):
                        for i in range(4):
                            dc = q * 4 + i
                            P.op("pe", _tr(C.pb[base + q][:, i * 128:(i + 1) * 128], yacc[:, dc, t * 128:(t + 1) * 128], C.ident[:]), reads=[("yacc", dc, t // 4), "ident"], writes=[keys[q]])
                    ep.front(t, half * HT + t * 128, C.ps_all[:, base * 512:base * 512 + 2048], keys)
                    if t >= 1:
                        ep.back(t - 1, half * HT + (t - 1) * 128)
                ep.back(ntl - 1, half * HT + (ntl - 1) * 128)
        P.barrier()


def _tt(out, in0, in1, op):
    return lambda e: e.tensor_tensor(out=out, in0=in0, in1=in1, op=op)


def _ts(out, in0, s1, s2=None, op0=ALU.mult, op1=None, accum=None):
    if accum is not None:
        return lambda e: e.tensor_scalar(out=out, in0=in0, scalar1=s1, scalar2=s2, op0=op0, op1=op1, accum_out=accum)
    if op1 is None:
        return lambda e: e.tensor_scalar(out=out, in0=in0, scalar1=s1, scalar2=None, op0=op0)
    return lambda e: e.tensor_scalar(out=out, in0=in0, scalar1=s1, scalar2=s2, op0=op0, op1=op1)


def _stt(out, in0, scalar, in1, op0, op1):
    return lambda e: e.scalar_tensor_tensor(out=out, in0=in0, scalar=scalar, in1=in1, op0=op0, op1=op1)


def _act(out, in_, func, bias=None, scale=None, accum=None):
    kw = {}
    if bias is not None:
        kw["bias"] = bias
    if scale is not None:
        kw["scale"] = scale
    if accum is not None:
        kw["accum_out"] = accum
    return lambda e: e.activation(out=out, in_=in_, func=func, **kw)


def _mm(out, lhsT, rhs, start, stop):
    return lambda e: e.matmul(out, lhsT=lhsT, rhs=rhs, start=start, stop=stop)


def _tr(out, in_, ident):
    return lambda e: e.transpose(out, in_, ident)


def _acp(out, in_):
    return lambda e: e.copy(out=out, in_=in_)


def _cp(out, in_):
    return lambda e: e.tensor_copy(out=out, in_=in_)


def _dma(out, in_):
    return lambda e: e.dma_start(out=out, in_=in_)


def _red(out, in_, op):
    return lambda e: e.tensor_reduce(out=out, in_=in_, axis=AX.X, op=op)


def _rcp(out, in_):
    return lambda e: e.reciprocal(out=out, in_=in_)


def _mset(out, v):
    return lambda e: e.memset(out, v)


def _sqrt(out, in_):
    return lambda e: e.sqrt(out=out, in_=in_)


def _rope(P, src, dst, cs, H, dh, t1, t2, rkeys, wkey, tk):
    hf = dh // 2
    s3 = src.rearrange("p (h d) -> p h d", h=H)
    d3 = dst.rearrange("p (h d) -> p h d", h=H)
    x1 = s3[:, :, 0:hf]; x2 = s3[:, :, hf:dh]
    cosB = cs[:, 0:1, :].to_broadcast([128, H, hf])
    sinB = cs[:, 1:2, :].to_broadcast([128, H, hf])
    a = t1[:, 0:H * hf].rearrange("p (h d) -> p h d", h=H)
    b = t2[:, 0:H * hf].rearrange("p (h d) -> p h d", h=H)
    k1, k2 = (tk, 1), (tk, 2)
    P.op("dve", _tt(a, x1, cosB, ALU.mult), reads=rkeys, writes=[k1])
    P.op("dve", _tt(b, x2, sinB, ALU.mult), reads=rkeys, writes=[k2])
    P.op("dve", _tt(d3[:, :, 0:hf], a, b, ALU.subtract), reads=[k1, k2], writes=[wkey])
    P.op("dve", _tt(a, x2, cosB, ALU.mult), reads=rkeys, writes=[k1])
    P.op("dve", _tt(b, x1, sinB, ALU.mult), reads=rkeys, writes=[k2])
    P.op("dve", _tt(d3[:, :, hf:dh], a, b, ALU.add), reads=[k1, k2, wkey], writes=[wkey])


def _transpose_blocks(C, src, nblk, dst3, rkeys, wkey, eng_toggle=0):
    P = C.P
    b0 = 0
    g = 0
    while b0 < nblk:
        n = min(4, nblk - b0)
        bi = (g + eng_toggle) % 2
        bank = C.pb[bi]; bk = f"pb{bi}"
        for i in range(n):
            P.op("pe", _tr(bank[:, i * 128:(i + 1) * 128], src[:, (b0 + i) * 128:(b0 + i + 1) * 128], C.ident[:]), reads=list(rkeys) + ["ident"], writes=[bk])
        srcp = bank[:, 0:n * 128].rearrange("p (i k) -> p i k", i=n)
        if bi == 0:
            P.op("act", _acp(dst3[:, b0:b0 + n, :], srcp), reads=[bk], writes=[wkey])
        else:
            P.op("dve", _cp(dst3[:, b0:b0 + n, :], srcp), reads=[bk], writes=[wkey])
        b0 += n
        g += 1


RET_G = [1.0 - 2.0 ** (-5.0 - h) for h in range(4)]


def mixer_ret(C, proj, pkv, mix, cst, cs_key="cs_own", s_in=None, s_out=None):
    P = C.P
    P.stage = C.pfx + "ret"
    with ExitStack() as st:
        decT = _sb(C, st, "decT", [128, 512]); xi = _sb(C, st, "xi", [128, 4]); zeta = _sb(C, st, "zeta", [128, 4]); gnB = _sb(C, st, "gnB", [128, 1024])
        P.dma("sp", _dma(decT[:], cst["decT"][:, :]), "decT", writes=["decT"])
        P.dma("sp", _dma(xi[:], cst["xi"][:, :]), "xi", writes=["xi"])
        P.dma("sp", _dma(zeta[:], cst["zeta"][:, :]), "zeta", writes=["zeta"])
        P.dma("sp", _dma(gnB[:], cst["gnB"][:, :]), "gnB", writes=["gnB"])
        S32 = _sb(C, st, "S32", [128, 4, 512]); Sb = _sb(C, st, "Sb", [128, 4, 512], BF16)
        if s_in is None:
            P.op("dve", _mset(S32[:], 0.0), writes=["S32"])
            P.op("dve", _mset(Sb[:], 0.0), writes=["Sb"])
        else:
            P.dma("sp", _dma(S32[:].rearrange("p h n -> p (h n)"), s_in[:, :]), "S32io", writes=["S32"])
            P.op("act", _acp(Sb[:], S32[:]), reads=["S32"], writes=["Sb"])
        qkvg = [_sb(C, st, "qkvg", [128, 4096]) for _ in range(2)]
        csb = [_sb(C, st, "csb", [128, 2, 128]) for _ in range(2)]
        qr = _sb(C, st, "qr", [128, 1024]); kr = _sb(C, st, "kr", [128, 1024]); qx = _sb(C, st, "qx", [128, 1024])
        kz = _sb(C, st, "kz", [128, 1024], BF16); vb = _sb(C, st, "vb", [128, 1024], BF16)
        qT = _sb(C, st, "qT", [128, 8, 128], BF16); kT = _sb(C, st, "kT", [128, 8, 128], BF16); qxT = _sb(C, st, "qxT", [128, 8, 128], BF16)
        am = _sb(C, st, "am", [128, 512], BF16)
        t1 = _sb(C, st, "rt1", [128, 512]); t2 = _sb(C, st, "rt2", [128, 512])
        junk = _sb(C, st, "rjunk", [128, 256])
        st4 = {n: _sb(C, st, "st" + n, [128, 4]) for n in ["s", "q", "m", "r", "nb"]}
        yn = _sb(C, st, "yn", [128, 1024]); sg = _sb(C, st, "sgt", [128, 1024])
        ret = [_sb(C, st, "ret", [128, 1024]) for _ in range(2)]
        SB = [C.pb[5], C.pb[6], C.pb[7], C.pb[2]]; SBK = ["pb5", "pb6", "pb7", "pb2"]
        for ci in range(0 if pkv is not None else 16, 32):
            own = ci >= 16
            t = ci % 16
            buf = qkvg[ci % 2]; bkey = ("qkvg", ci % 2)
            cs = csb[ci % 2]; ckey = ("csb", ci % 2)
            if own:
                P.dma("sp", _dma(buf[:], proj[t * 128:(t + 1) * 128, 0:4096]), bkey, writes=[bkey])
                P.dma("sp", _dma(cs[:], cst[cs_key][t * 128:(t + 1) * 128, :, :]), ckey, writes=[ckey])
            else:
                P.dma("sp", _dma(buf[:, 1024:3072], pkv[t * 128:(t + 1) * 128, :]), bkey, writes=[bkey])
                P.dma("sp", _dma(cs[:], cst["cs_pre"][t * 128:(t + 1) * 128, :, :]), ckey, writes=[ckey])
            _rope(P, buf[:, 1024:2048], kr[:], cs, 4, 256, t1, t2, [bkey, ckey], "kr", "rt")
            P.op("dve", _tt(kz[:].rearrange("p (h d) -> p h d", h=4), kr[:].rearrange("p (h d) -> p h d", h=4), zeta[:, :, None].to_broadcast([128, 4, 256]), ALU.mult), reads=["kr", "zeta"], writes=["kz"])
            P.op("act", _acp(vb[:], buf[:, 2048:3072]), reads=[bkey], writes=["vb"])
            if own:
                _rope(P, buf[:, 0:1024], qr[:], cs, 4, 256, t1, t2, [bkey, ckey], "qr", "rt")
                P.op("dve", _tt(qx[:].rearrange("p (h d) -> p h d", h=4), qr[:].rearrange("p (h d) -> p h d", h=4), xi[:, :, None].to_broadcast([128, 4, 256]), ALU.mult), reads=["qr", "xi"], writes=["qx"])
                _transpose_blocks(C, qr, 8, qT, ["qr"], "qT", 0)
                _transpose_blocks(C, kr, 8, kT, ["kr"], "kT", 0)
                _transpose_blocks(C, qx, 8, qxT, ["qx"], "qxT", 0)
                for h in range(4):
                    for j in range(2):
                        P.op("pe", _mm(C.pb[2][:, h * 128:(h + 1) * 128], kT[:, 2 * h + j, :], qT[:, 2 * h + j, :], j == 0, j == 1), reads=["kT", "qT"], writes=["pb2"])
                P.op("dve", _tt(am[:], C.pb[2][:, :], decT[:], ALU.mult), reads=["pb2", "decT"], writes=["am"])
                for h in range(4):
                    yb = C.pb[3 + h // 2]; ybk = f"pb{3 + h // 2}"
                    o = yb[:, (h % 2) * 256:(h % 2) * 256 + 256]
                    P.op("pe", _mm(o, am[:, h * 128:(h + 1) * 128], vb[:, h * 256:(h + 1) * 256], True, False), reads=["am", "vb"], writes=[ybk])
                    P.op("pe", _mm(o, qxT[:, 2 * h, :], Sb[:, h, 0:256], False, False), reads=["qxT", "Sb"], writes=[ybk])
                    P.op("pe", _mm(o, qxT[:, 2 * h + 1, :], Sb[:, h, 256:512], False, True), reads=["qxT", "Sb"], writes=[ybk])
            for h in range(4):
                for j in range(2):
                    P.op("pe", _mm(SB[h][:, j * 256:(j + 1) * 256], kz[:, h * 256 + j * 128:h * 256 + (j + 1) * 128], vb[:, h * 256:(h + 1) * 256], True, True), reads=["kz", "vb"], writes=[SBK[h]])
            for h in range(4):
                P.op("dve", _stt(S32[:, h, :], S32[:, h, :], RET_G[h] ** 128, SB[h][:, :], ALU.mult, ALU.add), reads=[SBK[h], "S32"], writes=["S32"])
            P.op("act", _acp(Sb[:], S32[:]), reads=["S32"], writes=["Sb"])
            if not own:
                continue
            s_, q_, m_, r_, nb_ = st4["s"], st4["q"], st4["m"], st4["r"], st4["nb"]
            for h in range(4):
                yb = C.pb[3 + h // 2]; ybk = f"pb{3 + h // 2}"
                o = yb[:, (h % 2) * 256:(h % 2) * 256 + 256]
                P.op("act", _act(junk[:], o, AF.Identity, accum=s_[:, h:h + 1]), reads=[ybk], writes=["rjunk", "st_s"])
                P.op("act", _act(junk[:], o, AF.Square, accum=q_[:, h:h + 1]), reads=[ybk], writes=["rjunk", "st_q"])
            P.op("dve", _ts(m_[:], s_[:], 1.0 / 256), reads=["st_s"], writes=["st_m"])
            P.op("dve", _tt(r_[:], m_[:], m_[:], ALU.mult), reads=["st_m"], writes=["st_r"])
            P.op("dve", _stt(r_[:], q_[:], 1.0 / 256, r_[:], ALU.mult, ALU.subtract), reads=["st_q", "st_r"], writes=["st_r"])
            P.op("dve", _ts(r_[:], r_[:], EPS, None, ALU.add), reads=["st_r"], writes=["st_r"])
            P.op("act", _sqrt(r_[:], r_[:]), reads=["st_r"], writes=["st_r"])
            P.op("dve", _rcp(r_[:], r_[:]), reads=["st_r"], writes=["st_r"])
            P.op("dve", _stt(nb_[:], m_[:], -1.0, r_[:], ALU.mult, ALU.mult), reads=["st_m", "st_r"], writes=["st_nb"])
            for h in range(4):
                yb = C.pb[3 + h // 2]; ybk = f"pb{3 + h // 2}"
                o = yb[:, (h % 2) * 256:(h % 2) * 256 + 256]
                P.op("act", _act(yn[:, h * 256:(h + 1) * 256], o, AF.Identity, bias=nb_[:, h:h + 1], scale=r_[:, h:h + 1]), reads=[ybk, "st_r", "st_nb"], writes=["yn"])
            P.op("act", _act(sg[:], buf[:, 3072:4096], AF.Silu), reads=[bkey], writes=["sgt"])
            P.op("pool", _tt(yn[:], yn[:], gnB[:], ALU.mult), reads=["yn", "gnB"], writes=["yn"])
            rt = ret[t % 2]; rk = ("ret", t % 2)
            P.op("pool", _tt(rt[:], yn[:], sg[:], ALU.mult), reads=["yn", "sgt"], writes=[rk])
            P.dma("sp", _dma(mix[t * 128:(t + 1) * 128, 0:1024], rt[:]), rk, reads=[rk], writes=[("mixr", t)])
        if s_out is not None:
            P.dma("sp", _dma(s_out[:, :], S32[:].rearrange("p h n -> p (h n)")), "S32io", reads=["S32"], writes=["s_out"])
    P.barrier()


def mixer_conv(C, proj, phalo, mix, cst):
    P = C.P
    P.stage = C.pfx + "conv"
    NTK = T + 128
    with ExitStack() as st:
        cw = _sb(C, st, "cw", [128, 8, 31]); cv = _sb(C, st, "cv", [128, 3, 8])
        P.dma("sp", _dma(cw[:], cst["conv_w"][:, :, :]), "cw", writes=["cw"])
        P.dma("sp", _dma(cv[:], cst["conv_v"][:, :, :]), "cv", writes=["cv"])
        uT = _sb(C, st, "uT", [128, 8, NTK])
        yT = _sb(C, st, "yT", [128, 8, T])
        gg = [_sb(C, st, "gg", [128, 2048]) for _ in range(2)]
        sig = _sb(C, st, "sig", [128, 1024]); u = _sb(C, st, "u", [128, 1024])
        for ti in range(17):
            buf = gg[ti % 2]; bkey = ("gg", ti % 2)
            if ti == 0:
                P.dma("sp", _dma(buf[:], phalo[:, :]), bkey, writes=[bkey])
            else:
                P.dma("sp", _dma(buf[:], proj[(ti - 1) * 128:ti * 128, 4096:6144]), bkey, writes=[bkey])
            P.op("act", _act(sig[:], buf[:, 1024:2048], AF.Sigmoid), reads=[bkey], writes=["sig"])
            P.op("dve", _tt(u[:], buf[:, 0:1024], sig[:], ALU.mult), reads=[bkey, "sig"], writes=["u"])
            _transpose_blocks(C, u, 8, uT[:, :, ti * 128:(ti + 1) * 128], ["u"], "uT", ti)
        for j in range(8):
            yk = ("yT", j)
            P.op("dve", _ts(yT[:, j, :], uT[:, j, 98:98 + T], cw[:, j, 0:1], cv[:, 0, j:j + 1], ALU.mult, ALU.add), reads=["uT", "cw", "cv"], writes=[yk])
            for k in range(1, 31):
                P.op("dve", _stt(yT[:, j, :], uT[:, j, 98 + k:98 + k + T], cw[:, j, k:k + 1], yT[:, j, :], ALU.mult, ALU.add), reads=["uT", "cw", yk], writes=[yk])
        mean = _sb(C, st, "cmean", [128, 512]); rstd = _sb(C, st, "crstd", [128, 512]); sq = [_sb(C, st, "csq", [128, 512]) for _ in range(2)]
        z = [_sb(C, st, "cz", [128, 512]) for _ in range(2)]
        for tb in range(4):
            sl = slice(tb * 512, (tb + 1) * 512)
            for j in range(8):
                P.op("pe", _mm(C.pb[2][:, :], C.ones[:], yT[:, j, sl], j == 0, j == 7), reads=[("yT", j), "ones"], writes=["pb2"])
            for j in range(8):
                sqt = sq[j % 2]; sqk = ("csq", j % 2)
                P.op("act", _act(sqt[:], yT[:, j, sl], AF.Square), reads=[("yT", j)], writes=[sqk])
                P.op("pe", _mm(C.pb[3][:, :], C.ones[:], sqt[:], j == 0, j == 7), reads=[sqk, "ones"], writes=["pb3"])
            P.op("dve", _ts(mean[:], C.pb[2][:, :], 1.0 / 1024), reads=["pb2"], writes=["cmean"])
            P.op("dve", _tt(rstd[:], mean[:], mean[:], ALU.mult), reads=["cmean"], writes=["crstd"])
            P.op("dve", _stt(rstd[:], C.pb[3][:, :], 1.0 / 1024, rstd[:], ALU.mult, ALU.subtract), reads=["pb3", "crstd"], writes=["crstd"])
            P.op("dve", _ts(rstd[:], rstd[:], EPS, None, ALU.add), reads=["crstd"], writes=["crstd"])
            P.op("act", _sqrt(rstd[:], rstd[:]), reads=["crstd"], writes=["crstd"])
            P.op("dve", _rcp(rstd[:], rstd[:]), reads=["crstd"], writes=["crstd"])
            for j in range(8):
                zt = z[j % 2]; zk = ("cz", j % 2)
                P.op("dve", _tt(zt[:], yT[:, j, sl], mean[:], ALU.subtract), reads=[("yT", j), "cmean"], writes=[zk])
                P.op("dve", _tt(zt[:], zt[:], rstd[:], ALU.mult), reads=[zk, "crstd"], writes=[zk])
                P.op("act", _act(yT[:, j, sl], zt[:], AF.Silu, bias=cv[:, 2, j:j + 1], scale=cv[:, 1, j:j + 1]), reads=[zk, "cv"], writes=[("yT", j)])
        co = [_sb(C, st, "co", [128, 1024]) for _ in range(2)]
        for t in range(16):
            cot = co[t % 2]; ck = ("co", t % 2)
            for g in range(2):
                bank = C.pb[g]; bk = f"pb{g}"
                for i in range(4):
                    j = g * 4 + i
                    P.op("pe", _tr(bank[:, i * 128:(i + 1) * 128], yT[:, j, t * 128:(t + 1) * 128], C.ident[:]), reads=[("yT", j), "ident"], writes=[bk])
                if g == 0:
                    P.op("act", _acp(cot[:, 0:512], bank[:, :]), reads=[bk], writes=[ck])
                else:
                    P.op("dve", _cp(cot[:, 512:1024], bank[:, :]), reads=[bk], writes=[ck])
            P.dma("sp", _dma(mix[t * 128:(t + 1) * 128, 1024:2048], cot[:]), ck, reads=[ck], writes=[("mixc", t)])
    P.barrier()


def _bcast_rows(v):
    v = np.asarray(v, np.float32)
    return np.ascontiguousarray(np.broadcast_to(v[None, :], (128, v.shape[0])))


def _rope_tab(pos, half):
    inv = (10000.0 ** (-np.arange(half, dtype=np.float32) / np.float32(half))).astype(np.float32)
    ang = pos.astype(np.float32)[:, None] * inv[None, :]
    return np.ascontiguousarray(np.stack([np.cos(ang), np.sin(ang)], axis=1).astype(np.float32))


def _common_tail(C, x, mix, w_out, g1, b1, g2, b2, w_gu, w_down, rw, rbB, m, ha, f, out):
    linear_ln(C, mix, w_out, x, g1, b1, ha, "mix")
    moe(C, ha, w_gu, w_down, rw, rbB, f, "moe", ln=(g2, b2, out))


def build_layer0(debug=False):
    nc = bass.Bass("TRN2", target_bir_lowering=False)
    dt = lambda name, shape, kind="ExternalInput": nc.dram_tensor(name, shape, F32, kind=kind).ap()
    x = dt("x", [T, D]); xp = dt("xp", [T, D]); w_in = dt("w_in", [D, 6144]); w_out = dt("w_out", [D, D])
    g1 = dt("mix_g", [128, D]); b1 = dt("mix_b", [128, D]); g2 = dt("ffn_g", [128, D]); b2 = dt("ffn_b", [128, D])
    w_gu = dt("w_gu", [16, D, 1024]); w_down = dt("w_down", [16, 512, D])
    rw = dt("rw", [D, 16]); rbB = dt("rbB", [128, 16]); ident = dt("ident", [128, 128])
    cst = {"cs_own": dt("cs_own", [T, 2, 128]), "cs_pre": dt("cs_pre", [T, 2, 128]), "decT": dt("decT", [128, 512]),
           "xi": dt("xi", [128, 4]), "zeta": dt("zeta", [128, 4]), "gnB": dt("gnB", [128, 1024]),
           "conv_w": dt("conv_w", [128, 8, 31]), "conv_v": dt("conv_v", [128, 3, 8])}
    out = dt("out", [T, D], "ExternalOutput")
    dk = "ExternalOutput" if debug else "Internal"
    proj = dt("proj", [T, 6144], "Internal"); pkv = dt("pkv", [T, 2048], "Internal"); phalo = dt("phalo", [128, 2048], "Internal")
    mix = dt("mix", [T, D], dk); m = dt("m", [T, D], dk)
    ha = dt("ha", [T, D], dk); f = dt("f", [T, D], "Internal")
    with ExitStack() as es:
        C = _mk_ctx(nc, es)
        _load_consts(C, ident)
        linear(C, x, T, w_in, [(0, 6144)], proj, "in")
        linear(C, xp, T, w_in, [(1024, 3072)], pkv, "pkv")
        linear(C, xp[T - 128:T, :], 128, w_in, [(4096, 6144)], phalo, "ph")
        mixer_ret(C, proj, pkv, mix, cst)
        mixer_conv(C, proj, phalo, mix, cst)
        _common_tail(C, x, mix, w_out, g1, b1, g2, b2, w_gu, w_down, rw, rbB, m, ha, f, out)
        C.P.emit()
    return nc


def layer0_inputs(inp, h, core):
    b, half = core // 2, core % 2
    x = h[b, half * T:(half + 1) * T]
    xp = h[b, 0:T] if half == 1 else np.zeros((T, D), np.float32)
    g = np.array(RET_G, np.float64)
    j = np.arange(128, dtype=np.float64)
    diff = j[None, :] - j[:, None]
    decT = np.concatenate([np.where(diff >= 0, g[h_] ** np.maximum(diff, 0), 0.0) / 16.0 for h_ in range(4)], axis=1)
    xi = np.stack([g[h_] ** (j + 1.0) for h_ in range(4)], axis=1)
    zeta = np.stack([g[h_] ** (127.0 - j) / 16.0 for h_ in range(4)], axis=1)
    conv_w = np.asarray(inp["even_conv_w"][0], np.float32)
    cvec = np.stack([inp["even_conv_b"][0], inp["even_conv_ln_g"][0], inp["even_conv_ln_b"][0]], axis=0).astype(np.float32)
    return {
        "x": np.ascontiguousarray(x), "xp": np.ascontiguousarray(xp),
        "w_in": np.asarray(inp["even_w_in"][0], np.float32), "w_out": np.asarray(inp["even_w_out"][0], np.float32),
        "mix_g": _bcast_rows(inp["mix_ln_g"][0]), "mix_b": _bcast_rows(inp["mix_ln_b"][0]),
        "ffn_g": _bcast_rows(inp["ffn_ln_g"][0]), "ffn_b": _bcast_rows(inp["ffn_ln_b"][0]),
        "w_gu": np.asarray(inp["moe_w_gu"][0], np.float32), "w_down": np.asarray(inp["moe_w_down"][0], np.float32),
        "rw": np.asarray(inp["router_w"], np.float32), "rbB": _bcast_rows(inp["router_b"]), "ident": np.eye(128, dtype=np.float32),
        "cs_own": _rope_tab(np.arange(half * T, (half + 1) * T), 128), "cs_pre": _rope_tab(np.arange(0, T), 128),
        "decT": np.ascontiguousarray(decT.astype(np.float32)), "xi": np.ascontiguousarray(xi.astype(np.float32)),
        "zeta": np.ascontiguousarray(zeta.astype(np.float32)), "gnB": _bcast_rows(inp["even_ret_gn_g"][0]),
        "conv_w": np.ascontiguousarray(conv_w.T.reshape(8, 128, 31).transpose(1, 0, 2)),
        "conv_v": np.ascontiguousarray(cvec.reshape(3, 8, 128).transpose(2, 0, 1)),
    }


def mixer_dsa(C, qproj, kvproj, attn_out, cst):
    P = C.P
    P.stage = C.pfx + "dsa_kprep"
    SC = 128.0 ** -0.5
    with ExitStack() as st:
        kT = _sb(C, st, "dkT", [128, 4, 4096], BF16)
        vb = _sb(C, st, "dvb", [128, 32, 4, 129], BF16)
        kiT2 = _sb(C, st, "dkiT", [128, 1, 4096], BF16)
        iota = _sb(C, st, "diota", [128, 4096])
        qpos = _sb(C, st, "dqpos", [128, 16])
        P.dma("sp", _dma(iota[:], cst["iota"][:, :]), "diota", writes=["iota"])
        P.dma("sp", _dma(qpos[:], cst["qpos"][:, :]), "dqpos", writes=["qpos"])
        P.op("dve", _mset(vb[:], 1.0), writes=["vb"])
        with ExitStack() as s1:
            kvt = [_sb(C, s1, "kvt", [128, 1088]) for _ in range(2)]
            csk = [_sb(C, s1, "csk", [128, 2, 64]) for _ in range(2)]
            csi = [_sb(C, s1, "csi", [128, 2, 32]) for _ in range(2)]
            kr = _sb(C, s1, "dkr", [128, 512]); kir2 = _sb(C, s1, "dkir", [128, 128])
            t1 = _sb(C, s1, "dt1", [128, 256]); t2 = _sb(C, s1, "dt2", [128, 256])
            for kt in range(32):
                i = kt % 2
                bk_, ck_, ik_ = ("kvt", i), ("csk", i), ("csi", i)
                rows = slice(kt * 128, (kt + 1) * 128)
                P.dma("sp", _dma(kvt[i][:], kvproj[rows, :]), bk_, writes=[bk_])
                P.dma("sp", _dma(csk[i][:], cst["cs_k"][rows, :, :]), ck_, writes=[ck_])
                P.dma("sp", _dma(csi[i][:], cst["cs_ki"][rows, :, :]), ik_, writes=[ik_])
                _rope(P, kvt[i][:, 0:512], kr[:], csk[i], 4, 128, t1, t2, [bk_, ck_], "dkr", "dt")
                _transpose_blocks(C, kr, 4, kT[:, :, kt * 128:(kt + 1) * 128], ["dkr"], "kT", kt)
                P.op("act", _acp(vb[:, kt, :, 0:128], kvt[i][:, 512:1024].rearrange("p (h d) -> p h d", h=4)), reads=[bk_], writes=["vb"])
                _rope(P, kvt[i][:, 1024:1088], kir2[:, 0:64], csi[i], 1, 64, t1, t2, [bk_, ik_], "dkir", "dt")
                P.op("dve", _cp(kir2[:, 64:128], kir2[:, 0:64]), reads=["dkir"], writes=["dkir"])
                _transpose_blocks(C, kir2, 1, kiT2[:, :, kt * 128:(kt + 1) * 128], ["dkir"], "kiT", kt + 1)
        P.barrier()
        P.stage = C.pfx + "dsa_q"
        qt = _sb(C, st, "dqt", [128, 3088])
        csq = [_sb(C, st, "csq", [128, 2, 64]) for _ in range(2)]
        csqi = [_sb(C, st, "csqi", [128, 2, 32]) for _ in range(2)]
        qr = _sb(C, st, "dqr", [128, 2048]); qir = _sb(C, st, "dqir", [128, 1024])
        qT = [_sb(C, st, "dqT", [128, 16, 128], BF16) for _ in range(2)]
        qiT = _sb(C, st, "dqiT", [128, 8, 128], BF16)
        wab = _sb(C, st, "dwab", [128, 16]); sgn = _sb(C, st, "dsgn", [128, 16])
        t1 = _sb(C, st, "dq1", [128, 1024]); t2 = _sb(C, st, "dq2", [128, 1024])
        acc = _sb(C, st, "dacc", [128, 4096]); scr = _sb(C, st, "dscr", [128, 4096])
        tmp = [_sb(C, st, "dtmp", [128, 1024]) for _ in range(2)]
        selT = [_sb(C, st, "dselT", [128, 32, 128], BF16) for _ in range(2)]
        pt = [_sb(C, st, "dp", [128, 1024], BF16) for _ in range(2)]
        obuf = _sb(C, st, "dobuf", [128, 16, 129])
        rec = _sb(C, st, "drec", [128, 16])
        sm = {n: _sb(C, st, "d_" + n, [128, 1]) for n in ["lo", "hi", "w", "mid", "cnt", "ge"]}
        NIT = 26
        hw = _sb(C, st, "d_hw", [128, NIT + 1]); pw2 = _sb(C, st, "d_pw2", [128, NIT + 1])
        for k_ in range(NIT + 1):
            P.op("dve", _mset(pw2[:, k_:k_ + 1], 2.0 ** -(k_ + 1)), writes=["pw2"])
        identb = _sb(C, st, "didb", [128, 128], BF16)
        P.op("dve", _cp(identb[:], C.ident[:]), reads=["ident"], writes=["identb"])
        cnts = {"it": 0, "ig": 0}

        def NN(j):
            return 2048 + 128 * (j + 1)

        def phaseA(j):
            N = NN(j); i = j % 2
            rows = slice(j * 128, (j + 1) * 128)
            qTk = ("qT", i)
            P.dma("sp", _dma(qt[:], qproj[rows, :]), "dqt", writes=["dqt"])
            P.dma("sp", _dma(csq[i][:], cst["cs_q"][rows, :, :]), ("csq", i), writes=[("csq", i)])
            P.dma("sp", _dma(csqi[i][:], cst["cs_qi"][rows, :, :]), ("csqi", i), writes=[("csqi", i)])
            _rope(P, qt[:, 0:2048], qr[:], csq[i], 16, 128, t1, t2, ["dqt", ("csq", i)], "dqr", "dq")
            _transpose_blocks(C, qr, 16, qT[i], ["dqr"], qTk, 0)
            _rope(P, qt[:, 2048:3072], qir[:], csqi[i], 16, 64, t1, t2, ["dqt", ("csqi", i)], "dqir", "dq")
            _transpose_blocks(C, qir, 8, qiT, ["dqir"], "qiT", 0)
            P.op("dve", _ts(sgn[:], qt[:, 3072:3088], 0.0, 2.0, ALU.is_ge, ALU.mult), reads=["dqt"], writes=["sgn"])
            P.op("dve", _ts(sgn[:], sgn[:], -1.0, None, ALU.add), reads=["sgn"], writes=["sgn"])
            P.op("dve", _tt(wab[:], qt[:, 3072:3088], sgn[:], ALU.mult), reads=["dqt", "sgn"], writes=["wab"])
            P.op("dve", _ts(wab[:], wab[:], 0.03125), reads=["wab"], writes=["wab"])
            P.op("dve", _ts(acc[:, 0:N], iota[:, 0:N], qpos[:, j:j + 1], NEG, ALU.is_gt, ALU.mult), reads=["iota", "qpos"], writes=["acc"])
            for h in range(16):
                p0 = (h % 2) * 64
                for g0 in range(0, N, 1024):
                    w = min(1024, N - g0)
                    ig = cnts["ig"]
                    base = (ig % 4) * 2
                    nb_ = (w + 511) // 512
                    bkeys = [f"pb{base + c}" for c in range(nb_)]
                    for c4 in range(nb_):
                        ww = min(512, w - c4 * 512)
                        P.op("pe", _mm(C.pb[base + c4][:, 0:ww], qiT[p0:p0 + 64, h // 2, :], kiT2[p0:p0 + 64, 0, g0 + c4 * 512:g0 + c4 * 512 + ww], True, True),
                             reads=["qiT", "kiT"], writes=[bkeys[c4]])
                    tm = tmp[ig % 2]; tk = ("dtmp", ig % 2)
                    P.op("act", _act(tm[:, 0:w], C.ps_all[:, base * 512:base * 512 + w], AF.Relu, scale=wab[:, h:h + 1]), reads=bkeys + ["wab"], writes=[tk] + bkeys)
                    P.op("dve", _stt(acc[:, g0:g0 + w], tm[:, 0:w], sgn[:, h:h + 1], acc[:, g0:g0 + w], ALU.mult, ALU.add), reads=[tk, "sgn", "acc"], writes=["acc"])
                    cnts["ig"] += 1

        def phaseB(j):
            N = NN(j); NB = N // 128; i = j % 2
            lo, hi, wd, mid, cnt, ge = (sm[n] for n in ["lo", "hi", "w", "mid", "cnt", "ge"])
            P.op("dve", _red(hi[:], acc[:, 0:N], ALU.max), reads=["acc"], writes=["hi"])
            P.op("dve", _ts(scr[:, 0:N], iota[:, 0:N], qpos[:, j:j + 1], -2.0 * NEG, ALU.is_gt, ALU.mult), reads=["iota", "qpos"], writes=["scr"])
            P.op("dve", _tt(scr[:, 0:N], scr[:, 0:N], acc[:, 0:N], ALU.add), reads=["scr", "acc"], writes=["scr"])
            P.op("dve", _red(lo[:], scr[:, 0:N], ALU.min), reads=["scr"], writes=["lo"])
            P.op("dve", _tt(wd[:], hi[:], lo[:], ALU.subtract), reads=["hi", "lo"], writes=["w"])
            P.op("dve", _ts(hw[:], pw2[:], wd[:, 0:1]), reads=["w", "pw2"], writes=["hw"])
            for k in range(NIT):
                P.op("dve", _tt(mid[:], lo[:], hw[:, k:k + 1], ALU.add), reads=["lo", "hw"], writes=["mid"])
                P.op("dve", _ts(scr[:, 0:N], acc[:, 0:N], mid[:, 0:1], 0.0, ALU.is_ge, ALU.add, accum=cnt[:, 0:1]), reads=["acc", "mid"], writes=["scr", "cnt"])
                P.op("dve", _ts(ge[:], cnt[:], 255.5, hw[:, k:k + 1], ALU.is_ge, ALU.mult), reads=["cnt", "hw"], writes=["ge"])
                P.op("dve", _tt(lo[:], lo[:], ge[:], ALU.add), reads=["lo", "ge"], writes=["lo"])
            P.op("dve", _ts(scr[:, 0:N], acc[:, 0:N], lo[:, 0:1], -30000.0, ALU.is_lt, ALU.mult), reads=["acc", "lo"], writes=["scr"])
            _transpose_blocks(C, scr, NB, selT[i], ["scr"], ("selT", i), 0)

        def phaseCmain(j):
            N = NN(j); NB = N // 128; i = j % 2
            qT2 = qT[i][:].rearrange("p h q -> p (h q)")
            qTk, sTk = ("qT", i), ("selT", i)
            for kv in range(4):
                for kb in range(0, NB, 2):
                    nk = min(2, NB - kb)
                    it = cnts["it"]
                    base = (it % 2) * 2
                    Lks = [f"pb{base + b}" for b in range(nk)]
                    pp = pt[it % 2]; ppk = ("dp", it % 2)
                    for b in range(nk):
                        P.op("pe", _mm(C.pb[base + b][:, :], kT[:, kv, (kb + b) * 128:(kb + b + 1) * 128], qT2[:, kv * 512:(kv + 1) * 512], True, False), reads=["kT", qTk], writes=[Lks[b]])
                        for g in range(4):
                            P.op("pe", _mm(C.pb[base + b][:, g * 128:(g + 1) * 128], identb[:], selT[i][:, kb + b, :], False, g == 3), reads=["identb", sTk], writes=[Lks[b]])
                    P.op("act", _act(pp[:, 0:nk * 512], C.ps_all[:, base * 512:(base + nk) * 512], AF.Exp, scale=SC), reads=Lks, writes=[ppk] + Lks)
                    for b in range(nk):
                        for g in range(4):
                            P.op("pe", _mm(C.pb[4 + g][:, 0:129], pp[:, b * 512 + g * 128:b * 512 + (g + 1) * 128], vb[:, kb + b, kv, :], kb + b == 0, kb + b == NB - 1), reads=[ppk, "vb"], writes=[f"pb{4 + g}"])
                    cnts["it"] += 1
                for g in range(4):
                    P.op("act", _acp(obuf[:, kv * 4 + g, :], C.pb[4 + g][:, 0:129]), reads=[f"pb{4 + g}"], writes=["obuf", f"pb{4 + g}"])

        def phaseCfin(j):
            rows = slice(j * 128, (j + 1) * 128)
            P.op("dve", _rcp(rec[:], obuf[:, :, 128]), reads=["obuf"], writes=["rec"])
            P.op("dve", _tt(obuf[:, :, 0:128], obuf[:, :, 0:128], rec[:, :, None].to_broadcast([128, 16, 128]), ALU.mult), reads=["obuf", "rec"], writes=["obuf"])
            P.dma("sp", _dma(attn_out[rows, :].rearrange("p (h d) -> p h d", h=16), obuf[:, :, 0:128]), "dobuf", reads=["obuf"], writes=[("aout", j)])

        phaseA(0)
        phaseB(0)
        for j in range(16):
            if j + 1 < 16:
                phaseA(j + 1)
            phaseCmain(j)
            if j + 1 < 16:
                phaseB(j + 1)
            phaseCfin(j)
    P.barrier()


def build_layer1(debug=False):
    nc = bass.Bass("TRN2", target_bir_lowering=False)
    dt = lambda name, shape, kind="ExternalInput": nc.dram_tensor(name, shape, F32, kind=kind).ap()
    x = dt("x", [T, D]); xf = dt("xf", [2 * T, D]); w_in = dt("w_in", [D, 4176]); w_out = dt("w_out", [D, D])
    g1 = dt("mix_g", [128, D]); b1 = dt("mix_b", [128, D]); g2 = dt("ffn_g", [128, D]); b2 = dt("ffn_b", [128, D])
    w_gu = dt("w_gu", [16, D, 1024]); w_down = dt("w_down", [16, 512, D])
    rw = dt("rw", [D, 16]); rbB = dt("rbB", [128, 16]); ident = dt("ident", [128, 128])
    cst = {"cs_k": dt("cs_k", [2 * T, 2, 64]), "cs_ki": dt("cs_ki", [2 * T, 2, 32]), "cs_q": dt("cs_q", [T, 2, 64]), "cs_qi": dt("cs_qi", [T, 2, 32]),
           "iota": dt("iota", [128, 4096]), "qpos": dt("qpos", [128, 16])}
    out = dt("out", [T, D], "ExternalOutput")
    dk = "ExternalOutput" if debug else "Internal"
    qproj = dt("qproj", [T, 3088], "Internal"); kvproj = dt("kvproj", [2 * T, 1088], "Internal")
    mix = dt("mix", [T, D], dk); m = dt("m", [T, D], dk)
    ha = dt("ha", [T, D], dk); f = dt("f", [T, D], "Internal")
    with ExitStack() as es:
        C = _mk_ctx(nc, es)
        _load_consts(C, ident)
        linear(C, x, T, w_in, [(0, 2048), (3072, 4096), (4160, 4176)], qproj, "q1")
        linear(C, xf, 2 * T, w_in, [(2048, 3072), (4096, 4160)], kvproj, "kv1")
        mixer_dsa(C, qproj, kvproj, mix, cst)
        _common_tail(C, x, mix, w_out, g1, b1, g2, b2, w_gu, w_down, rw, rbB, m, ha, f, out)
        C.P.emit()
    return nc


def layer1_inputs(inp, h, core):
    b, half = core // 2, core % 2
    pos_own = np.arange(half * T, (half + 1) * T)
    qpos = (half * T + np.arange(16)[None, :] * 128 + np.arange(128)[:, None]).astype(np.float32)
    return {
        "x": np.ascontiguousarray(h[b, half * T:(half + 1) * T]), "xf": np.ascontiguousarray(h[b]),
        "w_in": np.asarray(inp["odd_w_in"][0], np.float32), "w_out": np.asarray(inp["odd_w_out"][0], np.float32),
        "mix_g": _bcast_rows(inp["mix_ln_g"][1]), "mix_b": _bcast_rows(inp["mix_ln_b"][1]),
        "ffn_g": _bcast_rows(inp["ffn_ln_g"][1]), "ffn_b": _bcast_rows(inp["ffn_ln_b"][1]),
        "w_gu": np.asarray(inp["moe_w_gu"][1], np.float32), "w_down": np.asarray(inp["moe_w_down"][1], np.float32),
        "rw": np.asarray(inp["router_w"], np.float32), "rbB": _bcast_rows(inp["router_b"]), "ident": np.eye(128, dtype=np.float32),
        "cs_k": _rope_tab(np.arange(2 * T), 64), "cs_ki": _rope_tab(np.arange(2 * T), 32),
        "cs_q": _rope_tab(pos_own, 64), "cs_qi": _rope_tab(pos_own, 32),
        "iota": np.ascontiguousarray(np.broadcast_to(np.arange(4096, dtype=np.float32)[None, :], (128, 4096))),
        "qpos": np.ascontiguousarray(qpos),
    }


def build_fused(profile=False):
    nc = bass.Bass("TRN2", target_bir_lowering=False)
    dt = lambda name, shape, kind="ExternalInput": nc.dram_tensor(name, shape, F32, kind=kind).ap()
    xA = dt("x", [T, D]); xB = dt("xp", [T, D]); zhalo = dt("zhalo", [128, 2048])
    rw = dt("rw", [D, 16]); rbB = dt("rbB", [128, 16]); ident = dt("ident", [128, 128])
    L = []
    for l in range(2):
        L.append({"w_in": dt(f"w_in{l}", [D, 6144 if l == 0 else 4176]), "w_out": dt(f"w_out{l}", [D, D]),
                  "g1": dt(f"mix_g{l}", [128, D]), "b1": dt(f"mix_b{l}", [128, D]), "g2": dt(f"ffn_g{l}", [128, D]), "b2": dt(f"ffn_b{l}", [128, D]),
                  "w_gu": dt(f"w_gu{l}", [16, D, 1024]), "w_down": dt(f"w_down{l}", [16, 512, D])})
    cst = {"cs_own": dt("cs_own", [T, 2, 128]), "cs_pre": dt("cs_pre", [T, 2, 128]), "decT": dt("decT", [128, 512]),
           "xi": dt("xi", [128, 4]), "zeta": dt("zeta", [128, 4]), "gnB": dt("gnB", [128, 1024]),
           "conv_w": dt("conv_w", [128, 8, 31]), "conv_v": dt("conv_v", [128, 3, 8]),
           "cs_k": dt("cs_k", [2 * T, 2, 64]), "cs_ki": dt("cs_ki", [2 * T, 2, 32]), "cs_q": dt("cs_q", [T, 2, 64]), "cs_qi": dt("cs_qi", [T, 2, 32]),
           "iota": dt("iota", [128, 4096]), "qpos": dt("qpos", [128, 16])}
    out = dt("out", [T, D], "ExternalOutput")
    projB = dt("projB", [T, 6144], "Internal"); projA = dt("projA", [T, 6144], "Internal")
    sstate = dt("sstate", [128, 2048], "Internal"); h0f = dt("h0f", [2 * T, D], "Internal")
    qproj = dt("qproj", [T, 3088], "Internal"); kvproj = dt("kvproj", [2 * T, 1088], "Internal")
    mix = dt("mix", [T, D], "Internal"); m = dt("m", [T, D], "Internal"); ha = dt("ha", [T, D], "Internal"); f = dt("f", [T, D], "Internal")
    with ExitStack() as es:
        C = _mk_ctx(nc, es)
        C.P.profile = profile
        _load_consts(C, ident)
        l0 = L[0]
        for (xx, proj, cs_key, s_in, s_out, halo, dst) in [(xB, projB, "cs_pre", None, sstate, zhalo, h0f[0:T, :]),
                                                            (xA, projA, "cs_own", sstate, None, projB[T - 128:T, 4096:6144], h0f[T:2 * T, :])]:
            C.pfx = "B_" if s_in is None else "A_"
            linear(C, xx, T, l0["w_in"], [(0, 6144)], proj, "in")
            mixer_ret(C, proj, None, mix, cst, cs_key=cs_key, s_in=s_in, s_out=s_out)
            mixer_conv(C, proj, halo, mix, cst)
            _common_tail(C, xx, mix, l0["w_out"], l0["g1"], l0["b1"], l0["g2"], l0["b2"], l0["w_gu"], l0["w_down"], rw, rbB, m, ha, f, dst)
        l1 = L[1]
        C.pfx = "L1_"
        x1 = h0f[T:2 * T, :]
        linear(C, x1, T, l1["w_in"], [(0, 2048), (3072, 4096), (4160, 4176)], qproj, "q1")
        linear(C, h0f, 2 * T, l1["w_in"], [(2048, 3072), (4096, 4160)], kvproj, "kv1")
        mixer_dsa(C, qproj, kvproj, mix, cst)
        _common_tail(C, x1, mix, l1["w_out"], l1["g1"], l1["b1"], l1["g2"], l1["b2"], l1["w_gu"], l1["w_down"], rw, rbB, m, ha, f, out)
        C.P.emit()
    return nc


def fused_inputs(inp, core):
    b, half = core // 2, core % 2
    x = np.asarray(inp["x"], np.float32)
    a = layer0_inputs(inp, x, core)
    r = {k: a[k] for k in ["x", "xp", "rw", "rbB", "ident", "cs_own", "cs_pre", "decT", "xi", "zeta", "gnB", "conv_w", "conv_v"]}
    r["zhalo"] = np.zeros((128, 2048), np.float32)
    for l, pre in enumerate(["even", "odd"]):
        r[f"w_in{l}"] = np.asarray(inp[pre + "_w_in"][0], np.float32); r[f"w_out{l}"] = np.asarray(inp[pre + "_w_out"][0], np.float32)
        r[f"mix_g{l}"] = _bcast_rows(inp["mix_ln_g"][l]); r[f"mix_b{l}"] = _bcast_rows(inp["mix_ln_b"][l])
        r[f"ffn_g{l}"] = _bcast_rows(inp["ffn_ln_g"][l]); r[f"ffn_b{l}"] = _bcast_rows(inp["ffn_ln_b"][l])
        r[f"w_gu{l}"] = np.asarray(inp["moe_w_gu"][l], np.float32); r[f"w_down{l}"] = np.asarray(inp["moe_w_down"][l], np.float32)
    posA = np.arange(half * T, (half + 1) * T)
    posB = np.arange(0, T)
    kp = np.concatenate([posB if half == 1 else np.full(T, 1.0e9), posA]).astype(np.float32)
    ropepos = np.concatenate([posB, posA])
    r["cs_k"] = _rope_tab(ropepos, 64); r["cs_ki"] = _rope_tab(ropepos, 32)
    r["cs_q"] = _rope_tab(posA, 64); r["cs_qi"] = _rope_tab(posA, 32)
    r["iota"] = np.ascontiguousarray(np.broadcast_to(kp[None, :], (128, 4096)))
    r["qpos"] = np.ascontiguousarray((half * T + np.arange(16)[None, :] * 128 + np.arange(128)[:, None]).astype(np.float32))
    return r


_NC_CACHE = {}


def kernel(**inputs):
    inp = {k: np.asarray(v) for k, v in inputs.items()}
    B = inp["x"].shape[0]
    if "fused" not in _NC_CACHE:
        _NC_CACHE["fused"] = build_fused()
    nc = _NC_CACHE["fused"]
    in_maps = [fused_inputs(inp, core) for core in range(8)]
    res = run_bass_kernel_spmd(nc, in_maps, core_ids=list(range(8)))
    h = np.stack([np.concatenate([res.results[2 * b]["out"], res.results[2 * b + 1]["out"]], axis=0) for b in range(B)], axis=0)
    return np.ascontiguousarray(h.astype(np.float32))
```

```python
import math
import numpy as np
from contextlib import ExitStack
import concourse.bass as bass
import concourse.mybir as mybir
from concourse.bass_utils import run_bass_kernel_spmd

F32 = mybir.dt.float32
BF16 = mybir.dt.bfloat16
AF = mybir.ActivationFunctionType
ALU = mybir.AluOpType
AX = mybir.AxisListType

ENGS = ("pe", "act", "dve", "pool", "sp")
D = 2048
T = 2048
NT = 16
ALPHA = 4.0 ** 0.25
EPS = 1e-5
NEG = -1.0e30


class Op:
    __slots__ = ("eng", "fn", "deps", "dma_slot", "dma_val", "signal", "sigval", "idx", "stage")


class Prog:
    def __init__(self, nc):
        self.nc = nc
        self.ops = []
        self.last_w = {}
        self.readers = {}
        self.slot_cnt = {}
        self.slot_last = {}
        self.last_on = {}
        self.stage = "init"
        self.profile = False

    def _rec(self, eng, fn, reads, writes, dma_slot=None, extra_deps=()):
        op = Op()
        op.eng = eng; op.fn = fn; op.dma_slot = dma_slot; op.signal = False; op.sigval = 0
        op.idx = len(self.ops)
        op.stage = self.stage
        deps = set(extra_deps)
        for k in reads:
            w = self.last_w.get(k)
            if w is not None:
                deps.add(w)
        for k in writes:
            w = self.last_w.get(k)
            if w is not None:
                deps.add(w)
            rd = self.readers.get(k)
            if rd:
                deps.update(rd[0].values())
                deps.update(rd[1])
        op.deps = deps
        if dma_slot is not None:
            c = self.slot_cnt.get(dma_slot, 0) + 1
            self.slot_cnt[dma_slot] = c
            op.dma_val = 16 * c
            self.slot_last[dma_slot] = op.idx
        else:
            op.dma_val = 0
            self.last_on[eng] = op.idx
        for k in reads:
            rd = self.readers.setdefault(k, ({}, []))
            if dma_slot is not None:
                rd[1].append(op.idx)
            else:
                rd[0][eng] = op.idx
        for k in writes:
            self.last_w[k] = op.idx
            self.readers[k] = ({}, [])
        self.ops.append(op)
        return op

    def op(self, eng, fn, reads=(), writes=()):
        return self._rec(eng, fn, reads, writes)

    def dma(self, eng, fn, slot, reads=(), writes=()):
        return self._rec(eng, fn, reads, writes, dma_slot=slot)

    def barrier(self):
        deps = set(self.last_on.values()) | set(self.slot_last.values())
        for e in ENGS:
            self._rec(e, None, (), (), extra_deps=deps)
        self.last_w = {}
        self.readers = {}

    def emit(self):
        nc = self.nc
        self.barrier()
        ops = self.ops
        for op in ops:
            for d in op.deps:
                p = ops[d]
                if p.dma_slot is None and p.fn is not None and (p.eng != op.eng or p.eng != "pe"):
                    p.signal = True
        cnt = {e: 0 for e in ENGS}
        for op in ops:
            if op.signal:
                cnt[op.eng] += 1
                op.sigval = cnt[op.eng]
        slots = list(self.slot_cnt.keys())
        EPOCH = 30000
        with ExitStack() as es:
            eng_sems = {}
            for e in ENGS:
                n_ep = cnt[e] // EPOCH + 1
                eng_sems[e] = [es.enter_context(nc.semaphore(f"s_{e}_{i}")) for i in range(n_ep)]
            slot_sem = {s: es.enter_context(nc.semaphore(f"d_{i}")) for i, s in enumerate(slots)}
            block = es.enter_context(nc.Block())

            def run_engine(ename, eng):
                waited = {}
                cur = None
                for op in ops:
                    if op.eng != ename:
                        continue
                    if self.profile and op.stage != cur:
                        if cur is not None:
                            nc.pop_named_scope(cur)
                        cur = op.stage
                        nc.push_named_scope(cur)
                    for d in sorted(op.deps):
                        p = ops[d]
                        if p.dma_slot is not None:
                            sem = slot_sem[p.dma_slot]; val = p.dma_val
                        else:
                            if p.eng == ename and ename == "pe":
                                continue
                            if p.fn is None:
                                continue
                            ep = (p.sigval - 1) // EPOCH
                            sem = eng_sems[p.eng][ep]; val = p.sigval - ep * EPOCH
                        if waited.get(sem.num, 0) >= val:
                            continue
                        waited[sem.num] = val
                        eng.wait_ge(sem, val)
                    if op.fn is None:
                        continue
                    ins = op.fn(eng)
                    if op.dma_slot is not None:
                        ins.then_inc(slot_sem[op.dma_slot], 16)
                    elif op.signal:
                        ep = (op.sigval - 1) // EPOCH
                        ins.then_inc(eng_sems[ename][ep], 1)
                if self.profile and cur is not None:
                    nc.pop_named_scope(cur)

            block.sync(lambda e: run_engine("sp", e))
            block.tensor(lambda e: run_engine("pe", e))
            block.scalar(lambda e: run_engine("act", e))
            block.vector(lambda e: run_engine("dve", e))
            block.gpsimd(lambda e: run_engine("pool", e))


class Ctx:
    pass


def _mk_ctx(nc, es):
    C = Ctx()
    C.nc = nc
    C.P = Prog(nc)
    C.es = es
    C.ps_all = es.enter_context(nc.psum_tensor("ps_all", [128, 4096], F32))
    C.pb = [C.ps_all[:, i * 512:(i + 1) * 512] for i in range(8)]
    C.uid = 0
    C.pfx = ""
    return C


def _sb(C, st, name, shape, dt=F32):
    C.uid += 1
    return st.enter_context(C.nc.sbuf_tensor(f"{name}_{C.uid}", shape, dt))


def _load_consts(C, ident_d):
    nc, P = C.nc, C.P
    C.ident = C.es.enter_context(nc.sbuf_tensor("ident_sb", [128, 128], F32))
    C.ones = C.es.enter_context(nc.sbuf_tensor("ones_sb", [128, 128], F32))
    P.dma("sp", lambda e: e.dma_start(out=C.ident[:], in_=ident_d[:, :]), "ident", writes=["ident"])
    P.op("dve", lambda e: e.memset(C.ones[:], 1.0), writes=["ones"])


def build_xT(C, st, src, n_tok, tag, f32_copy=None):
    nc, P = C.nc, C.P
    nt = n_tok // 128
    xT = _sb(C, st, "xT" + tag, [128, 16, n_tok], BF16)
    stg = [_sb(C, st, "xs" + tag, [128, D]) for _ in range(2)]
    for t in range(nt):
        s = stg[t % 2]
        sk = ("xs", tag, t % 2)
        P.dma("sp", (lambda s=s, t=t: lambda e: e.dma_start(out=s[:], in_=src[t * 128:(t + 1) * 128, :]))(), sk, writes=[sk])
        for q in range(4):
            bank = C.pb[q % 2]
            bk = f"pb{q % 2}"
            for i in range(4):
                c = q * 4 + i
                P.op("pe", (lambda s=s, c=c, bank=bank, i=i: lambda e: e.transpose(bank[:, i * 128:(i + 1) * 128], s[:, c * 128:(c + 1) * 128], C.ident[:]))(),
                     reads=[sk, "ident"], writes=[bk])
            eng = "act" if q % 2 == 0 else "dve"
            dst = xT[:, q * 4:(q + 1) * 4, t * 128:(t + 1) * 128]
            srcp = bank[:].rearrange("p (i k) -> p i k", i=4)
            if eng == "act":
                P.op("act", (lambda dst=dst, srcp=srcp: lambda e: e.copy(out=dst, in_=srcp))(), reads=[bk], writes=[("xT", tag)])
            else:
                P.op("dve", (lambda dst=dst, srcp=srcp: lambda e: e.tensor_copy(out=dst, in_=srcp))(), reads=[bk], writes=[("xT", tag)])
            if f32_copy is not None:
                dst2 = f32_copy[:, q * 4:(q + 1) * 4, t * 128:(t + 1) * 128]
                P.op("dve", (lambda dst2=dst2, srcp=srcp: lambda e: e.tensor_copy(out=dst2, in_=srcp))(), reads=[bk], writes=[("xT32", tag)])
    return xT


def linear(C, src, n_tok, w, col_ranges, dst, tag):
    nc, P = C.nc, C.P
    P.stage = C.pfx + "lin_" + tag
    nt = n_tok // 128
    with ExitStack() as st:
        xT = build_xT(C, st, src, n_tok, tag)
        wb = [_sb(C, st, "wb" + tag, [128, 16, 512], BF16) for _ in range(2)]
        ost = [_sb(C, st, "os" + tag, [128, 512]) for _ in range(4)]
        groups = []
        o = 0
        for (c0, c1) in col_ranges:
            c = c0
            while c < c1:
                n = min(512, c1 - c)
                groups.append((c, n, o))
                c += n; o += n
        wv = w.rearrange("(c p) n -> p c n", p=128)
        k = 0
        for gi, (c0, n, o0) in enumerate(groups):
            wt = wb[gi % 2]
            wk = ("wb", tag, gi % 2)
            P.dma("pool", (lambda wt=wt, c0=c0, n=n: lambda e: e.dma_start(out=wt[:, :, 0:n], in_=wv[:, :, c0:c0 + n]))(), wk, writes=[wk])
            for t in range(nt):
                bank = C.pb[2 + k % 4]; bk = f"pb{2 + k % 4}"
                for c in range(16):
                    P.op("pe", (lambda bank=bank, c=c, t=t, wt=wt, n=n: lambda e: e.matmul(bank[:, 0:n], lhsT=xT[:, c, t * 128:(t + 1) * 128], rhs=wt[:, c, 0:n], start=(c == 0), stop=(c == 15)))(),
                         reads=[("xT", tag), wk], writes=[bk])
                os_ = ost[k % 4]; ok = ("os", tag, k % 4)
                if k % 2 == 0:
                    P.op("act", (lambda os_=os_, bank=bank, n=n: lambda e: e.copy(out=os_[:, 0:n], in_=bank[:, 0:n]))(), reads=[bk], writes=[ok])
                else:
                    P.op("dve", (lambda os_=os_, bank=bank, n=n: lambda e: e.tensor_copy(out=os_[:, 0:n], in_=bank[:, 0:n]))(), reads=[bk], writes=[ok])
                P.dma("sp", (lambda os_=os_, t=t, o0=o0, n=n: lambda e: e.dma_start(out=dst[t * 128:(t + 1) * 128, o0:o0 + n], in_=os_[:, 0:n]))(), ok, reads=[ok], writes=[("dst", tag, t, gi)])
                k += 1
    P.barrier()


def resid_ln(C, x, m, gB_d, bB_d, out, n_tok, tag):
    nc, P = C.nc, C.P
    P.stage = C.pfx + "ln_" + tag
    nt = n_tok // 128
    with ExitStack() as st:
        gB = _sb(C, st, "gB", [128, D]); bB = _sb(C, st, "bB", [128, D])
        P.dma("sp", _dma(gB[:], gB_d[:, :]), ("gB", tag), writes=[("gB", tag)])
        P.dma("sp", _dma(bB[:], bB_d[:, :]), ("bB", tag), writes=[("bB", tag)])
        xs = [_sb(C, st, "rx", [128, D]) for _ in range(2)]
        ms = [_sb(C, st, "rm", [128, D]) for _ in range(2)]
        rs = [_sb(C, st, "rr", [128, D]) for _ in range(2)]
        mv = [_sb(C, st, "rmv", [128, 2]) for _ in range(2)]
        rstd = [_sb(C, st, "rrs", [128, 1]) for _ in range(2)]
        nb = [_sb(C, st, "rnb", [128, 1]) for _ in range(2)]

        def front(t):
            i = t % 2
            xk, mk, rk, vk = ("rx", tag, i), ("rm", tag, i), ("rr", tag, i), ("rmv", i)
            P.dma("sp", _dma(xs[i][:], x[t * 128:(t + 1) * 128, :]), xk, writes=[xk])
            P.dma("sp", _dma(ms[i][:], m[t * 128:(t + 1) * 128, :]), mk, writes=[mk])
            P.op("dve", _stt(rs[i][:], xs[i][:], ALPHA, ms[i][:], ALU.mult, ALU.add), reads=[xk, mk], writes=[rk])
            P.op("act", _act(xs[i][:], rs[i][:], AF.Identity, accum=mv[i][:, 0:1]), reads=[rk], writes=[vk, xk])
            P.op("act", _act(xs[i][:], rs[i][:], AF.Square, accum=mv[i][:, 1:2]), reads=[rk], writes=[vk, xk])

        def back(t):
            i = t % 2
            xk, mk, rk, vk, sk, nk = ("rx", tag, i), ("rm", tag, i), ("rr", tag, i), ("rmv", i), ("rrs", i), ("rnb", i)
            P.op("dve", _ts(mv[i][:], mv[i][:], 1.0 / D), reads=[vk], writes=[vk])
            P.op("dve", _tt(nb[i][:], mv[i][:, 0:1], mv[i][:, 0:1], ALU.mult), reads=[vk], writes=[nk])
            P.op("dve", _tt(rstd[i][:], mv[i][:, 1:2], nb[i][:], ALU.subtract), reads=[vk, nk], writes=[sk])
            P.op("dve", _ts(rstd[i][:], rstd[i][:], EPS, None, ALU.add), reads=[sk], writes=[sk])
            P.op("act", _sqrt(rstd[i][:], rstd[i][:]), reads=[sk], writes=[sk])
            P.op("dve", _rcp(rstd[i][:], rstd[i][:]), reads=[sk], writes=[sk])
            P.op("dve", _stt(nb[i][:], mv[i][:, 0:1], -1.0, rstd[i][:], ALU.mult, ALU.mult), reads=[vk, sk], writes=[nk])
            P.op("act", _act(rs[i][:], rs[i][:], AF.Identity, bias=nb[i][:, 0:1], scale=rstd[i][:, 0:1]), reads=[rk, sk, nk], writes=[rk])
            P.op("dve", _tt(ms[i][:], rs[i][:], gB[:], ALU.mult), reads=[rk, ("gB", tag)], writes=[mk])
            P.op("pool", _tt(ms[i][:], ms[i][:], bB[:], ALU.add), reads=[mk, ("bB", tag)], writes=[mk])
            P.dma("sp", _dma(out[t * 128:(t + 1) * 128, :], ms[i][:]), mk, reads=[mk], writes=[("rout", tag, t)])

        for t in range(nt + 1):
            if t < nt:
                front(t)
            if t >= 1:
                back(t - 1)
    P.barrier()


class LNEpi:
    def __init__(self, C, st, x, gB_d, bB_d, out, tag):
        self.C, self.x, self.out, self.tag = C, x, out, tag
        P = C.P
        self.gB = _sb(C, st, "egB", [128, D]); self.bB = _sb(C, st, "ebB", [128, D])
        P.dma("sp", _dma(self.gB[:], gB_d[:, :]), ("egB", tag), writes=[("egB", tag)])
        P.dma("sp", _dma(self.bB[:], bB_d[:, :]), ("ebB", tag), writes=[("ebB", tag)])
        self.xs = [_sb(C, st, "ex", [128, D]) for _ in range(2)]
        self.rs = [_sb(C, st, "er", [128, D]) for _ in range(2)]
        self.mv = [_sb(C, st, "emv", [128, 2]) for _ in range(2)]
        self.rstd = [_sb(C, st, "ers", [128, 1]) for _ in range(2)]
        self.nb = [_sb(C, st, "enb", [128, 1]) for _ in range(2)]

    def front(self, t, row0, msrc, mkeys):
        P, tag, i = self.C.P, self.tag, t % 2
        xk, rk, vk = ("ex", tag, i), ("er", tag, i), ("emv", tag, i)
        P.dma("sp", _dma(self.xs[i][:], self.x[row0:row0 + 128, :]), xk, writes=[xk])
        P.op("dve", _stt(self.rs[i][:], self.xs[i][:], ALPHA, msrc, ALU.mult, ALU.add), reads=[xk] + list(mkeys), writes=[rk] + list(mkeys))
        P.op("act", _act(self.xs[i][:], self.rs[i][:], AF.Identity, accum=self.mv[i][:, 0:1]), reads=[rk], writes=[vk, xk])
        P.op("act", _act(self.xs[i][:], self.rs[i][:], AF.Square, accum=self.mv[i][:, 1:2]), reads=[rk], writes=[vk, xk])

    def back(self, t, row0):
        P, tag, i = self.C.P, self.tag, t % 2
        mv, rstd, nb, rs, xs = self.mv[i], self.rstd[i], self.nb[i], self.rs[i], self.xs[i]
        xk, rk, vk, sk, nk = ("ex", tag, i), ("er", tag, i), ("emv", tag, i), ("ers", tag, i), ("enb", tag, i)
        P.op("dve", _ts(mv[:], mv[:], 1.0 / D), reads=[vk], writes=[vk])
        P.op("dve", _tt(nb[:], mv[:, 0:1], mv[:, 0:1], ALU.mult), reads=[vk], writes=[nk])
        P.op("dve", _tt(rstd[:], mv[:, 1:2], nb[:], ALU.subtract), reads=[vk, nk], writes=[sk])
        P.op("dve", _ts(rstd[:], rstd[:], EPS, None, ALU.add), reads=[sk], writes=[sk])
        P.op("act", _sqrt(rstd[:], rstd[:]), reads=[sk], writes=[sk])
        P.op("dve", _rcp(rstd[:], rstd[:]), reads=[sk], writes=[sk])
        P.op("dve", _stt(nb[:], mv[:, 0:1], -1.0, rstd[:], ALU.mult, ALU.mult), reads=[vk, sk], writes=[nk])
        P.op("act", _act(rs[:], rs[:], AF.Identity, bias=nb[:, 0:1], scale=rstd[:, 0:1]), reads=[rk, sk, nk], writes=[rk])
        P.op("dve", _tt(xs[:], rs[:], self.gB[:], ALU.mult), reads=[rk, ("egB", tag)], writes=[xk])
        P.op("pool", _tt(xs[:], xs[:], self.bB[:], ALU.add), reads=[xk, ("ebB", tag)], writes=[xk])
        P.dma("sp", _dma(self.out[row0:row0 + 128, :], xs[:]), xk, reads=[xk], writes=[("eout", tag, t)])


def linear_ln(C, src, w, x, gB_d, bB_d, out, tag):
    nc, P = C.nc, C.P
    P.stage = C.pfx + "linln_" + tag
    nt = T // 128
    with ExitStack() as st:
        xT = _sb(C, st, "lxT", [128, 16, T], BF16)
        wt = _sb(C, st, "lw", [128, 16, D], BF16)
        wv = w.rearrange("(c p) n -> p c n", p=128)
        for g in range(4):
            P.dma("pool", _dma(wt[:, :, g * 512:(g + 1) * 512], wv[:, :, g * 512:(g + 1) * 512]), ("lw", g), writes=[("lw", g)])
        with ExitStack() as s1:
            stg = [_sb(C, s1, "lxs", [128, D]) for _ in range(2)]
            for t in range(nt):
                sk = ("lxs", t % 2)
                P.dma("sp", _dma(stg[t % 2][:], src[t * 128:(t + 1) * 128, :]), sk, writes=[sk])
                _transpose_blocks(C, stg[t % 2], 16, xT[:, :, t * 128:(t + 1) * 128], [sk], "lxT", 0)
        P.barrier()
        ep = LNEpi(C, st, x, gB_d, bB_d, out, tag)
        for t in range(nt):
            base = (t % 2) * 4
            keys = [f"pb{base + g}" for g in range(4)]
            for g in range(4):
                for c in range(16):
                    P.op("pe", _mm(C.pb[base + g][:, :], xT[:, c, t * 128:(t + 1) * 128], wt[:, c, g * 512:(g + 1) * 512], c == 0, c == 15), reads=["lxT", ("lw", g)], writes=[keys[g]])
            ep.front(t, t * 128, C.ps_all[:, base * 512:base * 512 + 2048], keys)
            if t >= 1:
                ep.back(t - 1, (t - 1) * 128)
        ep.back(nt - 1, (nt - 1) * 128)
    P.barrier()


MOE_STOP = None
MOE_DBG = None


def moe(C, h, w_gu, w_down, rw_d, rbB_d, f_out, tag, ln=None):
    nc, P = C.nc, C.P
    HT = 1024
    for half_ in range(2 if MOE_STOP is None else 1):
        _moe_half(C, h, w_gu, w_down, rw_d, rbB_d, f_out, tag, half_, ln)


def _moe_half(C, h, w_gu, w_down, rw_d, rbB_d, f_out, tag, half, ln=None):
    nc, P = C.nc, C.P
    P.stage = C.pfx + "moe"
    HT = 1024
    if True:
        with ExitStack() as st:
            hsrc = h[half * HT:(half + 1) * HT, :]
            gateT = _sb(C, st, "gateT", [16, HT], BF16)
            sel16 = _sb(C, st, "sel16", [16, 16, 128], BF16)
            xT = _sb(C, st, "mxT", [128, 16, HT], BF16)
            yacc = _sb(C, st, "yacc", [128, 16, HT])
            dbg_sb = _sb(C, st, "dbg_sb", [128, 512])
            P.op("dve", lambda e: e.memset(dbg_sb[:], 0.0), writes=["dbg"])
            st1 = ExitStack()
            xT32 = _sb(C, st1, "xT32", [128, 16, 128])
            rw = _sb(C, st1, "rw", [128, 16, 16])
            rbB = _sb(C, st1, "rbB", [128, 16])
            gate = _sb(C, st1, "gate", [128, 8, 16])
            P.dma("sp", lambda e: e.dma_start(out=rw[:], in_=rw_d.rearrange("(c p) n -> p c n", p=128)), ("rw", tag, half), writes=["rw"])
            P.dma("sp", lambda e: e.dma_start(out=rbB[:], in_=rbB_d[:, :]), ("rbB", tag, half), writes=["rbB"])
            for ex_ in range(16):
                P.op("dve", (lambda ex_=ex_: lambda e: e.tensor_copy(out=sel16[:, ex_, :], in_=C.ident[0:16, ex_:ex_ + 1].to_broadcast([16, 128])))(), reads=["ident"], writes=["sel16"])
            stg = [_sb(C, st1, "mxs", [128, D]) for _ in range(2)]
            sm = {n: _sb(C, st1, "g" + n, [128, 16]) for n in ["aff", "sel", "msk", "t1", "t2", "w"]}
            sm4 = {n: _sb(C, st1, "g4" + n, [128, 4]) for n in ["m1", "m2", "gs", "gm"]}
            sm1 = {n: _sb(C, st1, "g1" + n, [128, 1]) for n in ["a", "b"]}
            for t in range(HT // 128):
                s = stg[t % 2]; sk = ("mxs", t % 2)
                P.dma("sp", (lambda s=s, t=t: lambda e: e.dma_start(out=s[:], in_=hsrc[t * 128:(t + 1) * 128, :]))(), sk, writes=[sk])
                for q in range(4):
                    bank = C.pb[q % 2]; bk = f"pb{q % 2}"
                    for i in range(4):
                        c = q * 4 + i
                        P.op("pe", (lambda s=s, c=c, bank=bank, i=i: lambda e: e.transpose(bank[:, i * 128:(i + 1) * 128], s[:, c * 128:(c + 1) * 128], C.ident[:]))(), reads=[sk, "ident"], writes=[bk])
                    srcp = bank[:].rearrange("p (i k) -> p i k", i=4)
                    P.op("act", (lambda q=q, t=t, srcp=srcp: lambda e: e.copy(out=xT[:, q * 4:(q + 1) * 4, t * 128:(t + 1) * 128], in_=srcp))(), reads=[bk], writes=["mxT", bk])
                    P.op("dve", (lambda q=q, srcp=srcp: lambda e: e.tensor_copy(out=xT32[:, q * 4:(q + 1) * 4, :], in_=srcp))(), reads=[bk], writes=["xT32", bk])
                lb = C.pb[2]
                for c in range(16):
                    P.op("pe", (lambda c=c: lambda e: e.matmul(lb[:, 0:16], lhsT=xT32[:, c, :], rhs=rw[:, c, :], start=(c == 0), stop=(c == 15)))(), reads=["xT32", "rw"], writes=["pb2"])
                aff, sel, msk, t1, t2, wv = sm["aff"], sm["sel"], sm["msk"], sm["t1"], sm["t2"], sm["w"]
                m1, m2, gs, gm = sm4["m1"], sm4["m2"], sm4["gs"], sm4["gm"]
                a1, b1 = sm1["a"], sm1["b"]
                G = ["gsm"]
                P.op("act", lambda e: e.activation(out=aff[:], in_=lb[:, 0:16], func=AF.Sigmoid), reads=["pb2"], writes=G)
                P.op("dve", lambda e: e.tensor_tensor(out=sel[:], in0=aff[:], in1=rbB[:], op=ALU.add), reads=G + ["rbB"], writes=G)
                sel3 = sel[:].rearrange("p (g k) -> p g k", g=4)
                t13 = t1[:].rearrange("p (g k) -> p g k", g=4)
                P.op("dve", lambda e: e.tensor_reduce(out=m1[:], in_=sel3, axis=AX.X, op=ALU.max), reads=G, writes=G)
                P.op("dve", lambda e: e.tensor_tensor(out=t13, in0=sel3, in1=m1[:, :, None].to_broadcast([128, 4, 4]), op=ALU.is_equal), reads=G, writes=G)
                P.op("dve", lambda e: e.scalar_tensor_tensor(out=t1[:], in0=t1[:], scalar=NEG, in1=sel[:], op0=ALU.mult, op1=ALU.add), reads=G, writes=G)
                P.op("dve", lambda e: e.tensor_reduce(out=m2[:], in_=t13, axis=AX.X, op=ALU.max), reads=G, writes=G)
                P.op("dve", lambda e: e.tensor_tensor(out=gs[:], in0=m1[:], in1=m2[:], op=ALU.add), reads=G, writes=G)
                P.op("dve", lambda e: e.tensor_reduce(out=a1[:], in_=gs[:], axis=AX.X, op=ALU.max), reads=G, writes=G)
                P.op("dve", lambda e: e.tensor_scalar(out=gm[:], in0=gs[:], scalar1=a1[:, 0:1], scalar2=None, op0=ALU.is_equal), reads=G, writes=G)
                msk3 = msk[:].rearrange("p (g k) -> p g k", g=4)
                P.op("dve", lambda e: e.tensor_copy(out=msk3, in_=gm[:, :, None].to_broadcast([128, 4, 4])), reads=G, writes=G)
                P.op("dve", lambda e: e.tensor_tensor(out=t1[:], in0=sel[:], in1=msk[:], op=ALU.mult), reads=G, writes=G)
                P.op("dve", lambda e: e.tensor_scalar(out=t2[:], in0=msk[:], scalar1=-1.0, scalar2=-NEG, op0=ALU.add, op1=ALU.mult), reads=G, writes=G)
                P.op("dve", lambda e: e.tensor_tensor(out=t1[:], in0=t1[:], in1=t2[:], op=ALU.add), reads=G, writes=G)
                P.op("dve", lambda e: e.tensor_reduce(out=a1[:], in_=t1[:], axis=AX.X, op=ALU.max), reads=G, writes=G)
                P.op("dve", lambda e: e.tensor_scalar(out=wv[:], in0=t1[:], scalar1=a1[:, 0:1], scalar2=None, op0=ALU.is_equal), reads=G, writes=G)
                P.op("dve", lambda e: e.scalar_tensor_tensor(out=t1[:], in0=wv[:], scalar=NEG, in1=t1[:], op0=ALU.mult, op1=ALU.add), reads=G, writes=G)
                P.op("dve", lambda e: e.tensor_reduce(out=b1[:], in_=t1[:], axis=AX.X, op=ALU.max), reads=G, writes=G)
                P.op("dve", lambda e: e.tensor_scalar(out=t2[:], in0=t1[:], scalar1=b1[:, 0:1], scalar2=None, op0=ALU.is_equal), reads=G, writes=G)
                P.op("dve", lambda e: e.tensor_tensor(out=wv[:], in0=wv[:], in1=t2[:], op=ALU.add), reads=G, writes=G)
                P.op("dve", lambda e: e.tensor_tensor(out=wv[:], in0=wv[:], in1=aff[:], op=ALU.mult), reads=G, writes=G)
                P.op("dve", lambda e: e.tensor_reduce(out=a1[:], in_=wv[:], axis=AX.X, op=ALU.add), reads=G, writes=G)
                P.op("dve", lambda e: e.reciprocal(out=a1[:], in_=a1[:]), reads=G, writes=G)
                P.op("dve", (lambda t=t: lambda e: e.tensor_scalar(out=gate[:, t, :], in0=wv[:], scalar1=a1[:, 0:1], scalar2=None, op0=ALU.mult))(), reads=G, writes=["gate"])
                if t == 0 and MOE_DBG is not None:
                    P.op("dve", lambda e: e.tensor_copy(out=dbg_sb[:, 0:16], in_=gate[:, 0, :]), reads=["gate"], writes=["dbg"])
                    P.op("dve", lambda e: e.tensor_copy(out=dbg_sb[:, 400:416], in_=aff[:]), reads=G, writes=["dbg"])
                    P.op("dve", lambda e: e.tensor_copy(out=dbg_sb[:, 416:432], in_=wv[:]), reads=G, writes=["dbg"])
                P.op("pe", (lambda t=t: lambda e: e.transpose(C.pb[3][0:16, 0:128], gate[:, t, :], C.ident[:]))(), reads=["gate", "ident"], writes=["pb3"])
                P.op("act", (lambda t=t: lambda e: e.copy(out=gateT[:, t * 128:(t + 1) * 128], in_=C.pb[3][0:16, 0:128]))(), reads=["pb3"], writes=["gateT"])
            P.barrier()
            st1.close()
            if MOE_STOP == "build":
                return
            st2 = ExitStack()
            wgu = [_sb(C, st2, "wgu", [128, 16, 1024], BF16) for _ in range(2)]
            wdn = _sb(C, st2, "wdn", [128, 4, D], BF16)
            gB = _sb(C, st2, "mgB", [128, HT], BF16)
            hm = [_sb(C, st2, "hm", [128, 4, 512], BF16) for _ in range(2)]
            sa = [_sb(C, st2, "sa", [128, 512], BF16) for _ in range(2)]
            sg = [_sb(C, st2, "sg", [128, 512], BF16) for _ in range(2)]
            gv = w_gu.rearrange("x (c p) n -> x p c n", p=128)
            dv = w_down.rearrange("x (c p) n -> x p c n", p=128)
            P.dma("pool", lambda e: e.dma_start(out=wgu[0][:], in_=gv[0]), ("wgu", 0), writes=[("wgu", 0)])
            kk = 0
            for ex in range(16 if MOE_STOP is None else 1):
                wg = wgu[ex % 2]; wgk = ("wgu", ex % 2)
                if ex + 1 < 16:
                    P.dma("pool", (lambda ex=ex: lambda e: e.dma_start(out=wgu[(ex + 1) % 2][:], in_=gv[ex + 1]))(), ("wgu", (ex + 1) % 2), writes=[("wgu", (ex + 1) % 2)])
                P.dma("pool", (lambda ex=ex: lambda e: e.dma_start(out=wdn[:], in_=dv[ex]))(), "wdn", writes=["wdn"])
                for tb in range(HT // 512):
                    P.op("pe", (lambda ex=ex, tb=tb: lambda e: e.matmul(C.pb[2][:, :], lhsT=sel16[:, ex, :], rhs=gateT[:, tb * 512:(tb + 1) * 512], start=True, stop=True))(), reads=["sel16", "gateT"], writes=["pb2"])
                    P.op("act", (lambda tb=tb: lambda e: e.copy(out=gB[:, tb * 512:(tb + 1) * 512], in_=C.pb[2][:, :]))(), reads=["pb2"], writes=[("mgB", tb)])
                for tb in range(HT // 512):
                    hmt = hm[tb % 2]; hk = ("hm", tb % 2)
                    for ffc in range(4):
                        pa = C.pb[4 + (kk % 2) * 2]; pak = f"pb{4 + (kk % 2) * 2}"
                        pbb = C.pb[5 + (kk % 2) * 2]; pbk = f"pb{5 + (kk % 2) * 2}"
                        for c in range(16):
                            P.op("pe", (lambda pa=pa, wg=wg, c=c, ffc=ffc, tb=tb: lambda e: e.matmul(pa[:, :], lhsT=wg[:, c, ffc * 128:(ffc + 1) * 128], rhs=xT[:, c, tb * 512:(tb + 1) * 512], start=(c == 0), stop=(c == 15)))(), reads=[wgk, "mxT"], writes=[pak])
                        for c in range(16):
                            P.op("pe", (lambda pbb=pbb, wg=wg, c=c, ffc=ffc, tb=tb: lambda e: e.matmul(pbb[:, :], lhsT=wg[:, c, 512 + ffc * 128:512 + (ffc + 1) * 128], rhs=xT[:, c, tb * 512:(tb + 1) * 512], start=(c == 0), stop=(c == 15)))(), reads=[wgk, "mxT"], writes=[pbk])
                        sat = sa[kk % 2]; sgt = sg[kk % 2]; sak = ("sa", kk % 2); sgk = ("sg", kk % 2)
                        P.op("act", (lambda sat=sat, pa=pa: lambda e: e.activation(out=sat[:], in_=pa[:, :], func=AF.Silu))(), reads=[pak], writes=[sak])
                        P.op("dve", (lambda sgt=sgt, sat=sat, tb=tb: lambda e: e.tensor_tensor(out=sgt[:], in0=sat[:], in1=gB[:, tb * 512:(tb + 1) * 512], op=ALU.mult))(), reads=[sak, ("mgB", tb)], writes=[sgk])
                        P.op("dve", (lambda hmt=hmt, ffc=ffc, sgt=sgt, pbb=pbb: lambda e: e.tensor_tensor(out=hmt[:, ffc, :], in0=sgt[:], in1=pbb[:, :], op=ALU.mult))(), reads=[sgk, pbk], writes=[hk])
                        kk += 1
                for tb in range(HT // 512):
                    hmt = hm[tb % 2]; hk = ("hm", tb % 2)
                    for dc in range(16):
                        po = C.pb[dc % 4]; pok = f"pb{dc % 4}"
                        for ffc in range(4):
                            P.op("pe", (lambda po=po, ffc=ffc, dc=dc, hmt=hmt: lambda e: e.matmul(po[:, :], lhsT=wdn[:, ffc, dc * 128:(dc + 1) * 128], rhs=hmt[:, ffc, :], start=(ffc == 0), stop=(ffc == 3)))(), reads=["wdn", hk], writes=[pok])
                        ydst = yacc[:, dc, tb * 512:(tb + 1) * 512]
                        if ex == 0:
                            P.op("dve", (lambda ydst=ydst, po=po: lambda e: e.tensor_copy(out=ydst, in_=po[:, :]))(), reads=[pok], writes=[("yacc", dc, tb)])
                        else:
                            P.op("dve", (lambda ydst=ydst, po=po: lambda e: e.tensor_tensor(out=ydst, in0=ydst, in1=po[:, :], op=ALU.add))(), reads=[pok, ("yacc", dc, tb)], writes=[("yacc", dc, tb)])
            if MOE_DBG is not None:
                P.op("dve", lambda e: e.tensor_copy(out=dbg_sb[:, 16:144], in_=gB[:, 0:128]), reads=[("mgB", 0)], writes=["dbg"])
                P.op("dve", lambda e: e.tensor_copy(out=dbg_sb[:, 144:272], in_=hm[0][:, 0, 0:128]), reads=[("hm", 0)], writes=["dbg"])
                P.op("dve", lambda e: e.tensor_copy(out=dbg_sb[:, 272:400], in_=yacc[:, 0, 0:128]), reads=[("yacc", 0, 0)], writes=["dbg"])
                P.op("dve", lambda e: e.tensor_copy(out=dbg_sb[0:16, 432:512], in_=gateT[:, 0:80]), reads=["gateT"], writes=["dbg"])
                P.dma("sp", lambda e: e.dma_start(out=MOE_DBG[:, :], in_=dbg_sb[:]), "dbg", reads=["dbg"], writes=["dbgout"])
            P.barrier()
            st2.close()
            if ln is None:
                fo = [_sb(C, st, "fo", [128, D]) for _ in range(2)]
                for t in range(HT // 128):
                    fot = fo[t % 2]; fk = ("fo", t % 2)
                    for q in range(4):
                        bank = C.pb[q % 2]; bk = f"pb{q % 2}"
                        for i in range(4):
                            dc = q * 4 + i
                            P.op("pe", _tr(bank[:, i * 128:(i + 1) * 128], yacc[:, dc, t * 128:(t + 1) * 128], C.ident[:]), reads=[("yacc", dc, t // 4), "ident"], writes=[bk])
                        P.op("act", _acp(fot[:, q * 512:(q + 1) * 512], bank[:, :]), reads=[bk], writes=[fk])
                    P.dma("sp", _dma(f_out[half * HT + t * 128: half * HT + (t + 1) * 128, :], fot[:]), fk, reads=[fk], writes=[("fout", half, t)])
            else:
                gB_d, bB_d, out_d = ln
                ep = LNEpi(C, st, h, gB_d, bB_d, out_d, "moe")
                ntl = HT // 128
                for t in range(ntl):
                    base = (t % 2) * 4
                    keys = [f"pb{base + q}" for q in range(4)]
                    for q in range(4):
                        for i in range(4):
                            dc = q * 4 + i
                            P.op("pe", _tr(C.pb[base + q][:, i * 128:(i + 1) * 128], yacc[:, dc, t * 128:(t + 1) * 128], C.ident[:]), reads=[("yacc", dc, t // 4), "ident"], writes=[keys[q]])
                    ep.front(t, half * HT + t * 128, C.ps_all[:, base * 512:base * 512 + 2048], keys)
                    if t >= 1:
                        ep.back(t - 1, half * HT + (t - 1) * 128)
                ep.back(ntl - 1, half * HT + (ntl - 1) * 128)
        P.barrier()


def _tt(out, in0, in1, op):
    return lambda e: e.tensor_tensor(out=out, in0=in0, in1=in1, op=op)


def _ts(out, in0, s1, s2=None, op0=ALU.mult, op1=None, accum=None):
    if accum is not None:
        return lambda e: e.tensor_scalar(out=out, in0=in0, scalar1=s1, scalar2=s2, op0=op0, op1=op1, accum_out=accum)
    if op1 is None:
        return lambda e: e.tensor_scalar(out=out, in0=in0, scalar1=s1, scalar2=None, op0=op0)
    return lambda e: e.tensor_scalar(out=out, in0=in0, scalar1=s1, scalar2=s2, op0=op0, op1=op1)


def _stt(out, in0, scalar, in1, op0, op1):
    return lambda e: e.scalar_tensor_tensor(out=out, in0=in0, scalar=scalar, in1=in1, op0=op0, op1=op1)


def _act(out, in_, func, bias=None, scale=None, accum=None):
    kw = {}
    if bias is not None:
        kw["bias"] = bias
    if scale is not None:
        kw["scale"] = scale
    if accum is not None:
        kw["accum_out"] = accum
    return lambda e: e.activation(out=out, in_=in_, func=func, **kw)


def _mm(out, lhsT, rhs, start, stop):
    return lambda e: e.matmul(out, lhsT=lhsT, rhs=rhs, start=start, stop=stop)


def _tr(out, in_, ident):
    return lambda e: e.transpose(out, in_, ident)


def _acp(out, in_):
    return lambda e: e.copy(out=out, in_=in_)


def _cp(out, in_):
    return lambda e: e.tensor_copy(out=out, in_=in_)


def _dma(out, in_):
    return lambda e: e.dma_start(out=out, in_=in_)


def _red(out, in_, op):
    return lambda e: e.tensor_reduce(out=out, in_=in_, axis=AX.X, op=op)


def _rcp(out, in_):
    return lambda e: e.reciprocal(out=out, in_=in_)


def _mset(out, v):
    return lambda e: e.memset(out, v)


def _sqrt(out, in_):
    return lambda e: e.sqrt(out=out, in_=in_)


def _rope(P, src, dst, cs, H, dh, t1, t2, rkeys, wkey, tk):
    hf = dh // 2
    s3 = src.rearrange("p (h d) -> p h d", h=H)
    d3 = dst.rearrange("p (h d) -> p h d", h=H)
    x1 = s3[:, :, 0:hf]; x2 = s3[:, :, hf:dh]
    cosB = cs[:, 0:1, :].to_broadcast([128, H, hf])
    sinB = cs[:, 1:2, :].to_broadcast([128, H, hf])
    a = t1[:, 0:H * hf].rearrange("p (h d) -> p h d", h=H)
    b = t2[:, 0:H * hf].rearrange("p (h d) -> p h d", h=H)
    k1, k2 = (tk, 1), (tk, 2)
    P.op("dve", _tt(a, x1, cosB, ALU.mult), reads=rkeys, writes=[k1])
    P.op("dve", _tt(b, x2, sinB, ALU.mult), reads=rkeys, writes=[k2])
    P.op("dve", _tt(d3[:, :, 0:hf], a, b, ALU.subtract), reads=[k1, k2], writes=[wkey])
    P.op("dve", _tt(a, x2, cosB, ALU.mult), reads=rkeys, writes=[k1])
    P.op("dve", _tt(b, x1, sinB, ALU.mult), reads=rkeys, writes=[k2])
    P.op("dve", _tt(d3[:, :, hf:dh], a, b, ALU.add), reads=[k1, k2, wkey], writes=[wkey])


def _transpose_blocks(C, src, nblk, dst3, rkeys, wkey, eng_toggle=0):
    P = C.P
    b0 = 0
    g = 0
    while b0 < nblk:
        n = min(4, nblk - b0)
        bi = (g + eng_toggle) % 2
        bank = C.pb[bi]; bk = f"pb{bi}"
        for i in range(n):
            P.op("pe", _tr(bank[:, i * 128:(i + 1) * 128], src[:, (b0 + i) * 128:(b0 + i + 1) * 128], C.ident[:]), reads=list(rkeys) + ["ident"], writes=[bk])
        srcp = bank[:, 0:n * 128].rearrange("p (i k) -> p i k", i=n)
        if bi == 0:
            P.op("act", _acp(dst3[:, b0:b0 + n, :], srcp), reads=[bk], writes=[wkey])
        else:
            P.op("dve", _cp(dst3[:, b0:b0 + n, :], srcp), reads=[bk], writes=[wkey])
        b0 += n
        g += 1


RET_G = [1.0 - 2.0 ** (-5.0 - h) for h in range(4)]


def mixer_ret(C, proj, pkv, mix, cst, cs_key="cs_own", s_in=None, s_out=None):
    P = C.P
    P.stage = C.pfx + "ret"
    with ExitStack() as st:
        decT = _sb(C, st, "decT", [128, 512]); xi = _sb(C, st, "xi", [128, 4]); zeta = _sb(C, st, "zeta", [128, 4]); gnB = _sb(C, st, "gnB", [128, 1024])
        P.dma("sp", _dma(decT[:], cst["decT"][:, :]), "decT", writes=["decT"])
        P.dma("sp", _dma(xi[:], cst["xi"][:, :]), "xi", writes=["xi"])
        P.dma("sp", _dma(zeta[:], cst["zeta"][:, :]), "zeta", writes=["zeta"])
        P.dma("sp", _dma(gnB[:], cst["gnB"][:, :]), "gnB", writes=["gnB"])
        S32 = _sb(C, st, "S32", [128, 4, 512]); Sb = _sb(C, st, "Sb", [128, 4, 512], BF16)
        if s_in is None:
            P.op("dve", _mset(S32[:], 0.0), writes=["S32"])
            P.op("dve", _mset(Sb[:], 0.0), writes=["Sb"])
        else:
            P.dma("sp", _dma(S32[:].rearrange("p h n -> p (h n)"), s_in[:, :]), "S32io", writes=["S32"])
            P.op("act", _acp(Sb[:], S32[:]), reads=["S32"], writes=["Sb"])
        qkvg = [_sb(C, st, "qkvg", [128, 4096]) for _ in range(2)]
        csb = [_sb(C, st, "csb", [128, 2, 128]) for _ in range(2)]
        qr = _sb(C, st, "qr", [128, 1024]); kr = _sb(C, st, "kr", [128, 1024]); qx = _sb(C, st, "qx", [128, 1024])
        kz = _sb(C, st, "kz", [128, 1024], BF16); vb = _sb(C, st, "vb", [128, 1024], BF16)
        qT = _sb(C, st, "qT", [128, 8, 128], BF16); kT = _sb(C, st, "kT", [128, 8, 128], BF16); qxT = _sb(C, st, "qxT", [128, 8, 128], BF16)
        am = _sb(C, st, "am", [128, 512], BF16)
        t1 = _sb(C, st, "rt1", [128, 512]); t2 = _sb(C, st, "rt2", [128, 512])
        junk = _sb(C, st, "rjunk", [128, 256])
        st4 = {n: _sb(C, st, "st" + n, [128, 4]) for n in ["s", "q", "m", "r", "nb"]}
        yn = _sb(C, st, "yn", [128, 1024]); sg = _sb(C, st, "sgt", [128, 1024])
        ret = [_sb(C, st, "ret", [128, 1024]) for _ in range(2)]
        SB = [C.pb[5], C.pb[6], C.pb[7], C.pb[2]]; SBK = ["pb5", "pb6", "pb7", "pb2"]
        kz2 = [kz, _sb(C, st, "kz2", [128, 1024], BF16)]; vb2 = [vb, _sb(C, st, "vb2", [128, 1024], BF16)]
        qT2_ = [qT, _sb(C, st, "qT2", [128, 8, 128], BF16)]; kT2_ = [kT, _sb(C, st, "kT2", [128, 8, 128], BF16)]; qxT2_ = [qxT, _sb(C, st, "qxT2", [128, 8, 128], BF16)]

        def front(ci):
            own = ci >= 16
            t = ci % 16; i = ci % 2
            buf = qkvg[i]; bkey = ("qkvg", i)
            cs = csb[i]; ckey = ("csb", i)
            if own:
                P.dma("sp", _dma(buf[:], proj[t * 128:(t + 1) * 128, 0:4096]), bkey, writes=[bkey])
                P.dma("sp", _dma(cs[:], cst[cs_key][t * 128:(t + 1) * 128, :, :]), ckey, writes=[ckey])
            else:
                P.dma("sp", _dma(buf[:, 1024:3072], pkv[t * 128:(t + 1) * 128, :]), bkey, writes=[bkey])
                P.dma("sp", _dma(cs[:], cst["cs_pre"][t * 128:(t + 1) * 128, :, :]), ckey, writes=[ckey])
            _rope(P, buf[:, 1024:2048], kr[:], cs, 4, 256, t1, t2, [bkey, ckey], "kr", "rt")
            P.op("dve", _tt(kz2[i][:].rearrange("p (h d) -> p h d", h=4), kr[:].rearrange("p (h d) -> p h d", h=4), zeta[:, :, None].to_broadcast([128, 4, 256]), ALU.mult), reads=["kr", "zeta"], writes=[("kz", i)])
            P.op("act", _acp(vb2[i][:], buf[:, 2048:3072]), reads=[bkey], writes=[("vb", i)])
            if own:
                _rope(P, buf[:, 0:1024], qr[:], cs, 4, 256, t1, t2, [bkey, ckey], "qr", "rt")
                P.op("dve", _tt(qx[:].rearrange("p (h d) -> p h d", h=4), qr[:].rearrange("p (h d) -> p h d", h=4), xi[:, :, None].to_broadcast([128, 4, 256]), ALU.mult), reads=["qr", "xi"], writes=["qx"])
                _transpose_blocks(C, qr, 8, qT2_[i], ["qr"], ("qT", i), 0)
                _transpose_blocks(C, kr, 8, kT2_[i], ["kr"], ("kT", i), 0)
                _transpose_blocks(C, qx, 8, qxT2_[i], ["qx"], ("qxT", i), 0)

        def back(ci):
            own = ci >= 16
            t = ci % 16; i = ci % 2
            buf = qkvg[i]; bkey = ("qkvg", i)
            kzi, vbi, qTi, kTi, qxTi = kz2[i], vb2[i], qT2_[i], kT2_[i], qxT2_[i]
            kzk, vbk, qTk, kTk, qxTk = ("kz", i), ("vb", i), ("qT", i), ("kT", i), ("qxT", i)
            if own:
                for h in range(4):
                    for j in range(2):
                        P.op("pe", _mm(C.pb[2][:, h * 128:(h + 1) * 128], kTi[:, 2 * h + j, :], qTi[:, 2 * h + j, :], j == 0, j == 1), reads=[kTk, qTk], writes=["pb2"])
                P.op("dve", _tt(am[:], C.pb[2][:, :], decT[:], ALU.mult), reads=["pb2", "decT"], writes=["am", "pb2"])
                for h in range(4):
                    yb = C.pb[3 + h // 2]; ybk = f"pb{3 + h // 2}"
                    o = yb[:, (h % 2) * 256:(h % 2) * 256 + 256]
                    P.op("pe", _mm(o, am[:, h * 128:(h + 1) * 128], vbi[:, h * 256:(h + 1) * 256], True, False), reads=["am", vbk], writes=[ybk])
                    P.op("pe", _mm(o, qxTi[:, 2 * h, :], Sb[:, h, 0:256], False, False), reads=[qxTk, "Sb"], writes=[ybk])
                    P.op("pe", _mm(o, qxTi[:, 2 * h + 1, :], Sb[:, h, 256:512], False, True), reads=[qxTk, "Sb"], writes=[ybk])
            for h in range(4):
                for j in range(2):
                    P.op("pe", _mm(SB[h][:, j * 256:(j + 1) * 256], kzi[:, h * 256 + j * 128:h * 256 + (j + 1) * 128], vbi[:, h * 256:(h + 1) * 256], True, True), reads=[kzk, vbk], writes=[SBK[h]])
            for h in range(4):
                P.op("dve", _stt(S32[:, h, :], S32[:, h, :], RET_G[h] ** 128, SB[h][:, :], ALU.mult, ALU.add), reads=[SBK[h], "S32"], writes=["S32", SBK[h]])
            P.op("act", _acp(Sb[:], S32[:]), reads=["S32"], writes=["Sb"])
            if not own:
                return
            s_, q_, m_, r_, nb_ = st4["s"], st4["q"], st4["m"], st4["r"], st4["nb"]
            for h in range(4):
                yb = C.pb[3 + h // 2]; ybk = f"pb{3 + h // 2}"
                o = yb[:, (h % 2) * 256:(h % 2) * 256 + 256]
                P.op("act", _act(junk[:], o, AF.Identity, accum=s_[:, h:h + 1]), reads=[ybk], writes=["rjunk", "st_s"])
                P.op("act", _act(junk[:], o, AF.Square, accum=q_[:, h:h + 1]), reads=[ybk], writes=["rjunk", "st_q"])
            P.op("dve", _ts(m_[:], s_[:], 1.0 / 256), reads=["st_s"], writes=["st_m"])
            P.op("dve", _tt(r_[:], m_[:], m_[:], ALU.mult), reads=["st_m"], writes=["st_r"])
            P.op("dve", _stt(r_[:], q_[:], 1.0 / 256, r_[:], ALU.mult, ALU.subtract), reads=["st_q", "st_r"], writes=["st_r"])
            P.op("dve", _ts(r_[:], r_[:], EPS, None, ALU.add), reads=["st_r"], writes=["st_r"])
            P.op("act", _sqrt(r_[:], r_[:]), reads=["st_r"], writes=["st_r"])
            P.op("dve", _rcp(r_[:], r_[:]), reads=["st_r"], writes=["st_r"])
            P.op("dve", _stt(nb_[:], m_[:], -1.0, r_[:], ALU.mult, ALU.mult), reads=["st_m", "st_r"], writes=["st_nb"])
            for h in range(4):
                yb = C.pb[3 + h // 2]; ybk = f"pb{3 + h // 2}"
                o = yb[:, (h % 2) * 256:(h % 2) * 256 + 256]
                P.op("act", _act(yn[:, h * 256:(h + 1) * 256], o, AF.Identity, bias=nb_[:, h:h + 1], scale=r_[:, h:h + 1]), reads=[ybk, "st_r", "st_nb"], writes=["yn", ybk])
            P.op("act", _act(sg[:], buf[:, 3072:4096], AF.Silu), reads=[bkey], writes=["sgt"])
            P.op("pool", _tt(yn[:], yn[:], gnB[:], ALU.mult), reads=["yn", "gnB"], writes=["yn"])
            rt = ret[t % 2]; rk = ("ret", t % 2)
            P.op("pool", _tt(rt[:], yn[:], sg[:], ALU.mult), reads=["yn", "sgt"], writes=[rk])
            P.dma("sp", _dma(mix[t * 128:(t + 1) * 128, 0:1024], rt[:]), rk, reads=[rk], writes=[("mixr", t)])

        cis = list(range(0 if pkv is not None else 16, 32))
        front(cis[0])
        for n_, ci in enumerate(cis):
            if n_ + 1 < len(cis):
                front(cis[n_ + 1])
            back(ci)
        if s_out is not None:
            P.dma("sp", _dma(s_out[:, :], S32[:].rearrange("p h n -> p (h n)")), "S32io", reads=["S32"], writes=["s_out"])
    P.barrier()


def mixer_conv(C, proj, phalo, mix, cst):
    P = C.P
    P.stage = C.pfx + "conv"
    NTK = T + 128
    with ExitStack() as st:
        cw = _sb(C, st, "cw", [128, 8, 31]); cv = _sb(C, st, "cv", [128, 3, 8])
        P.dma("sp", _dma(cw[:], cst["conv_w"][:, :, :]), "cw", writes=["cw"])
        P.dma("sp", _dma(cv[:], cst["conv_v"][:, :, :]), "cv", writes=["cv"])
        uT = _sb(C, st, "uT", [128, 8, NTK])
        yT = _sb(C, st, "yT", [128, 8, T])
        gg = [_sb(C, st, "gg", [128, 2048]) for _ in range(2)]
        sig = _sb(C, st, "sig", [128, 1024]); u = _sb(C, st, "u", [128, 1024])
        for ti in range(17):
            buf = gg[ti % 2]; bkey = ("gg", ti % 2)
            if ti == 0:
                P.dma("sp", _dma(buf[:], phalo[:, :]), bkey, writes=[bkey])
            else:
                P.dma("sp", _dma(buf[:], proj[(ti - 1) * 128:ti * 128, 4096:6144]), bkey, writes=[bkey])
            P.op("act", _act(sig[:], buf[:, 1024:2048], AF.Sigmoid), reads=[bkey], writes=["sig"])
            P.op("dve", _tt(u[:], buf[:, 0:1024], sig[:], ALU.mult), reads=[bkey, "sig"], writes=["u"])
            _transpose_blocks(C, u, 8, uT[:, :, ti * 128:(ti + 1) * 128], ["u"], "uT", ti)
        ptmp = _sb(C, st, "cptmp", [128, T])
        pool_chunks = ()
        for k in range(31):
            for j in range(8):
                yk = ("yT", j)
                src_k = uT[:, j, 98 + k:98 + k + T]
                if k == 0:
                    eng = "pool" if j in pool_chunks else "dve"
                    P.op(eng, _ts(yT[:, j, :], src_k, cw[:, j, 0:1], cv[:, 0, j:j + 1], ALU.mult, ALU.add), reads=["uT", "cw", "cv"], writes=[yk])
                elif j in pool_chunks:
                    P.op("pool", _ts(ptmp[:], src_k, cw[:, j, k:k + 1]), reads=["uT", "cw"], writes=["cptmp"])
                    P.op("pool", _tt(yT[:, j, :], yT[:, j, :], ptmp[:], ALU.add), reads=["cptmp", yk], writes=[yk])
                else:
                    P.op("dve", _stt(yT[:, j, :], src_k, cw[:, j, k:k + 1], yT[:, j, :], ALU.mult, ALU.add), reads=["uT", "cw", yk], writes=[yk])
        mean = _sb(C, st, "cmean", [128, 512]); rstd = _sb(C, st, "crstd", [128, 512]); sq = [_sb(C, st, "csq", [128, 512]) for _ in range(2)]
        z = [_sb(C, st, "cz", [128, 512]) for _ in range(2)]
        for tb in range(4):
            sl = slice(tb * 512, (tb + 1) * 512)
            for j in range(8):
                P.op("pe", _mm(C.pb[2][:, :], C.ones[:], yT[:, j, sl], j == 0, j == 7), reads=[("yT", j), "ones"], writes=["pb2"])
            for j in range(8):
                sqt = sq[j % 2]; sqk = ("csq", j % 2)
                P.op("act", _act(sqt[:], yT[:, j, sl], AF.Square), reads=[("yT", j)], writes=[sqk])
                P.op("pe", _mm(C.pb[3][:, :], C.ones[:], sqt[:], j == 0, j == 7), reads=[sqk, "ones"], writes=["pb3"])
            P.op("dve", _ts(mean[:], C.pb[2][:, :], 1.0 / 1024), reads=["pb2"], writes=["cmean"])
            P.op("dve", _tt(rstd[:], mean[:], mean[:], ALU.mult), reads=["cmean"], writes=["crstd"])
            P.op("dve", _stt(rstd[:], C.pb[3][:, :], 1.0 / 1024, rstd[:], ALU.mult, ALU.subtract), reads=["pb3", "crstd"], writes=["crstd"])
            P.op("dve", _ts(rstd[:], rstd[:], EPS, None, ALU.add), reads=["crstd"], writes=["crstd"])
            P.op("act", _sqrt(rstd[:], rstd[:]), reads=["crstd"], writes=["crstd"])
            P.op("dve", _rcp(rstd[:], rstd[:]), reads=["crstd"], writes=["crstd"])
            for j in range(8):
                zt = z[j % 2]; zk = ("cz", j % 2)
                P.op("dve", _tt(zt[:], yT[:, j, sl], mean[:], ALU.subtract), reads=[("yT", j), "cmean"], writes=[zk])
                P.op("dve", _tt(zt[:], zt[:], rstd[:], ALU.mult), reads=[zk, "crstd"], writes=[zk])
                P.op("act", _act(yT[:, j, sl], zt[:], AF.Silu, bias=cv[:, 2, j:j + 1], scale=cv[:, 1, j:j + 1]), reads=[zk, "cv"], writes=[("yT", j)])
        co = [_sb(C, st, "co", [128, 1024]) for _ in range(2)]
        for t in range(16):
            cot = co[t % 2]; ck = ("co", t % 2)
            for g in range(2):
                bank = C.pb[g]; bk = f"pb{g}"
                for i in range(4):
                    j = g * 4 + i
                    P.op("pe", _tr(bank[:, i * 128:(i + 1) * 128], yT[:, j, t * 128:(t + 1) * 128], C.ident[:]), reads=[("yT", j), "ident"], writes=[bk])
                if g == 0:
                    P.op("act", _acp(cot[:, 0:512], bank[:, :]), reads=[bk], writes=[ck])
                else:
                    P.op("dve", _cp(cot[:, 512:1024], bank[:, :]), reads=[bk], writes=[ck])
            P.dma("sp", _dma(mix[t * 128:(t + 1) * 128, 1024:2048], cot[:]), ck, reads=[ck], writes=[("mixc", t)])
    P.barrier()


def _bcast_rows(v):
    v = np.asarray(v, np.float32)
    return np.ascontiguousarray(np.broadcast_to(v[None, :], (128, v.shape[0])))


def _rope_tab(pos, half):
    inv = (10000.0 ** (-np.arange(half, dtype=np.float32) / np.float32(half))).astype(np.float32)
    ang = pos.astype(np.float32)[:, None] * inv[None, :]
    return np.ascontiguousarray(np.stack([np.cos(ang), np.sin(ang)], axis=1).astype(np.float32))


def _common_tail(C, x, mix, w_out, g1, b1, g2, b2, w_gu, w_down, rw, rbB, m, ha, f, out):
    linear_ln(C, mix, w_out, x, g1, b1, ha, "mix")
    moe(C, ha, w_gu, w_down, rw, rbB, f, "moe", ln=(g2, b2, out))


def build_layer0(debug=False):
    nc = bass.Bass("TRN2", target_bir_lowering=False)
    dt = lambda name, shape, kind="ExternalInput": nc.dram_tensor(name, shape, F32, kind=kind).ap()
    x = dt("x", [T, D]); xp = dt("xp", [T, D]); w_in = dt("w_in", [D, 6144]); w_out = dt("w_out", [D, D])
    g1 = dt("mix_g", [128, D]); b1 = dt("mix_b", [128, D]); g2 = dt("ffn_g", [128, D]); b2 = dt("ffn_b", [128, D])
    w_gu = dt("w_gu", [16, D, 1024]); w_down = dt("w_down", [16, 512, D])
    rw = dt("rw", [D, 16]); rbB = dt("rbB", [128, 16]); ident = dt("ident", [128, 128])
    cst = {"cs_own": dt("cs_own", [T, 2, 128]), "cs_pre": dt("cs_pre", [T, 2, 128]), "decT": dt("decT", [128, 512]),
           "xi": dt("xi", [128, 4]), "zeta": dt("zeta", [128, 4]), "gnB": dt("gnB", [128, 1024]),
           "conv_w": dt("conv_w", [128, 8, 31]), "conv_v": dt("conv_v", [128, 3, 8])}
    out = dt("out", [T, D], "ExternalOutput")
    dk = "ExternalOutput" if debug else "Internal"
    proj = dt("proj", [T, 6144], "Internal"); pkv = dt("pkv", [T, 2048], "Internal"); phalo = dt("phalo", [128, 2048], "Internal")
    mix = dt("mix", [T, D], dk); m = dt("m", [T, D], dk)
    ha = dt("ha", [T, D], dk); f = dt("f", [T, D], "Internal")
    with ExitStack() as es:
        C = _mk_ctx(nc, es)
        _load_consts(C, ident)
        linear(C, x, T, w_in, [(0, 6144)], proj, "in")
        linear(C, xp, T, w_in, [(1024, 3072)], pkv, "pkv")
        linear(C, xp[T - 128:T, :], 128, w_in, [(4096, 6144)], phalo, "ph")
        mixer_ret(C, proj, pkv, mix, cst)
        mixer_conv(C, proj, phalo, mix, cst)
        _common_tail(C, x, mix, w_out, g1, b1, g2, b2, w_gu, w_down, rw, rbB, m, ha, f, out)
        C.P.emit()
    return nc


def layer0_inputs(inp, h, core):
    b, half = core // 2, core % 2
    x = h[b, half * T:(half + 1) * T]
    xp = h[b, 0:T] if half == 1 else np.zeros((T, D), np.float32)
    g = np.array(RET_G, np.float64)
    j = np.arange(128, dtype=np.float64)
    diff = j[None, :] - j[:, None]
    decT = np.concatenate([np.where(diff >= 0, g[h_] ** np.maximum(diff, 0), 0.0) / 16.0 for h_ in range(4)], axis=1)
    xi = np.stack([g[h_] ** (j + 1.0) for h_ in range(4)], axis=1)
    zeta = np.stack([g[h_] ** (127.0 - j) / 16.0 for h_ in range(4)], axis=1)
    conv_w = np.asarray(inp["even_conv_w"][0], np.float32)
    cvec = np.stack([inp["even_conv_b"][0], inp["even_conv_ln_g"][0], inp["even_conv_ln_b"][0]], axis=0).astype(np.float32)
    return {
        "x": np.ascontiguousarray(x), "xp": np.ascontiguousarray(xp),
        "w_in": np.asarray(inp["even_w_in"][0], np.float32), "w_out": np.asarray(inp["even_w_out"][0], np.float32),
        "mix_g": _bcast_rows(inp["mix_ln_g"][0]), "mix_b": _bcast_rows(inp["mix_ln_b"][0]),
        "ffn_g": _bcast_rows(inp["ffn_ln_g"][0]), "ffn_b": _bcast_rows(inp["ffn_ln_b"][0]),
        "w_gu": np.asarray(inp["moe_w_gu"][0], np.float32), "w_down": np.asarray(inp["moe_w_down"][0], np.float32),
        "rw": np.asarray(inp["router_w"], np.float32), "rbB": _bcast_rows(inp["router_b"]), "ident": np.eye(128, dtype=np.float32),
        "cs_own": _rope_tab(np.arange(half * T, (half + 1) * T), 128), "cs_pre": _rope_tab(np.arange(0, T), 128),
        "decT": np.ascontiguousarray(decT.astype(np.float32)), "xi": np.ascontiguousarray(xi.astype(np.float32)),
        "zeta": np.ascontiguousarray(zeta.astype(np.float32)), "gnB": _bcast_rows(inp["even_ret_gn_g"][0]),
        "conv_w": np.ascontiguousarray(conv_w.T.reshape(8, 128, 31).transpose(1, 0, 2)),
        "conv_v": np.ascontiguousarray(cvec.reshape(3, 8, 128).transpose(2, 0, 1)),
    }


def mixer_dsa(C, qproj, kvproj, attn_out, cst):
    P = C.P
    P.stage = C.pfx + "dsa_kprep"
    SC = 128.0 ** -0.5
    with ExitStack() as st:
        kT = _sb(C, st, "dkT", [128, 4, 4096], BF16)
        vb = _sb(C, st, "dvb", [128, 32, 4, 129], BF16)
        kiT2 = _sb(C, st, "dkiT", [128, 1, 4096], BF16)
        iota = _sb(C, st, "diota", [128, 4096])
        qpos = _sb(C, st, "dqpos", [128, 16])
        P.dma("sp", _dma(iota[:], cst["iota"][:, :]), "diota", writes=["iota"])
        P.dma("sp", _dma(qpos[:], cst["qpos"][:, :]), "dqpos", writes=["qpos"])
        P.op("dve", _mset(vb[:], 1.0), writes=["vb"])
        with ExitStack() as s1:
            kvt = [_sb(C, s1, "kvt", [128, 1088]) for _ in range(2)]
            csk = [_sb(C, s1, "csk", [128, 2, 64]) for _ in range(2)]
            csi = [_sb(C, s1, "csi", [128, 2, 32]) for _ in range(2)]
            kr = _sb(C, s1, "dkr", [128, 512]); kir2 = _sb(C, s1, "dkir", [128, 128])
            t1 = _sb(C, s1, "dt1", [128, 256]); t2 = _sb(C, s1, "dt2", [128, 256])
            for kt in range(32):
                i = kt % 2
                bk_, ck_, ik_ = ("kvt", i), ("csk", i), ("csi", i)
                rows = slice(kt * 128, (kt + 1) * 128)
                P.dma("sp", _dma(kvt[i][:], kvproj[rows, :]), bk_, writes=[bk_])
                P.dma("sp", _dma(csk[i][:], cst["cs_k"][rows, :, :]), ck_, writes=[ck_])
                P.dma("sp", _dma(csi[i][:], cst["cs_ki"][rows, :, :]), ik_, writes=[ik_])
                _rope(P, kvt[i][:, 0:512], kr[:], csk[i], 4, 128, t1, t2, [bk_, ck_], "dkr", "dt")
                _transpose_blocks(C, kr, 4, kT[:, :, kt * 128:(kt + 1) * 128], ["dkr"], "kT", kt)
                P.op("act", _acp(vb[:, kt, :, 0:128], kvt[i][:, 512:1024].rearrange("p (h d) -> p h d", h=4)), reads=[bk_], writes=["vb"])
                _rope(P, kvt[i][:, 1024:1088], kir2[:, 0:64], csi[i], 1, 64, t1, t2, [bk_, ik_], "dkir", "dt")
                P.op("dve", _cp(kir2[:, 64:128], kir2[:, 0:64]), reads=["dkir"], writes=["dkir"])
                _transpose_blocks(C, kir2, 1, kiT2[:, :, kt * 128:(kt + 1) * 128], ["dkir"], "kiT", kt + 1)
        P.barrier()
        P.stage = C.pfx + "dsa_q"
        qt = _sb(C, st, "dqt", [128, 3088])
        csq = [_sb(C, st, "csq", [128, 2, 64]) for _ in range(2)]
        csqi = [_sb(C, st, "csqi", [128, 2, 32]) for _ in range(2)]
        qr = _sb(C, st, "dqr", [128, 2048]); qir = _sb(C, st, "dqir", [128, 1024])
        qT = [_sb(C, st, "dqT", [128, 16, 128], BF16) for _ in range(2)]
        qiT = _sb(C, st, "dqiT", [128, 8, 128], BF16)
        wab = _sb(C, st, "dwab", [128, 16]); sgn = _sb(C, st, "dsgn", [128, 16])
        t1 = _sb(C, st, "dq1", [128, 1024]); t2 = _sb(C, st, "dq2", [128, 1024])
        acc = _sb(C, st, "dacc", [128, 4096]); scr = _sb(C, st, "dscr", [128, 4096])
        tmp = [_sb(C, st, "dtmp", [128, 1024]) for _ in range(2)]
        selT = [_sb(C, st, "dselT", [128, 32, 128], BF16) for _ in range(2)]
        pt = [_sb(C, st, "dp", [128, 1024], BF16) for _ in range(2)]
        obuf = _sb(C, st, "dobuf", [128, 16, 129])
        rec = _sb(C, st, "drec", [128, 16])
        sm = {n: _sb(C, st, "d_" + n, [128, 1]) for n in ["lo", "hi", "w", "mid", "cnt", "ge"]}
        NIT = 26
        hw = _sb(C, st, "d_hw", [128, NIT + 1]); pw2 = _sb(C, st, "d_pw2", [128, NIT + 1])
        for k_ in range(NIT + 1):
            P.op("dve", _mset(pw2[:, k_:k_ + 1], 2.0 ** -(k_ + 1)), writes=["pw2"])
        identb = _sb(C, st, "didb", [128, 128], BF16)
        P.op("dve", _cp(identb[:], C.ident[:]), reads=["ident"], writes=["identb"])
        cnts = {"it": 0, "ig": 0}

        def NN(j):
            return 2048 + 128 * (j + 1)

        def phaseA(j):
            N = NN(j); i = j % 2
            rows = slice(j * 128, (j + 1) * 128)
            qTk = ("qT", i)
            P.dma("sp", _dma(qt[:], qproj[rows, :]), "dqt", writes=["dqt"])
            P.dma("sp", _dma(csq[i][:], cst["cs_q"][rows, :, :]), ("csq", i), writes=[("csq", i)])
            P.dma("sp", _dma(csqi[i][:], cst["cs_qi"][rows, :, :]), ("csqi", i), writes=[("csqi", i)])
            _rope(P, qt[:, 0:2048], qr[:], csq[i], 16, 128, t1, t2, ["dqt", ("csq", i)], "dqr", "dq")
            _transpose_blocks(C, qr, 16, qT[i], ["dqr"], qTk, 0)
            _rope(P, qt[:, 2048:3072], qir[:], csqi[i], 16, 64, t1, t2, ["dqt", ("csqi", i)], "dqir", "dq")
            _transpose_blocks(C, qir, 8, qiT, ["dqir"], "qiT", 0)
            P.op("dve", _ts(sgn[:], qt[:, 3072:3088], 0.0, 2.0, ALU.is_ge, ALU.mult), reads=["dqt"], writes=["sgn"])
            P.op("dve", _ts(sgn[:], sgn[:], -1.0, None, ALU.add), reads=["sgn"], writes=["sgn"])
            P.op("dve", _tt(wab[:], qt[:, 3072:3088], sgn[:], ALU.mult), reads=["dqt", "sgn"], writes=["wab"])
            P.op("dve", _ts(wab[:], wab[:], 0.03125), reads=["wab"], writes=["wab"])
            P.op("dve", _ts(acc[:, 0:N], iota[:, 0:N], qpos[:, j:j + 1], NEG, ALU.is_gt, ALU.mult), reads=["iota", "qpos"], writes=["acc"])
            for h in range(16):
                p0 = (h % 2) * 64
                for g0 in range(0, N, 1024):
                    w = min(1024, N - g0)
                    ig = cnts["ig"]
                    base = (ig % 4) * 2
                    nb_ = (w + 511) // 512
                    bkeys = [f"pb{base + c}" for c in range(nb_)]
                    for c4 in range(nb_):
                        ww = min(512, w - c4 * 512)
                        P.op("pe", _mm(C.pb[base + c4][:, 0:ww], qiT[p0:p0 + 64, h // 2, :], kiT2[p0:p0 + 64, 0, g0 + c4 * 512:g0 + c4 * 512 + ww], True, True),
                             reads=["qiT", "kiT"], writes=[bkeys[c4]])
                    tm = tmp[ig % 2]; tk = ("dtmp", ig % 2)
                    P.op("act", _act(tm[:, 0:w], C.ps_all[:, base * 512:base * 512 + w], AF.Relu, scale=wab[:, h:h + 1]), reads=bkeys + ["wab"], writes=[tk] + bkeys)
                    P.op("dve", _stt(acc[:, g0:g0 + w], tm[:, 0:w], sgn[:, h:h + 1], acc[:, g0:g0 + w], ALU.mult, ALU.add), reads=[tk, "sgn", "acc"], writes=["acc"])
                    cnts["ig"] += 1

        def phaseB(j):
            N = NN(j); NB = N // 128; i = j % 2
            lo, hi, wd, mid, cnt, ge = (sm[n] for n in ["lo", "hi", "w", "mid", "cnt", "ge"])
            P.op("dve", _red(hi[:], acc[:, 0:N], ALU.max), reads=["acc"], writes=["hi"])
            P.op("dve", _ts(scr[:, 0:N], iota[:, 0:N], qpos[:, j:j + 1], -2.0 * NEG, ALU.is_gt, ALU.mult), reads=["iota", "qpos"], writes=["scr"])
            P.op("dve", _tt(scr[:, 0:N], scr[:, 0:N], acc[:, 0:N], ALU.add), reads=["scr", "acc"], writes=["scr"])
            P.op("dve", _red(lo[:], scr[:, 0:N], ALU.min), reads=["scr"], writes=["lo"])
            P.op("dve", _tt(wd[:], hi[:], lo[:], ALU.subtract), reads=["hi", "lo"], writes=["w"])
            P.op("dve", _ts(hw[:], pw2[:], wd[:, 0:1]), reads=["w", "pw2"], writes=["hw"])
            for k in range(NIT):
                P.op("dve", _tt(mid[:], lo[:], hw[:, k:k + 1], ALU.add), reads=["lo", "hw"], writes=["mid"])
                P.op("dve", _ts(scr[:, 0:N], acc[:, 0:N], mid[:, 0:1], 0.0, ALU.is_ge, ALU.add, accum=cnt[:, 0:1]), reads=["acc", "mid"], writes=["scr", "cnt"])
                P.op("dve", _ts(ge[:], cnt[:], 255.5, hw[:, k:k + 1], ALU.is_ge, ALU.mult), reads=["cnt", "hw"], writes=["ge"])
                P.op("dve", _tt(lo[:], lo[:], ge[:], ALU.add), reads=["lo", "ge"], writes=["lo"])
            P.op("dve", _ts(scr[:, 0:N], acc[:, 0:N], lo[:, 0:1], -30000.0, ALU.is_lt, ALU.mult), reads=["acc", "lo"], writes=["scr"])
            _transpose_blocks(C, scr, NB, selT[i], ["scr"], ("selT", i), 0)

        def phaseCmain(j):
            N = NN(j); NB = N // 128; i = j % 2
            qT2 = qT[i][:].rearrange("p h q -> p (h q)")
            qTk, sTk = ("qT", i), ("selT", i)
            for kv in range(4):
                for kb in range(0, NB, 2):
                    nk = min(2, NB - kb)
                    it = cnts["it"]
                    base = (it % 2) * 2
                    Lks = [f"pb{base + b}" for b in range(nk)]
                    pp = pt[it % 2]; ppk = ("dp", it % 2)
                    for b in range(nk):
                        P.op("pe", _mm(C.pb[base + b][:, :], kT[:, kv, (kb + b) * 128:(kb + b + 1) * 128], qT2[:, kv * 512:(kv + 1) * 512], True, False), reads=["kT", qTk], writes=[Lks[b]])
                        for g in range(4):
                            P.op("pe", _mm(C.pb[base + b][:, g * 128:(g + 1) * 128], identb[:], selT[i][:, kb + b, :], False, g == 3), reads=["identb", sTk], writes=[Lks[b]])
                    P.op("act", _act(pp[:, 0:nk * 512], C.ps_all[:, base * 512:(base + nk) * 512], AF.Exp, scale=SC), reads=Lks, writes=[ppk] + Lks)
                    for b in range(nk):
                        for g in range(4):
                            P.op("pe", _mm(C.pb[4 + g][:, 0:129], pp[:, b * 512 + g * 128:b * 512 + (g + 1) * 128], vb[:, kb + b, kv, :], kb + b == 0, kb + b == NB - 1), reads=[ppk, "vb"], writes=[f"pb{4 + g}"])
                    cnts["it"] += 1
                for g in range(4):
                    P.op("act", _acp(obuf[:, kv * 4 + g, :], C.pb[4 + g][:, 0:129]), reads=[f"pb{4 + g}"], writes=["obuf", f"pb{4 + g}"])

        def phaseCfin(j):
            rows = slice(j * 128, (j + 1) * 128)
            P.op("dve", _rcp(rec[:], obuf[:, :, 128]), reads=["obuf"], writes=["rec"])
            P.op("dve", _tt(obuf[:, :, 0:128], obuf[:, :, 0:128], rec[:, :, None].to_broadcast([128, 16, 128]), ALU.mult), reads=["obuf", "rec"], writes=["obuf"])
            P.dma("sp", _dma(attn_out[rows, :].rearrange("p (h d) -> p h d", h=16), obuf[:, :, 0:128]), "dobuf", reads=["obuf"], writes=[("aout", j)])

        phaseA(0)
        phaseB(0)
        for j in range(16):
            if j + 1 < 16:
                phaseA(j + 1)
            phaseCmain(j)
            if j + 1 < 16:
                phaseB(j + 1)
            phaseCfin(j)
    P.barrier()


def build_layer1(debug=False):
    nc = bass.Bass("TRN2", target_bir_lowering=False)
    dt = lambda name, shape, kind="ExternalInput": nc.dram_tensor(name, shape, F32, kind=kind).ap()
    x = dt("x", [T, D]); xf = dt("xf", [2 * T, D]); w_in = dt("w_in", [D, 4176]); w_out = dt("w_out", [D, D])
    g1 = dt("mix_g", [128, D]); b1 = dt("mix_b", [128, D]); g2 = dt("ffn_g", [128, D]); b2 = dt("ffn_b", [128, D])
    w_gu = dt("w_gu", [16, D, 1024]); w_down = dt("w_down", [16, 512, D])
    rw = dt("rw", [D, 16]); rbB = dt("rbB", [128, 16]); ident = dt("ident", [128, 128])
    cst = {"cs_k": dt("cs_k", [2 * T, 2, 64]), "cs_ki": dt("cs_ki", [2 * T, 2, 32]), "cs_q": dt("cs_q", [T, 2, 64]), "cs_qi": dt("cs_qi", [T, 2, 32]),
           "iota": dt("iota", [128, 4096]), "qpos": dt("qpos", [128, 16])}
    out = dt("out", [T, D], "ExternalOutput")
    dk = "ExternalOutput" if debug else "Internal"
    qproj = dt("qproj", [T, 3088], "Internal"); kvproj = dt("kvproj", [2 * T, 1088], "Internal")
    mix = dt("mix", [T, D], dk); m = dt("m", [T, D], dk)
    ha = dt("ha", [T, D], dk); f = dt("f", [T, D], "Internal")
    with ExitStack() as es:
        C = _mk_ctx(nc, es)
        _load_consts(C, ident)
        linear(C, x, T, w_in, [(0, 2048), (3072, 4096), (4160, 4176)], qproj, "q1")
        linear(C, xf, 2 * T, w_in, [(2048, 3072), (4096, 4160)], kvproj, "kv1")
        mixer_dsa(C, qproj, kvproj, mix, cst)
        _common_tail(C, x, mix, w_out, g1, b1, g2, b2, w_gu, w_down, rw, rbB, m, ha, f, out)
        C.P.emit()
    return nc


def layer1_inputs(inp, h, core):
    b, half = core // 2, core % 2
    pos_own = np.arange(half * T, (half + 1) * T)
    qpos = (half * T + np.arange(16)[None, :] * 128 + np.arange(128)[:, None]).astype(np.float32)
    return {
        "x": np.ascontiguousarray(h[b, half * T:(half + 1) * T]), "xf": np.ascontiguousarray(h[b]),
        "w_in": np.asarray(inp["odd_w_in"][0], np.float32), "w_out": np.asarray(inp["odd_w_out"][0], np.float32),
        "mix_g": _bcast_rows(inp["mix_ln_g"][1]), "mix_b": _bcast_rows(inp["mix_ln_b"][1]),
        "ffn_g": _bcast_rows(inp["ffn_ln_g"][1]), "ffn_b": _bcast_rows(inp["ffn_ln_b"][1]),
        "w_gu": np.asarray(inp["moe_w_gu"][1], np.float32), "w_down": np.asarray(inp["moe_w_down"][1], np.float32),
        "rw": np.asarray(inp["router_w"], np.float32), "rbB": _bcast_rows(inp["router_b"]), "ident": np.eye(128, dtype=np.float32),
        "cs_k": _rope_tab(np.arange(2 * T), 64), "cs_ki": _rope_tab(np.arange(2 * T), 32),
        "cs_q": _rope_tab(pos_own, 64), "cs_qi": _rope_tab(pos_own, 32),
        "iota": np.ascontiguousarray(np.broadcast_to(np.arange(4096, dtype=np.float32)[None, :], (128, 4096))),
        "qpos": np.ascontiguousarray(qpos),
    }


def build_fused(profile=False):
    nc = bass.Bass("TRN2", target_bir_lowering=False)
    dt = lambda name, shape, kind="ExternalInput": nc.dram_tensor(name, shape, F32, kind=kind).ap()
    xA = dt("x", [T, D]); xB = dt("xp", [T, D]); zhalo = dt("zhalo", [128, 2048])
    rw = dt("rw", [D, 16]); rbB = dt("rbB", [128, 16]); ident = dt("ident", [128, 128])
    L = []
    for l in range(2):
        L.append({"w_in": dt(f"w_in{l}", [D, 6144 if l == 0 else 4176]), "w_out": dt(f"w_out{l}", [D, D]),
                  "g1": dt(f"mix_g{l}", [128, D]), "b1": dt(f"mix_b{l}", [128, D]), "g2": dt(f"ffn_g{l}", [128, D]), "b2": dt(f"ffn_b{l}", [128, D]),
                  "w_gu": dt(f"w_gu{l}", [16, D, 1024]), "w_down": dt(f"w_down{l}", [16, 512, D])})
    cst = {"cs_own": dt("cs_own", [T, 2, 128]), "cs_pre": dt("cs_pre", [T, 2, 128]), "decT": dt("decT", [128, 512]),
           "xi": dt("xi", [128, 4]), "zeta": dt("zeta", [128, 4]), "gnB": dt("gnB", [128, 1024]),
           "conv_w": dt("conv_w", [128, 8, 31]), "conv_v": dt("conv_v", [128, 3, 8]),
           "cs_k": dt("cs_k", [2 * T, 2, 64]), "cs_ki": dt("cs_ki", [2 * T, 2, 32]), "cs_q": dt("cs_q", [T, 2, 64]), "cs_qi": dt("cs_qi", [T, 2, 32]),
           "iota": dt("iota", [128, 4096]), "qpos": dt("qpos", [128, 16])}
    out = dt("out", [T, D], "ExternalOutput")
    projB = dt("projB", [T, 6144], "Internal"); projA = dt("projA", [T, 6144], "Internal")
    sstate = dt("sstate", [128, 2048], "Internal"); h0f = dt("h0f", [2 * T, D], "Internal")
    qproj = dt("qproj", [T, 3088], "Internal"); kvproj = dt("kvproj", [2 * T, 1088], "Internal")
    mix = dt("mix", [T, D], "Internal"); m = dt("m", [T, D], "Internal"); ha = dt("ha", [T, D], "Internal"); f = dt("f", [T, D], "Internal")
    with ExitStack() as es:
        C = _mk_ctx(nc, es)
        C.P.profile = profile
        _load_consts(C, ident)
        l0 = L[0]
        for (xx, proj, cs_key, s_in, s_out, halo, dst) in [(xB, projB, "cs_pre", None, sstate, zhalo, h0f[0:T, :]),
                                                            (xA, projA, "cs_own", sstate, None, projB[T - 128:T, 4096:6144], h0f[T:2 * T, :])]:
            C.pfx = "B_" if s_in is None else "A_"
            linear(C, xx, T, l0["w_in"], [(0, 6144)], proj, "in")
            mixer_ret(C, proj, None, mix, cst, cs_key=cs_key, s_in=s_in, s_out=s_out)
            mixer_conv(C, proj, halo, mix, cst)
            _common_tail(C, xx, mix, l0["w_out"], l0["g1"], l0["b1"], l0["g2"], l0["b2"], l0["w_gu"], l0["w_down"], rw, rbB, m, ha, f, dst)
        l1 = L[1]
        C.pfx = "L1_"
        x1 = h0f[T:2 * T, :]
        linear(C, x1, T, l1["w_in"], [(0, 2048), (3072, 4096), (4160, 4176)], qproj, "q1")
        linear(C, h0f, 2 * T, l1["w_in"], [(2048, 3072), (4096, 4160)], kvproj, "kv1")
        mixer_dsa(C, qproj, kvproj, mix, cst)
        _common_tail(C, x1, mix, l1["w_out"], l1["g1"], l1["b1"], l1["g2"], l1["b2"], l1["w_gu"], l1["w_down"], rw, rbB, m, ha, f, out)
        C.P.emit()
    return nc


def fused_inputs(inp, core):
    b, half = core // 2, core % 2
    x = np.asarray(inp["x"], np.float32)
    a = layer0_inputs(inp, x, core)
    r = {k: a[k] for k in ["x", "xp", "rw", "rbB", "ident", "cs_own", "cs_pre", "decT", "xi", "zeta", "gnB", "conv_w", "conv_v"]}
    r["zhalo"] = np.zeros((128, 2048), np.float32)
    for l, pre in enumerate(["even", "odd"]):
        r[f"w_in{l}"] = np.asarray(inp[pre + "_w_in"][0], np.float32); r[f"w_out{l}"] = np.asarray(inp[pre + "_w_out"][0], np.float32)
        r[f"mix_g{l}"] = _bcast_rows(inp["mix_ln_g"][l]); r[f"mix_b{l}"] = _bcast_rows(inp["mix_ln_b"][l])
        r[f"ffn_g{l}"] = _bcast_rows(inp["ffn_ln_g"][l]); r[f"ffn_b{l}"] = _bcast_rows(inp["ffn_ln_b"][l])
        r[f"w_gu{l}"] = np.asarray(inp["moe_w_gu"][l], np.float32); r[f"w_down{l}"] = np.asarray(inp["moe_w_down"][l], np.float32)
    posA = np.arange(half * T, (half + 1) * T)
    posB = np.arange(0, T)
    kp = np.concatenate([posB if half == 1 else np.full(T, 1.0e9), posA]).astype(np.float32)
    ropepos = np.concatenate([posB, posA])
    r["cs_k"] = _rope_tab(ropepos, 64); r["cs_ki"] = _rope_tab(ropepos, 32)
    r["cs_q"] = _rope_tab(posA, 64); r["cs_qi"] = _rope_tab(posA, 32)
    r["iota"] = np.ascontiguousarray(np.broadcast_to(kp[None, :], (128, 4096)))
    r["qpos"] = np.ascontiguousarray((half * T + np.arange(16)[None, :] * 128 + np.arange(128)[:, None]).astype(np.float32))
    return r


_NC_CACHE = {}


def kernel(**inputs):
    inp = {k: np.asarray(v) for k, v in inputs.items()}
    B = inp["x"].shape[0]
    if "fused" not in _NC_CACHE:
        _NC_CACHE["fused"] = build_fused()
    nc = _NC_CACHE["fused"]
    in_maps = [fused_inputs(inp, core) for core in range(8)]
    res = run_bass_kernel_spmd(nc, in_maps, core_ids=list(range(8)))
    h = np.stack([np.concatenate([res.results[2 * b]["out"], res.results[2 * b + 1]["out"]], axis=0) for b in range(B)], axis=0)
    return np.ascontiguousarray(h.astype(np.float32))
```

```python
import math
import numpy as np
from contextlib import ExitStack
import concourse.bass as bass
import concourse.mybir as mybir
from concourse.bass_utils import run_bass_kernel_spmd

F32 = mybir.dt.float32
BF16 = mybir.dt.bfloat16
AF = mybir.ActivationFunctionType
ALU = mybir.AluOpType
AX = mybir.AxisListType

ENGS = ("pe", "act", "dve", "pool", "sp")
D = 2048
T = 2048
NT = 16
ALPHA = 4.0 ** 0.25
EPS = 1e-5
NEG = -1.0e30


class Op:
    __slots__ = ("eng", "fn", "deps", "dma_slot", "dma_val", "signal", "sigval", "idx", "stage")


class Prog:
    def __init__(self, nc):
        self.nc = nc
        self.ops = []
        self.last_w = {}
        self.readers = {}
        self.slot_cnt = {}
        self.slot_last = {}
        self.last_on = {}
        self.stage = "init"
        self.profile = False

    def _rec(self, eng, fn, reads, writes, dma_slot=None, extra_deps=()):
        op = Op()
        op.eng = eng; op.fn = fn; op.dma_slot = dma_slot; op.signal = False; op.sigval = 0
        op.idx = len(self.ops)
        op.stage = self.stage
        deps = set(extra_deps)
        for k in reads:
            w = self.last_w.get(k)
            if w is not None:
                deps.add(w)
        for k in writes:
            w = self.last_w.get(k)
            if w is not None:
                deps.add(w)
            rd = self.readers.get(k)
            if rd:
                deps.update(rd[0].values())
                deps.update(rd[1])
        op.deps = deps
        if dma_slot is not None:
            c = self.slot_cnt.get(dma_slot, 0) + 1
            self.slot_cnt[dma_slot] = c
            op.dma_val = 16 * c
            self.slot_last[dma_slot] = op.idx
        else:
            op.dma_val = 0
            self.last_on[eng] = op.idx
        for k in reads:
            rd = self.readers.setdefault(k, ({}, []))
            if dma_slot is not None:
                rd[1].append(op.idx)
            else:
                rd[0][eng] = op.idx
        for k in writes:
            self.last_w[k] = op.idx
            self.readers[k] = ({}, [])
        self.ops.append(op)
        return op

    def op(self, eng, fn, reads=(), writes=()):
        return self._rec(eng, fn, reads, writes)

    def dma(self, eng, fn, slot, reads=(), writes=()):
        return self._rec(eng, fn, reads, writes, dma_slot=slot)

    def barrier(self):
        deps = set(self.last_on.values()) | set(self.slot_last.values())
        for e in ENGS:
            self._rec(e, None, (), (), extra_deps=deps)
        self.last_w = {}
        self.readers = {}

    def emit(self):
        nc = self.nc
        self.barrier()
        ops = self.ops
        for op in ops:
            for d in op.deps:
                p = ops[d]
                if p.dma_slot is None and p.fn is not None and (p.eng != op.eng or p.eng != "pe"):
                    p.signal = True
        cnt = {e: 0 for e in ENGS}
        for op in ops:
            if op.signal:
                cnt[op.eng] += 1
                op.sigval = cnt[op.eng]
        slots = list(self.slot_cnt.keys())
        EPOCH = 30000
        with ExitStack() as es:
            eng_sems = {}
            for e in ENGS:
                n_ep = cnt[e] // EPOCH + 1
                eng_sems[e] = [es.enter_context(nc.semaphore(f"s_{e}_{i}")) for i in range(n_ep)]
            slot_sem = {s: es.enter_context(nc.semaphore(f"d_{i}")) for i, s in enumerate(slots)}
            block = es.enter_context(nc.Block())

            def run_engine(ename, eng):
                waited = {}
                cur = None
                for op in ops:
                    if op.eng != ename:
                        continue
                    if self.profile and op.stage != cur:
                        if cur is not None:
                            nc.pop_named_scope(cur)
                        cur = op.stage
                        nc.push_named_scope(cur)
                    for d in sorted(op.deps):
                        p = ops[d]
                        if p.dma_slot is not None:
                            sem = slot_sem[p.dma_slot]; val = p.dma_val
                        else:
                            if p.eng == ename and ename == "pe":
                                continue
                            if p.fn is None:
                                continue
                            ep = (p.sigval - 1) // EPOCH
                            sem = eng_sems[p.eng][ep]; val = p.sigval - ep * EPOCH
                        if waited.get(sem.num, 0) >= val:
                            continue
                        waited[sem.num] = val
                        eng.wait_ge(sem, val)
                    if op.fn is None:
                        continue
                    ins = op.fn(eng)
                    if op.dma_slot is not None:
                        ins.then_inc(slot_sem[op.dma_slot], 16)
                    elif op.signal:
                        ep = (op.sigval - 1) // EPOCH
                        ins.then_inc(eng_sems[ename][ep], 1)
                if self.profile and cur is not None:
                    nc.pop_named_scope(cur)

            block.sync(lambda e: run_engine("sp", e))
            block.tensor(lambda e: run_engine("pe", e))
            block.scalar(lambda e: run_engine("act", e))
            block.vector(lambda e: run_engine("dve", e))
            block.gpsimd(lambda e: run_engine("pool", e))


class Ctx:
    pass


def _mk_ctx(nc, es):
    C = Ctx()
    C.nc = nc
    C.P = Prog(nc)
    C.es = es
    C.ps_all = es.enter_context(nc.psum_tensor("ps_all", [128, 4096], F32))
    C.pb = [C.ps_all[:, i * 512:(i + 1) * 512] for i in range(8)]
    C.uid = 0
    C.pfx = ""
    return C


def _sb(C, st, name, shape, dt=F32):
    C.uid += 1
    return st.enter_context(C.nc.sbuf_tensor(f"{name}_{C.uid}", shape, dt))


def _load_consts(C, ident_d):
    nc, P = C.nc, C.P
    C.ident = C.es.enter_context(nc.sbuf_tensor("ident_sb", [128, 128], F32))
    C.ones = C.es.enter_context(nc.sbuf_tensor("ones_sb", [128, 128], F32))
    P.dma("sp", lambda e: e.dma_start(out=C.ident[:], in_=ident_d[:, :]), "ident", writes=["ident"])
    P.op("dve", lambda e: e.memset(C.ones[:], 1.0), writes=["ones"])


def build_xT(C, st, src, n_tok, tag, f32_copy=None):
    nc, P = C.nc, C.P
    nt = n_tok // 128
    xT = _sb(C, st, "xT" + tag, [128, 16, n_tok], BF16)
    stg = [_sb(C, st, "xs" + tag, [128, D]) for _ in range(2)]
    for t in range(nt):
        s = stg[t % 2]
        sk = ("xs", tag, t % 2)
        P.dma("sp", (lambda s=s, t=t: lambda e: e.dma_start(out=s[:], in_=src[t * 128:(t + 1) * 128, :]))(), sk, writes=[sk])
        for q in range(4):
            bank = C.pb[q % 2]
            bk = f"pb{q % 2}"
            for i in range(4):
                c = q * 4 + i
                P.op("pe", (lambda s=s, c=c, bank=bank, i=i: lambda e: e.transpose(bank[:, i * 128:(i + 1) * 128], s[:, c * 128:(c + 1) * 128], C.ident[:]))(),
                     reads=[sk, "ident"], writes=[bk])
            eng = "act" if q % 2 == 0 else "dve"
            dst = xT[:, q * 4:(q + 1) * 4, t * 128:(t + 1) * 128]
            srcp = bank[:].rearrange("p (i k) -> p i k", i=4)
            if eng == "act":
                P.op("act", (lambda dst=dst, srcp=srcp: lambda e: e.copy(out=dst, in_=srcp))(), reads=[bk], writes=[("xT", tag)])
            else:
                P.op("dve", (lambda dst=dst, srcp=srcp: lambda e: e.tensor_copy(out=dst, in_=srcp))(), reads=[bk], writes=[("xT", tag)])
            if f32_copy is not None:
                dst2 = f32_copy[:, q * 4:(q + 1) * 4, t * 128:(t + 1) * 128]
                P.op("dve", (lambda dst2=dst2, srcp=srcp: lambda e: e.tensor_copy(out=dst2, in_=srcp))(), reads=[bk], writes=[("xT32", tag)])
    return xT


def linear(C, src, n_tok, w, col_ranges, dst, tag):
    nc, P = C.nc, C.P
    P.stage = C.pfx + "lin_" + tag
    nt = n_tok // 128
    with ExitStack() as st:
        xT = build_xT(C, st, src, n_tok, tag)
        wb = [_sb(C, st, "wb" + tag, [128, 16, 512], BF16) for _ in range(2)]
        ost = [_sb(C, st, "os" + tag, [128, 512]) for _ in range(4)]
        groups = []
        o = 0
        for (c0, c1) in col_ranges:
            c = c0
            while c < c1:
                n = min(512, c1 - c)
                groups.append((c, n, o))
                c += n; o += n
        wv = w.rearrange("(c p) n -> p c n", p=128)
        k = 0
        for gi, (c0, n, o0) in enumerate(groups):
            wt = wb[gi % 2]
            wk = ("wb", tag, gi % 2)
            P.dma("pool", (lambda wt=wt, c0=c0, n=n: lambda e: e.dma_start(out=wt[:, :, 0:n], in_=wv[:, :, c0:c0 + n]))(), wk, writes=[wk])
            for t in range(nt):
                bank = C.pb[2 + k % 4]; bk = f"pb{2 + k % 4}"
                for c in range(16):
                    P.op("pe", (lambda bank=bank, c=c, t=t, wt=wt, n=n: lambda e: e.matmul(bank[:, 0:n], lhsT=xT[:, c, t * 128:(t + 1) * 128], rhs=wt[:, c, 0:n], start=(c == 0), stop=(c == 15)))(),
                         reads=[("xT", tag), wk], writes=[bk])
                os_ = ost[k % 4]; ok = ("os", tag, k % 4)
                if k % 2 == 0:
                    P.op("act", (lambda os_=os_, bank=bank, n=n: lambda e: e.copy(out=os_[:, 0:n], in_=bank[:, 0:n]))(), reads=[bk], writes=[ok])
                else:
                    P.op("dve", (lambda os_=os_, bank=bank, n=n: lambda e: e.tensor_copy(out=os_[:, 0:n], in_=bank[:, 0:n]))(), reads=[bk], writes=[ok])
                P.dma("sp", (lambda os_=os_, t=t, o0=o0, n=n: lambda e: e.dma_start(out=dst[t * 128:(t + 1) * 128, o0:o0 + n], in_=os_[:, 0:n]))(), ok, reads=[ok], writes=[("dst", tag, t, gi)])
                k += 1
    P.barrier()


def resid_ln(C, x, m, gB_d, bB_d, out, n_tok, tag):
    nc, P = C.nc, C.P
    P.stage = C.pfx + "ln_" + tag
    nt = n_tok // 128
    with ExitStack() as st:
        gB = _sb(C, st, "gB", [128, D]); bB = _sb(C, st, "bB", [128, D])
        P.dma("sp", _dma(gB[:], gB_d[:, :]), ("gB", tag), writes=[("gB", tag)])
        P.dma("sp", _dma(bB[:], bB_d[:, :]), ("bB", tag), writes=[("bB", tag)])
        xs = [_sb(C, st, "rx", [128, D]) for _ in range(2)]
        ms = [_sb(C, st, "rm", [128, D]) for _ in range(2)]
        rs = [_sb(C, st, "rr", [128, D]) for _ in range(2)]
        mv = [_sb(C, st, "rmv", [128, 2]) for _ in range(2)]
        rstd = [_sb(C, st, "rrs", [128, 1]) for _ in range(2)]
        nb = [_sb(C, st, "rnb", [128, 1]) for _ in range(2)]

        def front(t):
            i = t % 2
            xk, mk, rk, vk = ("rx", tag, i), ("rm", tag, i), ("rr", tag, i), ("rmv", i)
            P.dma("sp", _dma(xs[i][:], x[t * 128:(t + 1) * 128, :]), xk, writes=[xk])
            P.dma("sp", _dma(ms[i][:], m[t * 128:(t + 1) * 128, :]), mk, writes=[mk])
            P.op("dve", _stt(rs[i][:], xs[i][:], ALPHA, ms[i][:], ALU.mult, ALU.add), reads=[xk, mk], writes=[rk])
            P.op("act", _act(xs[i][:], rs[i][:], AF.Identity, accum=mv[i][:, 0:1]), reads=[rk], writes=[vk, xk])
            P.op("act", _act(xs[i][:], rs[i][:], AF.Square, accum=mv[i][:, 1:2]), reads=[rk], writes=[vk, xk])

        def back(t):
            i = t % 2
            xk, mk, rk, vk, sk, nk = ("rx", tag, i), ("rm", tag, i), ("rr", tag, i), ("rmv", i), ("rrs", i), ("rnb", i)
            P.op("dve", _ts(mv[i][:], mv[i][:], 1.0 / D), reads=[vk], writes=[vk])
            P.op("dve", _tt(nb[i][:], mv[i][:, 0:1], mv[i][:, 0:1], ALU.mult), reads=[vk], writes=[nk])
            P.op("dve", _tt(rstd[i][:], mv[i][:, 1:2], nb[i][:], ALU.subtract), reads=[vk, nk], writes=[sk])
            P.op("dve", _ts(rstd[i][:], rstd[i][:], EPS, None, ALU.add), reads=[sk], writes=[sk])
            P.op("act", _sqrt(rstd[i][:], rstd[i][:]), reads=[sk], writes=[sk])
            P.op("dve", _rcp(rstd[i][:], rstd[i][:]), reads=[sk], writes=[sk])
            P.op("dve", _stt(nb[i][:], mv[i][:, 0:1], -1.0, rstd[i][:], ALU.mult, ALU.mult), reads=[vk, sk], writes=[nk])
            P.op("act", _act(rs[i][:], rs[i][:], AF.Identity, bias=nb[i][:, 0:1], scale=rstd[i][:, 0:1]), reads=[rk, sk, nk], writes=[rk])
            P.op("dve", _tt(ms[i][:], rs[i][:], gB[:], ALU.mult), reads=[rk, ("gB", tag)], writes=[mk])
            P.op("pool", _tt(ms[i][:], ms[i][:], bB[:], ALU.add), reads=[mk, ("bB", tag)], writes=[mk])
            P.dma("sp", _dma(out[t * 128:(t + 1) * 128, :], ms[i][:]), mk, reads=[mk], writes=[("rout", tag, t)])

        for t in range(nt + 1):
            if t < nt:
                front(t)
            if t >= 1:
                back(t - 1)
    P.barrier()


class LNEpi:
    def __init__(self, C, st, x, gB_d, bB_d, out, tag):
        self.C, self.x, self.out, self.tag = C, x, out, tag
        P = C.P
        self.gB = _sb(C, st, "egB", [128, D]); self.bB = _sb(C, st, "ebB", [128, D])
        P.dma("sp", _dma(self.gB[:], gB_d[:, :]), ("egB", tag), writes=[("egB", tag)])
        P.dma("sp", _dma(self.bB[:], bB_d[:, :]), ("ebB", tag), writes=[("ebB", tag)])
        self.xs = [_sb(C, st, "ex", [128, D]) for _ in range(2)]
        self.rs = [_sb(C, st, "er", [128, D]) for _ in range(2)]
        self.mv = [_sb(C, st, "emv", [128, 2]) for _ in range(2)]
        self.rstd = [_sb(C, st, "ers", [128, 1]) for _ in range(2)]
        self.nb = [_sb(C, st, "enb", [128, 1]) for _ in range(2)]

    def front(self, t, row0, msrc, mkeys):
        P, tag, i = self.C.P, self.tag, t % 2
        xk, rk, vk = ("ex", tag, i), ("er", tag, i), ("emv", tag, i)
        P.dma("sp", _dma(self.xs[i][:], self.x[row0:row0 + 128, :]), xk, writes=[xk])
        P.op("dve", _stt(self.rs[i][:], self.xs[i][:], ALPHA, msrc, ALU.mult, ALU.add), reads=[xk] + list(mkeys), writes=[rk] + list(mkeys))
        P.op("act", _act(self.xs[i][:], self.rs[i][:], AF.Identity, accum=self.mv[i][:, 0:1]), reads=[rk], writes=[vk, xk])
        P.op("act", _act(self.xs[i][:], self.rs[i][:], AF.Square, accum=self.mv[i][:, 1:2]), reads=[rk], writes=[vk, xk])

    def back(self, t, row0):
        P, tag, i = self.C.P, self.tag, t % 2
        mv, rstd, nb, rs, xs = self.mv[i], self.rstd[i], self.nb[i], self.rs[i], self.xs[i]
        xk, rk, vk, sk, nk = ("ex", tag, i), ("er", tag, i), ("emv", tag, i), ("ers", tag, i), ("enb", tag, i)
        P.op("dve", _ts(mv[:], mv[:], 1.0 / D), reads=[vk], writes=[vk])
        P.op("dve", _tt(nb[:], mv[:, 0:1], mv[:, 0:1], ALU.mult), reads=[vk], writes=[nk])
        P.op("dve", _tt(rstd[:], mv[:, 1:2], nb[:], ALU.subtract), reads=[vk, nk], writes=[sk])
        P.op("dve", _ts(rstd[:], rstd[:], EPS, None, ALU.add), reads=[sk], writes=[sk])
        P.op("act", _sqrt(rstd[:], rstd[:]), reads=[sk], writes=[sk])
        P.op("dve", _rcp(rstd[:], rstd[:]), reads=[sk], writes=[sk])
        P.op("dve", _stt(nb[:], mv[:, 0:1], -1.0, rstd[:], ALU.mult, ALU.mult), reads=[vk, sk], writes=[nk])
        P.op("act", _act(rs[:], rs[:], AF.Identity, bias=nb[:, 0:1], scale=rstd[:, 0:1]), reads=[rk, sk, nk], writes=[rk])
        P.op("dve", _tt(xs[:], rs[:], self.gB[:], ALU.mult), reads=[rk, ("egB", tag)], writes=[xk])
        P.op("pool", _tt(xs[:], xs[:], self.bB[:], ALU.add), reads=[xk, ("ebB", tag)], writes=[xk])
        P.dma("sp", _dma(self.out[row0:row0 + 128, :], xs[:]), xk, reads=[xk], writes=[("eout", tag, t)])


def linear_ln(C, src, w, x, gB_d, bB_d, out, tag):
    nc, P = C.nc, C.P
    P.stage = C.pfx + "linln_" + tag
    nt = T // 128
    with ExitStack() as st:
        xT = _sb(C, st, "lxT", [128, 16, T], BF16)
        wt = _sb(C, st, "lw", [128, 16, D], BF16)
        wv = w.rearrange("(c p) n -> p c n", p=128)
        for g in range(4):
            P.dma("pool", _dma(wt[:, :, g * 512:(g + 1) * 512], wv[:, :, g * 512:(g + 1) * 512]), ("lw", g), writes=[("lw", g)])
        with ExitStack() as s1:
            stg = [_sb(C, s1, "lxs", [128, D]) for _ in range(2)]
            for t in range(nt):
                sk = ("lxs", t % 2)
                P.dma("sp", _dma(stg[t % 2][:], src[t * 128:(t + 1) * 128, :]), sk, writes=[sk])
                _transpose_blocks(C, stg[t % 2], 16, xT[:, :, t * 128:(t + 1) * 128], [sk], "lxT", 0)
        P.barrier()
        ep = LNEpi(C, st, x, gB_d, bB_d, out, tag)
        for t in range(nt):
            base = (t % 2) * 4
            keys = [f"pb{base + g}" for g in range(4)]
            for g in range(4):
                for c in range(16):
                    P.op("pe", _mm(C.pb[base + g][:, :], xT[:, c, t * 128:(t + 1) * 128], wt[:, c, g * 512:(g + 1) * 512], c == 0, c == 15), reads=["lxT", ("lw", g)], writes=[keys[g]])
            ep.front(t, t * 128, C.ps_all[:, base * 512:base * 512 + 2048], keys)
            if t >= 1:
                ep.back(t - 1, (t - 1) * 128)
        ep.back(nt - 1, (nt - 1) * 128)
    P.barrier()


MOE_STOP = None
MOE_DBG = None


def moe(C, h, w_gu, w_down, rw_d, rbB_d, f_out, tag, ln=None):
    nc, P = C.nc, C.P
    HT = 1024
    for half_ in range(2 if MOE_STOP is None else 1):
        _moe_half(C, h, w_gu, w_down, rw_d, rbB_d, f_out, tag, half_, ln)


def _moe_half(C, h, w_gu, w_down, rw_d, rbB_d, f_out, tag, half, ln=None):
    nc, P = C.nc, C.P
    P.stage = C.pfx + "moe"
    HT = 1024
    if True:
        with ExitStack() as st:
            hsrc = h[half * HT:(half + 1) * HT, :]
            gateT = _sb(C, st, "gateT", [16, HT], BF16)
            sel16 = _sb(C, st, "sel16", [16, 16, 128], BF16)
            xT = _sb(C, st, "mxT", [128, 16, HT], BF16)
            yacc = _sb(C, st, "yacc", [128, 16, HT])
            dbg_sb = _sb(C, st, "dbg_sb", [128, 512])
            P.op("dve", lambda e: e.memset(dbg_sb[:], 0.0), writes=["dbg"])
            st1 = ExitStack()
            xT32 = _sb(C, st1, "xT32", [128, 16, 128])
            rw = _sb(C, st1, "rw", [128, 16, 16])
            rbB = _sb(C, st1, "rbB", [128, 16])
            gate = _sb(C, st1, "gate", [128, 8, 16])
            P.dma("sp", lambda e: e.dma_start(out=rw[:], in_=rw_d.rearrange("(c p) n -> p c n", p=128)), ("rw", tag, half), writes=["rw"])
            P.dma("sp", lambda e: e.dma_start(out=rbB[:], in_=rbB_d[:, :]), ("rbB", tag, half), writes=["rbB"])
            for ex_ in range(16):
                P.op("dve", (lambda ex_=ex_: lambda e: e.tensor_copy(out=sel16[:, ex_, :], in_=C.ident[0:16, ex_:ex_ + 1].to_broadcast([16, 128])))(), reads=["ident"], writes=["sel16"])
            stg = [_sb(C, st1, "mxs", [128, D]) for _ in range(2)]
            sm = {n: _sb(C, st1, "g" + n, [128, 8 * 16]) for n in ["aff", "sel", "msk", "t1", "t2", "w"]}
            sm4 = {n: _sb(C, st1, "g4" + n, [128, 8 * 4]) for n in ["m1", "m2", "gs", "gm"]}
            sm1 = {n: _sb(C, st1, "g1" + n, [128, 8]) for n in ["a", "b"]}
            for t in range(HT // 128):
                s = stg[t % 2]; sk = ("mxs", t % 2)
                P.dma("sp", (lambda s=s, t=t: lambda e: e.dma_start(out=s[:], in_=hsrc[t * 128:(t + 1) * 128, :]))(), sk, writes=[sk])
                for q in range(4):
                    bank = C.pb[q % 2]; bk = f"pb{q % 2}"
                    for i in range(4):
                        c = q * 4 + i
                        P.op("pe", (lambda s=s, c=c, bank=bank, i=i: lambda e: e.transpose(bank[:, i * 128:(i + 1) * 128], s[:, c * 128:(c + 1) * 128], C.ident[:]))(), reads=[sk, "ident"], writes=[bk])
                    srcp = bank[:].rearrange("p (i k) -> p i k", i=4)
                    P.op("act", (lambda q=q, t=t, srcp=srcp: lambda e: e.copy(out=xT[:, q * 4:(q + 1) * 4, t * 128:(t + 1) * 128], in_=srcp))(), reads=[bk], writes=["mxT", bk])
                    P.op("dve", (lambda q=q, srcp=srcp: lambda e: e.tensor_copy(out=xT32[:, q * 4:(q + 1) * 4, :], in_=srcp))(), reads=[bk], writes=["xT32", bk])
                lb = C.pb[2]
                for c in range(16):
                    P.op("pe", (lambda c=c: lambda e: e.matmul(lb[:, 0:16], lhsT=xT32[:, c, :], rhs=rw[:, c, :], start=(c == 0), stop=(c == 15)))(), reads=["xT32", "rw"], writes=["pb2"])
                P.op("act", _act(sm["aff"][:, t * 16:(t + 1) * 16], lb[:, 0:16], AF.Sigmoid), reads=["pb2"], writes=["gsm", "pb2"])
            aff, sel, msk, t1, t2, wv = sm["aff"], sm["sel"], sm["msk"], sm["t1"], sm["t2"], sm["w"]
            m1, m2, gs, gm = sm4["m1"], sm4["m2"], sm4["gs"], sm4["gm"]
            a1, b1 = sm1["a"], sm1["b"]
            G = ["gsm"]
            NG = 8
            v16 = lambda x: x[:].rearrange("p (t e) -> p t e", t=NG)
            v4 = lambda x: x[:].rearrange("p (n k) -> p n k", k=4)
            g4 = lambda x: x[:].rearrange("p (t g) -> p t g", t=NG)
            P.op("dve", _tt(v16(sel), v16(aff), rbB[:, None, :].to_broadcast([128, NG, 16]), ALU.add), reads=G + ["rbB"], writes=G)
            P.op("dve", _red(m1[:], v4(sel), ALU.max), reads=G, writes=G)
            P.op("dve", _tt(v4(t1), v4(sel), m1[:, :, None].to_broadcast([128, NG * 4, 4]), ALU.is_equal), reads=G, writes=G)
            P.op("dve", _stt(t1[:], t1[:], NEG, sel[:], ALU.mult, ALU.add), reads=G, writes=G)
            P.op("dve", _red(m2[:], v4(t1), ALU.max), reads=G, writes=G)
            P.op("dve", _tt(gs[:], m1[:], m2[:], ALU.add), reads=G, writes=G)
            P.op("dve", _red(a1[:], g4(gs), ALU.max), reads=G, writes=G)
            P.op("dve", _tt(g4(gm), g4(gs), a1[:, :, None].to_broadcast([128, NG, 4]), ALU.is_equal), reads=G, writes=G)
            P.op("dve", _cp(v4(msk), gm[:, :, None].to_broadcast([128, NG * 4, 4])), reads=G, writes=G)
            P.op("dve", _tt(t1[:], sel[:], msk[:], ALU.mult), reads=G, writes=G)
            P.op("dve", _ts(t2[:], msk[:], -1.0, -NEG, ALU.add, ALU.mult), reads=G, writes=G)
            P.op("dve", _tt(t1[:], t1[:], t2[:], ALU.add), reads=G, writes=G)
            P.op("dve", _red(a1[:], v16(t1), ALU.max), reads=G, writes=G)
            P.op("dve", _tt(v16(wv), v16(t1), a1[:, :, None].to_broadcast([128, NG, 16]), ALU.is_equal), reads=G, writes=G)
            P.op("dve", _stt(t1[:], wv[:], NEG, t1[:], ALU.mult, ALU.add), reads=G, writes=G)
            P.op("dve", _red(b1[:], v16(t1), ALU.max), reads=G, writes=G)
            P.op("dve", _tt(v16(t2), v16(t1), b1[:, :, None].to_broadcast([128, NG, 16]), ALU.is_equal), reads=G, writes=G)
            P.op("dve", _tt(wv[:], wv[:], t2[:], ALU.add), reads=G, writes=G)
            P.op("dve", _tt(wv[:], wv[:], aff[:], ALU.mult), reads=G, writes=G)
            P.op("dve", _red(a1[:], v16(wv), ALU.add), reads=G, writes=G)
            P.op("dve", _rcp(a1[:], a1[:]), reads=G, writes=G)
            P.op("dve", _tt(gate[:], v16(wv), a1[:, :, None].to_broadcast([128, NG, 16]), ALU.mult), reads=G, writes=["gate"])
            for t in range(HT // 128):
                P.op("pe", _tr(C.pb[3][0:16, 0:128], gate[:, t, :], C.ident[:]), reads=["gate", "ident"], writes=["pb3"])
                P.op("act", _acp(gateT[:, t * 128:(t + 1) * 128], C.pb[3][0:16, 0:128]), reads=["pb3"], writes=["gateT", "pb3"])
            P.barrier()
            st1.close()
            if MOE_STOP == "build":
                return
            st2 = ExitStack()
            wgu = [_sb(C, st2, "wgu", [128, 16, 1024], BF16) for _ in range(2)]
            wdn = _sb(C, st2, "wdn", [128, 4, D], BF16)
            gB = _sb(C, st2, "mgB", [128, HT], BF16)
            hm = [_sb(C, st2, "hm", [128, 4, 512], BF16) for _ in range(2)]
            sa = [_sb(C, st2, "sa", [128, 512], BF16) for _ in range(2)]
            sg = [_sb(C, st2, "sg", [128, 512], BF16) for _ in range(2)]
            gv = w_gu.rearrange("x (c p) n -> x p c n", p=128)
            dv = w_down.rearrange("x (c p) n -> x p c n", p=128)
            P.dma("pool", lambda e: e.dma_start(out=wgu[0][:], in_=gv[0]), ("wgu", 0), writes=[("wgu", 0)])
            kk = 0
            for ex in range(16 if MOE_STOP is None else 1):
                wg = wgu[ex % 2]; wgk = ("wgu", ex % 2)
                if ex + 1 < 16:
                    P.dma("pool", (lambda ex=ex: lambda e: e.dma_start(out=wgu[(ex + 1) % 2][:], in_=gv[ex + 1]))(), ("wgu", (ex + 1) % 2), writes=[("wgu", (ex + 1) % 2)])
                P.dma("pool", (lambda ex=ex: lambda e: e.dma_start(out=wdn[:], in_=dv[ex]))(), "wdn", writes=["wdn"])
                for tb in range(HT // 512):
                    P.op("pe", (lambda ex=ex, tb=tb: lambda e: e.matmul(C.pb[2][:, :], lhsT=sel16[:, ex, :], rhs=gateT[:, tb * 512:(tb + 1) * 512], start=True, stop=True))(), reads=["sel16", "gateT"], writes=["pb2"])
                    P.op("act", (lambda tb=tb: lambda e: e.copy(out=gB[:, tb * 512:(tb + 1) * 512], in_=C.pb[2][:, :]))(), reads=["pb2"], writes=[("mgB", tb)])
                for tb in range(HT // 512):
                    hmt = hm[tb % 2]; hk = ("hm", tb % 2)
                    for ffc in range(4):
                        pa = C.pb[4 + (kk % 2) * 2]; pak = f"pb{4 + (kk % 2) * 2}"
                        pbb = C.pb[5 + (kk % 2) * 2]; pbk = f"pb{5 + (kk % 2) * 2}"
                        for c in range(16):
                            P.op("pe", (lambda pa=pa, wg=wg, c=c, ffc=ffc, tb=tb: lambda e: e.matmul(pa[:, :], lhsT=wg[:, c, ffc * 128:(ffc + 1) * 128], rhs=xT[:, c, tb * 512:(tb + 1) * 512], start=(c == 0), stop=(c == 15)))(), reads=[wgk, "mxT"], writes=[pak])
                        for c in range(16):
                            P.op("pe", (lambda pbb=pbb, wg=wg, c=c, ffc=ffc, tb=tb: lambda e: e.matmul(pbb[:, :], lhsT=wg[:, c, 512 + ffc * 128:512 + (ffc + 1) * 128], rhs=xT[:, c, tb * 512:(tb + 1) * 512], start=(c == 0), stop=(c == 15)))(), reads=[wgk, "mxT"], writes=[pbk])
                        sat = sa[kk % 2]; sgt = sg[kk % 2]; sak = ("sa", kk % 2); sgk = ("sg", kk % 2)
                        P.op("act", (lambda sat=sat, pa=pa: lambda e: e.activation(out=sat[:], in_=pa[:, :], func=AF.Silu))(), reads=[pak], writes=[sak])
                        P.op("dve", (lambda sgt=sgt, sat=sat, tb=tb: lambda e: e.tensor_tensor(out=sgt[:], in0=sat[:], in1=gB[:, tb * 512:(tb + 1) * 512], op=ALU.mult))(), reads=[sak, ("mgB", tb)], writes=[sgk])
                        P.op("dve", (lambda hmt=hmt, ffc=ffc, sgt=sgt, pbb=pbb: lambda e: e.tensor_tensor(out=hmt[:, ffc, :], in0=sgt[:], in1=pbb[:, :], op=ALU.mult))(), reads=[sgk, pbk], writes=[hk])
                        kk += 1
                for tb in range(HT // 512):
                    hmt = hm[tb % 2]; hk = ("hm", tb % 2)
                    for dc in range(16):
                        po = C.pb[dc % 4]; pok = f"pb{dc % 4}"
                        for ffc in range(4):
                            P.op("pe", (lambda po=po, ffc=ffc, dc=dc, hmt=hmt: lambda e: e.matmul(po[:, :], lhsT=wdn[:, ffc, dc * 128:(dc + 1) * 128], rhs=hmt[:, ffc, :], start=(ffc == 0), stop=(ffc == 3)))(), reads=["wdn", hk], writes=[pok])
                        ydst = yacc[:, dc, tb * 512:(tb + 1) * 512]
                        if ex == 0:
                            P.op("dve", (lambda ydst=ydst, po=po: lambda e: e.tensor_copy(out=ydst, in_=po[:, :]))(), reads=[pok], writes=[("yacc", dc, tb)])
                        else:
                            P.op("dve", (lambda ydst=ydst, po=po: lambda e: e.tensor_tensor(out=ydst, in0=ydst, in1=po[:, :], op=ALU.add))(), reads=[pok, ("yacc", dc, tb)], writes=[("yacc", dc, tb)])
            if MOE_DBG is not None:
                P.op("dve", lambda e: e.tensor_copy(out=dbg_sb[:, 16:144], in_=gB[:, 0:128]), reads=[("mgB", 0)], writes=["dbg"])
                P.op("dve", lambda e: e.tensor_copy(out=dbg_sb[:, 144:272], in_=hm[0][:, 0, 0:128]), reads=[("hm", 0)], writes=["dbg"])
                P.op("dve", lambda e: e.tensor_copy(out=dbg_sb[:, 272:400], in_=yacc[:, 0, 0:128]), reads=[("yacc", 0, 0)], writes=["dbg"])
                P.op("dve", lambda e: e.tensor_copy(out=dbg_sb[0:16, 432:512], in_=gateT[:, 0:80]), reads=["gateT"], writes=["dbg"])
                P.dma("sp", lambda e: e.dma_start(out=MOE_DBG[:, :], in_=dbg_sb[:]), "dbg", reads=["dbg"], writes=["dbgout"])
            P.barrier()
            st2.close()
            if ln is None:
                fo = [_sb(C, st, "fo", [128, D]) for _ in range(2)]
                for t in range(HT // 128):
                    fot = fo[t % 2]; fk = ("fo", t % 2)
                    for q in range(4):
                        bank = C.pb[q % 2]; bk = f"pb{q % 2}"
                        for i in range(4):
                            dc = q * 4 + i
                            P.op("pe", _tr(bank[:, i * 128:(i + 1) * 128], yacc[:, dc, t * 128:(t + 1) * 128], C.ident[:]), reads=[("yacc", dc, t // 4), "ident"], writes=[bk])
                        P.op("act", _acp(fot[:, q * 512:(q + 1) * 512], bank[:, :]), reads=[bk], writes=[fk])
                    P.dma("sp", _dma(f_out[half * HT + t * 128: half * HT + (t + 1) * 128, :], fot[:]), fk, reads=[fk], writes=[("fout", half, t)])
            else:
                gB_d, bB_d, out_d = ln
                ep = LNEpi(C, st, h, gB_d, bB_d, out_d, "moe")
                ntl = HT // 128
                for t in range(ntl):
                    base = (t % 2) * 4
                    keys = [f"pb{base + q}" for q in range(4)]
                    for q in range(4):
                        for i in range(4):
                            dc = q * 4 + i
                            P.op("pe", _tr(C.pb[base + q][:, i * 128:(i + 1) * 128], yacc[:, dc, t * 128:(t + 1) * 128], C.ident[:]), reads=[("yacc", dc, t // 4), "ident"], writes=[keys[q]])
                    ep.front(t, half * HT + t * 128, C.ps_all[:, base * 512:base * 512 + 2048], keys)
                    if t >= 1:
                        ep.back(t - 1, half * HT + (t - 1) * 128)
                ep.back(ntl - 1, half * HT + (ntl - 1) * 128)
        P.barrier()


def _tt(out, in0, in1, op):
    return lambda e: e.tensor_tensor(out=out, in0=in0, in1=in1, op=op)


def _ts(out, in0, s1, s2=None, op0=ALU.mult, op1=None, accum=None):
    if accum is not None:
        return lambda e: e.tensor_scalar(out=out, in0=in0, scalar1=s1, scalar2=s2, op0=op0, op1=op1, accum_out=accum)
    if op1 is None:
        return lambda e: e.tensor_scalar(out=out, in0=in0, scalar1=s1, scalar2=None, op0=op0)
    return lambda e: e.tensor_scalar(out=out, in0=in0, scalar1=s1, scalar2=s2, op0=op0, op1=op1)


def _stt(out, in0, scalar, in1, op0, op1):
    return lambda e: e.scalar_tensor_tensor(out=out, in0=in0, scalar=scalar, in1=in1, op0=op0, op1=op1)


def _act(out, in_, func, bias=None, scale=None, accum=None):
    kw = {}
    if bias is not None:
        kw["bias"] = bias
    if scale is not None:
        kw["scale"] = scale
    if accum is not None:
        kw["accum_out"] = accum
    return lambda e: e.activation(out=out, in_=in_, func=func, **kw)


def _mm(out, lhsT, rhs, start, stop):
    return lambda e: e.matmul(out, lhsT=lhsT, rhs=rhs, start=start, stop=stop)


def _tr(out, in_, ident):
    return lambda e: e.transpose(out, in_, ident)


def _acp(out, in_):
    return lambda e: e.copy(out=out, in_=in_)


def _cp(out, in_):
    return lambda e: e.tensor_copy(out=out, in_=in_)


def _dma(out, in_):
    return lambda e: e.dma_start(out=out, in_=in_)


def _red(out, in_, op):
    return lambda e: e.tensor_reduce(out=out, in_=in_, axis=AX.X, op=op)


def _rcp(out, in_):
    return lambda e: e.reciprocal(out=out, in_=in_)


def _mset(out, v):
    return lambda e: e.memset(out, v)


def _sqrt(out, in_):
    return lambda e: e.sqrt(out=out, in_=in_)


def _rope(P, src, dst, cs, H, dh, t1, t2, rkeys, wkey, tk):
    hf = dh // 2
    s3 = src.rearrange("p (h d) -> p h d", h=H)
    d3 = dst.rearrange("p (h d) -> p h d", h=H)
    x1 = s3[:, :, 0:hf]; x2 = s3[:, :, hf:dh]
    cosB = cs[:, 0:1, :].to_broadcast([128, H, hf])
    sinB = cs[:, 1:2, :].to_broadcast([128, H, hf])
    a = t1[:, 0:H * hf].rearrange("p (h d) -> p h d", h=H)
    b = t2[:, 0:H * hf].rearrange("p (h d) -> p h d", h=H)
    k1, k2 = (tk, 1), (tk, 2)
    P.op("dve", _tt(a, x1, cosB, ALU.mult), reads=rkeys, writes=[k1])
    P.op("dve", _tt(b, x2, sinB, ALU.mult), reads=rkeys, writes=[k2])
    P.op("dve", _tt(d3[:, :, 0:hf], a, b, ALU.subtract), reads=[k1, k2], writes=[wkey])
    P.op("dve", _tt(a, x2, cosB, ALU.mult), reads=rkeys, writes=[k1])
    P.op("dve", _tt(b, x1, sinB, ALU.mult), reads=rkeys, writes=[k2])
    P.op("dve", _tt(d3[:, :, hf:dh], a, b, ALU.add), reads=[k1, k2, wkey], writes=[wkey])


def _transpose_blocks(C, src, nblk, dst3, rkeys, wkey, eng_toggle=0):
    P = C.P
    b0 = 0
    g = 0
    while b0 < nblk:
        n = min(4, nblk - b0)
        bi = (g + eng_toggle) % 2
        bank = C.pb[bi]; bk = f"pb{bi}"
        for i in range(n):
            P.op("pe", _tr(bank[:, i * 128:(i + 1) * 128], src[:, (b0 + i) * 128:(b0 + i + 1) * 128], C.ident[:]), reads=list(rkeys) + ["ident"], writes=[bk])
        srcp = bank[:, 0:n * 128].rearrange("p (i k) -> p i k", i=n)
        if bi == 0:
            P.op("act", _acp(dst3[:, b0:b0 + n, :], srcp), reads=[bk], writes=[wkey])
        else:
            P.op("dve", _cp(dst3[:, b0:b0 + n, :], srcp), reads=[bk], writes=[wkey])
        b0 += n
        g += 1


RET_G = [1.0 - 2.0 ** (-5.0 - h) for h in range(4)]


def mixer_ret(C, proj, pkv, mix, cst, cs_key="cs_own", s_in=None, s_out=None):
    P = C.P
    P.stage = C.pfx + "ret"
    with ExitStack() as st:
        decT = _sb(C, st, "decT", [128, 512]); xi = _sb(C, st, "xi", [128, 4]); zeta = _sb(C, st, "zeta", [128, 4]); gnB = _sb(C, st, "gnB", [128, 1024])
        P.dma("sp", _dma(decT[:], cst["decT"][:, :]), "decT", writes=["decT"])
        P.dma("sp", _dma(xi[:], cst["xi"][:, :]), "xi", writes=["xi"])
        P.dma("sp", _dma(zeta[:], cst["zeta"][:, :]), "zeta", writes=["zeta"])
        P.dma("sp", _dma(gnB[:], cst["gnB"][:, :]), "gnB", writes=["gnB"])
        S32 = _sb(C, st, "S32", [128, 4, 512]); Sb = _sb(C, st, "Sb", [128, 4, 512], BF16)
        if s_in is None:
            P.op("dve", _mset(S32[:], 0.0), writes=["S32"])
            P.op("dve", _mset(Sb[:], 0.0), writes=["Sb"])
        else:
            P.dma("sp", _dma(S32[:].rearrange("p h n -> p (h n)"), s_in[:, :]), "S32io", writes=["S32"])
            P.op("act", _acp(Sb[:], S32[:]), reads=["S32"], writes=["Sb"])
        qkvg = [_sb(C, st, "qkvg", [128, 4096]) for _ in range(2)]
        csb = [_sb(C, st, "csb", [128, 2, 128]) for _ in range(2)]
        qr = _sb(C, st, "qr", [128, 1024]); kr = _sb(C, st, "kr", [128, 1024]); qx = _sb(C, st, "qx", [128, 1024])
        kz = _sb(C, st, "kz", [128, 1024], BF16); vb = _sb(C, st, "vb", [128, 1024], BF16)
        qT = _sb(C, st, "qT", [128, 8, 128], BF16); kT = _sb(C, st, "kT", [128, 8, 128], BF16); qxT = _sb(C, st, "qxT", [128, 8, 128], BF16)
        am = _sb(C, st, "am", [128, 512], BF16)
        t1 = _sb(C, st, "rt1", [128, 512]); t2 = _sb(C, st, "rt2", [128, 512])
        junk = _sb(C, st, "rjunk", [128, 256])
        st4 = {n: _sb(C, st, "st" + n, [128, 4]) for n in ["s", "q", "m", "r", "nb"]}
        yn = _sb(C, st, "yn", [128, 1024]); sg = _sb(C, st, "sgt", [128, 1024])
        ret = [_sb(C, st, "ret", [128, 1024]) for _ in range(2)]
        SB = [C.pb[5], C.pb[6], C.pb[7], C.pb[2]]; SBK = ["pb5", "pb6", "pb7", "pb2"]
        kz2 = [kz, _sb(C, st, "kz2", [128, 1024], BF16)]; vb2 = [vb, _sb(C, st, "vb2", [128, 1024], BF16)]
        qT2_ = [qT, _sb(C, st, "qT2", [128, 8, 128], BF16)]; kT2_ = [kT, _sb(C, st, "kT2", [128, 8, 128], BF16)]; qxT2_ = [qxT, _sb(C, st, "qxT2", [128, 8, 128], BF16)]

        def front(ci):
            own = ci >= 16
            t = ci % 16; i = ci % 2
            buf = qkvg[i]; bkey = ("qkvg", i)
            cs = csb[i]; ckey = ("csb", i)
            if own:
                P.dma("sp", _dma(buf[:], proj[t * 128:(t + 1) * 128, 0:4096]), bkey, writes=[bkey])
                P.dma("sp", _dma(cs[:], cst[cs_key][t * 128:(t + 1) * 128, :, :]), ckey, writes=[ckey])
            else:
                P.dma("sp", _dma(buf[:, 1024:3072], pkv[t * 128:(t + 1) * 128, :]), bkey, writes=[bkey])
                P.dma("sp", _dma(cs[:], cst["cs_pre"][t * 128:(t + 1) * 128, :, :]), ckey, writes=[ckey])
            _rope(P, buf[:, 1024:2048], kr[:], cs, 4, 256, t1, t2, [bkey, ckey], "kr", "rt")
            P.op("dve", _tt(kz2[i][:].rearrange("p (h d) -> p h d", h=4), kr[:].rearrange("p (h d) -> p h d", h=4), zeta[:, :, None].to_broadcast([128, 4, 256]), ALU.mult), reads=["kr", "zeta"], writes=[("kz", i)])
            P.op("act", _acp(vb2[i][:], buf[:, 2048:3072]), reads=[bkey], writes=[("vb", i)])
            if own:
                _rope(P, buf[:, 0:1024], qr[:], cs, 4, 256, t1, t2, [bkey, ckey], "qr", "rt")
                P.op("dve", _tt(qx[:].rearrange("p (h d) -> p h d", h=4), qr[:].rearrange("p (h d) -> p h d", h=4), xi[:, :, None].to_broadcast([128, 4, 256]), ALU.mult), reads=["qr", "xi"], writes=["qx"])
                _transpose_blocks(C, qr, 8, qT2_[i], ["qr"], ("qT", i), 0)
                _transpose_blocks(C, kr, 8, kT2_[i], ["kr"], ("kT", i), 0)
                _transpose_blocks(C, qx, 8, qxT2_[i], ["qx"], ("qxT", i), 0)

        def back(ci):
            own = ci >= 16
            t = ci % 16; i = ci % 2
            buf = qkvg[i]; bkey = ("qkvg", i)
            kzi, vbi, qTi, kTi, qxTi = kz2[i], vb2[i], qT2_[i], kT2_[i], qxT2_[i]
            kzk, vbk, qTk, kTk, qxTk = ("kz", i), ("vb", i), ("qT", i), ("kT", i), ("qxT", i)
            if own:
                for h in range(4):
                    for j in range(2):
                        P.op("pe", _mm(C.pb[2][:, h * 128:(h + 1) * 128], kTi[:, 2 * h + j, :], qTi[:, 2 * h + j, :], j == 0, j == 1), reads=[kTk, qTk], writes=["pb2"])
                P.op("dve", _tt(am[:], C.pb[2][:, :], decT[:], ALU.mult), reads=["pb2", "decT"], writes=["am", "pb2"])
                for h in range(4):
                    yb = C.pb[3 + h // 2]; ybk = f"pb{3 + h // 2}"
                    o = yb[:, (h % 2) * 256:(h % 2) * 256 + 256]
                    P.op("pe", _mm(o, am[:, h * 128:(h + 1) * 128], vbi[:, h * 256:(h + 1) * 256], True, False), reads=["am", vbk], writes=[ybk])
                    P.op("pe", _mm(o, qxTi[:, 2 * h, :], Sb[:, h, 0:256], False, False), reads=[qxTk, "Sb"], writes=[ybk])
                    P.op("pe", _mm(o, qxTi[:, 2 * h + 1, :], Sb[:, h, 256:512], False, True), reads=[qxTk, "Sb"], writes=[ybk])
            for h in range(4):
                for j in range(2):
                    P.op("pe", _mm(SB[h][:, j * 256:(j + 1) * 256], kzi[:, h * 256 + j * 128:h * 256 + (j + 1) * 128], vbi[:, h * 256:(h + 1) * 256], True, True), reads=[kzk, vbk], writes=[SBK[h]])
            for h in range(4):
                P.op("dve", _stt(S32[:, h, :], S32[:, h, :], RET_G[h] ** 128, SB[h][:, :], ALU.mult, ALU.add), reads=[SBK[h], "S32"], writes=["S32", SBK[h]])
            P.op("act", _acp(Sb[:], S32[:]), reads=["S32"], writes=["Sb"])
            if not own:
                return
            s_, q_, m_, r_, nb_ = st4["s"], st4["q"], st4["m"], st4["r"], st4["nb"]
            for h in range(4):
                yb = C.pb[3 + h // 2]; ybk = f"pb{3 + h // 2}"
                o = yb[:, (h % 2) * 256:(h % 2) * 256 + 256]
                P.op("act", _act(junk[:], o, AF.Identity, accum=s_[:, h:h + 1]), reads=[ybk], writes=["rjunk", "st_s"])
                P.op("act", _act(junk[:], o, AF.Square, accum=q_[:, h:h + 1]), reads=[ybk], writes=["rjunk", "st_q"])
            P.op("dve", _ts(m_[:], s_[:], 1.0 / 256), reads=["st_s"], writes=["st_m"])
            P.op("dve", _tt(r_[:], m_[:], m_[:], ALU.mult), reads=["st_m"], writes=["st_r"])
            P.op("dve", _stt(r_[:], q_[:], 1.0 / 256, r_[:], ALU.mult, ALU.subtract), reads=["st_q", "st_r"], writes=["st_r"])
            P.op("dve", _ts(r_[:], r_[:], EPS, None, ALU.add), reads=["st_r"], writes=["st_r"])
            P.op("act", _sqrt(r_[:], r_[:]), reads=["st_r"], writes=["st_r"])
            P.op("dve", _rcp(r_[:], r_[:]), reads=["st_r"], writes=["st_r"])
            P.op("dve", _stt(nb_[:], m_[:], -1.0, r_[:], ALU.mult, ALU.mult), reads=["st_m", "st_r"], writes=["st_nb"])
            for h in range(4):
                yb = C.pb[3 + h // 2]; ybk = f"pb{3 + h // 2}"
                o = yb[:, (h % 2) * 256:(h % 2) * 256 + 256]
                P.op("act", _act(yn[:, h * 256:(h + 1) * 256], o, AF.Identity, bias=nb_[:, h:h + 1], scale=r_[:, h:h + 1]), reads=[ybk, "st_r", "st_nb"], writes=["yn", ybk])
            P.op("act", _act(sg[:], buf[:, 3072:4096], AF.Silu), reads=[bkey], writes=["sgt"])
            P.op("pool", _tt(yn[:], yn[:], gnB[:], ALU.mult), reads=["yn", "gnB"], writes=["yn"])
            rt = ret[t % 2]; rk = ("ret", t % 2)
            P.op("pool", _tt(rt[:], yn[:], sg[:], ALU.mult), reads=["yn", "sgt"], writes=[rk])
            P.dma("sp", _dma(mix[t * 128:(t + 1) * 128, 0:1024], rt[:]), rk, reads=[rk], writes=[("mixr", t)])

        cis = list(range(0 if pkv is not None else 16, 32))
        front(cis[0])
        for n_, ci in enumerate(cis):
            if n_ + 1 < len(cis):
                front(cis[n_ + 1])
            back(ci)
        if s_out is not None:
            P.dma("sp", _dma(s_out[:, :], S32[:].rearrange("p h n -> p (h n)")), "S32io", reads=["S32"], writes=["s_out"])
    P.barrier()


def mixer_conv(C, proj, phalo, mix, cst):
    P = C.P
    P.stage = C.pfx + "conv"
    NTK = T + 128
    with ExitStack() as st:
        cw = _sb(C, st, "cw", [128, 8, 31]); cv = _sb(C, st, "cv", [128, 3, 8])
        P.dma("sp", _dma(cw[:], cst["conv_w"][:, :, :]), "cw", writes=["cw"])
        P.dma("sp", _dma(cv[:], cst["conv_v"][:, :, :]), "cv", writes=["cv"])
        uT = _sb(C, st, "uT", [128, 8, NTK])
        yT = _sb(C, st, "yT", [128, 8, T])
        gg = [_sb(C, st, "gg", [128, 2048]) for _ in range(2)]
        sig = _sb(C, st, "sig", [128, 1024]); u = _sb(C, st, "u", [128, 1024])
        for ti in range(17):
            buf = gg[ti % 2]; bkey = ("gg", ti % 2)
            if ti == 0:
                P.dma("sp", _dma(buf[:], phalo[:, :]), bkey, writes=[bkey])
            else:
                P.dma("sp", _dma(buf[:], proj[(ti - 1) * 128:ti * 128, 4096:6144]), bkey, writes=[bkey])
            P.op("act", _act(sig[:], buf[:, 1024:2048], AF.Sigmoid), reads=[bkey], writes=["sig"])
            P.op("dve", _tt(u[:], buf[:, 0:1024], sig[:], ALU.mult), reads=[bkey, "sig"], writes=["u"])
            _transpose_blocks(C, u, 8, uT[:, :, ti * 128:(ti + 1) * 128], ["u"], "uT", ti)
        ptmp = _sb(C, st, "cptmp", [128, T])
        pool_chunks = ()
        for k in range(31):
            for j in range(8):
                yk = ("yT", j)
                src_k = uT[:, j, 98 + k:98 + k + T]
                if k == 0:
                    eng = "pool" if j in pool_chunks else "dve"
                    P.op(eng, _ts(yT[:, j, :], src_k, cw[:, j, 0:1], cv[:, 0, j:j + 1], ALU.mult, ALU.add), reads=["uT", "cw", "cv"], writes=[yk])
                elif j in pool_chunks:
                    P.op("pool", _ts(ptmp[:], src_k, cw[:, j, k:k + 1]), reads=["uT", "cw"], writes=["cptmp"])
                    P.op("pool", _tt(yT[:, j, :], yT[:, j, :], ptmp[:], ALU.add), reads=["cptmp", yk], writes=[yk])
                else:
                    P.op("dve", _stt(yT[:, j, :], src_k, cw[:, j, k:k + 1], yT[:, j, :], ALU.mult, ALU.add), reads=["uT", "cw", yk], writes=[yk])
        mean = _sb(C, st, "cmean", [128, 512]); rstd = _sb(C, st, "crstd", [128, 512]); sq = [_sb(C, st, "csq", [128, 512]) for _ in range(2)]
        z = [_sb(C, st, "cz", [128, 512]) for _ in range(2)]
        for tb in range(4):
            sl = slice(tb * 512, (tb + 1) * 512)
            for j in range(8):
                P.op("pe", _mm(C.pb[2][:, :], C.ones[:], yT[:, j, sl], j == 0, j == 7), reads=[("yT", j), "ones"], writes=["pb2"])
            for j in range(8):
                sqt = sq[j % 2]; sqk = ("csq", j % 2)
                P.op("act", _act(sqt[:], yT[:, j, sl], AF.Square), reads=[("yT", j)], writes=[sqk])
                P.op("pe", _mm(C.pb[3][:, :], C.ones[:], sqt[:], j == 0, j == 7), reads=[sqk, "ones"], writes=["pb3"])
            P.op("dve", _ts(mean[:], C.pb[2][:, :], 1.0 / 1024), reads=["pb2"], writes=["cmean"])
            P.op("dve", _tt(rstd[:], mean[:], mean[:], ALU.mult), reads=["cmean"], writes=["crstd"])
            P.op("dve", _stt(rstd[:], C.pb[3][:, :], 1.0 / 1024, rstd[:], ALU.mult, ALU.subtract), reads=["pb3", "crstd"], writes=["crstd"])
            P.op("dve", _ts(rstd[:], rstd[:], EPS, None, ALU.add), reads=["crstd"], writes=["crstd"])
            P.op("act", _sqrt(rstd[:], rstd[:]), reads=["crstd"], writes=["crstd"])
            P.op("dve", _rcp(rstd[:], rstd[:]), reads=["crstd"], writes=["crstd"])
            for j in range(8):
                zt = z[j % 2]; zk = ("cz", j % 2)
                P.op("dve", _tt(zt[:], yT[:, j, sl], mean[:], ALU.subtract), reads=[("yT", j), "cmean"], writes=[zk])
                P.op("dve", _tt(zt[:], zt[:], rstd[:], ALU.mult), reads=[zk, "crstd"], writes=[zk])
                P.op("act", _act(yT[:, j, sl], zt[:], AF.Silu, bias=cv[:, 2, j:j + 1], scale=cv[:, 1, j:j + 1]), reads=[zk, "cv"], writes=[("yT", j)])
        co = [_sb(C, st, "co", [128, 1024]) for _ in range(2)]
        for t in range(16):
            cot = co[t % 2]; ck = ("co", t % 2)
            for g in range(2):
                bank = C.pb[g]; bk = f"pb{g}"
                for i in range(4):
                    j = g * 4 + i
                    P.op("pe", _tr(bank[:, i * 128:(i + 1) * 128], yT[:, j, t * 128:(t + 1) * 128], C.ident[:]), reads=[("yT", j), "ident"], writes=[bk])
                if g == 0:
                    P.op("act", _acp(cot[:, 0:512], bank[:, :]), reads=[bk], writes=[ck])
                else:
                    P.op("dve", _cp(cot[:, 512:1024], bank[:, :]), reads=[bk], writes=[ck])
            P.dma("sp", _dma(mix[t * 128:(t + 1) * 128, 1024:2048], cot[:]), ck, reads=[ck], writes=[("mixc", t)])
    P.barrier()


def _bcast_rows(v):
    v = np.asarray(v, np.float32)
    return np.ascontiguousarray(np.broadcast_to(v[None, :], (128, v.shape[0])))


def _rope_tab(pos, half):
    inv = (10000.0 ** (-np.arange(half, dtype=np.float32) / np.float32(half))).astype(np.float32)
    ang = pos.astype(np.float32)[:, None] * inv[None, :]
    return np.ascontiguousarray(np.stack([np.cos(ang), np.sin(ang)], axis=1).astype(np.float32))


def _common_tail(C, x, mix, w_out, g1, b1, g2, b2, w_gu, w_down, rw, rbB, m, ha, f, out):
    linear_ln(C, mix, w_out, x, g1, b1, ha, "mix")
    moe(C, ha, w_gu, w_down, rw, rbB, f, "moe", ln=(g2, b2, out))


def build_layer0(debug=False):
    nc = bass.Bass("TRN2", target_bir_lowering=False)
    dt = lambda name, shape, kind="ExternalInput": nc.dram_tensor(name, shape, F32, kind=kind).ap()
    x = dt("x", [T, D]); xp = dt("xp", [T, D]); w_in = dt("w_in", [D, 6144]); w_out = dt("w_out", [D, D])
    g1 = dt("mix_g", [128, D]); b1 = dt("mix_b", [128, D]); g2 = dt("ffn_g", [128, D]); b2 = dt("ffn_b", [128, D])
    w_gu = dt("w_gu", [16, D, 1024]); w_down = dt("w_down", [16, 512, D])
    rw = dt("rw", [D, 16]); rbB = dt("rbB", [128, 16]); ident = dt("ident", [128, 128])
    cst = {"cs_own": dt("cs_own", [T, 2, 128]), "cs_pre": dt("cs_pre", [T, 2, 128]), "decT": dt("decT", [128, 512]),
           "xi": dt("xi", [128, 4]), "zeta": dt("zeta", [128, 4]), "gnB": dt("gnB", [128, 1024]),
           "conv_w": dt("conv_w", [128, 8, 31]), "conv_v": dt("conv_v", [128, 3, 8])}
    out = dt("out", [T, D], "ExternalOutput")
    dk = "ExternalOutput" if debug else "Internal"
    proj = dt("proj", [T, 6144], "Internal"); pkv = dt("pkv", [T, 2048], "Internal"); phalo = dt("phalo", [128, 2048], "Internal")
    mix = dt("mix", [T, D], dk); m = dt("m", [T, D], dk)
    ha = dt("ha", [T, D], dk); f = dt("f", [T, D], "Internal")
    with ExitStack() as es:
        C = _mk_ctx(nc, es)
        _load_consts(C, ident)
        linear(C, x, T, w_in, [(0, 6144)], proj, "in")
        linear(C, xp, T, w_in, [(1024, 3072)], pkv, "pkv")
        linear(C, xp[T - 128:T, :], 128, w_in, [(4096, 6144)], phalo, "ph")
        mixer_ret(C, proj, pkv, mix, cst)
        mixer_conv(C, proj, phalo, mix, cst)
        _common_tail(C, x, mix, w_out, g1, b1, g2, b2, w_gu, w_down, rw, rbB, m, ha, f, out)
        C.P.emit()
    return nc


def layer0_inputs(inp, h, core):
    b, half = core // 2, core % 2
    x = h[b, half * T:(half + 1) * T]
    xp = h[b, 0:T] if half == 1 else np.zeros((T, D), np.float32)
    g = np.array(RET_G, np.float64)
    j = np.arange(128, dtype=np.float64)
    diff = j[None, :] - j[:, None]
    decT = np.concatenate([np.where(diff >= 0, g[h_] ** np.maximum(diff, 0), 0.0) / 16.0 for h_ in range(4)], axis=1)
    xi = np.stack([g[h_] ** (j + 1.0) for h_ in range(4)], axis=1)
    zeta = np.stack([g[h_] ** (127.0 - j) / 16.0 for h_ in range(4)], axis=1)
    conv_w = np.asarray(inp["even_conv_w"][0], np.float32)
    cvec = np.stack([inp["even_conv_b"][0], inp["even_conv_ln_g"][0], inp["even_conv_ln_b"][0]], axis=0).astype(np.float32)
    return {
        "x": np.ascontiguousarray(x), "xp": np.ascontiguousarray(xp),
        "w_in": np.asarray(inp["even_w_in"][0], np.float32), "w_out": np.asarray(inp["even_w_out"][0], np.float32),
        "mix_g": _bcast_rows(inp["mix_ln_g"][0]), "mix_b": _bcast_rows(inp["mix_ln_b"][0]),
        "ffn_g": _bcast_rows(inp["ffn_ln_g"][0]), "ffn_b": _bcast_rows(inp["ffn_ln_b"][0]),
        "w_gu": np.asarray(inp["moe_w_gu"][0], np.float32), "w_down": np.asarray(inp["moe_w_down"][0], np.float32),
        "rw": np.asarray(inp["router_w"], np.float32), "rbB": _bcast_rows(inp["router_b"]), "ident": np.eye(128, dtype=np.float32),
        "cs_own": _rope_tab(np.arange(half * T, (half + 1) * T), 128), "cs_pre": _rope_tab(np.arange(0, T), 128),
        "decT": np.ascontiguousarray(decT.astype(np.float32)), "xi": np.ascontiguousarray(xi.astype(np.float32)),
        "zeta": np.ascontiguousarray(zeta.astype(np.float32)), "gnB": _bcast_rows(inp["even_ret_gn_g"][0]),
        "conv_w": np.ascontiguousarray(conv_w.T.reshape(8, 128, 31).transpose(1, 0, 2)),
        "conv_v": np.ascontiguousarray(cvec.reshape(3, 8, 128).transpose(2, 0, 1)),
    }


def mixer_dsa(C, qproj, kvproj, attn_out, cst):
    P = C.P
    P.stage = C.pfx + "dsa_kprep"
    SC = 128.0 ** -0.5
    with ExitStack() as st:
        kT = _sb(C, st, "dkT", [128, 4, 4096], BF16)
        vb = _sb(C, st, "dvb", [128, 32, 4, 129], BF16)
        kiT2 = _sb(C, st, "dkiT", [128, 1, 4096], BF16)
        iota = _sb(C, st, "diota", [128, 4096])
        qpos = _sb(C, st, "dqpos", [128, 16])
        P.dma("sp", _dma(iota[:], cst["iota"][:, :]), "diota", writes=["iota"])
        P.dma("sp", _dma(qpos[:], cst["qpos"][:, :]), "dqpos", writes=["qpos"])
        P.op("dve", _mset(vb[:], 1.0), writes=["vb"])
        with ExitStack() as s1:
            kvt = [_sb(C, s1, "kvt", [128, 1088]) for _ in range(2)]
            csk = [_sb(C, s1, "csk", [128, 2, 64]) for _ in range(2)]
            csi = [_sb(C, s1, "csi", [128, 2, 32]) for _ in range(2)]
            kr = _sb(C, s1, "dkr", [128, 512]); kir2 = _sb(C, s1, "dkir", [128, 128])
            t1 = _sb(C, s1, "dt1", [128, 256]); t2 = _sb(C, s1, "dt2", [128, 256])
            for kt in range(32):
                i = kt % 2
                bk_, ck_, ik_ = ("kvt", i), ("csk", i), ("csi", i)
                rows = slice(kt * 128, (kt + 1) * 128)
                P.dma("sp", _dma(kvt[i][:], kvproj[rows, :]), bk_, writes=[bk_])
                P.dma("sp", _dma(csk[i][:], cst["cs_k"][rows, :, :]), ck_, writes=[ck_])
                P.dma("sp", _dma(csi[i][:], cst["cs_ki"][rows, :, :]), ik_, writes=[ik_])
                _rope(P, kvt[i][:, 0:512], kr[:], csk[i], 4, 128, t1, t2, [bk_, ck_], "dkr", "dt")
                _transpose_blocks(C, kr, 4, kT[:, :, kt * 128:(kt + 1) * 128], ["dkr"], "kT", kt)
                P.op("act", _acp(vb[:, kt, :, 0:128], kvt[i][:, 512:1024].rearrange("p (h d) -> p h d", h=4)), reads=[bk_], writes=["vb"])
                _rope(P, kvt[i][:, 1024:1088], kir2[:, 0:64], csi[i], 1, 64, t1, t2, [bk_, ik_], "dkir", "dt")
                P.op("dve", _cp(kir2[:, 64:128], kir2[:, 0:64]), reads=["dkir"], writes=["dkir"])
                _transpose_blocks(C, kir2, 1, kiT2[:, :, kt * 128:(kt + 1) * 128], ["dkir"], "kiT", kt + 1)
        P.barrier()
        P.stage = C.pfx + "dsa_q"
        qt = _sb(C, st, "dqt", [128, 3088])
        csq = [_sb(C, st, "csq", [128, 2, 64]) for _ in range(2)]
        csqi = [_sb(C, st, "csqi", [128, 2, 32]) for _ in range(2)]
        qr = _sb(C, st, "dqr", [128, 2048]); qir = _sb(C, st, "dqir", [128, 1024])
        qT = [_sb(C, st, "dqT", [128, 16, 128], BF16) for _ in range(2)]
        qiT = _sb(C, st, "dqiT", [128, 8, 128], BF16)
        wab = _sb(C, st, "dwab", [128, 16]); sgn = _sb(C, st, "dsgn", [128, 16])
        t1 = _sb(C, st, "dq1", [128, 1024]); t2 = _sb(C, st, "dq2", [128, 1024])
        acc = _sb(C, st, "dacc", [128, 4096]); scr = _sb(C, st, "dscr", [128, 4096])
        tmp = [_sb(C, st, "dtmp", [128, 1024]) for _ in range(2)]
        selT = [_sb(C, st, "dselT", [128, 32, 128], BF16) for _ in range(2)]
        pt = [_sb(C, st, "dp", [128, 1024], BF16) for _ in range(2)]
        obuf = _sb(C, st, "dobuf", [128, 16, 129])
        rec = _sb(C, st, "drec", [128, 16])
        sm = {n: _sb(C, st, "d_" + n, [128, 1]) for n in ["lo", "hi", "w", "mid", "cnt", "ge"]}
        NIT = 26
        hw = _sb(C, st, "d_hw", [128, NIT + 1]); pw2 = _sb(C, st, "d_pw2", [128, NIT + 1])
        for k_ in range(NIT + 1):
            P.op("dve", _mset(pw2[:, k_:k_ + 1], 2.0 ** -(k_ + 1)), writes=["pw2"])
        identb = _sb(C, st, "didb", [128, 128], BF16)
        P.op("dve", _cp(identb[:], C.ident[:]), reads=["ident"], writes=["identb"])
        cnts = {"it": 0, "ig": 0}

        def NN(j):
            return 2048 + 128 * (j + 1)

        def phaseA(j):
            N = NN(j); i = j % 2
            rows = slice(j * 128, (j + 1) * 128)
            qTk = ("qT", i)
            P.dma("sp", _dma(qt[:], qproj[rows, :]), "dqt", writes=["dqt"])
            P.dma("sp", _dma(csq[i][:], cst["cs_q"][rows, :, :]), ("csq", i), writes=[("csq", i)])
            P.dma("sp", _dma(csqi[i][:], cst["cs_qi"][rows, :, :]), ("csqi", i), writes=[("csqi", i)])
            _rope(P, qt[:, 0:2048], qr[:], csq[i], 16, 128, t1, t2, ["dqt", ("csq", i)], "dqr", "dq")
            _transpose_blocks(C, qr, 16, qT[i], ["dqr"], qTk, 0)
            _rope(P, qt[:, 2048:3072], qir[:], csqi[i], 16, 64, t1, t2, ["dqt", ("csqi", i)], "dqir", "dq")
            _transpose_blocks(C, qir, 8, qiT, ["dqir"], "qiT", 0)
            P.op("dve", _ts(sgn[:], qt[:, 3072:3088], 0.0, 2.0, ALU.is_ge, ALU.mult), reads=["dqt"], writes=["sgn"])
            P.op("dve", _ts(sgn[:], sgn[:], -1.0, None, ALU.add), reads=["sgn"], writes=["sgn"])
            P.op("dve", _tt(wab[:], qt[:, 3072:3088], sgn[:], ALU.mult), reads=["dqt", "sgn"], writes=["wab"])
            P.op("dve", _ts(wab[:], wab[:], 0.03125), reads=["wab"], writes=["wab"])
            P.op("dve", _ts(acc[:, 0:N], iota[:, 0:N], qpos[:, j:j + 1], NEG, ALU.is_gt, ALU.mult), reads=["iota", "qpos"], writes=["acc"])
            for h in range(16):
                p0 = (h % 2) * 64
                for g0 in range(0, N, 1024):
                    w = min(1024, N - g0)
                    ig = cnts["ig"]
                    base = (ig % 4) * 2
                    nb_ = (w + 511) // 512
                    bkeys = [f"pb{base + c}" for c in range(nb_)]
                    for c4 in range(nb_):
                        ww = min(512, w - c4 * 512)
                        P.op("pe", _mm(C.pb[base + c4][:, 0:ww], qiT[p0:p0 + 64, h // 2, :], kiT2[p0:p0 + 64, 0, g0 + c4 * 512:g0 + c4 * 512 + ww], True, True),
                             reads=["qiT", "kiT"], writes=[bkeys[c4]])
                    tm = tmp[ig % 2]; tk = ("dtmp", ig % 2)
                    P.op("act", _act(tm[:, 0:w], C.ps_all[:, base * 512:base * 512 + w], AF.Relu, scale=wab[:, h:h + 1]), reads=bkeys + ["wab"], writes=[tk] + bkeys)
                    P.op("dve", _stt(acc[:, g0:g0 + w], tm[:, 0:w], sgn[:, h:h + 1], acc[:, g0:g0 + w], ALU.mult, ALU.add), reads=[tk, "sgn", "acc"], writes=["acc"])
                    cnts["ig"] += 1

        def phaseB(j):
            N = NN(j); NB = N // 128; i = j % 2
            lo, hi, wd, mid, cnt, ge = (sm[n] for n in ["lo", "hi", "w", "mid", "cnt", "ge"])
            P.op("dve", _red(hi[:], acc[:, 0:N], ALU.max), reads=["acc"], writes=["hi"])
            P.op("dve", _ts(scr[:, 0:N], iota[:, 0:N], qpos[:, j:j + 1], -2.0 * NEG, ALU.is_gt, ALU.mult), reads=["iota", "qpos"], writes=["scr"])
            P.op("dve", _tt(scr[:, 0:N], scr[:, 0:N], acc[:, 0:N], ALU.add), reads=["scr", "acc"], writes=["scr"])
            P.op("dve", _red(lo[:], scr[:, 0:N], ALU.min), reads=["scr"], writes=["lo"])
            P.op("dve", _tt(wd[:], hi[:], lo[:], ALU.subtract), reads=["hi", "lo"], writes=["w"])
            P.op("dve", _ts(hw[:], pw2[:], wd[:, 0:1]), reads=["w", "pw2"], writes=["hw"])
            for k in range(NIT):
                P.op("dve", _tt(mid[:], lo[:], hw[:, k:k + 1], ALU.add), reads=["lo", "hw"], writes=["mid"])
                P.op("dve", _ts(scr[:, 0:N], acc[:, 0:N], mid[:, 0:1], 0.0, ALU.is_ge, ALU.add, accum=cnt[:, 0:1]), reads=["acc", "mid"], writes=["scr", "cnt"])
                P.op("dve", _ts(ge[:], cnt[:], 255.5, hw[:, k:k + 1], ALU.is_ge, ALU.mult), reads=["cnt", "hw"], writes=["ge"])
                P.op("dve", _tt(lo[:], lo[:], ge[:], ALU.add), reads=["lo", "ge"], writes=["lo"])
            P.op("dve", _ts(scr[:, 0:N], acc[:, 0:N], lo[:, 0:1], -30000.0, ALU.is_lt, ALU.mult), reads=["acc", "lo"], writes=["scr"])
            _transpose_blocks(C, scr, NB, selT[i], ["scr"], ("selT", i), 0)

        def phaseCmain(j):
            N = NN(j); NB = N // 128; i = j % 2
            qT2 = qT[i][:].rearrange("p h q -> p (h q)")
            qTk, sTk = ("qT", i), ("selT", i)
            for kv in range(4):
                for kb in range(0, NB, 2):
                    nk = min(2, NB - kb)
                    it = cnts["it"]
                    base = (it % 2) * 2
                    Lks = [f"pb{base + b}" for b in range(nk)]
                    pp = pt[it % 2]; ppk = ("dp", it % 2)
                    for b in range(nk):
                        P.op("pe", _mm(C.pb[base + b][:, :], kT[:, kv, (kb + b) * 128:(kb + b + 1) * 128], qT2[:, kv * 512:(kv + 1) * 512], True, False), reads=["kT", qTk], writes=[Lks[b]])
                        for g in range(4):
                            P.op("pe", _mm(C.pb[base + b][:, g * 128:(g + 1) * 128], identb[:], selT[i][:, kb + b, :], False, g == 3), reads=["identb", sTk], writes=[Lks[b]])
                    P.op("act", _act(pp[:, 0:nk * 512], C.ps_all[:, base * 512:(base + nk) * 512], AF.Exp, scale=SC), reads=Lks, writes=[ppk] + Lks)
                    for b in range(nk):
                        for g in range(4):
                            P.op("pe", _mm(C.pb[4 + g][:, 0:129], pp[:, b * 512 + g * 128:b * 512 + (g + 1) * 128], vb[:, kb + b, kv, :], kb + b == 0, kb + b == NB - 1), reads=[ppk, "vb"], writes=[f"pb{4 + g}"])
                    cnts["it"] += 1
                for g in range(4):
                    P.op("act", _acp(obuf[:, kv * 4 + g, :], C.pb[4 + g][:, 0:129]), reads=[f"pb{4 + g}"], writes=["obuf", f"pb{4 + g}"])

        def phaseCfin(j):
            rows = slice(j * 128, (j + 1) * 128)
            P.op("dve", _rcp(rec[:], obuf[:, :, 128]), reads=["obuf"], writes=["rec"])
            P.op("dve", _tt(obuf[:, :, 0:128], obuf[:, :, 0:128], rec[:, :, None].to_broadcast([128, 16, 128]), ALU.mult), reads=["obuf", "rec"], writes=["obuf"])
            P.dma("sp", _dma(attn_out[rows, :].rearrange("p (h d) -> p h d", h=16), obuf[:, :, 0:128]), "dobuf", reads=["obuf"], writes=[("aout", j)])

        phaseA(0)
        phaseB(0)
        for j in range(16):
            if j + 1 < 16:
                phaseA(j + 1)
            phaseCmain(j)
            if j + 1 < 16:
                phaseB(j + 1)
            phaseCfin(j)
    P.barrier()


def build_layer1(debug=False):
    nc = bass.Bass("TRN2", target_bir_lowering=False)
    dt = lambda name, shape, kind="ExternalInput": nc.dram_tensor(name, shape, F32, kind=kind).ap()
    x = dt("x", [T, D]); xf = dt("xf", [2 * T, D]); w_in = dt("w_in", [D, 4176]); w_out = dt("w_out", [D, D])
    g1 = dt("mix_g", [128, D]); b1 = dt("mix_b", [128, D]); g2 = dt("ffn_g", [128, D]); b2 = dt("ffn_b", [128, D])
    w_gu = dt("w_gu", [16, D, 1024]); w_down = dt("w_down", [16, 512, D])
    rw = dt("rw", [D, 16]); rbB = dt("rbB", [128, 16]); ident = dt("ident", [128, 128])
    cst = {"cs_k": dt("cs_k", [2 * T, 2, 64]), "cs_ki": dt("cs_ki", [2 * T, 2, 32]), "cs_q": dt("cs_q", [T, 2, 64]), "cs_qi": dt("cs_qi", [T, 2, 32]),
           "iota": dt("iota", [128, 4096]), "qpos": dt("qpos", [128, 16])}
    out = dt("out", [T, D], "ExternalOutput")
    dk = "ExternalOutput" if debug else "Internal"
    qproj = dt("qproj", [T, 3088], "Internal"); kvproj = dt("kvproj", [2 * T, 1088], "Internal")
    mix = dt("mix", [T, D], dk); m = dt("m", [T, D], dk)
    ha = dt("ha", [T, D], dk); f = dt("f", [T, D], "Internal")
    with ExitStack() as es:
        C = _mk_ctx(nc, es)
        _load_consts(C, ident)
        linear(C, x, T, w_in, [(0, 2048), (3072, 4096), (4160, 4176)], qproj, "q1")
        linear(C, xf, 2 * T, w_in, [(2048, 3072), (4096, 4160)], kvproj, "kv1")
        mixer_dsa(C, qproj, kvproj, mix, cst)
        _common_tail(C, x, mix, w_out, g1, b1, g2, b2, w_gu, w_down, rw, rbB, m, ha, f, out)
        C.P.emit()
    return nc


def layer1_inputs(inp, h, core):
    b, half = core // 2, core % 2
    pos_own = np.arange(half * T, (half + 1) * T)
    qpos = (half * T + np.arange(16)[None, :] * 128 + np.arange(128)[:, None]).astype(np.float32)
    return {
        "x": np.ascontiguousarray(h[b, half * T:(half + 1) * T]), "xf": np.ascontiguousarray(h[b]),
        "w_in": np.asarray(inp["odd_w_in"][0], np.float32), "w_out": np.asarray(inp["odd_w_out"][0], np.float32),
        "mix_g": _bcast_rows(inp["mix_ln_g"][1]), "mix_b": _bcast_rows(inp["mix_ln_b"][1]),
        "ffn_g": _bcast_rows(inp["ffn_ln_g"][1]), "ffn_b": _bcast_rows(inp["ffn_ln_b"][1]),
        "w_gu": np.asarray(inp["moe_w_gu"][1], np.float32), "w_down": np.asarray(inp["moe_w_down"][1], np.float32),
        "rw": np.asarray(inp["router_w"], np.float32), "rbB": _bcast_rows(inp["router_b"]), "ident": np.eye(128, dtype=np.float32),
        "cs_k": _rope_tab(np.arange(2 * T), 64), "cs_ki": _rope_tab(np.arange(2 * T), 32),
        "cs_q": _rope_tab(pos_own, 64), "cs_qi": _rope_tab(pos_own, 32),
        "iota": np.ascontiguousarray(np.broadcast_to(np.arange(4096, dtype=np.float32)[None, :], (128, 4096))),
        "qpos": np.ascontiguousarray(qpos),
    }


def build_fused(profile=False):
    nc = bass.Bass("TRN2", target_bir_lowering=False)
    dt = lambda name, shape, kind="ExternalInput": nc.dram_tensor(name, shape, F32, kind=kind).ap()
    xA = dt("x", [T, D]); xB = dt("xp", [T, D]); zhalo = dt("zhalo", [128, 2048])
    rw = dt("rw", [D, 16]); rbB = dt("rbB", [128, 16]); ident = dt("ident", [128, 128])
    L = []
    for l in range(2):
        L.append({"w_in": dt(f"w_in{l}", [D, 6144 if l == 0 else 4176]), "w_out": dt(f"w_out{l}", [D, D]),
                  "g1": dt(f"mix_g{l}", [128, D]), "b1": dt(f"mix_b{l}", [128, D]), "g2": dt(f"ffn_g{l}", [128, D]), "b2": dt(f"ffn_b{l}", [128, D]),
                  "w_gu": dt(f"w_gu{l}", [16, D, 1024]), "w_down": dt(f"w_down{l}", [16, 512, D])})
    cst = {"cs_own": dt("cs_own", [T, 2, 128]), "cs_pre": dt("cs_pre", [T, 2, 128]), "decT": dt("decT", [128, 512]),
           "xi": dt("xi", [128, 4]), "zeta": dt("zeta", [128, 4]), "gnB": dt("gnB", [128, 1024]),
           "conv_w": dt("conv_w", [128, 8, 31]), "conv_v": dt("conv_v", [128, 3, 8]),
           "cs_k": dt("cs_k", [2 * T, 2, 64]), "cs_ki": dt("cs_ki", [2 * T, 2, 32]), "cs_q": dt("cs_q", [T, 2, 64]), "cs_qi": dt("cs_qi", [T, 2, 32]),
           "iota": dt("iota", [128, 4096]), "qpos": dt("qpos", [128, 16])}
    out = dt("out", [T, D], "ExternalOutput")
    projB = dt("projB", [T, 6144], "Internal"); projA = dt("projA", [T, 6144], "Internal")
    sstate = dt("sstate", [128, 2048], "Internal"); h0f = dt("h0f", [2 * T, D], "Internal")
    qproj = dt("qproj", [T, 3088], "Internal"); kvproj = dt("kvproj", [2 * T, 1088], "Internal")
    mix = dt("mix", [T, D], "Internal"); m = dt("m", [T, D], "Internal"); ha = dt("ha", [T, D], "Internal"); f = dt("f", [T, D], "Internal")
    with ExitStack() as es:
        C = _mk_ctx(nc, es)
        C.P.profile = profile
        _load_consts(C, ident)
        l0 = L[0]
        for (xx, proj, cs_key, s_in, s_out, halo, dst) in [(xB, projB, "cs_pre", None, sstate, zhalo, h0f[0:T, :]),
                                                            (xA, projA, "cs_own", sstate, None, projB[T - 128:T, 4096:6144], h0f[T:2 * T, :])]:
            C.pfx = "B_" if s_in is None else "A_"
            linear(C, xx, T, l0["w_in"], [(0, 6144)], proj, "in")
            mixer_ret(C, proj, None, mix, cst, cs_key=cs_key, s_in=s_in, s_out=s_out)
            mixer_conv(C, proj, halo, mix, cst)
            _common_tail(C, xx, mix, l0["w_out"], l0["g1"], l0["b1"], l0["g2"], l0["b2"], l0["w_gu"], l0["w_down"], rw, rbB, m, ha, f, dst)
        l1 = L[1]
        C.pfx = "L1_"
        x1 = h0f[T:2 * T, :]
        linear(C, x1, T, l1["w_in"], [(0, 2048), (3072, 4096), (4160, 4176)], qproj, "q1")
        linear(C, h0f, 2 * T, l1["w_in"], [(2048, 3072), (4096, 4160)], kvproj, "kv1")
        mixer_dsa(C, qproj, kvproj, mix, cst)
        _common_tail(C, x1, mix, l1["w_out"], l1["g1"], l1["b1"], l1["g2"], l1["b2"], l1["w_gu"], l1["w_down"], rw, rbB, m, ha, f, out)
        C.P.emit()
    return nc


def fused_inputs(inp, core):
    b, half = core // 2, core % 2
    x = np.asarray(inp["x"], np.float32)
    a = layer0_inputs(inp, x, core)
    r = {k: a[k] for k in ["x", "xp", "rw", "rbB", "ident", "cs_own", "cs_pre", "decT", "xi", "zeta", "gnB", "conv_w", "conv_v"]}
    r["zhalo"] = np.zeros((128, 2048), np.float32)
    for l, pre in enumerate(["even", "odd"]):
        r[f"w_in{l}"] = np.asarray(inp[pre + "_w_in"][0], np.float32); r[f"w_out{l}"] = np.asarray(inp[pre + "_w_out"][0], np.float32)
        r[f"mix_g{l}"] = _bcast_rows(inp["mix_ln_g"][l]); r[f"mix_b{l}"] = _bcast_rows(inp["mix_ln_b"][l])
        r[f"ffn_g{l}"] = _bcast_rows(inp["ffn_ln_g"][l]); r[f"ffn_b{l}"] = _bcast_rows(inp["ffn_ln_b"][l])
        r[f"w_gu{l}"] = np.asarray(inp["moe_w_gu"][l], np.float32); r[f"w_down{l}"] = np.asarray(inp["moe_w_down"][l], np.float32)
    posA = np.arange(half * T, (half + 1) * T)
    posB = np.arange(0, T)
    kp = np.concatenate([posB if half == 1 else np.full(T, 1.0e9), posA]).astype(np.float32)
    ropepos = np.concatenate([posB, posA])
    r["cs_k"] = _rope_tab(ropepos, 64); r["cs_ki"] = _rope_tab(ropepos, 32)
    r["cs_q"] = _rope_tab(posA, 64); r["cs_qi"] = _rope_tab(posA, 32)
    r["iota"] = np.ascontiguousarray(np.broadcast_to(kp[None, :], (128, 4096)))
    r["qpos"] = np.ascontiguousarray((half * T + np.arange(16)[None, :] * 128 + np.arange(128)[:, None]).astype(np.float32))
    return r


_NC_CACHE = {}


def kernel(**inputs):
    inp = {k: np.asarray(v) for k, v in inputs.items()}
    B = inp["x"].shape[0]
    if "fused" not in _NC_CACHE:
        _NC_CACHE["fused"] = build_fused()
    nc = _NC_CACHE["fused"]
    in_maps = [fused_inputs(inp, core) for core in range(8)]
    res = run_bass_kernel_spmd(nc, in_maps, core_ids=list(range(8)))
    h = np.stack([np.concatenate([res.results[2 * b]["out"], res.results[2 * b + 1]["out"]], axis=0) for b in range(B)], axis=0)
    return np.ascontiguousarray(h.astype(np.float32))
```

```python
import math
import numpy as np
from contextlib import ExitStack
import concourse.bass as bass
import concourse.mybir as mybir
from concourse.bass_utils import run_bass_kernel_spmd

F32 = mybir.dt.float32
BF16 = mybir.dt.bfloat16
AF = mybir.ActivationFunctionType
ALU = mybir.AluOpType
AX = mybir.AxisListType

ENGS = ("pe", "act", "dve", "pool", "sp")
D = 2048
T = 2048
NT = 16
ALPHA = 4.0 ** 0.25
EPS = 1e-5
NEG = -1.0e30


class Op:
    __slots__ = ("eng", "fn", "deps", "dma_slot", "dma_val", "signal", "sigval", "idx", "stage")


class Prog:
    def __init__(self, nc):
        self.nc = nc
        self.ops = []
        self.last_w = {}
        self.readers = {}
        self.slot_cnt = {}
        self.slot_last = {}
        self.last_on = {}
        self.stage = "init"
        self.profile = False

    def _rec(self, eng, fn, reads, writes, dma_slot=None, extra_deps=()):
        op = Op()
        op.eng = eng; op.fn = fn; op.dma_slot = dma_slot; op.signal = False; op.sigval = 0
        op.idx = len(self.ops)
        op.stage = self.stage
        deps = set(extra_deps)
        for k in reads:
            w = self.last_w.get(k)
            if w is not None:
                deps.add(w)
        for k in writes:
            w = self.last_w.get(k)
            if w is not None:
                deps.add(w)
            rd = self.readers.get(k)
            if rd:
                deps.update(rd[0].values())
                deps.update(rd[1])
        op.deps = deps
        if dma_slot is not None:
            c = self.slot_cnt.get(dma_slot, 0) + 1
            self.slot_cnt[dma_slot] = c
            op.dma_val = 16 * c
            self.slot_last[dma_slot] = op.idx
        else:
            op.dma_val = 0
            self.last_on[eng] = op.idx
        for k in reads:
            rd = self.readers.setdefault(k, ({}, []))
            if dma_slot is not None:
                rd[1].append(op.idx)
            else:
                rd[0][eng] = op.idx
        for k in writes:
            self.last_w[k] = op.idx
            self.readers[k] = ({}, [])
        self.ops.append(op)
        return op

    def op(self, eng, fn, reads=(), writes=()):
        return self._rec(eng, fn, reads, writes)

    def dma(self, eng, fn, slot, reads=(), writes=()):
        return self._rec(eng, fn, reads, writes, dma_slot=slot)

    def barrier(self):
        deps = set(self.last_on.values()) | set(self.slot_last.values())
        for e in ENGS:
            self._rec(e, None, (), (), extra_deps=deps)
        self.last_w = {}
        self.readers = {}

    def emit(self):
        nc = self.nc
        self.barrier()
        ops = self.ops
        for op in ops:
            for d in op.deps:
                p = ops[d]
                if p.dma_slot is None and p.fn is not None and (p.eng != op.eng or p.eng != "pe"):
                    p.signal = True
        cnt = {e: 0 for e in ENGS}
        for op in ops:
            if op.signal:
                cnt[op.eng] += 1
                op.sigval = cnt[op.eng]
        slots = list(self.slot_cnt.keys())
        EPOCH = 30000
        with ExitStack() as es:
            eng_sems = {}
            for e in ENGS:
                n_ep = cnt[e] // EPOCH + 1
                eng_sems[e] = [es.enter_context(nc.semaphore(f"s_{e}_{i}")) for i in range(n_ep)]
            slot_sem = {s: es.enter_context(nc.semaphore(f"d_{i}")) for i, s in enumerate(slots)}
            block = es.enter_context(nc.Block())

            def run_engine(ename, eng):
                waited = {}
                cur = None
                for op in ops:
                    if op.eng != ename:
                        continue
                    if self.profile and op.stage != cur:
                        if cur is not None:
                            nc.pop_named_scope(cur)
                        cur = op.stage
                        nc.push_named_scope(cur)
                    for d in sorted(op.deps):
                        p = ops[d]
                        if p.dma_slot is not None:
                            sem = slot_sem[p.dma_slot]; val = p.dma_val
                        else:
                            if p.eng == ename and ename == "pe":
                                continue
                            if p.fn is None:
                                continue
                            ep = (p.sigval - 1) // EPOCH
                            sem = eng_sems[p.eng][ep]; val = p.sigval - ep * EPOCH
                        if waited.get(sem.num, 0) >= val:
                            continue
                        waited[sem.num] = val
                        eng.wait_ge(sem, val)
                    if op.fn is None:
                        continue
                    ins = op.fn(eng)
                    if op.dma_slot is not None:
                        ins.then_inc(slot_sem[op.dma_slot], 16)
                    elif op.signal:
                        ep = (op.sigval - 1) // EPOCH
                        ins.then_inc(eng_sems[ename][ep], 1)
                if self.profile and cur is not None:
                    nc.pop_named_scope(cur)

            block.sync(lambda e: run_engine("sp", e))
            block.tensor(lambda e: run_engine("pe", e))
            block.scalar(lambda e: run_engine("act", e))
            block.vector(lambda e: run_engine("dve", e))
            block.gpsimd(lambda e: run_engine("pool", e))


class Ctx:
    pass


def _mk_ctx(nc, es):
    C = Ctx()
    C.nc = nc
    C.P = Prog(nc)
    C.es = es
    C.ps_all = es.enter_context(nc.psum_tensor("ps_all", [128, 4096], F32))
    C.pb = [C.ps_all[:, i * 512:(i + 1) * 512] for i in range(8)]
    C.uid = 0
    C.pfx = ""
    return C


def _sb(C, st, name, shape, dt=F32):
    C.uid += 1
    return st.enter_context(C.nc.sbuf_tensor(f"{name}_{C.uid}", shape, dt))


def _load_consts(C, ident_d):
    nc, P = C.nc, C.P
    C.ident = C.es.enter_context(nc.sbuf_tensor("ident_sb", [128, 128], F32))
    C.ones = C.es.enter_context(nc.sbuf_tensor("ones_sb", [128, 128], F32))
    P.dma("sp", lambda e: e.dma_start(out=C.ident[:], in_=ident_d[:, :]), "ident", writes=["ident"])
    P.op("dve", lambda e: e.memset(C.ones[:], 1.0), writes=["ones"])


def build_xT(C, st, src, n_tok, tag, f32_copy=None):
    nc, P = C.nc, C.P
    nt = n_tok // 128
    xT = _sb(C, st, "xT" + tag, [128, 16, n_tok], BF16)
    stg = [_sb(C, st, "xs" + tag, [128, D]) for _ in range(2)]
    for t in range(nt):
        s = stg[t % 2]
        sk = ("xs", tag, t % 2)
        P.dma("sp", (lambda s=s, t=t: lambda e: e.dma_start(out=s[:], in_=src[t * 128:(t + 1) * 128, :]))(), sk, writes=[sk])
        for q in range(4):
            bank = C.pb[q % 2]
            bk = f"pb{q % 2}"
            for i in range(4):
                c = q * 4 + i
                P.op("pe", (lambda s=s, c=c, bank=bank, i=i: lambda e: e.transpose(bank[:, i * 128:(i + 1) * 128], s[:, c * 128:(c + 1) * 128], C.ident[:]))(),
                     reads=[sk, "ident"], writes=[bk])
            eng = "act" if q % 2 == 0 else "dve"
            dst = xT[:, q * 4:(q + 1) * 4, t * 128:(t + 1) * 128]
            srcp = bank[:].rearrange("p (i k) -> p i k", i=4)
            if eng == "act":
                P.op("act", (lambda dst=dst, srcp=srcp: lambda e: e.copy(out=dst, in_=srcp))(), reads=[bk], writes=[("xT", tag)])
            else:
                P.op("dve", (lambda dst=dst, srcp=srcp: lambda e: e.tensor_copy(out=dst, in_=srcp))(), reads=[bk], writes=[("xT", tag)])
            if f32_copy is not None:
                dst2 = f32_copy[:, q * 4:(q + 1) * 4, t * 128:(t + 1) * 128]
                P.op("dve", (lambda dst2=dst2, srcp=srcp: lambda e: e.tensor_copy(out=dst2, in_=srcp))(), reads=[bk], writes=[("xT32", tag)])
    return xT


def linear(C, src, n_tok, w, col_ranges, dst, tag):
    nc, P = C.nc, C.P
    P.stage = C.pfx + "lin_" + tag
    nt = n_tok // 128
    with ExitStack() as st:
        xT = build_xT(C, st, src, n_tok, tag)
        wb = [_sb(C, st, "wb" + tag, [128, 16, 512], BF16) for _ in range(2)]
        ost = [_sb(C, st, "os" + tag, [128, 512]) for _ in range(4)]
        groups = []
        o = 0
        for (c0, c1) in col_ranges:
            c = c0
            while c < c1:
                n = min(512, c1 - c)
                groups.append((c, n, o))
                c += n; o += n
        wv = w.rearrange("(c p) n -> p c n", p=128)
        k = 0
        for gi, (c0, n, o0) in enumerate(groups):
            wt = wb[gi % 2]
            wk = ("wb", tag, gi % 2)
            P.dma("pool", (lambda wt=wt, c0=c0, n=n: lambda e: e.dma_start(out=wt[:, :, 0:n], in_=wv[:, :, c0:c0 + n]))(), wk, writes=[wk])
            for t in range(nt):
                bank = C.pb[2 + k % 4]; bk = f"pb{2 + k % 4}"
                for c in range(16):
                    P.op("pe", (lambda bank=bank, c=c, t=t, wt=wt, n=n: lambda e: e.matmul(bank[:, 0:n], lhsT=xT[:, c, t * 128:(t + 1) * 128], rhs=wt[:, c, 0:n], start=(c == 0), stop=(c == 15)))(),
                         reads=[("xT", tag), wk], writes=[bk])
                os_ = ost[k % 4]; ok = ("os", tag, k % 4)
                if k % 2 == 0:
                    P.op("act", (lambda os_=os_, bank=bank, n=n: lambda e: e.copy(out=os_[:, 0:n], in_=bank[:, 0:n]))(), reads=[bk], writes=[ok])
                else:
                    P.op("dve", (lambda os_=os_, bank=bank, n=n: lambda e: e.tensor_copy(out=os_[:, 0:n], in_=bank[:, 0:n]))(), reads=[bk], writes=[ok])
                P.dma("sp", (lambda os_=os_, t=t, o0=o0, n=n: lambda e: e.dma_start(out=dst[t * 128:(t + 1) * 128, o0:o0 + n], in_=os_[:, 0:n]))(), ok, reads=[ok], writes=[("dst", tag, t, gi)])
                k += 1
    P.barrier()


def resid_ln(C, x, m, gB_d, bB_d, out, n_tok, tag):
    nc, P = C.nc, C.P
    P.stage = C.pfx + "ln_" + tag
    nt = n_tok // 128
    with ExitStack() as st:
        gB = _sb(C, st, "gB", [128, D]); bB = _sb(C, st, "bB", [128, D])
        P.dma("sp", _dma(gB[:], gB_d[:, :]), ("gB", tag), writes=[("gB", tag)])
        P.dma("sp", _dma(bB[:], bB_d[:, :]), ("bB", tag), writes=[("bB", tag)])
        xs = [_sb(C, st, "rx", [128, D]) for _ in range(2)]
        ms = [_sb(C, st, "rm", [128, D]) for _ in range(2)]
        rs = [_sb(C, st, "rr", [128, D]) for _ in range(2)]
        mv = [_sb(C, st, "rmv", [128, 2]) for _ in range(2)]
        rstd = [_sb(C, st, "rrs", [128, 1]) for _ in range(2)]
        nb = [_sb(C, st, "rnb", [128, 1]) for _ in range(2)]

        def front(t):
            i = t % 2
            xk, mk, rk, vk = ("rx", tag, i), ("rm", tag, i), ("rr", tag, i), ("rmv", i)
            P.dma("sp", _dma(xs[i][:], x[t * 128:(t + 1) * 128, :]), xk, writes=[xk])
            P.dma("sp", _dma(ms[i][:], m[t * 128:(t + 1) * 128, :]), mk, writes=[mk])
            P.op("dve", _stt(rs[i][:], xs[i][:], ALPHA, ms[i][:], ALU.mult, ALU.add), reads=[xk, mk], writes=[rk])
            P.op("act", _act(xs[i][:], rs[i][:], AF.Identity, accum=mv[i][:, 0:1]), reads=[rk], writes=[vk, xk])
            P.op("act", _act(xs[i][:], rs[i][:], AF.Square, accum=mv[i][:, 1:2]), reads=[rk], writes=[vk, xk])

        def back(t):
            i = t % 2
            xk, mk, rk, vk, sk, nk = ("rx", tag, i), ("rm", tag, i), ("rr", tag, i), ("rmv", i), ("rrs", i), ("rnb", i)
            P.op("dve", _ts(mv[i][:], mv[i][:], 1.0 / D), reads=[vk], writes=[vk])
            P.op("dve", _tt(nb[i][:], mv[i][:, 0:1], mv[i][:, 0:1], ALU.mult), reads=[vk], writes=[nk])
            P.op("dve", _tt(rstd[i][:], mv[i][:, 1:2], nb[i][:], ALU.subtract), reads=[vk, nk], writes=[sk])
            P.op("dve", _ts(rstd[i][:], rstd[i][:], EPS, None, ALU.add), reads=[sk], writes=[sk])
            P.op("act", _sqrt(rstd[i][:], rstd[i][:]), reads=[sk], writes=[sk])
            P.op("dve", _rcp(rstd[i][:], rstd[i][:]), reads=[sk], writes=[sk])
            P.op("dve", _stt(nb[i][:], mv[i][:, 0:1], -1.0, rstd[i][:], ALU.mult, ALU.mult), reads=[vk, sk], writes=[nk])
            P.op("act", _act(rs[i][:], rs[i][:], AF.Identity, bias=nb[i][:, 0:1], scale=rstd[i][:, 0:1]), reads=[rk, sk, nk], writes=[rk])
            P.op("dve", _tt(ms[i][:], rs[i][:], gB[:], ALU.mult), reads=[rk, ("gB", tag)], writes=[mk])
            P.op("pool", _tt(ms[i][:], ms[i][:], bB[:], ALU.add), reads=[mk, ("bB", tag)], writes=[mk])
            P.dma("sp", _dma(out[t * 128:(t + 1) * 128, :], ms[i][:]), mk, reads=[mk], writes=[("rout", tag, t)])

        for t in range(nt + 1):
            if t < nt:
                front(t)
            if t >= 1:
                back(t - 1)
    P.barrier()


class LNEpi:
    def __init__(self, C, st, x, gB_d, bB_d, out, tag):
        self.C, self.x, self.out, self.tag = C, x, out, tag
        P = C.P
        self.gB = _sb(C, st, "egB", [128, D]); self.bB = _sb(C, st, "ebB", [128, D])
        P.dma("sp", _dma(self.gB[:], gB_d[:, :]), ("egB", tag), writes=[("egB", tag)])
        P.dma("sp", _dma(self.bB[:], bB_d[:, :]), ("ebB", tag), writes=[("ebB", tag)])
        self.xs = [_sb(C, st, "ex", [128, D]) for _ in range(2)]
        self.rs = [_sb(C, st, "er", [128, D]) for _ in range(2)]
        self.mv = [_sb(C, st, "emv", [128, 2]) for _ in range(2)]
        self.rstd = [_sb(C, st, "ers", [128, 1]) for _ in range(2)]
        self.nb = [_sb(C, st, "enb", [128, 1]) for _ in range(2)]

    def front(self, t, row0, msrc, mkeys):
        P, tag, i = self.C.P, self.tag, t % 2
        xk, rk, vk = ("ex", tag, i), ("er", tag, i), ("emv", tag, i)
        P.dma("sp", _dma(self.xs[i][:], self.x[row0:row0 + 128, :]), xk, writes=[xk])
        P.op("dve", _stt(self.rs[i][:], self.xs[i][:], ALPHA, msrc, ALU.mult, ALU.add), reads=[xk] + list(mkeys), writes=[rk] + list(mkeys))
        P.op("act", _act(self.xs[i][:], self.rs[i][:], AF.Identity, accum=self.mv[i][:, 0:1]), reads=[rk], writes=[vk, xk])
        P.op("act", _act(self.xs[i][:], self.rs[i][:], AF.Square, accum=self.mv[i][:, 1:2]), reads=[rk], writes=[vk, xk])

    def back(self, t, row0):
        P, tag, i = self.C.P, self.tag, t % 2
        mv, rstd, nb, rs, xs = self.mv[i], self.rstd[i], self.nb[i], self.rs[i], self.xs[i]
        xk, rk, vk, sk, nk = ("ex", tag, i), ("er", tag, i), ("emv", tag, i), ("ers", tag, i), ("enb", tag, i)
        P.op("dve", _ts(mv[:], mv[:], 1.0 / D), reads=[vk], writes=[vk])
        P.op("dve", _tt(nb[:], mv[:, 0:1], mv[:, 0:1], ALU.mult), reads=[vk], writes=[nk])
        P.op("dve", _tt(rstd[:], mv[:, 1:2], nb[:], ALU.subtract), reads=[vk, nk], writes=[sk])
        P.op("dve", _ts(rstd[:], rstd[:], EPS, None, ALU.add), reads=[sk], writes=[sk])
        P.op("act", _sqrt(rstd[:], rstd[:]), reads=[sk], writes=[sk])
        P.op("dve", _rcp(rstd[:], rstd[:]), reads=[sk], writes=[sk])
        P.op("dve", _stt(nb[:], mv[:, 0:1], -1.0, rstd[:], ALU.mult, ALU.mult), reads=[vk, sk], writes=[nk])
        P.op("act", _act(rs[:], rs[:], AF.Identity, bias=nb[:, 0:1], scale=rstd[:, 0:1]), reads=[rk, sk, nk], writes=[rk])
        P.op("dve", _tt(xs[:], rs[:], self.gB[:], ALU.mult), reads=[rk, ("egB", tag)], writes=[xk])
        P.op("dve", _tt(xs[:], xs[:], self.bB[:], ALU.add), reads=[xk, ("ebB", tag)], writes=[xk])
        P.dma("sp", _dma(self.out[row0:row0 + 128, :], xs[:]), xk, reads=[xk], writes=[("eout", tag, t)])


def linear_ln(C, src, w, x, gB_d, bB_d, out, tag):
    nc, P = C.nc, C.P
    P.stage = C.pfx + "linln_" + tag
    nt = T // 128
    with ExitStack() as st:
        xT = _sb(C, st, "lxT", [128, 16, T], BF16)
        wt = _sb(C, st, "lw", [128, 16, D], BF16)
        wv = w.rearrange("(c p) n -> p c n", p=128)
        for g in range(4):
            P.dma("pool", _dma(wt[:, :, g * 512:(g + 1) * 512], wv[:, :, g * 512:(g + 1) * 512]), ("lw", g), writes=[("lw", g)])
        with ExitStack() as s1:
            stg = [_sb(C, s1, "lxs", [128, D]) for _ in range(2)]
            for t in range(nt):
                sk = ("lxs", t % 2)
                P.dma("sp", _dma(stg[t % 2][:], src[t * 128:(t + 1) * 128, :]), sk, writes=[sk])
                _transpose_blocks(C, stg[t % 2], 16, xT[:, :, t * 128:(t + 1) * 128], [sk], "lxT", 0)
        P.barrier()
        ep = LNEpi(C, st, x, gB_d, bB_d, out, tag)
        for t in range(nt):
            base = (t % 2) * 4
            keys = [f"pb{base + g}" for g in range(4)]
            for g in range(4):
                for c in range(16):
                    P.op("pe", _mm(C.pb[base + g][:, :], xT[:, c, t * 128:(t + 1) * 128], wt[:, c, g * 512:(g + 1) * 512], c == 0, c == 15), reads=["lxT", ("lw", g)], writes=[keys[g]])
            ep.front(t, t * 128, C.ps_all[:, base * 512:base * 512 + 2048], keys)
            if t >= 1:
                ep.back(t - 1, (t - 1) * 128)
        ep.back(nt - 1, (nt - 1) * 128)
    P.barrier()


MOE_STOP = None
MOE_DBG = None


def moe(C, h, w_gu, w_down, rw_d, rbB_d, f_out, tag, ln=None):
    nc, P = C.nc, C.P
    HT = 1024
    for half_ in range(2 if MOE_STOP is None else 1):
        _moe_half(C, h, w_gu, w_down, rw_d, rbB_d, f_out, tag, half_, ln)


def _moe_half(C, h, w_gu, w_down, rw_d, rbB_d, f_out, tag, half, ln=None):
    nc, P = C.nc, C.P
    P.stage = C.pfx + "moe"
    HT = 1024
    if True:
        with ExitStack() as st:
            hsrc = h[half * HT:(half + 1) * HT, :]
            gateT = _sb(C, st, "gateT", [16, HT], BF16)
            sel16 = _sb(C, st, "sel16", [16, 16, 128], BF16)
            xT = _sb(C, st, "mxT", [128, 16, HT], BF16)
            yacc = _sb(C, st, "yacc", [128, 16, HT])
            dbg_sb = _sb(C, st, "dbg_sb", [128, 512])
            P.op("dve", lambda e: e.memset(dbg_sb[:], 0.0), writes=["dbg"])
            st1 = ExitStack()
            xT32 = _sb(C, st1, "xT32", [128, 16, 128])
            rw = _sb(C, st1, "rw", [128, 16, 16])
            rbB = _sb(C, st1, "rbB", [128, 16])
            gate = _sb(C, st1, "gate", [128, 8, 16])
            P.dma("sp", lambda e: e.dma_start(out=rw[:], in_=rw_d.rearrange("(c p) n -> p c n", p=128)), ("rw", tag, half), writes=["rw"])
            P.dma("sp", lambda e: e.dma_start(out=rbB[:], in_=rbB_d[:, :]), ("rbB", tag, half), writes=["rbB"])
            for ex_ in range(16):
                P.op("dve", (lambda ex_=ex_: lambda e: e.tensor_copy(out=sel16[:, ex_, :], in_=C.ident[0:16, ex_:ex_ + 1].to_broadcast([16, 128])))(), reads=["ident"], writes=["sel16"])
            stg = [_sb(C, st1, "mxs", [128, D]) for _ in range(2)]
            sm = {n: _sb(C, st1, "g" + n, [128, 8 * 16]) for n in ["aff", "sel", "msk", "t1", "t2", "w"]}
            sm4 = {n: _sb(C, st1, "g4" + n, [128, 8 * 4]) for n in ["m1", "m2", "gs", "gm"]}
            sm1 = {n: _sb(C, st1, "g1" + n, [128, 8]) for n in ["a", "b"]}
            for t in range(HT // 128):
                s = stg[t % 2]; sk = ("mxs", t % 2)
                P.dma("sp", (lambda s=s, t=t: lambda e: e.dma_start(out=s[:], in_=hsrc[t * 128:(t + 1) * 128, :]))(), sk, writes=[sk])
                for q in range(4):
                    bank = C.pb[q % 2]; bk = f"pb{q % 2}"
                    for i in range(4):
                        c = q * 4 + i
                        P.op("pe", (lambda s=s, c=c, bank=bank, i=i: lambda e: e.transpose(bank[:, i * 128:(i + 1) * 128], s[:, c * 128:(c + 1) * 128], C.ident[:]))(), reads=[sk, "ident"], writes=[bk])
                    srcp = bank[:].rearrange("p (i k) -> p i k", i=4)
                    P.op("act", (lambda q=q, t=t, srcp=srcp: lambda e: e.copy(out=xT[:, q * 4:(q + 1) * 4, t * 128:(t + 1) * 128], in_=srcp))(), reads=[bk], writes=["mxT", bk])
                    P.op("dve", (lambda q=q, srcp=srcp: lambda e: e.tensor_copy(out=xT32[:, q * 4:(q + 1) * 4, :], in_=srcp))(), reads=[bk], writes=["xT32", bk])
                lb = C.pb[2]
                for c in range(16):
                    P.op("pe", (lambda c=c: lambda e: e.matmul(lb[:, 0:16], lhsT=xT32[:, c, :], rhs=rw[:, c, :], start=(c == 0), stop=(c == 15)))(), reads=["xT32", "rw"], writes=["pb2"])
                P.op("act", _act(sm["aff"][:, t * 16:(t + 1) * 16], lb[:, 0:16], AF.Sigmoid), reads=["pb2"], writes=["gsm", "pb2"])
            aff, sel, msk, t1, t2, wv = sm["aff"], sm["sel"], sm["msk"], sm["t1"], sm["t2"], sm["w"]
            m1, m2, gs, gm = sm4["m1"], sm4["m2"], sm4["gs"], sm4["gm"]
            a1, b1 = sm1["a"], sm1["b"]
            G = ["gsm"]
            NG = 8
            v16 = lambda x: x[:].rearrange("p (t e) -> p t e", t=NG)
            v4 = lambda x: x[:].rearrange("p (n k) -> p n k", k=4)
            g4 = lambda x: x[:].rearrange("p (t g) -> p t g", t=NG)
            P.op("dve", _tt(v16(sel), v16(aff), rbB[:, None, :].to_broadcast([128, NG, 16]), ALU.add), reads=G + ["rbB"], writes=G)
            P.op("dve", _red(m1[:], v4(sel), ALU.max), reads=G, writes=G)
            P.op("dve", _tt(v4(t1), v4(sel), m1[:, :, None].to_broadcast([128, NG * 4, 4]), ALU.is_equal), reads=G, writes=G)
            P.op("dve", _stt(t1[:], t1[:], NEG, sel[:], ALU.mult, ALU.add), reads=G, writes=G)
            P.op("dve", _red(m2[:], v4(t1), ALU.max), reads=G, writes=G)
            P.op("dve", _tt(gs[:], m1[:], m2[:], ALU.add), reads=G, writes=G)
            P.op("dve", _red(a1[:], g4(gs), ALU.max), reads=G, writes=G)
            P.op("dve", _tt(g4(gm), g4(gs), a1[:, :, None].to_broadcast([128, NG, 4]), ALU.is_equal), reads=G, writes=G)
            P.op("dve", _cp(v4(msk), gm[:, :, None].to_broadcast([128, NG * 4, 4])), reads=G, writes=G)
            P.op("dve", _tt(t1[:], sel[:], msk[:], ALU.mult), reads=G, writes=G)
            P.op("dve", _ts(t2[:], msk[:], -1.0, -NEG, ALU.add, ALU.mult), reads=G, writes=G)
            P.op("dve", _tt(t1[:], t1[:], t2[:], ALU.add), reads=G, writes=G)
            P.op("dve", _red(a1[:], v16(t1), ALU.max), reads=G, writes=G)
            P.op("dve", _tt(v16(wv), v16(t1), a1[:, :, None].to_broadcast([128, NG, 16]), ALU.is_equal), reads=G, writes=G)
            P.op("dve", _stt(t1[:], wv[:], NEG, t1[:], ALU.mult, ALU.add), reads=G, writes=G)
            P.op("dve", _red(b1[:], v16(t1), ALU.max), reads=G, writes=G)
            P.op("dve", _tt(v16(t2), v16(t1), b1[:, :, None].to_broadcast([128, NG, 16]), ALU.is_equal), reads=G, writes=G)
            P.op("dve", _tt(wv[:], wv[:], t2[:], ALU.add), reads=G, writes=G)
            P.op("dve", _tt(wv[:], wv[:], aff[:], ALU.mult), reads=G, writes=G)
            P.op("dve", _red(a1[:], v16(wv), ALU.add), reads=G, writes=G)
            P.op("dve", _rcp(a1[:], a1[:]), reads=G, writes=G)
            P.op("dve", _tt(gate[:], v16(wv), a1[:, :, None].to_broadcast([128, NG, 16]), ALU.mult), reads=G, writes=["gate"])
            for t in range(HT // 128):
                P.op("pe", _tr(C.pb[3][0:16, 0:128], gate[:, t, :], C.ident[:]), reads=["gate", "ident"], writes=["pb3"])
                P.op("act", _acp(gateT[:, t * 128:(t + 1) * 128], C.pb[3][0:16, 0:128]), reads=["pb3"], writes=["gateT", "pb3"])
            P.barrier()
            st1.close()
            if MOE_STOP == "build":
                return
            st2 = ExitStack()
            wgu = [_sb(C, st2, "wgu", [128, 16, 1024], BF16) for _ in range(2)]
            wdn = _sb(C, st2, "wdn", [128, 4, D], BF16)
            gB = _sb(C, st2, "mgB", [128, HT], BF16)
            hm = [_sb(C, st2, "hm", [128, 4, 512], BF16) for _ in range(2)]
            sa = [_sb(C, st2, "sa", [128, 512], BF16) for _ in range(2)]
            sg = [_sb(C, st2, "sg", [128, 512], BF16) for _ in range(2)]
            gv = w_gu.rearrange("x (c p) n -> x p c n", p=128)
            dv = w_down.rearrange("x (c p) n -> x p c n", p=128)
            P.dma("pool", lambda e: e.dma_start(out=wgu[0][:], in_=gv[0]), ("wgu", 0), writes=[("wgu", 0)])
            kk = 0
            for ex in range(16 if MOE_STOP is None else 1):
                wg = wgu[ex % 2]; wgk = ("wgu", ex % 2)
                if ex + 1 < 16:
                    P.dma("pool", (lambda ex=ex: lambda e: e.dma_start(out=wgu[(ex + 1) % 2][:], in_=gv[ex + 1]))(), ("wgu", (ex + 1) % 2), writes=[("wgu", (ex + 1) % 2)])
                P.dma("pool", (lambda ex=ex: lambda e: e.dma_start(out=wdn[:], in_=dv[ex]))(), "wdn", writes=["wdn"])
                for tb in range(HT // 512):
                    P.op("pe", (lambda ex=ex, tb=tb: lambda e: e.matmul(C.pb[2][:, :], lhsT=sel16[:, ex, :], rhs=gateT[:, tb * 512:(tb + 1) * 512], start=True, stop=True))(), reads=["sel16", "gateT"], writes=["pb2"])
                    P.op("act", (lambda tb=tb: lambda e: e.copy(out=gB[:, tb * 512:(tb + 1) * 512], in_=C.pb[2][:, :]))(), reads=["pb2"], writes=[("mgB", tb)])
                for tb in range(HT // 512):
                    hmt = hm[tb % 2]; hk = ("hm", tb % 2)
                    for ffc in range(4):
                        pa = C.pb[4 + (kk % 2) * 2]; pak = f"pb{4 + (kk % 2) * 2}"
                        pbb = C.pb[5 + (kk % 2) * 2]; pbk = f"pb{5 + (kk % 2) * 2}"
                        for c in range(16):
                            P.op("pe", (lambda pa=pa, wg=wg, c=c, ffc=ffc, tb=tb: lambda e: e.matmul(pa[:, :], lhsT=wg[:, c, ffc * 128:(ffc + 1) * 128], rhs=xT[:, c, tb * 512:(tb + 1) * 512], start=(c == 0), stop=(c == 15)))(), reads=[wgk, "mxT"], writes=[pak])
                        for c in range(16):
                            P.op("pe", (lambda pbb=pbb, wg=wg, c=c, ffc=ffc, tb=tb: lambda e: e.matmul(pbb[:, :], lhsT=wg[:, c, 512 + ffc * 128:512 + (ffc + 1) * 128], rhs=xT[:, c, tb * 512:(tb + 1) * 512], start=(c == 0), stop=(c == 15)))(), reads=[wgk, "mxT"], writes=[pbk])
                        sat = sa[kk % 2]; sgt = sg[kk % 2]; sak = ("sa", kk % 2); sgk = ("sg", kk % 2)
                        P.op("act", (lambda sat=sat, pa=pa: lambda e: e.activation(out=sat[:], in_=pa[:, :], func=AF.Silu))(), reads=[pak], writes=[sak])
                        P.op("dve", (lambda sgt=sgt, sat=sat, tb=tb: lambda e: e.tensor_tensor(out=sgt[:], in0=sat[:], in1=gB[:, tb * 512:(tb + 1) * 512], op=ALU.mult))(), reads=[sak, ("mgB", tb)], writes=[sgk])
                        P.op("dve", (lambda hmt=hmt, ffc=ffc, sgt=sgt, pbb=pbb: lambda e: e.tensor_tensor(out=hmt[:, ffc, :], in0=sgt[:], in1=pbb[:, :], op=ALU.mult))(), reads=[sgk, pbk], writes=[hk])
                        kk += 1
                for tb in range(HT // 512):
                    hmt = hm[tb % 2]; hk = ("hm", tb % 2)
                    for dc in range(16):
                        po = C.pb[dc % 4]; pok = f"pb{dc % 4}"
                        for ffc in range(4):
                            P.op("pe", (lambda po=po, ffc=ffc, dc=dc, hmt=hmt: lambda e: e.matmul(po[:, :], lhsT=wdn[:, ffc, dc * 128:(dc + 1) * 128], rhs=hmt[:, ffc, :], start=(ffc == 0), stop=(ffc == 3)))(), reads=["wdn", hk], writes=[pok])
                        ydst = yacc[:, dc, tb * 512:(tb + 1) * 512]
                        if ex == 0:
                            P.op("dve", (lambda ydst=ydst, po=po: lambda e: e.tensor_copy(out=ydst, in_=po[:, :]))(), reads=[pok], writes=[("yacc", dc, tb)])
                        else:
                            P.op("dve", (lambda ydst=ydst, po=po: lambda e: e.tensor_tensor(out=ydst, in0=ydst, in1=po[:, :], op=ALU.add))(), reads=[pok, ("yacc", dc, tb)], writes=[("yacc", dc, tb)])
            if MOE_DBG is not None:
                P.op("dve", lambda e: e.tensor_copy(out=dbg_sb[:, 16:144], in_=gB[:, 0:128]), reads=[("mgB", 0)], writes=["dbg"])
                P.op("dve", lambda e: e.tensor_copy(out=dbg_sb[:, 144:272], in_=hm[0][:, 0, 0:128]), reads=[("hm", 0)], writes=["dbg"])
                P.op("dve", lambda e: e.tensor_copy(out=dbg_sb[:, 272:400], in_=yacc[:, 0, 0:128]), reads=[("yacc", 0, 0)], writes=["dbg"])
                P.op("dve", lambda e: e.tensor_copy(out=dbg_sb[0:16, 432:512], in_=gateT[:, 0:80]), reads=["gateT"], writes=["dbg"])
                P.dma("sp", lambda e: e.dma_start(out=MOE_DBG[:, :], in_=dbg_sb[:]), "dbg", reads=["dbg"], writes=["dbgout"])
            P.barrier()
            st2.close()
            if ln is None:
                fo = [_sb(C, st, "fo", [128, D]) for _ in range(2)]
                for t in range(HT // 128):
                    fot = fo[t % 2]; fk = ("fo", t % 2)
                    for q in range(4):
                        bank = C.pb[q % 2]; bk = f"pb{q % 2}"
                        for i in range(4):
                            dc = q * 4 + i
                            P.op("pe", _tr(bank[:, i * 128:(i + 1) * 128], yacc[:, dc, t * 128:(t + 1) * 128], C.ident[:]), reads=[("yacc", dc, t // 4), "ident"], writes=[bk])
                        P.op("act", _acp(fot[:, q * 512:(q + 1) * 512], bank[:, :]), reads=[bk], writes=[fk])
                    P.dma("sp", _dma(f_out[half * HT + t * 128: half * HT + (t + 1) * 128, :], fot[:]), fk, reads=[fk], writes=[("fout", half, t)])
            else:
                gB_d, bB_d, out_d = ln
                ep = LNEpi(C, st, h, gB_d, bB_d, out_d, "moe")
                ntl = HT // 128
                for t in range(ntl):
                    base = (t % 2) * 4
                    keys = [f"pb{base + q}" for q in range(4)]
                    for q in range(4):
                        for i in range(4):
                            dc = q * 4 + i
                            P.op("pe", _tr(C.pb[base + q][:, i * 128:(i + 1) * 128], yacc[:, dc, t * 128:(t + 1) * 128], C.ident[:]), reads=[("yacc", dc, t // 4), "ident"], writes=[keys[q]])
                    ep.front(t, half * HT + t * 128, C.ps_all[:, base * 512:base * 512 + 2048], keys)
                    if t >= 1:
                        ep.back(t - 1, half * HT + (t - 1) * 128)
                ep.back(ntl - 1, half * HT + (ntl - 1) * 128)
        P.barrier()


def _tt(out, in0, in1, op):
    return lambda e: e.tensor_tensor(out=out, in0=in0, in1=in1, op=op)


def _ts(out, in0, s1, s2=None, op0=ALU.mult, op1=None, accum=None):
    if accum is not None:
        return lambda e: e.tensor_scalar(out=out, in0=in0, scalar1=s1, scalar2=s2, op0=op0, op1=op1, accum_out=accum)
    if op1 is None:
        return lambda e: e.tensor_scalar(out=out, in0=in0, scalar1=s1, scalar2=None, op0=op0)
    return lambda e: e.tensor_scalar(out=out, in0=in0, scalar1=s1, scalar2=s2, op0=op0, op1=op1)


def _stt(out, in0, scalar, in1, op0, op1):
    return lambda e: e.scalar_tensor_tensor(out=out, in0=in0, scalar=scalar, in1=in1, op0=op0, op1=op1)


def _act(out, in_, func, bias=None, scale=None, accum=None):
    kw = {}
    if bias is not None:
        kw["bias"] = bias
    if scale is not None:
        kw["scale"] = scale
    if accum is not None:
        kw["accum_out"] = accum
    return lambda e: e.activation(out=out, in_=in_, func=func, **kw)


def _mm(out, lhsT, rhs, start, stop):
    return lambda e: e.matmul(out, lhsT=lhsT, rhs=rhs, start=start, stop=stop)


def _tr(out, in_, ident):
    return lambda e: e.transpose(out, in_, ident)


def _acp(out, in_):
    return lambda e: e.copy(out=out, in_=in_)


def _cp(out, in_):
    return lambda e: e.tensor_copy(out=out, in_=in_)


def _dma(out, in_):
    return lambda e: e.dma_start(out=out, in_=in_)


def _red(out, in_, op):
    return lambda e: e.tensor_reduce(out=out, in_=in_, axis=AX.X, op=op)


def _rcp(out, in_):
    return lambda e: e.reciprocal(out=out, in_=in_)


def _mset(out, v):
    return lambda e: e.memset(out, v)


def _sqrt(out, in_):
    return lambda e: e.sqrt(out=out, in_=in_)


def _rope(P, src, dst, cs, H, dh, t1, t2, rkeys, wkey, tk):
    hf = dh // 2
    s3 = src.rearrange("p (h d) -> p h d", h=H)
    d3 = dst.rearrange("p (h d) -> p h d", h=H)
    x1 = s3[:, :, 0:hf]; x2 = s3[:, :, hf:dh]
    cosB = cs[:, 0:1, :].to_broadcast([128, H, hf])
    sinB = cs[:, 1:2, :].to_broadcast([128, H, hf])
    a = t1[:, 0:H * hf].rearrange("p (h d) -> p h d", h=H)
    b = t2[:, 0:H * hf].rearrange("p (h d) -> p h d", h=H)
    k1, k2 = (tk, 1), (tk, 2)
    P.op("dve", _tt(a, x1, cosB, ALU.mult), reads=rkeys, writes=[k1])
    P.op("dve", _tt(b, x2, sinB, ALU.mult), reads=rkeys, writes=[k2])
    P.op("dve", _tt(d3[:, :, 0:hf], a, b, ALU.subtract), reads=[k1, k2], writes=[wkey])
    P.op("dve", _tt(a, x2, cosB, ALU.mult), reads=rkeys, writes=[k1])
    P.op("dve", _tt(b, x1, sinB, ALU.mult), reads=rkeys, writes=[k2])
    P.op("dve", _tt(d3[:, :, hf:dh], a, b, ALU.add), reads=[k1, k2, wkey], writes=[wkey])


def _transpose_blocks(C, src, nblk, dst3, rkeys, wkey, eng_toggle=0):
    P = C.P
    b0 = 0
    g = 0
    while b0 < nblk:
        n = min(4, nblk - b0)
        bi = (g + eng_toggle) % 2
        bank = C.pb[bi]; bk = f"pb{bi}"
        for i in range(n):
            P.op("pe", _tr(bank[:, i * 128:(i + 1) * 128], src[:, (b0 + i) * 128:(b0 + i + 1) * 128], C.ident[:]), reads=list(rkeys) + ["ident"], writes=[bk])
        srcp = bank[:, 0:n * 128].rearrange("p (i k) -> p i k", i=n)
        if bi == 0:
            P.op("act", _acp(dst3[:, b0:b0 + n, :], srcp), reads=[bk], writes=[wkey])
        else:
            P.op("dve", _cp(dst3[:, b0:b0 + n, :], srcp), reads=[bk], writes=[wkey])
        b0 += n
        g += 1


RET_G = [1.0 - 2.0 ** (-5.0 - h) for h in range(4)]


def mixer_ret(C, proj, pkv, mix, cst, cs_key="cs_own", s_in=None, s_out=None):
    P = C.P
    P.stage = C.pfx + "ret"
    with ExitStack() as st:
        decT = _sb(C, st, "decT", [128, 512]); xi = _sb(C, st, "xi", [128, 4]); zeta = _sb(C, st, "zeta", [128, 4]); gnB = _sb(C, st, "gnB", [128, 1024])
        P.dma("sp", _dma(decT[:], cst["decT"][:, :]), "decT", writes=["decT"])
        P.dma("sp", _dma(xi[:], cst["xi"][:, :]), "xi", writes=["xi"])
        P.dma("sp", _dma(zeta[:], cst["zeta"][:, :]), "zeta", writes=["zeta"])
        P.dma("sp", _dma(gnB[:], cst["gnB"][:, :]), "gnB", writes=["gnB"])
        S32 = _sb(C, st, "S32", [128, 4, 512]); Sb = _sb(C, st, "Sb", [128, 4, 512], BF16)
        if s_in is None:
            P.op("dve", _mset(S32[:], 0.0), writes=["S32"])
            P.op("dve", _mset(Sb[:], 0.0), writes=["Sb"])
        else:
            P.dma("sp", _dma(S32[:].rearrange("p h n -> p (h n)"), s_in[:, :]), "S32io", writes=["S32"])
            P.op("act", _acp(Sb[:], S32[:]), reads=["S32"], writes=["Sb"])
        qkvg = [_sb(C, st, "qkvg", [128, 4096]) for _ in range(2)]
        csb = [_sb(C, st, "csb", [128, 2, 128]) for _ in range(2)]
        qr = _sb(C, st, "qr", [128, 1024]); kr = _sb(C, st, "kr", [128, 1024]); qx = _sb(C, st, "qx", [128, 1024])
        kz = _sb(C, st, "kz", [128, 1024], BF16); vb = _sb(C, st, "vb", [128, 1024], BF16)
        qT = _sb(C, st, "qT", [128, 8, 128], BF16); kT = _sb(C, st, "kT", [128, 8, 128], BF16); qxT = _sb(C, st, "qxT", [128, 8, 128], BF16)
        am = _sb(C, st, "am", [128, 512], BF16)
        t1 = _sb(C, st, "rt1", [128, 512]); t2 = _sb(C, st, "rt2", [128, 512])
        junk = _sb(C, st, "rjunk", [128, 256])
        st4 = {n: _sb(C, st, "st" + n, [128, 4]) for n in ["s", "q", "m", "r", "nb"]}
        yn = _sb(C, st, "yn", [128, 1024]); sg = _sb(C, st, "sgt", [128, 1024])
        ret = [_sb(C, st, "ret", [128, 1024]) for _ in range(2)]
        SB = [C.pb[5], C.pb[6], C.pb[7], C.pb[2]]; SBK = ["pb5", "pb6", "pb7", "pb2"]
        kz2 = [kz, _sb(C, st, "kz2", [128, 1024], BF16)]; vb2 = [vb, _sb(C, st, "vb2", [128, 1024], BF16)]
        qT2_ = [qT, _sb(C, st, "qT2", [128, 8, 128], BF16)]; kT2_ = [kT, _sb(C, st, "kT2", [128, 8, 128], BF16)]; qxT2_ = [qxT, _sb(C, st, "qxT2", [128, 8, 128], BF16)]

        def front(ci):
            own = ci >= 16
            t = ci % 16; i = ci % 2
            buf = qkvg[i]; bkey = ("qkvg", i)
            cs = csb[i]; ckey = ("csb", i)
            if own:
                P.dma("sp", _dma(buf[:], proj[t * 128:(t + 1) * 128, 0:4096]), bkey, writes=[bkey])
                P.dma("sp", _dma(cs[:], cst[cs_key][t * 128:(t + 1) * 128, :, :]), ckey, writes=[ckey])
            else:
                P.dma("sp", _dma(buf[:, 1024:3072], pkv[t * 128:(t + 1) * 128, :]), bkey, writes=[bkey])
                P.dma("sp", _dma(cs[:], cst["cs_pre"][t * 128:(t + 1) * 128, :, :]), ckey, writes=[ckey])
            _rope(P, buf[:, 1024:2048], kr[:], cs, 4, 256, t1, t2, [bkey, ckey], "kr", "rt")
            P.op("dve", _tt(kz2[i][:].rearrange("p (h d) -> p h d", h=4), kr[:].rearrange("p (h d) -> p h d", h=4), zeta[:, :, None].to_broadcast([128, 4, 256]), ALU.mult), reads=["kr", "zeta"], writes=[("kz", i)])
            P.op("act", _acp(vb2[i][:], buf[:, 2048:3072]), reads=[bkey], writes=[("vb", i)])
            if own:
                _rope(P, buf[:, 0:1024], qr[:], cs, 4, 256, t1, t2, [bkey, ckey], "qr", "rt")
                P.op("dve", _tt(qx[:].rearrange("p (h d) -> p h d", h=4), qr[:].rearrange("p (h d) -> p h d", h=4), xi[:, :, None].to_broadcast([128, 4, 256]), ALU.mult), reads=["qr", "xi"], writes=["qx"])
                _transpose_blocks(C, qr, 8, qT2_[i], ["qr"], ("qT", i), 0)
                _transpose_blocks(C, kr, 8, kT2_[i], ["kr"], ("kT", i), 0)
                _transpose_blocks(C, qx, 8, qxT2_[i], ["qx"], ("qxT", i), 0)

        def back(ci):
            own = ci >= 16
            t = ci % 16; i = ci % 2
            buf = qkvg[i]; bkey = ("qkvg", i)
            kzi, vbi, qTi, kTi, qxTi = kz2[i], vb2[i], qT2_[i], kT2_[i], qxT2_[i]
            kzk, vbk, qTk, kTk, qxTk = ("kz", i), ("vb", i), ("qT", i), ("kT", i), ("qxT", i)
            if own:
                for h in range(4):
                    for j in range(2):
                        P.op("pe", _mm(C.pb[2][:, h * 128:(h + 1) * 128], kTi[:, 2 * h + j, :], qTi[:, 2 * h + j, :], j == 0, j == 1), reads=[kTk, qTk], writes=["pb2"])
                P.op("dve", _tt(am[:], C.pb[2][:, :], decT[:], ALU.mult), reads=["pb2", "decT"], writes=["am", "pb2"])
                for h in range(4):
                    yb = C.pb[3 + h // 2]; ybk = f"pb{3 + h // 2}"
                    o = yb[:, (h % 2) * 256:(h % 2) * 256 + 256]
                    P.op("pe", _mm(o, am[:, h * 128:(h + 1) * 128], vbi[:, h * 256:(h + 1) * 256], True, False), reads=["am", vbk], writes=[ybk])
                    P.op("pe", _mm(o, qxTi[:, 2 * h, :], Sb[:, h, 0:256], False, False), reads=[qxTk, "Sb"], writes=[ybk])
                    P.op("pe", _mm(o, qxTi[:, 2 * h + 1, :], Sb[:, h, 256:512], False, True), reads=[qxTk, "Sb"], writes=[ybk])
            for h in range(4):
                for j in range(2):
                    P.op("pe", _mm(SB[h][:, j * 256:(j + 1) * 256], kzi[:, h * 256 + j * 128:h * 256 + (j + 1) * 128], vbi[:, h * 256:(h + 1) * 256], True, True), reads=[kzk, vbk], writes=[SBK[h]])
            for h in range(4):
                P.op("dve", _stt(S32[:, h, :], S32[:, h, :], RET_G[h] ** 128, SB[h][:, :], ALU.mult, ALU.add), reads=[SBK[h], "S32"], writes=["S32", SBK[h]])
            P.op("act", _acp(Sb[:], S32[:]), reads=["S32"], writes=["Sb"])
            if not own:
                return
            s_, q_, m_, r_, nb_ = st4["s"], st4["q"], st4["m"], st4["r"], st4["nb"]
            for h in range(4):
                yb = C.pb[3 + h // 2]; ybk = f"pb{3 + h // 2}"
                o = yb[:, (h % 2) * 256:(h % 2) * 256 + 256]
                P.op("act", _act(junk[:], o, AF.Identity, accum=s_[:, h:h + 1]), reads=[ybk], writes=["rjunk", "st_s"])
                P.op("act", _act(junk[:], o, AF.Square, accum=q_[:, h:h + 1]), reads=[ybk], writes=["rjunk", "st_q"])
            P.op("dve", _ts(m_[:], s_[:], 1.0 / 256), reads=["st_s"], writes=["st_m"])
            P.op("dve", _tt(r_[:], m_[:], m_[:], ALU.mult), reads=["st_m"], writes=["st_r"])
            P.op("dve", _stt(r_[:], q_[:], 1.0 / 256, r_[:], ALU.mult, ALU.subtract), reads=["st_q", "st_r"], writes=["st_r"])
            P.op("dve", _ts(r_[:], r_[:], EPS, None, ALU.add), reads=["st_r"], writes=["st_r"])
            P.op("act", _sqrt(r_[:], r_[:]), reads=["st_r"], writes=["st_r"])
            P.op("dve", _rcp(r_[:], r_[:]), reads=["st_r"], writes=["st_r"])
            P.op("dve", _stt(nb_[:], m_[:], -1.0, r_[:], ALU.mult, ALU.mult), reads=["st_m", "st_r"], writes=["st_nb"])
            for h in range(4):
                yb = C.pb[3 + h // 2]; ybk = f"pb{3 + h // 2}"
                o = yb[:, (h % 2) * 256:(h % 2) * 256 + 256]
                P.op("act", _act(yn[:, h * 256:(h + 1) * 256], o, AF.Identity, bias=nb_[:, h:h + 1], scale=r_[:, h:h + 1]), reads=[ybk, "st_r", "st_nb"], writes=["yn", ybk])
            P.op("act", _act(sg[:], buf[:, 3072:4096], AF.Silu), reads=[bkey], writes=["sgt"])
            P.op("dve", _tt(yn[:], yn[:], gnB[:], ALU.mult), reads=["yn", "gnB"], writes=["yn"])
            rt = ret[t % 2]; rk = ("ret", t % 2)
            P.op("dve", _tt(rt[:], yn[:], sg[:], ALU.mult), reads=["yn", "sgt"], writes=[rk])
            P.dma("sp", _dma(mix[t * 128:(t + 1) * 128, 0:1024], rt[:]), rk, reads=[rk], writes=[("mixr", t)])

        cis = list(range(0 if pkv is not None else 16, 32))
        front(cis[0])
        for n_, ci in enumerate(cis):
            if n_ + 1 < len(cis):
                front(cis[n_ + 1])
            back(ci)
        if s_out is not None:
            P.dma("sp", _dma(s_out[:, :], S32[:].rearrange("p h n -> p (h n)")), "S32io", reads=["S32"], writes=["s_out"])
    P.barrier()


def mixer_conv(C, proj, phalo, mix, cst):
    P = C.P
    P.stage = C.pfx + "conv"
    NTK = T + 128
    with ExitStack() as st:
        cw = _sb(C, st, "cw", [128, 8, 31]); cv = _sb(C, st, "cv", [128, 3, 8])
        P.dma("sp", _dma(cw[:], cst["conv_w"][:, :, :]), "cw", writes=["cw"])
        P.dma("sp", _dma(cv[:], cst["conv_v"][:, :, :]), "cv", writes=["cv"])
        uT = _sb(C, st, "uT", [128, 8, NTK])
        yT = _sb(C, st, "yT", [128, 8, T])
        gg = [_sb(C, st, "gg", [128, 2048]) for _ in range(2)]
        sig = _sb(C, st, "sig", [128, 1024]); u = _sb(C, st, "u", [128, 1024])
        for ti in range(17):
            buf = gg[ti % 2]; bkey = ("gg", ti % 2)
            if ti == 0:
                P.dma("sp", _dma(buf[:], phalo[:, :]), bkey, writes=[bkey])
            else:
                P.dma("sp", _dma(buf[:], proj[(ti - 1) * 128:ti * 128, 4096:6144]), bkey, writes=[bkey])
            P.op("act", _act(sig[:], buf[:, 1024:2048], AF.Sigmoid), reads=[bkey], writes=["sig"])
            P.op("dve", _tt(u[:], buf[:, 0:1024], sig[:], ALU.mult), reads=[bkey, "sig"], writes=["u"])
            _transpose_blocks(C, u, 8, uT[:, :, ti * 128:(ti + 1) * 128], ["u"], "uT", ti)
        ptmp = _sb(C, st, "cptmp", [128, T])
        pool_chunks = ()
        for k in range(31):
            for j in range(8):
                yk = ("yT", j)
                src_k = uT[:, j, 98 + k:98 + k + T]
                if k == 0:
                    eng = "pool" if j in pool_chunks else "dve"
                    P.op(eng, _ts(yT[:, j, :], src_k, cw[:, j, 0:1], cv[:, 0, j:j + 1], ALU.mult, ALU.add), reads=["uT", "cw", "cv"], writes=[yk])
                elif j in pool_chunks:
                    P.op("pool", _ts(ptmp[:], src_k, cw[:, j, k:k + 1]), reads=["uT", "cw"], writes=["cptmp"])
                    P.op("pool", _tt(yT[:, j, :], yT[:, j, :], ptmp[:], ALU.add), reads=["cptmp", yk], writes=[yk])
                else:
                    P.op("dve", _stt(yT[:, j, :], src_k, cw[:, j, k:k + 1], yT[:, j, :], ALU.mult, ALU.add), reads=["uT", "cw", yk], writes=[yk])
        mean = _sb(C, st, "cmean", [128, 512]); rstd = _sb(C, st, "crstd", [128, 512]); sq = [_sb(C, st, "csq", [128, 512]) for _ in range(2)]
        z = [_sb(C, st, "cz", [128, 512]) for _ in range(2)]
        for tb in range(4):
            sl = slice(tb * 512, (tb + 1) * 512)
            for j in range(8):
                P.op("pe", _mm(C.pb[2][:, :], C.ones[:], yT[:, j, sl], j == 0, j == 7), reads=[("yT", j), "ones"], writes=["pb2"])
            for j in range(8):
                sqt = sq[j % 2]; sqk = ("csq", j % 2)
                P.op("act", _act(sqt[:], yT[:, j, sl], AF.Square), reads=[("yT", j)], writes=[sqk])
                P.op("pe", _mm(C.pb[3][:, :], C.ones[:], sqt[:], j == 0, j == 7), reads=[sqk, "ones"], writes=["pb3"])
            P.op("dve", _ts(mean[:], C.pb[2][:, :], 1.0 / 1024), reads=["pb2"], writes=["cmean"])
            P.op("dve", _tt(rstd[:], mean[:], mean[:], ALU.mult), reads=["cmean"], writes=["crstd"])
            P.op("dve", _stt(rstd[:], C.pb[3][:, :], 1.0 / 1024, rstd[:], ALU.mult, ALU.subtract), reads=["pb3", "crstd"], writes=["crstd"])
            P.op("dve", _ts(rstd[:], rstd[:], EPS, None, ALU.add), reads=["crstd"], writes=["crstd"])
            P.op("act", _sqrt(rstd[:], rstd[:]), reads=["crstd"], writes=["crstd"])
            P.op("dve", _rcp(rstd[:], rstd[:]), reads=["crstd"], writes=["crstd"])
            for j in range(8):
                zt = z[j % 2]; zk = ("cz", j % 2)
                P.op("dve", _tt(zt[:], yT[:, j, sl], mean[:], ALU.subtract), reads=[("yT", j), "cmean"], writes=[zk])
                P.op("dve", _tt(zt[:], zt[:], rstd[:], ALU.mult), reads=[zk, "crstd"], writes=[zk])
                P.op("act", _act(yT[:, j, sl], zt[:], AF.Silu, bias=cv[:, 2, j:j + 1], scale=cv[:, 1, j:j + 1]), reads=[zk, "cv"], writes=[("yT", j)])
        co = [_sb(C, st, "co", [128, 1024]) for _ in range(2)]
        for t in range(16):
            cot = co[t % 2]; ck = ("co", t % 2)
            for g in range(2):
                bank = C.pb[g]; bk = f"pb{g}"
                for i in range(4):
                    j = g * 4 + i
                    P.op("pe", _tr(bank[:, i * 128:(i + 1) * 128], yT[:, j, t * 128:(t + 1) * 128], C.ident[:]), reads=[("yT", j), "ident"], writes=[bk])
                if g == 0:
                    P.op("act", _acp(cot[:, 0:512], bank[:, :]), reads=[bk], writes=[ck])
                else:
                    P.op("dve", _cp(cot[:, 512:1024], bank[:, :]), reads=[bk], writes=[ck])
            P.dma("sp", _dma(mix[t * 128:(t + 1) * 128, 1024:2048], cot[:]), ck, reads=[ck], writes=[("mixc", t)])
    P.barrier()


def _bcast_rows(v):
    v = np.asarray(v, np.float32)
    return np.ascontiguousarray(np.broadcast_to(v[None, :], (128, v.shape[0])))


def _rope_tab(pos, half):
    inv = (10000.0 ** (-np.arange(half, dtype=np.float32) / np.float32(half))).astype(np.float32)
    ang = pos.astype(np.float32)[:, None] * inv[None, :]
    return np.ascontiguousarray(np.stack([np.cos(ang), np.sin(ang)], axis=1).astype(np.float32))


def _common_tail(C, x, mix, w_out, g1, b1, g2, b2, w_gu, w_down, rw, rbB, m, ha, f, out):
    linear_ln(C, mix, w_out, x, g1, b1, ha, "mix")
    moe(C, ha, w_gu, w_down, rw, rbB, f, "moe", ln=(g2, b2, out))


def build_layer0(debug=False):
    nc = bass.Bass("TRN2", target_bir_lowering=False)
    dt = lambda name, shape, kind="ExternalInput": nc.dram_tensor(name, shape, F32, kind=kind).ap()
    x = dt("x", [T, D]); xp = dt("xp", [T, D]); w_in = dt("w_in", [D, 6144]); w_out = dt("w_out", [D, D])
    g1 = dt("mix_g", [128, D]); b1 = dt("mix_b", [128, D]); g2 = dt("ffn_g", [128, D]); b2 = dt("ffn_b", [128, D])
    w_gu = dt("w_gu", [16, D, 1024]); w_down = dt("w_down", [16, 512, D])
    rw = dt("rw", [D, 16]); rbB = dt("rbB", [128, 16]); ident = dt("ident", [128, 128])
    cst = {"cs_own": dt("cs_own", [T, 2, 128]), "cs_pre": dt("cs_pre", [T, 2, 128]), "decT": dt("decT", [128, 512]),
           "xi": dt("xi", [128, 4]), "zeta": dt("zeta", [128, 4]), "gnB": dt("gnB", [128, 1024]),
           "conv_w": dt("conv_w", [128, 8, 31]), "conv_v": dt("conv_v", [128, 3, 8])}
    out = dt("out", [T, D], "ExternalOutput")
    dk = "ExternalOutput" if debug else "Internal"
    proj = dt("proj", [T, 6144], "Internal"); pkv = dt("pkv", [T, 2048], "Internal"); phalo = dt("phalo", [128, 2048], "Internal")
    mix = dt("mix", [T, D], dk); m = dt("m", [T, D], dk)
    ha = dt("ha", [T, D], dk); f = dt("f", [T, D], "Internal")
    with ExitStack() as es:
        C = _mk_ctx(nc, es)
        _load_consts(C, ident)
        linear(C, x, T, w_in, [(0, 6144)], proj, "in")
        linear(C, xp, T, w_in, [(1024, 3072)], pkv, "pkv")
        linear(C, xp[T - 128:T, :], 128, w_in, [(4096, 6144)], phalo, "ph")
        mixer_ret(C, proj, pkv, mix, cst)
        mixer_conv(C, proj, phalo, mix, cst)
        _common_tail(C, x, mix, w_out, g1, b1, g2, b2, w_gu, w_down, rw, rbB, m, ha, f, out)
        C.P.emit()
    return nc


def layer0_inputs(inp, h, core):
    b, half = core // 2, core % 2
    x = h[b, half * T:(half + 1) * T]
    xp = h[b, 0:T] if half == 1 else np.zeros((T, D), np.float32)
    g = np.array(RET_G, np.float64)
    j = np.arange(128, dtype=np.float64)
    diff = j[None, :] - j[:, None]
    decT = np.concatenate([np.where(diff >= 0, g[h_] ** np.maximum(diff, 0), 0.0) / 16.0 for h_ in range(4)], axis=1)
    xi = np.stack([g[h_] ** (j + 1.0) for h_ in range(4)], axis=1)
    zeta = np.stack([g[h_] ** (127.0 - j) / 16.0 for h_ in range(4)], axis=1)
    conv_w = np.asarray(inp["even_conv_w"][0], np.float32)
    cvec = np.stack([inp["even_conv_b"][0], inp["even_conv_ln_g"][0], inp["even_conv_ln_b"][0]], axis=0).astype(np.float32)
    return {
        "x": np.ascontiguousarray(x), "xp": np.ascontiguousarray(xp),
        "w_in": np.asarray(inp["even_w_in"][0], np.float32), "w_out": np.asarray(inp["even_w_out"][0], np.float32),
        "mix_g": _bcast_rows(inp["mix_ln_g"][0]), "mix_b": _bcast_rows(inp["mix_ln_b"][0]),
        "ffn_g": _bcast_rows(inp["ffn_ln_g"][0]), "ffn_b": _bcast_rows(inp["ffn_ln_b"][0]),
        "w_gu": np.asarray(inp["moe_w_gu"][0], np.float32), "w_down": np.asarray(inp["moe_w_down"][0], np.float32),
        "rw": np.asarray(inp["router_w"], np.float32), "rbB": _bcast_rows(inp["router_b"]), "ident": np.eye(128, dtype=np.float32),
        "cs_own": _rope_tab(np.arange(half * T, (half + 1) * T), 128), "cs_pre": _rope_tab(np.arange(0, T), 128),
        "decT": np.ascontiguousarray(decT.astype(np.float32)), "xi": np.ascontiguousarray(xi.astype(np.float32)),
        "zeta": np.ascontiguousarray(zeta.astype(np.float32)), "gnB": _bcast_rows(inp["even_ret_gn_g"][0]),
        "conv_w": np.ascontiguousarray(conv_w.T.reshape(8, 128, 31).transpose(1, 0, 2)),
        "conv_v": np.ascontiguousarray(cvec.reshape(3, 8, 128).transpose(2, 0, 1)),
    }


def mixer_dsa(C, qproj, kvproj, attn_out, cst):
    P = C.P
    P.stage = C.pfx + "dsa_kprep"
    SC = 128.0 ** -0.5
    with ExitStack() as st:
        kT = _sb(C, st, "dkT", [128, 4, 4096], BF16)
        vb = _sb(C, st, "dvb", [128, 32, 4, 129], BF16)
        kiT2 = _sb(C, st, "dkiT", [128, 1, 4096], BF16)
        iota = _sb(C, st, "diota", [128, 4096])
        qpos = _sb(C, st, "dqpos", [128, 16])
        P.dma("sp", _dma(iota[:], cst["iota"][:, :]), "diota", writes=["iota"])
        P.dma("sp", _dma(qpos[:], cst["qpos"][:, :]), "dqpos", writes=["qpos"])
        P.op("dve", _mset(vb[:], 1.0), writes=["vb"])
        with ExitStack() as s1:
            kvt = [_sb(C, s1, "kvt", [128, 1088]) for _ in range(2)]
            csk = [_sb(C, s1, "csk", [128, 2, 64]) for _ in range(2)]
            csi = [_sb(C, s1, "csi", [128, 2, 32]) for _ in range(2)]
            kr = _sb(C, s1, "dkr", [128, 512]); kir2 = _sb(C, s1, "dkir", [128, 128])
            t1 = _sb(C, s1, "dt1", [128, 256]); t2 = _sb(C, s1, "dt2", [128, 256])
            for kt in range(32):
                i = kt % 2
                bk_, ck_, ik_ = ("kvt", i), ("csk", i), ("csi", i)
                rows = slice(kt * 128, (kt + 1) * 128)
                P.dma("sp", _dma(kvt[i][:], kvproj[rows, :]), bk_, writes=[bk_])
                P.dma("sp", _dma(csk[i][:], cst["cs_k"][rows, :, :]), ck_, writes=[ck_])
                P.dma("sp", _dma(csi[i][:], cst["cs_ki"][rows, :, :]), ik_, writes=[ik_])
                _rope(P, kvt[i][:, 0:512], kr[:], csk[i], 4, 128, t1, t2, [bk_, ck_], "dkr", "dt")
                _transpose_blocks(C, kr, 4, kT[:, :, kt * 128:(kt + 1) * 128], ["dkr"], "kT", kt)
                P.op("act", _acp(vb[:, kt, :, 0:128], kvt[i][:, 512:1024].rearrange("p (h d) -> p h d", h=4)), reads=[bk_], writes=["vb"])
                _rope(P, kvt[i][:, 1024:1088], kir2[:, 0:64], csi[i], 1, 64, t1, t2, [bk_, ik_], "dkir", "dt")
                P.op("dve", _cp(kir2[:, 64:128], kir2[:, 0:64]), reads=["dkir"], writes=["dkir"])
                _transpose_blocks(C, kir2, 1, kiT2[:, :, kt * 128:(kt + 1) * 128], ["dkir"], "kiT", kt + 1)
        P.barrier()
        P.stage = C.pfx + "dsa_q"
        qt = _sb(C, st, "dqt", [128, 3088])
        csq = [_sb(C, st, "csq", [128, 2, 64]) for _ in range(2)]
        csqi = [_sb(C, st, "csqi", [128, 2, 32]) for _ in range(2)]
        qr = _sb(C, st, "dqr", [128, 2048]); qir = _sb(C, st, "dqir", [128, 1024])
        qT = [_sb(C, st, "dqT", [128, 16, 128], BF16) for _ in range(2)]
        qiT = _sb(C, st, "dqiT", [128, 8, 128], BF16)
        wab = _sb(C, st, "dwab", [128, 16]); sgn = _sb(C, st, "dsgn", [128, 16])
        t1 = _sb(C, st, "dq1", [128, 1024]); t2 = _sb(C, st, "dq2", [128, 1024])
        acc = _sb(C, st, "dacc", [128, 4096]); scr = _sb(C, st, "dscr", [128, 4096])
        tmp = [_sb(C, st, "dtmp", [128, 1024]) for _ in range(2)]
        selT = [_sb(C, st, "dselT", [128, 32, 128], BF16) for _ in range(2)]
        pt = [_sb(C, st, "dp", [128, 1024], BF16) for _ in range(2)]
        obuf = _sb(C, st, "dobuf", [128, 16, 129])
        rec = _sb(C, st, "drec", [128, 16])
        sm = {n: _sb(C, st, "d_" + n, [128, 1]) for n in ["lo", "hi", "w", "mid", "cnt", "ge"]}
        NIT = 26
        hw = _sb(C, st, "d_hw", [128, NIT + 1]); pw2 = _sb(C, st, "d_pw2", [128, NIT + 1])
        for k_ in range(NIT + 1):
            P.op("dve", _mset(pw2[:, k_:k_ + 1], 2.0 ** -(k_ + 1)), writes=["pw2"])
        identb = _sb(C, st, "didb", [128, 128], BF16)
        P.op("dve", _cp(identb[:], C.ident[:]), reads=["ident"], writes=["identb"])
        cnts = {"it": 0, "ig": 0}

        def NN(j):
            return 2048 + 128 * (j + 1)

        def phaseA(j):
            N = NN(j); i = j % 2
            rows = slice(j * 128, (j + 1) * 128)
            qTk = ("qT", i)
            P.dma("sp", _dma(qt[:], qproj[rows, :]), "dqt", writes=["dqt"])
            P.dma("sp", _dma(csq[i][:], cst["cs_q"][rows, :, :]), ("csq", i), writes=[("csq", i)])
            P.dma("sp", _dma(csqi[i][:], cst["cs_qi"][rows, :, :]), ("csqi", i), writes=[("csqi", i)])
            _rope(P, qt[:, 0:2048], qr[:], csq[i], 16, 128, t1, t2, ["dqt", ("csq", i)], "dqr", "dq")
            _transpose_blocks(C, qr, 16, qT[i], ["dqr"], qTk, 0)
            _rope(P, qt[:, 2048:3072], qir[:], csqi[i], 16, 64, t1, t2, ["dqt", ("csqi", i)], "dqir", "dq")
            _transpose_blocks(C, qir, 8, qiT, ["dqir"], "qiT", 0)
            P.op("dve", _ts(sgn[:], qt[:, 3072:3088], 0.0, 2.0, ALU.is_ge, ALU.mult), reads=["dqt"], writes=["sgn"])
            P.op("dve", _ts(sgn[:], sgn[:], -1.0, None, ALU.add), reads=["sgn"], writes=["sgn"])
            P.op("dve", _tt(wab[:], qt[:, 3072:3088], sgn[:], ALU.mult), reads=["dqt", "sgn"], writes=["wab"])
            P.op("dve", _ts(wab[:], wab[:], 0.03125), reads=["wab"], writes=["wab"])
            P.op("dve", _ts(acc[:, 0:N], iota[:, 0:N], qpos[:, j:j + 1], NEG, ALU.is_gt, ALU.mult), reads=["iota", "qpos"], writes=["acc"])
            for h in range(16):
                p0 = (h % 2) * 64
                for g0 in range(0, N, 1024):
                    w = min(1024, N - g0)
                    ig = cnts["ig"]
                    base = (ig % 4) * 2
                    nb_ = (w + 511) // 512
                    bkeys = [f"pb{base + c}" for c in range(nb_)]
                    for c4 in range(nb_):
                        ww = min(512, w - c4 * 512)
                        P.op("pe", _mm(C.pb[base + c4][:, 0:ww], qiT[p0:p0 + 64, h // 2, :], kiT2[p0:p0 + 64, 0, g0 + c4 * 512:g0 + c4 * 512 + ww], True, True),
                             reads=["qiT", "kiT"], writes=[bkeys[c4]])
                    tm = tmp[ig % 2]; tk = ("dtmp", ig % 2)
                    P.op("act", _act(tm[:, 0:w], C.ps_all[:, base * 512:base * 512 + w], AF.Relu, scale=wab[:, h:h + 1]), reads=bkeys + ["wab"], writes=[tk] + bkeys)
                    P.op("dve", _stt(acc[:, g0:g0 + w], tm[:, 0:w], sgn[:, h:h + 1], acc[:, g0:g0 + w], ALU.mult, ALU.add), reads=[tk, "sgn", "acc"], writes=["acc"])
                    cnts["ig"] += 1

        def phaseB(j):
            N = NN(j); NB = N // 128; i = j % 2
            lo, hi, wd, mid, cnt, ge = (sm[n] for n in ["lo", "hi", "w", "mid", "cnt", "ge"])
            P.op("dve", _red(hi[:], acc[:, 0:N], ALU.max), reads=["acc"], writes=["hi"])
            P.op("dve", _ts(scr[:, 0:N], iota[:, 0:N], qpos[:, j:j + 1], -2.0 * NEG, ALU.is_gt, ALU.mult), reads=["iota", "qpos"], writes=["scr"])
            P.op("dve", _tt(scr[:, 0:N], scr[:, 0:N], acc[:, 0:N], ALU.add), reads=["scr", "acc"], writes=["scr"])
            P.op("dve", _red(lo[:], scr[:, 0:N], ALU.min), reads=["scr"], writes=["lo"])
            P.op("dve", _tt(wd[:], hi[:], lo[:], ALU.subtract), reads=["hi", "lo"], writes=["w"])
            P.op("dve", _ts(hw[:], pw2[:], wd[:, 0:1]), reads=["w", "pw2"], writes=["hw"])
            for k in range(NIT):
                P.op("dve", _tt(mid[:], lo[:], hw[:, k:k + 1], ALU.add), reads=["lo", "hw"], writes=["mid"])
                P.op("dve", _ts(scr[:, 0:N], acc[:, 0:N], mid[:, 0:1], 0.0, ALU.is_ge, ALU.add, accum=cnt[:, 0:1]), reads=["acc", "mid"], writes=["scr", "cnt"])
                P.op("dve", _ts(ge[:], cnt[:], 255.5, hw[:, k:k + 1], ALU.is_ge, ALU.mult), reads=["cnt", "hw"], writes=["ge"])
                P.op("dve", _tt(lo[:], lo[:], ge[:], ALU.add), reads=["lo", "ge"], writes=["lo"])
            P.op("dve", _ts(scr[:, 0:N], acc[:, 0:N], lo[:, 0:1], -30000.0, ALU.is_lt, ALU.mult), reads=["acc", "lo"], writes=["scr"])
            _transpose_blocks(C, scr, NB, selT[i], ["scr"], ("selT", i), 0)

        def phaseCmain(j):
            N = NN(j); NB = N // 128; i = j % 2
            qT2 = qT[i][:].rearrange("p h q -> p (h q)")
            qTk, sTk = ("qT", i), ("selT", i)
            for kv in range(4):
                for kb in range(0, NB, 2):
                    nk = min(2, NB - kb)
                    it = cnts["it"]
                    base = (it % 2) * 2
                    Lks = [f"pb{base + b}" for b in range(nk)]
                    pp = pt[it % 2]; ppk = ("dp", it % 2)
                    for b in range(nk):
                        P.op("pe", _mm(C.pb[base + b][:, :], kT[:, kv, (kb + b) * 128:(kb + b + 1) * 128], qT2[:, kv * 512:(kv + 1) * 512], True, False), reads=["kT", qTk], writes=[Lks[b]])
                        for g in range(4):
                            P.op("pe", _mm(C.pb[base + b][:, g * 128:(g + 1) * 128], identb[:], selT[i][:, kb + b, :], False, g == 3), reads=["identb", sTk], writes=[Lks[b]])
                    P.op("act", _act(pp[:, 0:nk * 512], C.ps_all[:, base * 512:(base + nk) * 512], AF.Exp, scale=SC), reads=Lks, writes=[ppk] + Lks)
                    for b in range(nk):
                        for g in range(4):
                            P.op("pe", _mm(C.pb[4 + g][:, 0:129], pp[:, b * 512 + g * 128:b * 512 + (g + 1) * 128], vb[:, kb + b, kv, :], kb + b == 0, kb + b == NB - 1), reads=[ppk, "vb"], writes=[f"pb{4 + g}"])
                    cnts["it"] += 1
                for g in range(4):
                    P.op("act", _acp(obuf[:, kv * 4 + g, :], C.pb[4 + g][:, 0:129]), reads=[f"pb{4 + g}"], writes=["obuf", f"pb{4 + g}"])

        def phaseCfin(j):
            rows = slice(j * 128, (j + 1) * 128)
            P.op("dve", _rcp(rec[:], obuf[:, :, 128]), reads=["obuf"], writes=["rec"])
            P.op("dve", _tt(obuf[:, :, 0:128], obuf[:, :, 0:128], rec[:, :, None].to_broadcast([128, 16, 128]), ALU.mult), reads=["obuf", "rec"], writes=["obuf"])
            P.dma("sp", _dma(attn_out[rows, :].rearrange("p (h d) -> p h d", h=16), obuf[:, :, 0:128]), "dobuf", reads=["obuf"], writes=[("aout", j)])

        phaseA(0)
        phaseB(0)
        for j in range(16):
            if j + 1 < 16:
                phaseA(j + 1)
            phaseCmain(j)
            if j + 1 < 16:
                phaseB(j + 1)
            phaseCfin(j)
    P.barrier()


def build_layer1(debug=False):
    nc = bass.Bass("TRN2", target_bir_lowering=False)
    dt = lambda name, shape, kind="ExternalInput": nc.dram_tensor(name, shape, F32, kind=kind).ap()
    x = dt("x", [T, D]); xf = dt("xf", [2 * T, D]); w_in = dt("w_in", [D, 4176]); w_out = dt("w_out", [D, D])
    g1 = dt("mix_g", [128, D]); b1 = dt("mix_b", [128, D]); g2 = dt("ffn_g", [128, D]); b2 = dt("ffn_b", [128, D])
    w_gu = dt("w_gu", [16, D, 1024]); w_down = dt("w_down", [16, 512, D])
    rw = dt("rw", [D, 16]); rbB = dt("rbB", [128, 16]); ident = dt("ident", [128, 128])
    cst = {"cs_k": dt("cs_k", [2 * T, 2, 64]), "cs_ki": dt("cs_ki", [2 * T, 2, 32]), "cs_q": dt("cs_q", [T, 2, 64]), "cs_qi": dt("cs_qi", [T, 2, 32]),
           "iota": dt("iota", [128, 4096]), "qpos": dt("qpos", [128, 16])}
    out = dt("out", [T, D], "ExternalOutput")
    dk = "ExternalOutput" if debug else "Internal"
    qproj = dt("qproj", [T, 3088], "Internal"); kvproj = dt("kvproj", [2 * T, 1088], "Internal")
    mix = dt("mix", [T, D], dk); m = dt("m", [T, D], dk)
    ha = dt("ha", [T, D], dk); f = dt("f", [T, D], "Internal")
    with ExitStack() as es:
        C = _mk_ctx(nc, es)
        _load_consts(C, ident)
        linear(C, x, T, w_in, [(0, 2048), (3072, 4096), (4160, 4176)], qproj, "q1")
        linear(C, xf, 2 * T, w_in, [(2048, 3072), (4096, 4160)], kvproj, "kv1")
        mixer_dsa(C, qproj, kvproj, mix, cst)
        _common_tail(C, x, mix, w_out, g1, b1, g2, b2, w_gu, w_down, rw, rbB, m, ha, f, out)
        C.P.emit()
    return nc


def layer1_inputs(inp, h, core):
    b, half = core // 2, core % 2
    pos_own = np.arange(half * T, (half + 1) * T)
    qpos = (half * T + np.arange(16)[None, :] * 128 + np.arange(128)[:, None]).astype(np.float32)
    return {
        "x": np.ascontiguousarray(h[b, half * T:(half + 1) * T]), "xf": np.ascontiguousarray(h[b]),
        "w_in": np.asarray(inp["odd_w_in"][0], np.float32), "w_out": np.asarray(inp["odd_w_out"][0], np.float32),
        "mix_g": _bcast_rows(inp["mix_ln_g"][1]), "mix_b": _bcast_rows(inp["mix_ln_b"][1]),
        "ffn_g": _bcast_rows(inp["ffn_ln_g"][1]), "ffn_b": _bcast_rows(inp["ffn_ln_b"][1]),
        "w_gu": np.asarray(inp["moe_w_gu"][1], np.float32), "w_down": np.asarray(inp["moe_w_down"][1], np.float32),
        "rw": np.asarray(inp["router_w"], np.float32), "rbB": _bcast_rows(inp["router_b"]), "ident": np.eye(128, dtype=np.float32),
        "cs_k": _rope_tab(np.arange(2 * T), 64), "cs_ki": _rope_tab(np.arange(2 * T), 32),
        "cs_q": _rope_tab(pos_own, 64), "cs_qi": _rope_tab(pos_own, 32),
        "iota": np.ascontiguousarray(np.broadcast_to(np.arange(4096, dtype=np.float32)[None, :], (128, 4096))),
        "qpos": np.ascontiguousarray(qpos),
    }


def build_fused(profile=False):
    nc = bass.Bass("TRN2", target_bir_lowering=False)
    dt = lambda name, shape, kind="ExternalInput": nc.dram_tensor(name, shape, F32, kind=kind).ap()
    xA = dt("x", [T, D]); xB = dt("xp", [T, D]); zhalo = dt("zhalo", [128, 2048])
    rw = dt("rw", [D, 16]); rbB = dt("rbB", [128, 16]); ident = dt("ident", [128, 128])
    L = []
    for l in range(2):
        L.append({"w_in": dt(f"w_in{l}", [D, 6144 if l == 0 else 4176]), "w_out": dt(f"w_out{l}", [D, D]),
                  "g1": dt(f"mix_g{l}", [128, D]), "b1": dt(f"mix_b{l}", [128, D]), "g2": dt(f"ffn_g{l}", [128, D]), "b2": dt(f"ffn_b{l}", [128, D]),
                  "w_gu": dt(f"w_gu{l}", [16, D, 1024]), "w_down": dt(f"w_down{l}", [16, 512, D])})
    cst = {"cs_own": dt("cs_own", [T, 2, 128]), "cs_pre": dt("cs_pre", [T, 2, 128]), "decT": dt("decT", [128, 512]),
           "xi": dt("xi", [128, 4]), "zeta": dt("zeta", [128, 4]), "gnB": dt("gnB", [128, 1024]),
           "conv_w": dt("conv_w", [128, 8, 31]), "conv_v": dt("conv_v", [128, 3, 8]),
           "cs_k": dt("cs_k", [2 * T, 2, 64]), "cs_ki": dt("cs_ki", [2 * T, 2, 32]), "cs_q": dt("cs_q", [T, 2, 64]), "cs_qi": dt("cs_qi", [T, 2, 32]),
           "iota": dt("iota", [128, 4096]), "qpos": dt("qpos", [128, 16])}
    out = dt("out", [T, D], "ExternalOutput")
    projB = dt("projB", [T, 6144], "Internal"); projA = dt("projA", [T, 6144], "Internal")
    sstate = dt("sstate", [128, 2048], "Internal"); h0f = dt("h0f", [2 * T, D], "Internal")
    qproj = dt("qproj", [T, 3088], "Internal"); kvproj = dt("kvproj", [2 * T, 1088], "Internal")
    mix = dt("mix", [T, D], "Internal"); m = dt("m", [T, D], "Internal"); ha = dt("ha", [T, D], "Internal"); f = dt("f", [T, D], "Internal")
    with ExitStack() as es:
        C = _mk_ctx(nc, es)
        C.P.profile = profile
        _load_consts(C, ident)
        l0 = L[0]
        for (xx, proj, cs_key, s_in, s_out, halo, dst) in [(xB, projB, "cs_pre", None, sstate, zhalo, h0f[0:T, :]),
                                                            (xA, projA, "cs_own", sstate, None, projB[T - 128:T, 4096:6144], h0f[T:2 * T, :])]:
            C.pfx = "B_" if s_in is None else "A_"
            linear(C, xx, T, l0["w_in"], [(0, 6144)], proj, "in")
            mixer_ret(C, proj, None, mix, cst, cs_key=cs_key, s_in=s_in, s_out=s_out)
            mixer_conv(C, proj, halo, mix, cst)
            _common_tail(C, xx, mix, l0["w_out"], l0["g1"], l0["b1"], l0["g2"], l0["b2"], l0["w_gu"], l0["w_down"], rw, rbB, m, ha, f, dst)
        l1 = L[1]
        C.pfx = "L1_"
        x1 = h0f[T:2 * T, :]
        linear(C, x1, T, l1["w_in"], [(0, 2048), (3072, 4096), (4160, 4176)], qproj, "q1")
        linear(C, h0f, 2 * T, l1["w_in"], [(2048, 3072), (4096, 4160)], kvproj, "kv1")
        mixer_dsa(C, qproj, kvproj, mix, cst)
        _common_tail(C, x1, mix, l1["w_out"], l1["g1"], l1["b1"], l1["g2"], l1["b2"], l1["w_gu"], l1["w_down"], rw, rbB, m, ha, f, out)
        C.P.emit()
    return nc


def fused_inputs(inp, core):
    b, half = core // 2, core % 2
    x = np.asarray(inp["x"], np.float32)
    a = layer0_inputs(inp, x, core)
    r = {k: a[k] for k in ["x", "xp", "rw", "rbB", "ident", "cs_own", "cs_pre", "decT", "xi", "zeta", "gnB", "conv_w", "conv_v"]}
    r["zhalo"] = np.zeros((128, 2048), np.float32)
    for l, pre in enumerate(["even", "odd"]):
        r[f"w_in{l}"] = np.asarray(inp[pre + "_w_in"][0], np.float32); r[f"w_out{l}"] = np.asarray(inp[pre + "_w_out"][0], np.float32)
        r[f"mix_g{l}"] = _bcast_rows(inp["mix_ln_g"][l]); r[f"mix_b{l}"] = _bcast_rows(inp["mix_ln_b"][l])
        r[f"ffn_g{l}"] = _bcast_rows(inp["ffn_ln_g"][l]); r[f"ffn_b{l}"] = _bcast_rows(inp["ffn_ln_b"][l])
        r[f"w_gu{l}"] = np.asarray(inp["moe_w_gu"][l], np.float32); r[f"w_down{l}"] = np.asarray(inp["moe_w_down"][l], np.float32)
    posA = np.arange(half * T, (half + 1) * T)
    posB = np.arange(0, T)
    kp = np.concatenate([posB if half == 1 else np.full(T, 1.0e9), posA]).astype(np.float32)
    ropepos = np.concatenate([posB, posA])
    r["cs_k"] = _rope_tab(ropepos, 64); r["cs_ki"] = _rope_tab(ropepos, 32)
    r["cs_q"] = _rope_tab(posA, 64); r["cs_qi"] = _rope_tab(posA, 32)
    r["iota"] = np.ascontiguousarray(np.broadcast_to(kp[None, :], (128, 4096)))
    r["qpos"] = np.ascontiguousarray((half * T + np.arange(16)[None, :] * 128 + np.arange(128)[:, None]).astype(np.float32))
    return r


_NC_CACHE = {}


def kernel(**inputs):
    inp = {k: np.asarray(v) for k, v in inputs.items()}
    B = inp["x"].shape[0]
    if "fused" not in _NC_CACHE:
        _NC_CACHE["fused"] = build_fused()
    nc = _NC_CACHE["fused"]
    in_maps = [fused_inputs(inp, core) for core in range(8)]
    res = run_bass_kernel_spmd(nc, in_maps, core_ids=list(range(8)))
    h = np.stack([np.concatenate([res.results[2 * b]["out"], res.results[2 * b + 1]["out"]], axis=0) for b in range(B)], axis=0)
    return np.ascontiguousarray(h.astype(np.float32))
```

```python
import math
import numpy as np
from contextlib import ExitStack
import concourse.bass as bass
import concourse.mybir as mybir
from concourse.bass_utils import run_bass_kernel_spmd

F32 = mybir.dt.float32
BF16 = mybir.dt.bfloat16
AF = mybir.ActivationFunctionType
ALU = mybir.AluOpType
AX = mybir.AxisListType

ENGS = ("pe", "act", "dve", "pool", "sp")
D = 2048
T = 2048
NT = 16
ALPHA = 4.0 ** 0.25
EPS = 1e-5
NEG = -1.0e30


class Op:
    __slots__ = ("eng", "fn", "deps", "dma_slot", "dma_val", "signal", "sigval", "idx", "stage")


class Prog:
    def __init__(self, nc):
        self.nc = nc
        self.ops = []
        self.last_w = {}
        self.readers = {}
        self.slot_cnt = {}
        self.slot_last = {}
        self.last_on = {}
        self.stage = "init"
        self.profile = False

    def _rec(self, eng, fn, reads, writes, dma_slot=None, extra_deps=()):
        op = Op()
        op.eng = eng; op.fn = fn; op.dma_slot = dma_slot; op.signal = False; op.sigval = 0
        op.idx = len(self.ops)
        op.stage = self.stage
        deps = set(extra_deps)
        for k in reads:
            w = self.last_w.get(k)
            if w is not None:
                deps.add(w)
        for k in writes:
            w = self.last_w.get(k)
            if w is not None:
                deps.add(w)
            rd = self.readers.get(k)
            if rd:
                deps.update(rd[0].values())
                deps.update(rd[1])
        op.deps = deps
        if dma_slot is not None:
            c = self.slot_cnt.get(dma_slot, 0) + 1
            self.slot_cnt[dma_slot] = c
            op.dma_val = 16 * c
            self.slot_last[dma_slot] = op.idx
        else:
            op.dma_val = 0
            self.last_on[eng] = op.idx
        for k in reads:
            rd = self.readers.setdefault(k, ({}, []))
            if dma_slot is not None:
                rd[1].append(op.idx)
            else:
                rd[0][eng] = op.idx
        for k in writes:
            self.last_w[k] = op.idx
            self.readers[k] = ({}, [])
        self.ops.append(op)
        return op

    def op(self, eng, fn, reads=(), writes=()):
        return self._rec(eng, fn, reads, writes)

    def dma(self, eng, fn, slot, reads=(), writes=()):
        return self._rec(eng, fn, reads, writes, dma_slot=slot)

    def barrier(self):
        deps = set(self.last_on.values()) | set(self.slot_last.values())
        for e in ENGS:
            self._rec(e, None, (), (), extra_deps=deps)
        self.last_w = {}
        self.readers = {}

    def emit(self):
        nc = self.nc
        self.barrier()
        ops = self.ops
        for op in ops:
            for d in op.deps:
                p = ops[d]
                if p.dma_slot is None and p.fn is not None and (p.eng != op.eng or p.eng != "pe"):
                    p.signal = True
        cnt = {e: 0 for e in ENGS}
        for op in ops:
            if op.signal:
                cnt[op.eng] += 1
                op.sigval = cnt[op.eng]
        slots = list(self.slot_cnt.keys())
        EPOCH = 30000
        with ExitStack() as es:
            eng_sems = {}
            for e in ENGS:
                n_ep = cnt[e] // EPOCH + 1
                eng_sems[e] = [es.enter_context(nc.semaphore(f"s_{e}_{i}")) for i in range(n_ep)]
            slot_sem = {s: es.enter_context(nc.semaphore(f"d_{i}")) for i, s in enumerate(slots)}
            block = es.enter_context(nc.Block())

            def run_engine(ename, eng):
                waited = {}
                cur = None
                for op in ops:
                    if op.eng != ename:
                        continue
                    if self.profile and op.stage != cur:
                        if cur is not None:
                            nc.pop_named_scope(cur)
                        cur = op.stage
                        nc.push_named_scope(cur)
                    for d in sorted(op.deps):
                        p = ops[d]
                        if p.dma_slot is not None:
                            sem = slot_sem[p.dma_slot]; val = p.dma_val
                        else:
                            if p.eng == ename and ename == "pe":
                                continue
                            if p.fn is None:
                                continue
                            ep = (p.sigval - 1) // EPOCH
                            sem = eng_sems[p.eng][ep]; val = p.sigval - ep * EPOCH
                        if waited.get(sem.num, 0) >= val:
                            continue
                        waited[sem.num] = val
                        eng.wait_ge(sem, val)
                    if op.fn is None:
                        continue
                    ins = op.fn(eng)
                    if op.dma_slot is not None:
                        ins.then_inc(slot_sem[op.dma_slot], 16)
                    elif op.signal:
                        ep = (op.sigval - 1) // EPOCH
                        ins.then_inc(eng_sems[ename][ep], 1)
                if self.profile and cur is not None:
                    nc.pop_named_scope(cur)

            block.sync(lambda e: run_engine("sp", e))
            block.tensor(lambda e: run_engine("pe", e))
            block.scalar(lambda e: run_engine("act", e))
            block.vector(lambda e: run_engine("dve", e))
            block.gpsimd(lambda e: run_engine("pool", e))


class Ctx:
    pass


def _mk_ctx(nc, es):
    C = Ctx()
    C.nc = nc
    C.P = Prog(nc)
    C.es = es
    C.ps_all = es.enter_context(nc.psum_tensor("ps_all", [128, 4096], F32))
    C.pb = [C.ps_all[:, i * 512:(i + 1) * 512] for i in range(8)]
    C.uid = 0
    C.pfx = ""
    return C


def _sb(C, st, name, shape, dt=F32):
    C.uid += 1
    return st.enter_context(C.nc.sbuf_tensor(f"{name}_{C.uid}", shape, dt))


def _load_consts(C, ident_d):
    nc, P = C.nc, C.P
    C.ident = C.es.enter_context(nc.sbuf_tensor("ident_sb", [128, 128], F32))
    C.ones = C.es.enter_context(nc.sbuf_tensor("ones_sb", [128, 128], F32))
    P.dma("sp", lambda e: e.dma_start(out=C.ident[:], in_=ident_d[:, :]), "ident", writes=["ident"])
    P.op("dve", lambda e: e.memset(C.ones[:], 1.0), writes=["ones"])


def build_xT(C, st, src, n_tok, tag, f32_copy=None):
    nc, P = C.nc, C.P
    nt = n_tok // 128
    xT = _sb(C, st, "xT" + tag, [128, 16, n_tok], BF16)
    stg = [_sb(C, st, "xs" + tag, [128, D]) for _ in range(2)]
    for t in range(nt):
        s = stg[t % 2]
        sk = ("xs", tag, t % 2)
        P.dma("sp", (lambda s=s, t=t: lambda e: e.dma_start(out=s[:], in_=src[t * 128:(t + 1) * 128, :]))(), sk, writes=[sk])
        for q in range(4):
            bank = C.pb[q % 2]
            bk = f"pb{q % 2}"
            for i in range(4):
                c = q * 4 + i
                P.op("pe", (lambda s=s, c=c, bank=bank, i=i: lambda e: e.transpose(bank[:, i * 128:(i + 1) * 128], s[:, c * 128:(c + 1) * 128], C.ident[:]))(),
                     reads=[sk, "ident"], writes=[bk])
            eng = "act" if q % 2 == 0 else "dve"
            dst = xT[:, q * 4:(q + 1) * 4, t * 128:(t + 1) * 128]
            srcp = bank[:].rearrange("p (i k) -> p i k", i=4)
            if eng == "act":
                P.op("act", (lambda dst=dst, srcp=srcp: lambda e: e.copy(out=dst, in_=srcp))(), reads=[bk], writes=[("xT", tag)])
            else:
                P.op("dve", (lambda dst=dst, srcp=srcp: lambda e: e.tensor_copy(out=dst, in_=srcp))(), reads=[bk], writes=[("xT", tag)])
            if f32_copy is not None:
                dst2 = f32_copy[:, q * 4:(q + 1) * 4, t * 128:(t + 1) * 128]
                P.op("dve", (lambda dst2=dst2, srcp=srcp: lambda e: e.tensor_copy(out=dst2, in_=srcp))(), reads=[bk], writes=[("xT32", tag)])
    return xT


def linear(C, src, n_tok, w, col_ranges, dst, tag):
    nc, P = C.nc, C.P
    P.stage = C.pfx + "lin_" + tag
    nt = n_tok // 128
    with ExitStack() as st:
        xT = build_xT(C, st, src, n_tok, tag)
        wb = [_sb(C, st, "wb" + tag, [128, 16, 512], BF16) for _ in range(2)]
        ost = [_sb(C, st, "os" + tag, [128, 512]) for _ in range(4)]
        groups = []
        o = 0
        for (c0, c1) in col_ranges:
            c = c0
            while c < c1:
                n = min(512, c1 - c)
                groups.append((c, n, o))
                c += n; o += n
        wv = w.rearrange("(c p) n -> p c n", p=128)
        k = 0
        for gi, (c0, n, o0) in enumerate(groups):
            wt = wb[gi % 2]
            wk = ("wb", tag, gi % 2)
            P.dma("pool", (lambda wt=wt, c0=c0, n=n: lambda e: e.dma_start(out=wt[:, :, 0:n], in_=wv[:, :, c0:c0 + n]))(), wk, writes=[wk])
            for t in range(nt):
                bank = C.pb[2 + k % 4]; bk = f"pb{2 + k % 4}"
                for c in range(16):
                    P.op("pe", (lambda bank=bank, c=c, t=t, wt=wt, n=n: lambda e: e.matmul(bank[:, 0:n], lhsT=xT[:, c, t * 128:(t + 1) * 128], rhs=wt[:, c, 0:n], start=(c == 0), stop=(c == 15)))(),
                         reads=[("xT", tag), wk], writes=[bk])
                os_ = ost[k % 4]; ok = ("os", tag, k % 4)
                if k % 2 == 0:
                    P.op("act", (lambda os_=os_, bank=bank, n=n: lambda e: e.copy(out=os_[:, 0:n], in_=bank[:, 0:n]))(), reads=[bk], writes=[ok])
                else:
                    P.op("dve", (lambda os_=os_, bank=bank, n=n: lambda e: e.tensor_copy(out=os_[:, 0:n], in_=bank[:, 0:n]))(), reads=[bk], writes=[ok])
                P.dma("sp", (lambda os_=os_, t=t, o0=o0, n=n: lambda e: e.dma_start(out=dst[t * 128:(t + 1) * 128, o0:o0 + n], in_=os_[:, 0:n]))(), ok, reads=[ok], writes=[("dst", tag, t, gi)])
                k += 1
    P.barrier()


def resid_ln(C, x, m, gB_d, bB_d, out, n_tok, tag):
    nc, P = C.nc, C.P
    P.stage = C.pfx + "ln_" + tag
    nt = n_tok // 128
    with ExitStack() as st:
        gB = _sb(C, st, "gB", [128, D]); bB = _sb(C, st, "bB", [128, D])
        P.dma("sp", _dma(gB[:], gB_d[:, :]), ("gB", tag), writes=[("gB", tag)])
        P.dma("sp", _dma(bB[:], bB_d[:, :]), ("bB", tag), writes=[("bB", tag)])
        xs = [_sb(C, st, "rx", [128, D]) for _ in range(2)]
        ms = [_sb(C, st, "rm", [128, D]) for _ in range(2)]
        rs = [_sb(C, st, "rr", [128, D]) for _ in range(2)]
        mv = [_sb(C, st, "rmv", [128, 2]) for _ in range(2)]
        rstd = [_sb(C, st, "rrs", [128, 1]) for _ in range(2)]
        nb = [_sb(C, st, "rnb", [128, 1]) for _ in range(2)]

        def front(t):
            i = t % 2
            xk, mk, rk, vk = ("rx", tag, i), ("rm", tag, i), ("rr", tag, i), ("rmv", i)
            P.dma("sp", _dma(xs[i][:], x[t * 128:(t + 1) * 128, :]), xk, writes=[xk])
            P.dma("sp", _dma(ms[i][:], m[t * 128:(t + 1) * 128, :]), mk, writes=[mk])
            P.op("dve", _stt(rs[i][:], xs[i][:], ALPHA, ms[i][:], ALU.mult, ALU.add), reads=[xk, mk], writes=[rk])
            P.op("act", _act(xs[i][:], rs[i][:], AF.Identity, accum=mv[i][:, 0:1]), reads=[rk], writes=[vk, xk])
            P.op("act", _act(xs[i][:], rs[i][:], AF.Square, accum=mv[i][:, 1:2]), reads=[rk], writes=[vk, xk])

        def back(t):
            i = t % 2
            xk, mk, rk, vk, sk, nk = ("rx", tag, i), ("rm", tag, i), ("rr", tag, i), ("rmv", i), ("rrs", i), ("rnb", i)
            P.op("dve", _ts(mv[i][:], mv[i][:], 1.0 / D), reads=[vk], writes=[vk])
            P.op("dve", _tt(nb[i][:], mv[i][:, 0:1], mv[i][:, 0:1], ALU.mult), reads=[vk], writes=[nk])
            P.op("dve", _tt(rstd[i][:], mv[i][:, 1:2], nb[i][:], ALU.subtract), reads=[vk, nk], writes=[sk])
            P.op("dve", _ts(rstd[i][:], rstd[i][:], EPS, None, ALU.add), reads=[sk], writes=[sk])
            P.op("act", _sqrt(rstd[i][:], rstd[i][:]), reads=[sk], writes=[sk])
            P.op("dve", _rcp(rstd[i][:], rstd[i][:]), reads=[sk], writes=[sk])
            P.op("dve", _stt(nb[i][:], mv[i][:, 0:1], -1.0, rstd[i][:], ALU.mult, ALU.mult), reads=[vk, sk], writes=[nk])
            P.op("act", _act(rs[i][:], rs[i][:], AF.Identity, bias=nb[i][:, 0:1], scale=rstd[i][:, 0:1]), reads=[rk, sk, nk], writes=[rk])
            P.op("dve", _tt(ms[i][:], rs[i][:], gB[:], ALU.mult), reads=[rk, ("gB", tag)], writes=[mk])
            P.op("pool", _tt(ms[i][:], ms[i][:], bB[:], ALU.add), reads=[mk, ("bB", tag)], writes=[mk])
            P.dma("sp", _dma(out[t * 128:(t + 1) * 128, :], ms[i][:]), mk, reads=[mk], writes=[("rout", tag, t)])

        for t in range(nt + 1):
            if t < nt:
                front(t)
            if t >= 1:
                back(t - 1)
    P.barrier()


class LNEpi:
    def __init__(self, C, st, x, gB_d, bB_d, out, tag):
        self.C, self.x, self.out, self.tag = C, x, out, tag
        P = C.P
        self.gB = _sb(C, st, "egB", [128, D]); self.bB = _sb(C, st, "ebB", [128, D])
        P.dma("sp", _dma(self.gB[:], gB_d[:, :]), ("egB", tag), writes=[("egB", tag)])
        P.dma("sp", _dma(self.bB[:], bB_d[:, :]), ("ebB", tag), writes=[("ebB", tag)])
        self.xs = [_sb(C, st, "ex", [128, D]) for _ in range(2)]
        self.rs = [_sb(C, st, "er", [128, D]) for _ in range(2)]
        self.mv = [_sb(C, st, "emv", [128, 2]) for _ in range(2)]
        self.rstd = [_sb(C, st, "ers", [128, 1]) for _ in range(2)]
        self.nb = [_sb(C, st, "enb", [128, 1]) for _ in range(2)]

    def front(self, t, row0, msrc, mkeys):
        P, tag, i = self.C.P, self.tag, t % 2
        xk, rk, vk = ("ex", tag, i), ("er", tag, i), ("emv", tag, i)
        P.dma("sp", _dma(self.xs[i][:], self.x[row0:row0 + 128, :]), xk, writes=[xk])
        P.op("dve", _stt(self.rs[i][:], self.xs[i][:], ALPHA, msrc, ALU.mult, ALU.add), reads=[xk] + list(mkeys), writes=[rk] + list(mkeys))
        P.op("act", _act(self.xs[i][:], self.rs[i][:], AF.Identity, accum=self.mv[i][:, 0:1]), reads=[rk], writes=[vk, xk])
        P.op("act", _act(self.xs[i][:], self.rs[i][:], AF.Square, accum=self.mv[i][:, 1:2]), reads=[rk], writes=[vk, xk])

    def back(self, t, row0):
        P, tag, i = self.C.P, self.tag, t % 2
        mv, rstd, nb, rs, xs = self.mv[i], self.rstd[i], self.nb[i], self.rs[i], self.xs[i]
        xk, rk, vk, sk, nk = ("ex", tag, i), ("er", tag, i), ("emv", tag, i), ("ers", tag, i), ("enb", tag, i)
        P.op("dve", _ts(mv[:], mv[:], 1.0 / D), reads=[vk], writes=[vk])
        P.op("dve", _tt(nb[:], mv[:, 0:1], mv[:, 0:1], ALU.mult), reads=[vk], writes=[nk])
        P.op("dve", _tt(rstd[:], mv[:, 1:2], nb[:], ALU.subtract), reads=[vk, nk], writes=[sk])
        P.op("dve", _ts(rstd[:], rstd[:], EPS, None, ALU.add), reads=[sk], writes=[sk])
        P.op("act", _sqrt(rstd[:], rstd[:]), reads=[sk], writes=[sk])
        P.op("dve", _rcp(rstd[:], rstd[:]), reads=[sk], writes=[sk])
        P.op("dve", _stt(nb[:], mv[:, 0:1], -1.0, rstd[:], ALU.mult, ALU.mult), reads=[vk, sk], writes=[nk])
        P.op("act", _act(rs[:], rs[:], AF.Identity, bias=nb[:, 0:1], scale=rstd[:, 0:1]), reads=[rk, sk, nk], writes=[rk])
        P.op("dve", _tt(xs[:], rs[:], self.gB[:], ALU.mult), reads=[rk, ("egB", tag)], writes=[xk])
        P.op("dve", _tt(xs[:], xs[:], self.bB[:], ALU.add), reads=[xk, ("ebB", tag)], writes=[xk])
        P.dma("sp", _dma(self.out[row0:row0 + 128, :], xs[:]), xk, reads=[xk], writes=[("eout", tag, t)])


def linear_ln(C, src, w, x, gB_d, bB_d, out, tag):
    nc, P = C.nc, C.P
    P.stage = C.pfx + "linln_" + tag
    nt = T // 128
    with ExitStack() as st:
        xT = _sb(C, st, "lxT", [128, 16, T], BF16)
        wt = _sb(C, st, "lw", [128, 16, D], BF16)
        wv = w.rearrange("(c p) n -> p c n", p=128)
        for g in range(4):
            P.dma("pool", _dma(wt[:, :, g * 512:(g + 1) * 512], wv[:, :, g * 512:(g + 1) * 512]), ("lw", g), writes=[("lw", g)])
        with ExitStack() as s1:
            stg = [_sb(C, s1, "lxs", [128, D]) for _ in range(2)]
            for t in range(nt):
                sk = ("lxs", t % 2)
                P.dma("sp", _dma(stg[t % 2][:], src[t * 128:(t + 1) * 128, :]), sk, writes=[sk])
                _transpose_blocks(C, stg[t % 2], 16, xT[:, :, t * 128:(t + 1) * 128], [sk], "lxT", 0)
        P.barrier()
        ep = LNEpi(C, st, x, gB_d, bB_d, out, tag)
        for t in range(nt):
            base = (t % 2) * 4
            keys = [f"pb{base + g}" for g in range(4)]
            for g in range(4):
                for c in range(16):
                    P.op("pe", _mm(C.pb[base + g][:, :], xT[:, c, t * 128:(t + 1) * 128], wt[:, c, g * 512:(g + 1) * 512], c == 0, c == 15), reads=["lxT", ("lw", g)], writes=[keys[g]])
            ep.front(t, t * 128, C.ps_all[:, base * 512:base * 512 + 2048], keys)
            if t >= 1:
                ep.back(t - 1, (t - 1) * 128)
        ep.back(nt - 1, (nt - 1) * 128)
    P.barrier()


MOE_STOP = None
MOE_DBG = None


def moe(C, h, w_gu, w_down, rw_d, rbB_d, f_out, tag, ln=None):
    nc, P = C.nc, C.P
    HT = 1024
    for half_ in range(2 if MOE_STOP is None else 1):
        _moe_half(C, h, w_gu, w_down, rw_d, rbB_d, f_out, tag, half_, ln)


def _moe_half(C, h, w_gu, w_down, rw_d, rbB_d, f_out, tag, half, ln=None):
    nc, P = C.nc, C.P
    P.stage = C.pfx + "moe"
    HT = 1024
    if True:
        with ExitStack() as st:
            hsrc = h[half * HT:(half + 1) * HT, :]
            gateT = _sb(C, st, "gateT", [16, HT], BF16)
            sel16 = _sb(C, st, "sel16", [16, 16, 128], BF16)
            xT = _sb(C, st, "mxT", [128, 16, HT], BF16)
            yacc = _sb(C, st, "yacc", [128, 16, HT])
            dbg_sb = _sb(C, st, "dbg_sb", [128, 512])
            P.op("dve", lambda e: e.memset(dbg_sb[:], 0.0), writes=["dbg"])
            st1 = ExitStack()
            xT32 = _sb(C, st1, "xT32", [128, 16, 128])
            rw = _sb(C, st1, "rw", [128, 16, 16])
            rbB = _sb(C, st1, "rbB", [128, 16])
            gate = _sb(C, st1, "gate", [128, 8, 16])
            P.dma("sp", lambda e: e.dma_start(out=rw[:], in_=rw_d.rearrange("(c p) n -> p c n", p=128)), ("rw", tag, half), writes=["rw"])
            P.dma("sp", lambda e: e.dma_start(out=rbB[:], in_=rbB_d[:, :]), ("rbB", tag, half), writes=["rbB"])
            for ex_ in range(16):
                P.op("dve", (lambda ex_=ex_: lambda e: e.tensor_copy(out=sel16[:, ex_, :], in_=C.ident[0:16, ex_:ex_ + 1].to_broadcast([16, 128])))(), reads=["ident"], writes=["sel16"])
            stg = [_sb(C, st1, "mxs", [128, D]) for _ in range(2)]
            sm = {n: _sb(C, st1, "g" + n, [128, 8 * 16]) for n in ["aff", "sel", "msk", "t1", "t2", "w"]}
            sm4 = {n: _sb(C, st1, "g4" + n, [128, 8 * 4]) for n in ["m1", "m2", "gs", "gm"]}
            sm1 = {n: _sb(C, st1, "g1" + n, [128, 8]) for n in ["a", "b"]}
            for t in range(HT // 128):
                s = stg[t % 2]; sk = ("mxs", t % 2)
                P.dma("sp", (lambda s=s, t=t: lambda e: e.dma_start(out=s[:], in_=hsrc[t * 128:(t + 1) * 128, :]))(), sk, writes=[sk])
                for q in range(4):
                    bank = C.pb[q % 2]; bk = f"pb{q % 2}"
                    for i in range(4):
                        c = q * 4 + i
                        P.op("pe", (lambda s=s, c=c, bank=bank, i=i: lambda e: e.transpose(bank[:, i * 128:(i + 1) * 128], s[:, c * 128:(c + 1) * 128], C.ident[:]))(), reads=[sk, "ident"], writes=[bk])
                    srcp = bank[:].rearrange("p (i k) -> p i k", i=4)
                    P.op("act", (lambda q=q, t=t, srcp=srcp: lambda e: e.copy(out=xT[:, q * 4:(q + 1) * 4, t * 128:(t + 1) * 128], in_=srcp))(), reads=[bk], writes=["mxT", bk])
                    P.op("dve", (lambda q=q, srcp=srcp: lambda e: e.tensor_copy(out=xT32[:, q * 4:(q + 1) * 4, :], in_=srcp))(), reads=[bk], writes=["xT32", bk])
                lb = C.pb[2]
                for c in range(16):
                    P.op("pe", (lambda c=c: lambda e: e.matmul(lb[:, 0:16], lhsT=xT32[:, c, :], rhs=rw[:, c, :], start=(c == 0), stop=(c == 15)))(), reads=["xT32", "rw"], writes=["pb2"])
                P.op("act", _act(sm["aff"][:, t * 16:(t + 1) * 16], lb[:, 0:16], AF.Sigmoid), reads=["pb2"], writes=["gsm", "pb2"])
            aff, sel, msk, t1, t2, wv = sm["aff"], sm["sel"], sm["msk"], sm["t1"], sm["t2"], sm["w"]
            m1, m2, gs, gm = sm4["m1"], sm4["m2"], sm4["gs"], sm4["gm"]
            a1, b1 = sm1["a"], sm1["b"]
            G = ["gsm"]
            NG = 8
            v16 = lambda x: x[:].rearrange("p (t e) -> p t e", t=NG)
            v4 = lambda x: x[:].rearrange("p (n k) -> p n k", k=4)
            g4 = lambda x: x[:].rearrange("p (t g) -> p t g", t=NG)
            P.op("dve", _tt(v16(sel), v16(aff), rbB[:, None, :].to_broadcast([128, NG, 16]), ALU.add), reads=G + ["rbB"], writes=G)
            P.op("dve", _red(m1[:], v4(sel), ALU.max), reads=G, writes=G)
            P.op("dve", _tt(v4(t1), v4(sel), m1[:, :, None].to_broadcast([128, NG * 4, 4]), ALU.is_equal), reads=G, writes=G)
            P.op("dve", _stt(t1[:], t1[:], NEG, sel[:], ALU.mult, ALU.add), reads=G, writes=G)
            P.op("dve", _red(m2[:], v4(t1), ALU.max), reads=G, writes=G)
            P.op("dve", _tt(gs[:], m1[:], m2[:], ALU.add), reads=G, writes=G)
            P.op("dve", _red(a1[:], g4(gs), ALU.max), reads=G, writes=G)
            P.op("dve", _tt(g4(gm), g4(gs), a1[:, :, None].to_broadcast([128, NG, 4]), ALU.is_equal), reads=G, writes=G)
            P.op("dve", _cp(v4(msk), gm[:, :, None].to_broadcast([128, NG * 4, 4])), reads=G, writes=G)
            P.op("dve", _tt(t1[:], sel[:], msk[:], ALU.mult), reads=G, writes=G)
            P.op("dve", _ts(t2[:], msk[:], -1.0, -NEG, ALU.add, ALU.mult), reads=G, writes=G)
            P.op("dve", _tt(t1[:], t1[:], t2[:], ALU.add), reads=G, writes=G)
            P.op("dve", _red(a1[:], v16(t1), ALU.max), reads=G, writes=G)
            P.op("dve", _tt(v16(wv), v16(t1), a1[:, :, None].to_broadcast([128, NG, 16]), ALU.is_equal), reads=G, writes=G)
            P.op("dve", _stt(t1[:], wv[:], NEG, t1[:], ALU.mult, ALU.add), reads=G, writes=G)
            P.op("dve", _red(b1[:], v16(t1), ALU.max), reads=G, writes=G)
            P.op("dve", _tt(v16(t2), v16(t1), b1[:, :, None].to_broadcast([128, NG, 16]), ALU.is_equal), reads=G, writes=G)
            P.op("dve", _tt(wv[:], wv[:], t2[:], ALU.add), reads=G, writes=G)
            P.op("dve", _tt(wv[:], wv[:], aff[:], ALU.mult), reads=G, writes=G)
            P.op("dve", _red(a1[:], v16(wv), ALU.add), reads=G, writes=G)
            P.op("dve", _rcp(a1[:], a1[:]), reads=G, writes=G)
            P.op("dve", _tt(gate[:], v16(wv), a1[:, :, None].to_broadcast([128, NG, 16]), ALU.mult), reads=G, writes=["gate"])
            for t in range(HT // 128):
                P.op("pe", _tr(C.pb[3][0:16, 0:128], gate[:, t, :], C.ident[:]), reads=["gate", "ident"], writes=["pb3"])
                P.op("act", _acp(gateT[:, t * 128:(t + 1) * 128], C.pb[3][0:16, 0:128]), reads=["pb3"], writes=["gateT", "pb3"])
            P.barrier()
            st1.close()
            if MOE_STOP == "build":
                return
            st2 = ExitStack()
            wgu = [_sb(C, st2, "wgu", [128, 16, 1024], BF16) for _ in range(2)]
            wdn = _sb(C, st2, "wdn", [128, 4, D], BF16)
            gB = _sb(C, st2, "mgB", [128, HT], BF16)
            hm = [_sb(C, st2, "hm", [128, 4, 512], BF16) for _ in range(2)]
            sa = [_sb(C, st2, "sa", [128, 512], BF16) for _ in range(2)]
            sg = [_sb(C, st2, "sg", [128, 512], BF16) for _ in range(2)]
            gv = w_gu.rearrange("x (c p) n -> x p c n", p=128)
            dv = w_down.rearrange("x (c p) n -> x p c n", p=128)
            P.dma("pool", lambda e: e.dma_start(out=wgu[0][:], in_=gv[0]), ("wgu", 0), writes=[("wgu", 0)])
            kk = 0
            for ex in range(16 if MOE_STOP is None else 1):
                wg = wgu[ex % 2]; wgk = ("wgu", ex % 2)
                if ex + 1 < 16:
                    P.dma("pool", (lambda ex=ex: lambda e: e.dma_start(out=wgu[(ex + 1) % 2][:], in_=gv[ex + 1]))(), ("wgu", (ex + 1) % 2), writes=[("wgu", (ex + 1) % 2)])
                P.dma("pool", (lambda ex=ex: lambda e: e.dma_start(out=wdn[:], in_=dv[ex]))(), "wdn", writes=["wdn"])
                for tb in range(HT // 512):
                    P.op("pe", (lambda ex=ex, tb=tb: lambda e: e.matmul(C.pb[2][:, :], lhsT=sel16[:, ex, :], rhs=gateT[:, tb * 512:(tb + 1) * 512], start=True, stop=True))(), reads=["sel16", "gateT"], writes=["pb2"])
                    P.op("act", (lambda tb=tb: lambda e: e.copy(out=gB[:, tb * 512:(tb + 1) * 512], in_=C.pb[2][:, :]))(), reads=["pb2"], writes=[("mgB", tb)])
                for tb in range(HT // 512):
                    hmt = hm[tb % 2]; hk = ("hm", tb % 2)
                    for ffc in range(4):
                        pa = C.pb[4 + (kk % 2) * 2]; pak = f"pb{4 + (kk % 2) * 2}"
                        pbb = C.pb[5 + (kk % 2) * 2]; pbk = f"pb{5 + (kk % 2) * 2}"
                        for c in range(16):
                            P.op("pe", (lambda pa=pa, wg=wg, c=c, ffc=ffc, tb=tb: lambda e: e.matmul(pa[:, :], lhsT=wg[:, c, ffc * 128:(ffc + 1) * 128], rhs=xT[:, c, tb * 512:(tb + 1) * 512], start=(c == 0), stop=(c == 15)))(), reads=[wgk, "mxT"], writes=[pak])
                        for c in range(16):
                            P.op("pe", (lambda pbb=pbb, wg=wg, c=c, ffc=ffc, tb=tb: lambda e: e.matmul(pbb[:, :], lhsT=wg[:, c, 512 + ffc * 128:512 + (ffc + 1) * 128], rhs=xT[:, c, tb * 512:(tb + 1) * 512], start=(c == 0), stop=(c == 15)))(), reads=[wgk, "mxT"], writes=[pbk])
                        sat = sa[kk % 2]; sgt = sg[kk % 2]; sak = ("sa", kk % 2); sgk = ("sg", kk % 2)
                        P.op("act", (lambda sat=sat, pa=pa: lambda e: e.activation(out=sat[:], in_=pa[:, :], func=AF.Silu))(), reads=[pak], writes=[sak])
                        P.op("dve", (lambda sgt=sgt, sat=sat, tb=tb: lambda e: e.tensor_tensor(out=sgt[:], in0=sat[:], in1=gB[:, tb * 512:(tb + 1) * 512], op=ALU.mult))(), reads=[sak, ("mgB", tb)], writes=[sgk])
                        P.op("dve", (lambda hmt=hmt, ffc=ffc, sgt=sgt, pbb=pbb: lambda e: e.tensor_tensor(out=hmt[:, ffc, :], in0=sgt[:], in1=pbb[:, :], op=ALU.mult))(), reads=[sgk, pbk], writes=[hk])
                        kk += 1
                for tb in range(HT // 512):
                    hmt = hm[tb % 2]; hk = ("hm", tb % 2)
                    for dc in range(16):
                        po = C.pb[dc % 4]; pok = f"pb{dc % 4}"
                        for ffc in range(4):
                            P.op("pe", (lambda po=po, ffc=ffc, dc=dc, hmt=hmt: lambda e: e.matmul(po[:, :], lhsT=wdn[:, ffc, dc * 128:(dc + 1) * 128], rhs=hmt[:, ffc, :], start=(ffc == 0), stop=(ffc == 3)))(), reads=["wdn", hk], writes=[pok])
                        ydst = yacc[:, dc, tb * 512:(tb + 1) * 512]
                        if ex == 0:
                            P.op("dve", (lambda ydst=ydst, po=po: lambda e: e.tensor_copy(out=ydst, in_=po[:, :]))(), reads=[pok], writes=[("yacc", dc, tb)])
                        else:
                            P.op("dve", (lambda ydst=ydst, po=po: lambda e: e.tensor_tensor(out=ydst, in0=ydst, in1=po[:, :], op=ALU.add))(), reads=[pok, ("yacc", dc, tb)], writes=[("yacc", dc, tb)])
            if MOE_DBG is not None:
                P.op("dve", lambda e: e.tensor_copy(out=dbg_sb[:, 16:144], in_=gB[:, 0:128]), reads=[("mgB", 0)], writes=["dbg"])
                P.op("dve", lambda e: e.tensor_copy(out=dbg_sb[:, 144:272], in_=hm[0][:, 0, 0:128]), reads=[("hm", 0)], writes=["dbg"])
                P.op("dve", lambda e: e.tensor_copy(out=dbg_sb[:, 272:400], in_=yacc[:, 0, 0:128]), reads=[("yacc", 0, 0)], writes=["dbg"])
                P.op("dve", lambda e: e.tensor_copy(out=dbg_sb[0:16, 432:512], in_=gateT[:, 0:80]), reads=["gateT"], writes=["dbg"])
                P.dma("sp", lambda e: e.dma_start(out=MOE_DBG[:, :], in_=dbg_sb[:]), "dbg", reads=["dbg"], writes=["dbgout"])
            P.barrier()
            st2.close()
            if ln is None:
                fo = [_sb(C, st, "fo", [128, D]) for _ in range(2)]
                for t in range(HT // 128):
                    fot = fo[t % 2]; fk = ("fo", t % 2)
                    for q in range(4):
                        bank = C.pb[q % 2]; bk = f"pb{q % 2}"
                        for i in range(4):
                            dc = q * 4 + i
                            P.op("pe", _tr(bank[:, i * 128:(i + 1) * 128], yacc[:, dc, t * 128:(t + 1) * 128], C.ident[:]), reads=[("yacc", dc, t // 4), "ident"], writes=[bk])
                        P.op("act", _acp(fot[:, q * 512:(q + 1) * 512], bank[:, :]), reads=[bk], writes=[fk])
                    P.dma("sp", _dma(f_out[half * HT + t * 128: half * HT + (t + 1) * 128, :], fot[:]), fk, reads=[fk], writes=[("fout", half, t)])
            else:
                gB_d, bB_d, out_d = ln
                ep = LNEpi(C, st, h, gB_d, bB_d, out_d, "moe")
                ntl = HT // 128
                for t in range(ntl):
                    base = (t % 2) * 4
                    keys = [f"pb{base + q}" for q in range(4)]
                    for q in range(4):
                        for i in range(4):
                            dc = q * 4 + i
                            P.op("pe", _tr(C.pb[base + q][:, i * 128:(i + 1) * 128], yacc[:, dc, t * 128:(t + 1) * 128], C.ident[:]), reads=[("yacc", dc, t // 4), "ident"], writes=[keys[q]])
                    ep.front(t, half * HT + t * 128, C.ps_all[:, base * 512:base * 512 + 2048], keys)
                    if t >= 1:
                        ep.back(t - 1, half * HT + (t - 1) * 128)
                ep.back(ntl - 1, half * HT + (ntl - 1) * 128)
        P.barrier()


def _tt(out, in0, in1, op):
    return lambda e: e.tensor_tensor(out=out, in0=in0, in1=in1, op=op)


def _ts(out, in0, s1, s2=None, op0=ALU.mult, op1=None, accum=None):
    if accum is not None:
        return lambda e: e.tensor_scalar(out=out, in0=in0, scalar1=s1, scalar2=s2, op0=op0, op1=op1, accum_out=accum)
    if op1 is None:
        return lambda e: e.tensor_scalar(out=out, in0=in0, scalar1=s1, scalar2=None, op0=op0)
    return lambda e: e.tensor_scalar(out=out, in0=in0, scalar1=s1, scalar2=s2, op0=op0, op1=op1)


def _stt(out, in0, scalar, in1, op0, op1):
    return lambda e: e.scalar_tensor_tensor(out=out, in0=in0, scalar=scalar, in1=in1, op0=op0, op1=op1)


def _act(out, in_, func, bias=None, scale=None, accum=None):
    kw = {}
    if bias is not None:
        kw["bias"] = bias
    if scale is not None:
        kw["scale"] = scale
    if accum is not None:
        kw["accum_out"] = accum
    return lambda e: e.activation(out=out, in_=in_, func=func, **kw)


def _mm(out, lhsT, rhs, start, stop):
    return lambda e: e.matmul(out, lhsT=lhsT, rhs=rhs, start=start, stop=stop)


def _tr(out, in_, ident):
    return lambda e: e.transpose(out, in_, ident)


def _acp(out, in_):
    return lambda e: e.copy(out=out, in_=in_)


def _cp(out, in_):
    return lambda e: e.tensor_copy(out=out, in_=in_)


def _dma(out, in_):
    return lambda e: e.dma_start(out=out, in_=in_)


def _red(out, in_, op):
    return lambda e: e.tensor_reduce(out=out, in_=in_, axis=AX.X, op=op)


def _rcp(out, in_):
    return lambda e: e.reciprocal(out=out, in_=in_)


def _mset(out, v):
    return lambda e: e.memset(out, v)


def _sqrt(out, in_):
    return lambda e: e.sqrt(out=out, in_=in_)


def _rope(P, src, dst, cs, H, dh, t1, t2, rkeys, wkey, tk):
    hf = dh // 2
    s3 = src.rearrange("p (h d) -> p h d", h=H)
    d3 = dst.rearrange("p (h d) -> p h d", h=H)
    x1 = s3[:, :, 0:hf]; x2 = s3[:, :, hf:dh]
    cosB = cs[:, 0:1, :].to_broadcast([128, H, hf])
    sinB = cs[:, 1:2, :].to_broadcast([128, H, hf])
    a = t1[:, 0:H * hf].rearrange("p (h d) -> p h d", h=H)
    b = t2[:, 0:H * hf].rearrange("p (h d) -> p h d", h=H)
    k1, k2 = (tk, 1), (tk, 2)
    P.op("dve", _tt(a, x1, cosB, ALU.mult), reads=rkeys, writes=[k1])
    P.op("dve", _tt(b, x2, sinB, ALU.mult), reads=rkeys, writes=[k2])
    P.op("dve", _tt(d3[:, :, 0:hf], a, b, ALU.subtract), reads=[k1, k2], writes=[wkey])
    P.op("dve", _tt(a, x2, cosB, ALU.mult), reads=rkeys, writes=[k1])
    P.op("dve", _tt(b, x1, sinB, ALU.mult), reads=rkeys, writes=[k2])
    P.op("dve", _tt(d3[:, :, hf:dh], a, b, ALU.add), reads=[k1, k2, wkey], writes=[wkey])


def _transpose_blocks(C, src, nblk, dst3, rkeys, wkey, eng_toggle=0):
    P = C.P
    b0 = 0
    g = 0
    while b0 < nblk:
        n = min(4, nblk - b0)
        bi = (g + eng_toggle) % 2
        bank = C.pb[bi]; bk = f"pb{bi}"
        for i in range(n):
            P.op("pe", _tr(bank[:, i * 128:(i + 1) * 128], src[:, (b0 + i) * 128:(b0 + i + 1) * 128], C.ident[:]), reads=list(rkeys) + ["ident"], writes=[bk])
        srcp = bank[:, 0:n * 128].rearrange("p (i k) -> p i k", i=n)
        if bi == 0:
            P.op("act", _acp(dst3[:, b0:b0 + n, :], srcp), reads=[bk], writes=[wkey])
        else:
            P.op("dve", _cp(dst3[:, b0:b0 + n, :], srcp), reads=[bk], writes=[wkey])
        b0 += n
        g += 1


RET_G = [1.0 - 2.0 ** (-5.0 - h) for h in range(4)]


def mixer_ret(C, proj, pkv, mix, cst, cs_key="cs_own", s_in=None, s_out=None):
    P = C.P
    P.stage = C.pfx + "ret"
    with ExitStack() as st:
        decT = _sb(C, st, "decT", [128, 512]); xi = _sb(C, st, "xi", [128, 4]); zeta = _sb(C, st, "zeta", [128, 4]); gnB = _sb(C, st, "gnB", [128, 1024])
        P.dma("sp", _dma(decT[:], cst["decT"][:, :]), "decT", writes=["decT"])
        P.dma("sp", _dma(xi[:], cst["xi"][:, :]), "xi", writes=["xi"])
        P.dma("sp", _dma(zeta[:], cst["zeta"][:, :]), "zeta", writes=["zeta"])
        P.dma("sp", _dma(gnB[:], cst["gnB"][:, :]), "gnB", writes=["gnB"])
        S32 = _sb(C, st, "S32", [128, 4, 512]); Sb = _sb(C, st, "Sb", [128, 4, 512], BF16)
        if s_in is None:
            P.op("dve", _mset(S32[:], 0.0), writes=["S32"])
            P.op("dve", _mset(Sb[:], 0.0), writes=["Sb"])
        else:
            P.dma("sp", _dma(S32[:].rearrange("p h n -> p (h n)"), s_in[:, :]), "S32io", writes=["S32"])
            P.op("act", _acp(Sb[:], S32[:]), reads=["S32"], writes=["Sb"])
        qkvg = [_sb(C, st, "qkvg", [128, 4096]) for _ in range(2)]
        csb = [_sb(C, st, "csb", [128, 2, 128]) for _ in range(2)]
        qr = _sb(C, st, "qr", [128, 1024]); kr = _sb(C, st, "kr", [128, 1024]); qx = _sb(C, st, "qx", [128, 1024])
        kz = _sb(C, st, "kz", [128, 1024], BF16); vb = _sb(C, st, "vb", [128, 1024], BF16)
        qT = _sb(C, st, "qT", [128, 8, 128], BF16); kT = _sb(C, st, "kT", [128, 8, 128], BF16); qxT = _sb(C, st, "qxT", [128, 8, 128], BF16)
        am = _sb(C, st, "am", [128, 512], BF16)
        t1 = _sb(C, st, "rt1", [128, 512]); t2 = _sb(C, st, "rt2", [128, 512])
        junk = _sb(C, st, "rjunk", [128, 256])
        st4 = {n: _sb(C, st, "st" + n, [128, 4]) for n in ["s", "q", "m", "r", "nb"]}
        yn = _sb(C, st, "yn", [128, 1024]); sg = _sb(C, st, "sgt", [128, 1024])
        ret = [_sb(C, st, "ret", [128, 1024]) for _ in range(2)]
        SB = [C.pb[5], C.pb[6], C.pb[7], C.pb[2]]; SBK = ["pb5", "pb6", "pb7", "pb2"]
        kz2 = [kz, _sb(C, st, "kz2", [128, 1024], BF16)]; vb2 = [vb, _sb(C, st, "vb2", [128, 1024], BF16)]
        qT2_ = [qT, _sb(C, st, "qT2", [128, 8, 128], BF16)]; kT2_ = [kT, _sb(C, st, "kT2", [128, 8, 128], BF16)]; qxT2_ = [qxT, _sb(C, st, "qxT2", [128, 8, 128], BF16)]

        def front(ci):
            own = ci >= 16
            t = ci % 16; i = ci % 2
            buf = qkvg[i]; bkey = ("qkvg", i)
            cs = csb[i]; ckey = ("csb", i)
            if own:
                P.dma("sp", _dma(buf[:], proj[t * 128:(t + 1) * 128, 0:4096]), bkey, writes=[bkey])
                P.dma("sp", _dma(cs[:], cst[cs_key][t * 128:(t + 1) * 128, :, :]), ckey, writes=[ckey])
            else:
                P.dma("sp", _dma(buf[:, 1024:3072], pkv[t * 128:(t + 1) * 128, :]), bkey, writes=[bkey])
                P.dma("sp", _dma(cs[:], cst["cs_pre"][t * 128:(t + 1) * 128, :, :]), ckey, writes=[ckey])
            _rope(P, buf[:, 1024:2048], kr[:], cs, 4, 256, t1, t2, [bkey, ckey], "kr", "rt")
            P.op("dve", _tt(kz2[i][:].rearrange("p (h d) -> p h d", h=4), kr[:].rearrange("p (h d) -> p h d", h=4), zeta[:, :, None].to_broadcast([128, 4, 256]), ALU.mult), reads=["kr", "zeta"], writes=[("kz", i)])
            P.op("act", _acp(vb2[i][:], buf[:, 2048:3072]), reads=[bkey], writes=[("vb", i)])
            if own:
                _rope(P, buf[:, 0:1024], qr[:], cs, 4, 256, t1, t2, [bkey, ckey], "qr", "rt")
                P.op("dve", _tt(qx[:].rearrange("p (h d) -> p h d", h=4), qr[:].rearrange("p (h d) -> p h d", h=4), xi[:, :, None].to_broadcast([128, 4, 256]), ALU.mult), reads=["qr", "xi"], writes=["qx"])
                _transpose_blocks(C, qr, 8, qT2_[i], ["qr"], ("qT", i), 0)
                _transpose_blocks(C, kr, 8, kT2_[i], ["kr"], ("kT", i), 0)
                _transpose_blocks(C, qx, 8, qxT2_[i], ["qx"], ("qxT", i), 0)

        def back(ci):
            own = ci >= 16
            t = ci % 16; i = ci % 2
            buf = qkvg[i]; bkey = ("qkvg", i)
            kzi, vbi, qTi, kTi, qxTi = kz2[i], vb2[i], qT2_[i], kT2_[i], qxT2_[i]
            kzk, vbk, qTk, kTk, qxTk = ("kz", i), ("vb", i), ("qT", i), ("kT", i), ("qxT", i)
            if own:
                for h in range(4):
                    for j in range(2):
                        P.op("pe", _mm(C.pb[2][:, h * 128:(h + 1) * 128], kTi[:, 2 * h + j, :], qTi[:, 2 * h + j, :], j == 0, j == 1), reads=[kTk, qTk], writes=["pb2"])
                P.op("dve", _tt(am[:], C.pb[2][:, :], decT[:], ALU.mult), reads=["pb2", "decT"], writes=["am", "pb2"])
                for h in range(4):
                    yb = C.pb[3 + h // 2]; ybk = f"pb{3 + h // 2}"
                    o = yb[:, (h % 2) * 256:(h % 2) * 256 + 256]
                    P.op("pe", _mm(o, am[:, h * 128:(h + 1) * 128], vbi[:, h * 256:(h + 1) * 256], True, False), reads=["am", vbk], writes=[ybk])
                    P.op("pe", _mm(o, qxTi[:, 2 * h, :], Sb[:, h, 0:256], False, False), reads=[qxTk, "Sb"], writes=[ybk])
                    P.op("pe", _mm(o, qxTi[:, 2 * h + 1, :], Sb[:, h, 256:512], False, True), reads=[qxTk, "Sb"], writes=[ybk])
            for h in range(4):
                for j in range(2):
                    P.op("pe", _mm(SB[h][:, j * 256:(j + 1) * 256], kzi[:, h * 256 + j * 128:h * 256 + (j + 1) * 128], vbi[:, h * 256:(h + 1) * 256], True, True), reads=[kzk, vbk], writes=[SBK[h]])
            for h in range(4):
                P.op("dve", _stt(S32[:, h, :], S32[:, h, :], RET_G[h] ** 128, SB[h][:, :], ALU.mult, ALU.add), reads=[SBK[h], "S32"], writes=["S32", SBK[h]])
            P.op("act", _acp(Sb[:], S32[:]), reads=["S32"], writes=["Sb"])
            if not own:
                return
            s_, q_, m_, r_, nb_ = st4["s"], st4["q"], st4["m"], st4["r"], st4["nb"]
            for h in range(4):
                yb = C.pb[3 + h // 2]; ybk = f"pb{3 + h // 2}"
                o = yb[:, (h % 2) * 256:(h % 2) * 256 + 256]
                P.op("act", _act(junk[:], o, AF.Identity, accum=s_[:, h:h + 1]), reads=[ybk], writes=["rjunk", "st_s"])
                P.op("act", _act(junk[:], o, AF.Square, accum=q_[:, h:h + 1]), reads=[ybk], writes=["rjunk", "st_q"])
            P.op("dve", _ts(m_[:], s_[:], 1.0 / 256), reads=["st_s"], writes=["st_m"])
            P.op("dve", _tt(r_[:], m_[:], m_[:], ALU.mult), reads=["st_m"], writes=["st_r"])
            P.op("dve", _stt(r_[:], q_[:], 1.0 / 256, r_[:], ALU.mult, ALU.subtract), reads=["st_q", "st_r"], writes=["st_r"])
            P.op("dve", _ts(r_[:], r_[:], EPS, None, ALU.add), reads=["st_r"], writes=["st_r"])
            P.op("act", _sqrt(r_[:], r_[:]), reads=["st_r"], writes=["st_r"])
            P.op("dve", _rcp(r_[:], r_[:]), reads=["st_r"], writes=["st_r"])
            P.op("dve", _stt(nb_[:], m_[:], -1.0, r_[:], ALU.mult, ALU.mult), reads=["st_m", "st_r"], writes=["st_nb"])
            for h in range(4):
                yb = C.pb[3 + h // 2]; ybk = f"pb{3 + h // 2}"
                o = yb[:, (h % 2) * 256:(h % 2) * 256 + 256]
                P.op("act", _act(yn[:, h * 256:(h + 1) * 256], o, AF.Identity, bias=nb_[:, h:h + 1], scale=r_[:, h:h + 1]), reads=[ybk, "st_r", "st_nb"], writes=["yn", ybk])
            P.op("act", _act(sg[:], buf[:, 3072:4096], AF.Silu), reads=[bkey], writes=["sgt"])
            P.op("dve", _tt(yn[:], yn[:], gnB[:], ALU.mult), reads=["yn", "gnB"], writes=["yn"])
            rt = ret[t % 2]; rk = ("ret", t % 2)
            P.op("dve", _tt(rt[:], yn[:], sg[:], ALU.mult), reads=["yn", "sgt"], writes=[rk])
            P.dma("sp", _dma(mix[t * 128:(t + 1) * 128, 0:1024], rt[:]), rk, reads=[rk], writes=[("mixr", t)])

        cis = list(range(0 if pkv is not None else 16, 32))
        front(cis[0])
        for n_, ci in enumerate(cis):
            if n_ + 1 < len(cis):
                front(cis[n_ + 1])
            back(ci)
        if s_out is not None:
            P.dma("sp", _dma(s_out[:, :], S32[:].rearrange("p h n -> p (h n)")), "S32io", reads=["S32"], writes=["s_out"])
    P.barrier()


def mixer_conv(C, proj, phalo, mix, cst):
    P = C.P
    P.stage = C.pfx + "conv"
    NTK = T + 128
    with ExitStack() as st:
        cw = _sb(C, st, "cw", [128, 8, 31]); cv = _sb(C, st, "cv", [128, 3, 8])
        P.dma("sp", _dma(cw[:], cst["conv_w"][:, :, :]), "cw", writes=["cw"])
        P.dma("sp", _dma(cv[:], cst["conv_v"][:, :, :]), "cv", writes=["cv"])
        uT = _sb(C, st, "uT", [128, 8, NTK])
        yT = _sb(C, st, "yT", [128, 8, T])
        gg = [_sb(C, st, "gg", [128, 2048]) for _ in range(2)]
        sig = _sb(C, st, "sig", [128, 1024]); u = _sb(C, st, "u", [128, 1024])
        for ti in range(17):
            buf = gg[ti % 2]; bkey = ("gg", ti % 2)
            if ti == 0:
                P.dma("sp", _dma(buf[:], phalo[:, :]), bkey, writes=[bkey])
            else:
                P.dma("sp", _dma(buf[:], proj[(ti - 1) * 128:ti * 128, 4096:6144]), bkey, writes=[bkey])
            P.op("act", _act(sig[:], buf[:, 1024:2048], AF.Sigmoid), reads=[bkey], writes=["sig"])
            P.op("dve", _tt(u[:], buf[:, 0:1024], sig[:], ALU.mult), reads=[bkey, "sig"], writes=["u"])
            _transpose_blocks(C, u, 8, uT[:, :, ti * 128:(ti + 1) * 128], ["u"], "uT", ti)
        ptmp = _sb(C, st, "cptmp", [128, T])
        pool_chunks = ()
        for k in range(31):
            for j in range(8):
                yk = ("yT", j)
                src_k = uT[:, j, 98 + k:98 + k + T]
                if k == 0:
                    eng = "pool" if j in pool_chunks else "dve"
                    P.op(eng, _ts(yT[:, j, :], src_k, cw[:, j, 0:1], cv[:, 0, j:j + 1], ALU.mult, ALU.add), reads=["uT", "cw", "cv"], writes=[yk])
                elif j in pool_chunks:
                    P.op("pool", _ts(ptmp[:], src_k, cw[:, j, k:k + 1]), reads=["uT", "cw"], writes=["cptmp"])
                    P.op("pool", _tt(yT[:, j, :], yT[:, j, :], ptmp[:], ALU.add), reads=["cptmp", yk], writes=[yk])
                else:
                    P.op("dve", _stt(yT[:, j, :], src_k, cw[:, j, k:k + 1], yT[:, j, :], ALU.mult, ALU.add), reads=["uT", "cw", yk], writes=[yk])
        mean = _sb(C, st, "cmean", [128, 512]); rstd = _sb(C, st, "crstd", [128, 512]); sq = [_sb(C, st, "csq", [128, 512]) for _ in range(2)]
        z = [_sb(C, st, "cz", [128, 512]) for _ in range(2)]
        for tb in range(4):
            sl = slice(tb * 512, (tb + 1) * 512)
            for j in range(8):
                P.op("pe", _mm(C.pb[2][:, :], C.ones[:], yT[:, j, sl], j == 0, j == 7), reads=[("yT", j), "ones"], writes=["pb2"])
            for j in range(8):
                sqt = sq[j % 2]; sqk = ("csq", j % 2)
                P.op("act", _act(sqt[:], yT[:, j, sl], AF.Square), reads=[("yT", j)], writes=[sqk])
                P.op("pe", _mm(C.pb[3][:, :], C.ones[:], sqt[:], j == 0, j == 7), reads=[sqk, "ones"], writes=["pb3"])
            P.op("dve", _ts(mean[:], C.pb[2][:, :], 1.0 / 1024), reads=["pb2"], writes=["cmean"])
            P.op("dve", _tt(rstd[:], mean[:], mean[:], ALU.mult), reads=["cmean"], writes=["crstd"])
            P.op("dve", _stt(rstd[:], C.pb[3][:, :], 1.0 / 1024, rstd[:], ALU.mult, ALU.subtract), reads=["pb3", "crstd"], writes=["crstd"])
            P.op("dve", _ts(rstd[:], rstd[:], EPS, None, ALU.add), reads=["crstd"], writes=["crstd"])
            P.op("act", _sqrt(rstd[:], rstd[:]), reads=["crstd"], writes=["crstd"])
            P.op("dve", _rcp(rstd[:], rstd[:]), reads=["crstd"], writes=["crstd"])
            for j in range(8):
                zt = z[j % 2]; zk = ("cz", j % 2)
                P.op("dve", _tt(zt[:], yT[:, j, sl], mean[:], ALU.subtract), reads=[("yT", j), "cmean"], writes=[zk])
                P.op("dve", _tt(zt[:], zt[:], rstd[:], ALU.mult), reads=[zk, "crstd"], writes=[zk])
                P.op("act", _act(yT[:, j, sl], zt[:], AF.Silu, bias=cv[:, 2, j:j + 1], scale=cv[:, 1, j:j + 1]), reads=[zk, "cv"], writes=[("yT", j)])
        co = [_sb(C, st, "co", [128, 1024]) for _ in range(2)]
        for t in range(16):
            cot = co[t % 2]; ck = ("co", t % 2)
            for g in range(2):
                bank = C.pb[g]; bk = f"pb{g}"
                for i in range(4):
                    j = g * 4 + i
                    P.op("pe", _tr(bank[:, i * 128:(i + 1) * 128], yT[:, j, t * 128:(t + 1) * 128], C.ident[:]), reads=[("yT", j), "ident"], writes=[bk])
                if g == 0:
                    P.op("act", _acp(cot[:, 0:512], bank[:, :]), reads=[bk], writes=[ck])
                else:
                    P.op("dve", _cp(cot[:, 512:1024], bank[:, :]), reads=[bk], writes=[ck])
            P.dma("sp", _dma(mix[t * 128:(t + 1) * 128, 1024:2048], cot[:]), ck, reads=[ck], writes=[("mixc", t)])
    P.barrier()


def _bcast_rows(v):
    v = np.asarray(v, np.float32)
    return np.ascontiguousarray(np.broadcast_to(v[None, :], (128, v.shape[0])))


def _rope_tab(pos, half):
    inv = (10000.0 ** (-np.arange(half, dtype=np.float32) / np.float32(half))).astype(np.float32)
    ang = pos.astype(np.float32)[:, None] * inv[None, :]
    return np.ascontiguousarray(np.stack([np.cos(ang), np.sin(ang)], axis=1).astype(np.float32))


def _common_tail(C, x, mix, w_out, g1, b1, g2, b2, w_gu, w_down, rw, rbB, m, ha, f, out):
    linear_ln(C, mix, w_out, x, g1, b1, ha, "mix")
    moe(C, ha, w_gu, w_down, rw, rbB, f, "moe", ln=(g2, b2, out))


def build_layer0(debug=False):
    nc = bass.Bass("TRN2", target_bir_lowering=False)
    dt = lambda name, shape, kind="ExternalInput": nc.dram_tensor(name, shape, F32, kind=kind).ap()
    x = dt("x", [T, D]); xp = dt("xp", [T, D]); w_in = dt("w_in", [D, 6144]); w_out = dt("w_out", [D, D])
    g1 = dt("mix_g", [128, D]); b1 = dt("mix_b", [128, D]); g2 = dt("ffn_g", [128, D]); b2 = dt("ffn_b", [128, D])
    w_gu = dt("w_gu", [16, D, 1024]); w_down = dt("w_down", [16, 512, D])
    rw = dt("rw", [D, 16]); rbB = dt("rbB", [128, 16]); ident = dt("ident", [128, 128])
    cst = {"cs_own": dt("cs_own", [T, 2, 128]), "cs_pre": dt("cs_pre", [T, 2, 128]), "decT": dt("decT", [128, 512]),
           "xi": dt("xi", [128, 4]), "zeta": dt("zeta", [128, 4]), "gnB": dt("gnB", [128, 1024]),
           "conv_w": dt("conv_w", [128, 8, 31]), "conv_v": dt("conv_v", [128, 3, 8])}
    out = dt("out", [T, D], "ExternalOutput")
    dk = "ExternalOutput" if debug else "Internal"
    proj = dt("proj", [T, 6144], "Internal"); pkv = dt("pkv", [T, 2048], "Internal"); phalo = dt("phalo", [128, 2048], "Internal")
    mix = dt("mix", [T, D], dk); m = dt("m", [T, D], dk)
    ha = dt("ha", [T, D], dk); f = dt("f", [T, D], "Internal")
    with ExitStack() as es:
        C = _mk_ctx(nc, es)
        _load_consts(C, ident)
        linear(C, x, T, w_in, [(0, 6144)], proj, "in")
        linear(C, xp, T, w_in, [(1024, 3072)], pkv, "pkv")
        linear(C, xp[T - 128:T, :], 128, w_in, [(4096, 6144)], phalo, "ph")
        mixer_ret(C, proj, pkv, mix, cst)
        mixer_conv(C, proj, phalo, mix, cst)
        _common_tail(C, x, mix, w_out, g1, b1, g2, b2, w_gu, w_down, rw, rbB, m, ha, f, out)
        C.P.emit()
    return nc


def layer0_inputs(inp, h, core):
    b, half = core // 2, core % 2
    x = h[b, half * T:(half + 1) * T]
    xp = h[b, 0:T] if half == 1 else np.zeros((T, D), np.float32)
    g = np.array(RET_G, np.float64)
    j = np.arange(128, dtype=np.float64)
    diff = j[None, :] - j[:, None]
    decT = np.concatenate([np.where(diff >= 0, g[h_] ** np.maximum(diff, 0), 0.0) / 16.0 for h_ in range(4)], axis=1)
    xi = np.stack([g[h_] ** (j + 1.0) for h_ in range(4)], axis=1)
    zeta = np.stack([g[h_] ** (127.0 - j) / 16.0 for h_ in range(4)], axis=1)
    conv_w = np.asarray(inp["even_conv_w"][0], np.float32)
    cvec = np.stack([inp["even_conv_b"][0], inp["even_conv_ln_g"][0], inp["even_conv_ln_b"][0]], axis=0).astype(np.float32)
    return {
        "x": np.ascontiguousarray(x), "xp": np.ascontiguousarray(xp),
        "w_in": np.asarray(inp["even_w_in"][0], np.float32), "w_out": np.asarray(inp["even_w_out"][0], np.float32),
        "mix_g": _bcast_rows(inp["mix_ln_g"][0]), "mix_b": _bcast_rows(inp["mix_ln_b"][0]),
        "ffn_g": _bcast_rows(inp["ffn_ln_g"][0]), "ffn_b": _bcast_rows(inp["ffn_ln_b"][0]),
        "w_gu": np.asarray(inp["moe_w_gu"][0], np.float32), "w_down": np.asarray(inp["moe_w_down"][0], np.float32),
        "rw": np.asarray(inp["router_w"], np.float32), "rbB": _bcast_rows(inp["router_b"]), "ident": np.eye(128, dtype=np.float32),
        "cs_own": _rope_tab(np.arange(half * T, (half + 1) * T), 128), "cs_pre": _rope_tab(np.arange(0, T), 128),
        "decT": np.ascontiguousarray(decT.astype(np.float32)), "xi": np.ascontiguousarray(xi.astype(np.float32)),
        "zeta": np.ascontiguousarray(zeta.astype(np.float32)), "gnB": _bcast_rows(inp["even_ret_gn_g"][0]),
        "conv_w": np.ascontiguousarray(conv_w.T.reshape(8, 128, 31).transpose(1, 0, 2)),
        "conv_v": np.ascontiguousarray(cvec.reshape(3, 8, 128).transpose(2, 0, 1)),
    }


def mixer_dsa(C, qproj, kvproj, attn_out, cst):
    P = C.P
    P.stage = C.pfx + "dsa_kprep"
    SC = 128.0 ** -0.5
    with ExitStack() as st:
        kT = _sb(C, st, "dkT", [128, 4, 4096], BF16)
        vb = _sb(C, st, "dvb", [128, 32, 4, 129], BF16)
        kiT2 = _sb(C, st, "dkiT", [128, 1, 4096], BF16)
        iota = _sb(C, st, "diota", [128, 4096])
        qpos = _sb(C, st, "dqpos", [128, 16])
        P.dma("sp", _dma(iota[:], cst["iota"][:, :]), "diota", writes=["iota"])
        P.dma("sp", _dma(qpos[:], cst["qpos"][:, :]), "dqpos", writes=["qpos"])
        P.op("dve", _mset(vb[:], 1.0), writes=["vb"])
        with ExitStack() as s1:
            kvt = [_sb(C, s1, "kvt", [128, 1088]) for _ in range(2)]
            csk = [_sb(C, s1, "csk", [128, 2, 64]) for _ in range(2)]
            csi = [_sb(C, s1, "csi", [128, 2, 32]) for _ in range(2)]
            kr = _sb(C, s1, "dkr", [128, 512]); kir2 = _sb(C, s1, "dkir", [128, 128])
            t1 = _sb(C, s1, "dt1", [128, 256]); t2 = _sb(C, s1, "dt2", [128, 256])
            for kt in range(32):
                i = kt % 2
                bk_, ck_, ik_ = ("kvt", i), ("csk", i), ("csi", i)
                rows = slice(kt * 128, (kt + 1) * 128)
                P.dma("sp", _dma(kvt[i][:], kvproj[rows, :]), bk_, writes=[bk_])
                P.dma("sp", _dma(csk[i][:], cst["cs_k"][rows, :, :]), ck_, writes=[ck_])
                P.dma("sp", _dma(csi[i][:], cst["cs_ki"][rows, :, :]), ik_, writes=[ik_])
                _rope(P, kvt[i][:, 0:512], kr[:], csk[i], 4, 128, t1, t2, [bk_, ck_], "dkr", "dt")
                _transpose_blocks(C, kr, 4, kT[:, :, kt * 128:(kt + 1) * 128], ["dkr"], "kT", kt)
                P.op("act", _acp(vb[:, kt, :, 0:128], kvt[i][:, 512:1024].rearrange("p (h d) -> p h d", h=4)), reads=[bk_], writes=["vb"])
                _rope(P, kvt[i][:, 1024:1088], kir2[:, 0:64], csi[i], 1, 64, t1, t2, [bk_, ik_], "dkir", "dt")
                P.op("dve", _cp(kir2[:, 64:128], kir2[:, 0:64]), reads=["dkir"], writes=["dkir"])
                _transpose_blocks(C, kir2, 1, kiT2[:, :, kt * 128:(kt + 1) * 128], ["dkir"], "kiT", kt + 1)
        P.barrier()
        P.stage = C.pfx + "dsa_q"
        qt = _sb(C, st, "dqt", [128, 3088])
        csq = [_sb(C, st, "csq", [128, 2, 64]) for _ in range(2)]
        csqi = [_sb(C, st, "csqi", [128, 2, 32]) for _ in range(2)]
        qr = _sb(C, st, "dqr", [128, 2048]); qir = _sb(C, st, "dqir", [128, 1024])
        qT = [_sb(C, st, "dqT", [128, 16, 128], BF16) for _ in range(2)]
        qiT = _sb(C, st, "dqiT", [128, 8, 128], BF16)
        wab = _sb(C, st, "dwab", [128, 16]); sgn = _sb(C, st, "dsgn", [128, 16])
        t1 = _sb(C, st, "dq1", [128, 1024]); t2 = _sb(C, st, "dq2", [128, 1024])
        acc = _sb(C, st, "dacc", [128, 4096]); scr = _sb(C, st, "dscr", [128, 4096])
        tmp = [_sb(C, st, "dtmp", [128, 1024]) for _ in range(2)]
        selT = [_sb(C, st, "dselT", [128, 32, 128], BF16) for _ in range(2)]
        pt = [_sb(C, st, "dp", [128, 1024], BF16) for _ in range(2)]
        obuf = _sb(C, st, "dobuf", [128, 16, 129])
        rec = _sb(C, st, "drec", [128, 16])
        sm = {n: _sb(C, st, "d_" + n, [128, 1]) for n in ["lo", "hi", "w", "mid", "cnt", "ge"]}
        NIT = 26
        hw = _sb(C, st, "d_hw", [128, NIT + 1]); pw2 = _sb(C, st, "d_pw2", [128, NIT + 1])
        for k_ in range(NIT + 1):
            P.op("dve", _mset(pw2[:, k_:k_ + 1], 2.0 ** -(k_ + 1)), writes=["pw2"])
        identb = _sb(C, st, "didb", [128, 128], BF16)
        P.op("dve", _cp(identb[:], C.ident[:]), reads=["ident"], writes=["identb"])
        cnts = {"it": 0, "ig": 0}
        ACCK = [("acc", g_) for g_ in (0, 1024, 2048, 3072)]

        def NN(j):
            return 2048 + 128 * (j + 1)

        def phaseA(j):
            N = NN(j); i = j % 2
            rows = slice(j * 128, (j + 1) * 128)
            qTk = ("qT", i)
            P.dma("sp", _dma(qt[:], qproj[rows, :]), "dqt", writes=["dqt"])
            P.dma("sp", _dma(csq[i][:], cst["cs_q"][rows, :, :]), ("csq", i), writes=[("csq", i)])
            P.dma("sp", _dma(csqi[i][:], cst["cs_qi"][rows, :, :]), ("csqi", i), writes=[("csqi", i)])
            _rope(P, qt[:, 0:2048], qr[:], csq[i], 16, 128, t1, t2, ["dqt", ("csq", i)], "dqr", "dq")
            _transpose_blocks(C, qr, 16, qT[i], ["dqr"], qTk, 0)
            _rope(P, qt[:, 2048:3072], qir[:], csqi[i], 16, 64, t1, t2, ["dqt", ("csqi", i)], "dqir", "dq")
            _transpose_blocks(C, qir, 8, qiT, ["dqir"], "qiT", 0)
            P.op("dve", _ts(sgn[:], qt[:, 3072:3088], 0.0, 2.0, ALU.is_ge, ALU.mult), reads=["dqt"], writes=["sgn"])
            P.op("dve", _ts(sgn[:], sgn[:], -1.0, None, ALU.add), reads=["sgn"], writes=["sgn"])
            P.op("dve", _tt(wab[:], qt[:, 3072:3088], sgn[:], ALU.mult), reads=["dqt", "sgn"], writes=["wab"])
            P.op("dve", _ts(wab[:], wab[:], 0.03125), reads=["wab"], writes=["wab"])
            P.op("dve", _ts(acc[:, 0:N], iota[:, 0:N], qpos[:, j:j + 1], NEG, ALU.is_gt, ALU.mult), reads=["iota", "qpos"], writes=ACCK)
            for h in range(16):
                p0 = (h % 2) * 64
                for g0 in range(0, N, 1024):
                    w = min(1024, N - g0)
                    ig = cnts["ig"]
                    base = (ig % 4) * 2
                    nb_ = (w + 511) // 512
                    bkeys = [f"pb{base + c}" for c in range(nb_)]
                    for c4 in range(nb_):
                        ww = min(512, w - c4 * 512)
                        P.op("pe", _mm(C.pb[base + c4][:, 0:ww], qiT[p0:p0 + 64, h // 2, :], kiT2[p0:p0 + 64, 0, g0 + c4 * 512:g0 + c4 * 512 + ww], True, True),
                             reads=["qiT", "kiT"], writes=[bkeys[c4]])
                    tm = tmp[ig % 2]; tk = ("dtmp", ig % 2)
                    P.op("act", _act(tm[:, 0:w], C.ps_all[:, base * 512:base * 512 + w], AF.Relu, scale=wab[:, h:h + 1]), reads=bkeys + ["wab"], writes=[tk] + bkeys)
                    P.op("dve", _stt(acc[:, g0:g0 + w], tm[:, 0:w], sgn[:, h:h + 1], acc[:, g0:g0 + w], ALU.mult, ALU.add), reads=[tk, "sgn", ("acc", g0)], writes=[("acc", g0)])
                    cnts["ig"] += 1

        def phaseB(j):
            N = NN(j); NB = N // 128; i = j % 2
            lo, hi, wd, mid, cnt, ge = (sm[n] for n in ["lo", "hi", "w", "mid", "cnt", "ge"])
            P.op("dve", _red(hi[:], acc[:, 0:N], ALU.max), reads=ACCK, writes=["hi"])
            P.op("dve", _ts(scr[:, 0:N], iota[:, 0:N], qpos[:, j:j + 1], -2.0 * NEG, ALU.is_gt, ALU.mult), reads=["iota", "qpos"], writes=["scr"])
            P.op("dve", _tt(scr[:, 0:N], scr[:, 0:N], acc[:, 0:N], ALU.add), reads=["scr"] + ACCK, writes=["scr"])
            P.op("dve", _red(lo[:], scr[:, 0:N], ALU.min), reads=["scr"], writes=["lo"])
            P.op("dve", _tt(wd[:], hi[:], lo[:], ALU.subtract), reads=["hi", "lo"], writes=["w"])
            P.op("dve", _ts(hw[:], pw2[:], wd[:, 0:1]), reads=["w", "pw2"], writes=["hw"])
            for k in range(NIT):
                P.op("dve", _tt(mid[:], lo[:], hw[:, k:k + 1], ALU.add), reads=["lo", "hw"], writes=["mid"])
                P.op("dve", _ts(scr[:, 0:N], acc[:, 0:N], mid[:, 0:1], 0.0, ALU.is_ge, ALU.add, accum=cnt[:, 0:1]), reads=ACCK + ["mid"], writes=["scr", "cnt"])
                P.op("dve", _ts(ge[:], cnt[:], 255.5, hw[:, k:k + 1], ALU.is_ge, ALU.mult), reads=["cnt", "hw"], writes=["ge"])
                P.op("dve", _tt(lo[:], lo[:], ge[:], ALU.add), reads=["lo", "ge"], writes=["lo"])
            P.op("dve", _ts(scr[:, 0:N], acc[:, 0:N], lo[:, 0:1], -30000.0, ALU.is_lt, ALU.mult), reads=ACCK + ["lo"], writes=["scr"])
            _transpose_blocks(C, scr, NB, selT[i], ["scr"], ("selT", i), 0)

        def phaseCmain(j):
            N = NN(j); NB = N // 128; i = j % 2
            qT2 = qT[i][:].rearrange("p h q -> p (h q)")
            qTk, sTk = ("qT", i), ("selT", i)
            for kv in range(4):
                for kb in range(0, NB, 2):
                    nk = min(2, NB - kb)
                    it = cnts["it"]
                    base = (it % 2) * 2
                    Lks = [f"pb{base + b}" for b in range(nk)]
                    pp = pt[it % 2]; ppk = ("dp", it % 2)
                    for b in range(nk):
                        P.op("pe", _mm(C.pb[base + b][:, :], kT[:, kv, (kb + b) * 128:(kb + b + 1) * 128], qT2[:, kv * 512:(kv + 1) * 512], True, False), reads=["kT", qTk], writes=[Lks[b]])
                        for g in range(4):
                            P.op("pe", _mm(C.pb[base + b][:, g * 128:(g + 1) * 128], identb[:], selT[i][:, kb + b, :], False, g == 3), reads=["identb", sTk], writes=[Lks[b]])
                    P.op("act", _act(pp[:, 0:nk * 512], C.ps_all[:, base * 512:(base + nk) * 512], AF.Exp, scale=SC), reads=Lks, writes=[ppk] + Lks)
                    for b in range(nk):
                        for g in range(4):
                            P.op("pe", _mm(C.pb[4 + g][:, 0:129], pp[:, b * 512 + g * 128:b * 512 + (g + 1) * 128], vb[:, kb + b, kv, :], kb + b == 0, kb + b == NB - 1), reads=[ppk, "vb"], writes=[f"pb{4 + g}"])
                    cnts["it"] += 1
                for g in range(4):
                    P.op("act", _acp(obuf[:, kv * 4 + g, :], C.pb[4 + g][:, 0:129]), reads=[f"pb{4 + g}"], writes=["obuf", f"pb{4 + g}"])

        def phaseCfin(j):
            rows = slice(j * 128, (j + 1) * 128)
            P.op("dve", _rcp(rec[:], obuf[:, :, 128]), reads=["obuf"], writes=["rec"])
            P.op("dve", _tt(obuf[:, :, 0:128], obuf[:, :, 0:128], rec[:, :, None].to_broadcast([128, 16, 128]), ALU.mult), reads=["obuf", "rec"], writes=["obuf"])
            P.dma("sp", _dma(attn_out[rows, :].rearrange("p (h d) -> p h d", h=16), obuf[:, :, 0:128]), "dobuf", reads=["obuf"], writes=[("aout", j)])

        phaseA(0)
        phaseB(0)
        for j in range(16):
            if j + 1 < 16:
                phaseA(j + 1)
            phaseCmain(j)
            if j + 1 < 16:
                phaseB(j + 1)
            phaseCfin(j)
    P.barrier()


def build_layer1(debug=False):
    nc = bass.Bass("TRN2", target_bir_lowering=False)
    dt = lambda name, shape, kind="ExternalInput": nc.dram_tensor(name, shape, F32, kind=kind).ap()
    x = dt("x", [T, D]); xf = dt("xf", [2 * T, D]); w_in = dt("w_in", [D, 4176]); w_out = dt("w_out", [D, D])
    g1 = dt("mix_g", [128, D]); b1 = dt("mix_b", [128, D]); g2 = dt("ffn_g", [128, D]); b2 = dt("ffn_b", [128, D])
    w_gu = dt("w_gu", [16, D, 1024]); w_down = dt("w_down", [16, 512, D])
    rw = dt("rw", [D, 16]); rbB = dt("rbB", [128, 16]); ident = dt("ident", [128, 128])
    cst = {"cs_k": dt("cs_k", [2 * T, 2, 64]), "cs_ki": dt("cs_ki", [2 * T, 2, 32]), "cs_q": dt("cs_q", [T, 2, 64]), "cs_qi": dt("cs_qi", [T, 2, 32]),
           "iota": dt("iota", [128, 4096]), "qpos": dt("qpos", [128, 16])}
    out = dt("out", [T, D], "ExternalOutput")
    dk = "ExternalOutput" if debug else "Internal"
    qproj = dt("qproj", [T, 3088], "Internal"); kvproj = dt("kvproj", [2 * T, 1088], "Internal")
    mix = dt("mix", [T, D], dk); m = dt("m", [T, D], dk)
    ha = dt("ha", [T, D], dk); f = dt("f", [T, D], "Internal")
    with ExitStack() as es:
        C = _mk_ctx(nc, es)
        _load_consts(C, ident)
        linear(C, x, T, w_in, [(0, 2048), (3072, 4096), (4160, 4176)], qproj, "q1")
        linear(C, xf, 2 * T, w_in, [(2048, 3072), (4096, 4160)], kvproj, "kv1")
        mixer_dsa(C, qproj, kvproj, mix, cst)
        _common_tail(C, x, mix, w_out, g1, b1, g2, b2, w_gu, w_down, rw, rbB, m, ha, f, out)
        C.P.emit()
    return nc


def layer1_inputs(inp, h, core):
    b, half = core // 2, core % 2
    pos_own = np.arange(half * T, (half + 1) * T)
    qpos = (half * T + np.arange(16)[None, :] * 128 + np.arange(128)[:, None]).astype(np.float32)
    return {
        "x": np.ascontiguousarray(h[b, half * T:(half + 1) * T]), "xf": np.ascontiguousarray(h[b]),
        "w_in": np.asarray(inp["odd_w_in"][0], np.float32), "w_out": np.asarray(inp["odd_w_out"][0], np.float32),
        "mix_g": _bcast_rows(inp["mix_ln_g"][1]), "mix_b": _bcast_rows(inp["mix_ln_b"][1]),
        "ffn_g": _bcast_rows(inp["ffn_ln_g"][1]), "ffn_b": _bcast_rows(inp["ffn_ln_b"][1]),
        "w_gu": np.asarray(inp["moe_w_gu"][1], np.float32), "w_down": np.asarray(inp["moe_w_down"][1], np.float32),
        "rw": np.asarray(inp["router_w"], np.float32), "rbB": _bcast_rows(inp["router_b"]), "ident": np.eye(128, dtype=np.float32),
        "cs_k": _rope_tab(np.arange(2 * T), 64), "cs_ki": _rope_tab(np.arange(2 * T), 32),
        "cs_q": _rope_tab(pos_own, 64), "cs_qi": _rope_tab(pos_own, 32),
        "iota": np.ascontiguousarray(np.broadcast_to(np.arange(4096, dtype=np.float32)[None, :], (128, 4096))),
        "qpos": np.ascontiguousarray(qpos),
    }


def build_fused(profile=False):
    nc = bass.Bass("TRN2", target_bir_lowering=False)
    dt = lambda name, shape, kind="ExternalInput": nc.dram_tensor(name, shape, F32, kind=kind).ap()
    xA = dt("x", [T, D]); xB = dt("xp", [T, D]); zhalo = dt("zhalo", [128, 2048])
    rw = dt("rw", [D, 16]); rbB = dt("rbB", [128, 16]); ident = dt("ident", [128, 128])
    L = []
    for l in range(2):
        L.append({"w_in": dt(f"w_in{l}", [D, 6144 if l == 0 else 4176]), "w_out": dt(f"w_out{l}", [D, D]),
                  "g1": dt(f"mix_g{l}", [128, D]), "b1": dt(f"mix_b{l}", [128, D]), "g2": dt(f"ffn_g{l}", [128, D]), "b2": dt(f"ffn_b{l}", [128, D]),
                  "w_gu": dt(f"w_gu{l}", [16, D, 1024]), "w_down": dt(f"w_down{l}", [16, 512, D])})
    cst = {"cs_own": dt("cs_own", [T, 2, 128]), "cs_pre": dt("cs_pre", [T, 2, 128]), "decT": dt("decT", [128, 512]),
           "xi": dt("xi", [128, 4]), "zeta": dt("zeta", [128, 4]), "gnB": dt("gnB", [128, 1024]),
           "conv_w": dt("conv_w", [128, 8, 31]), "conv_v": dt("conv_v", [128, 3, 8]),
           "cs_k": dt("cs_k", [2 * T, 2, 64]), "cs_ki": dt("cs_ki", [2 * T, 2, 32]), "cs_q": dt("cs_q", [T, 2, 64]), "cs_qi": dt("cs_qi", [T, 2, 32]),
           "iota": dt("iota", [128, 4096]), "qpos": dt("qpos", [128, 16])}
    out = dt("out", [T, D], "ExternalOutput")
    projB = dt("projB", [T, 6144], "Internal"); projA = dt("projA", [T, 6144], "Internal")
    sstate = dt("sstate", [128, 2048], "Internal"); h0f = dt("h0f", [2 * T, D], "Internal")
    qproj = dt("qproj", [T, 3088], "Internal"); kvproj = dt("kvproj", [2 * T, 1088], "Internal")
    mix = dt("mix", [T, D], "Internal"); m = dt("m", [T, D], "Internal"); ha = dt("ha", [T, D], "Internal"); f = dt("f", [T, D], "Internal")
    with ExitStack() as es:
        C = _mk_ctx(nc, es)
        C.P.profile = profile
        _load_consts(C, ident)
        l0 = L[0]
        for (xx, proj, cs_key, s_in, s_out, halo, dst) in [(xB, projB, "cs_pre", None, sstate, zhalo, h0f[0:T, :]),
                                                            (xA, projA, "cs_own", sstate, None, projB[T - 128:T, 4096:6144], h0f[T:2 * T, :])]:
            C.pfx = "B_" if s_in is None else "A_"
            linear(C, xx, T, l0["w_in"], [(0, 6144)], proj, "in")
            mixer_ret(C, proj, None, mix, cst, cs_key=cs_key, s_in=s_in, s_out=s_out)
            mixer_conv(C, proj, halo, mix, cst)
            _common_tail(C, xx, mix, l0["w_out"], l0["g1"], l0["b1"], l0["g2"], l0["b2"], l0["w_gu"], l0["w_down"], rw, rbB, m, ha, f, dst)
        l1 = L[1]
        C.pfx = "L1_"
        x1 = h0f[T:2 * T, :]
        linear(C, x1, T, l1["w_in"], [(0, 2048), (3072, 4096), (4160, 4176)], qproj, "q1")
        linear(C, h0f, 2 * T, l1["w_in"], [(2048, 3072), (4096, 4160)], kvproj, "kv1")
        mixer_dsa(C, qproj, kvproj, mix, cst)
        _common_tail(C, x1, mix, l1["w_out"], l1["g1"], l1["b1"], l1["g2"], l1["b2"], l1["w_gu"], l1["w_down"], rw, rbB, m, ha, f, out)
        C.P.emit()
    return nc


def fused_inputs(inp, core):
    b, half = core // 2, core % 2
    x = np.asarray(inp["x"], np.float32)
    a = layer0_inputs(inp, x, core)
    r = {k: a[k] for k in ["x", "xp", "rw", "rbB", "ident", "cs_own", "cs_pre", "decT", "xi", "zeta", "gnB", "conv_w", "conv_v"]}
    r["zhalo"] = np.zeros((128, 2048), np.float32)
    for l, pre in enumerate(["even", "odd"]):
        r[f"w_in{l}"] = np.asarray(inp[pre + "_w_in"][0], np.float32); r[f"w_out{l}"] = np.asarray(inp[pre + "_w_out"][0], np.float32)
        r[f"mix_g{l}"] = _bcast_rows(inp["mix_ln_g"][l]); r[f"mix_b{l}"] = _bcast_rows(inp["mix_ln_b"][l])
        r[f"ffn_g{l}"] = _bcast_rows(inp["ffn_ln_g"][l]); r[f"ffn_b{l}"] = _bcast_rows(inp["ffn_ln_b"][l])
        r[f"w_gu{l}"] = np.asarray(inp["moe_w_gu"][l], np.float32); r[f"w_down{l}"] = np.asarray(inp["moe_w_down"][l], np.float32)
    posA = np.arange(half * T, (half + 1) * T)
    posB = np.arange(0, T)
    kp = np.concatenate([posB if half == 1 else np.full(T, 1.0e9), posA]).astype(np.float32)
    ropepos = np.concatenate([posB, posA])
    r["cs_k"] = _rope_tab(ropepos, 64); r["cs_ki"] = _rope_tab(ropepos, 32)
    r["cs_q"] = _rope_tab(posA, 64); r["cs_qi"] = _rope_tab(posA, 32)
    r["iota"] = np.ascontiguousarray(np.broadcast_to(kp[None, :], (128, 4096)))
    r["qpos"] = np.ascontiguousarray((half * T + np.arange(16)[None, :] * 128 + np.arange(128)[:, None]).astype(np.float32))
    return r


_NC_CACHE = {}


def kernel(**inputs):
    inp = {k: np.asarray(v) for k, v in inputs.items()}
    B = inp["x"].shape[0]
    if "fused" not in _NC_CACHE:
        _NC_CACHE["fused"] = build_fused()
    nc = _NC_CACHE["fused"]
    in_maps = [fused_inputs(inp, core) for core in range(8)]
    res = run_bass_kernel_spmd(nc, in_maps, core_ids=list(range(8)))
    h = np.stack([np.concatenate([res.results[2 * b]["out"], res.results[2 * b + 1]["out"]], axis=0) for b in range(B)], axis=0)
    return np.ascontiguousarray(h.astype(np.float32))
```
